# Optimizing a Trainium2 kernel written in Bass

```python
import jax, jax.numpy as jnp
from jax import lax
import numpy as np

D_MODEL = 1024
BATCH = 16
SEQ = 2048
DEPTH = 1

D_MIX = D_MODEL
MLA_HEADS = 8
MLA_V_DIM = (D_MIX // 2) // MLA_HEADS
MLA_NOPE_DIM = 64
MLA_ROPE_DIM = 32
MLA_QK_DIM = MLA_NOPE_DIM + MLA_ROPE_DIM
MLA_Q_LORA = 384
MLA_KV_LORA = 256
ROPE_THETA = 10000.0
Q_BLOCK = 128
HG_HEADS = 4
HG_HEAD_DIM = (D_MIX // 2) // HG_HEADS
HG_WIDTH = HG_HEADS * HG_HEAD_DIM
HG_CHUNK = 64
N_GROUPS = 4
EXPERTS_PER_GROUP = 8
N_EXPERTS = N_GROUPS * EXPERTS_PER_GROUP
TOP_K = 2
D_EXPERT = D_MODEL // 2
MOE_BLOCK = 128
EPS = 1e-6
IN_SPLITS = (MLA_Q_LORA, MLA_KV_LORA, MLA_ROPE_DIM, HG_WIDTH, HG_WIDTH, HG_WIDTH, HG_WIDTH, HG_WIDTH)
IN_COLS = MLA_Q_LORA + MLA_KV_LORA + MLA_ROPE_DIM + 5 * HG_WIDTH

kernel_name = 'hymba_mla_hgrn2_hmoe_adaln_encoder'


def rms_norm(x, g):
    xf = x.astype(jnp.float32)
    y = xf * lax.rsqrt(jnp.mean(xf * xf, axis=-1, keepdims=True) + EPS)
    return (y * g.astype(jnp.float32)).astype(x.dtype)


def modulate(h, shift, scale):
    return h * (1 + scale[:, None, :]) + shift[:, None, :]


def apply_rope(x, positions):
    half = MLA_ROPE_DIM // 2
    inv_freq = ROPE_THETA ** (-jnp.arange(half, dtype=jnp.float32) / half)
    ang = positions.astype(jnp.float32)[:, :, None, None] * inv_freq
    cos, sin = jnp.cos(ang), jnp.sin(ang)
    xf = x.astype(jnp.float32)
    x1, x2 = xf[..., :half], xf[..., half:]
    return jnp.concatenate([x1 * cos - x2 * sin, x2 * cos + x1 * sin], axis=-1).astype(x.dtype)


def mla_mixer(q_lat, kv_lat, k_rope, positions, qa_g, wq_up, kva_g, wkv_up, qn_g, kn_g):
    B, S, _ = q_lat.shape
    q = (rms_norm(q_lat, qa_g) @ wq_up).reshape(B, S, MLA_HEADS, MLA_QK_DIM)
    kv = (rms_norm(kv_lat, kva_g) @ wkv_up).reshape(B, S, MLA_HEADS, MLA_NOPE_DIM + MLA_V_DIM)
    k_nope, v = kv[..., :MLA_NOPE_DIM], kv[..., MLA_NOPE_DIM:]
    k_shared = jnp.broadcast_to(k_rope[:, :, None, :], (B, S, MLA_HEADS, MLA_ROPE_DIM))
    k = jnp.concatenate([k_nope, k_shared], axis=-1)
    q = rms_norm(q, qn_g)
    k = rms_norm(k, kn_g)
    q = jnp.concatenate([q[..., :MLA_NOPE_DIM], apply_rope(q[..., MLA_NOPE_DIM:], positions)], axis=-1)
    k = jnp.concatenate([k[..., :MLA_NOPE_DIM], apply_rope(k[..., MLA_NOPE_DIM:], positions)], axis=-1)
    scale = MLA_QK_DIM ** -0.5
    n_blocks = S // Q_BLOCK
    q_blocks = q.reshape(B, n_blocks, Q_BLOCK, MLA_HEADS, MLA_QK_DIM).transpose(1, 0, 2, 3, 4)

    def attend(qb):
        s = jnp.einsum('bqhd,bkhd->bhqk', qb, k).astype(jnp.float32) * scale
        p = jax.nn.softmax(s, axis=-1).astype(v.dtype)
        return jnp.einsum('bhqk,bkhv->bqhv', p, v)

    o = lax.map(attend, q_blocks)
    return o.transpose(1, 0, 2, 3, 4).reshape(B, S, MLA_HEADS * MLA_V_DIM)


def gla_chunk_scan(q, k, v, log_f):
    B, S, H, K = q.shape
    V = v.shape[-1]
    N = S // HG_CHUNK

    def to_chunks(t):
        return t.reshape(B, N, HG_CHUNK, H, t.shape[-1]).transpose(0, 3, 1, 2, 4)

    q, k, v, log_f = to_chunks(q), to_chunks(k), to_chunks(v), to_chunks(log_f)
    b = jnp.cumsum(log_f, axis=3)
    b_last = b[:, :, :, -1:, :]
    q_dec = q * jnp.exp(b)
    att = jnp.einsum('bhnck,bhnsk->bhncs', q_dec, k * jnp.exp(-b))
    mask = jnp.tril(jnp.ones((HG_CHUNK, HG_CHUNK), dtype=bool))
    att = jnp.where(mask, att, 0.0)
    o_intra = jnp.einsum('bhncs,bhnsv->bhncv', att, v)
    kv = jnp.einsum('bhnck,bhncv->bhnkv', k * jnp.exp(b_last - b), v)
    decay = jnp.exp(b_last[:, :, :, 0, :])

    def step(state, inp):
        d, u = inp
        return d[..., None] * state + u, state

    init = jnp.zeros((B, H, K, V), q.dtype)
    _, s_prev = lax.scan(step, init, (jnp.moveaxis(decay, 2, 0), jnp.moveaxis(kv, 2, 0)))
    s_prev = jnp.moveaxis(s_prev, 0, 2)
    o = o_intra + jnp.einsum('bhnck,bhnkv->bhncv', q_dec, s_prev)
    return o.transpose(0, 2, 3, 1, 4).reshape(B, S, H, V)


def hgrn2_mixer(hq, hf_fwd, hf_bwd, hi, hg, lower, norm_g):
    B, S, _ = hq.shape

    def heads(t):
        return t.reshape(B, S, HG_HEADS, HG_HEAD_DIM).astype(jnp.float32)

    q = jax.nn.silu(heads(hq))
    v = heads(hi)

    def gates(f_logits, lb):
        lb = lb.reshape(HG_HEADS, HG_HEAD_DIM)
        f = lb + (1.0 - lb) * jax.nn.sigmoid(heads(f_logits))
        return 1.0 - f, jnp.log(f)

    k_f, lf_f = gates(hf_fwd, lower[0])
    k_b, lf_b = gates(hf_bwd, lower[1])
    o_fwd = gla_chunk_scan(q, k_f, v, lf_f)

    def flip(t):
        return jnp.flip(t, axis=1)

    o_bwd = flip(gla_chunk_scan(flip(q), flip(k_b), flip(v), flip(lf_b)))
    o = rms_norm(o_fwd + o_bwd, norm_g) * jax.nn.silu(heads(hg))
    return o.reshape(B, S, HG_WIDTH).astype(hq.dtype)


def hierarchical_moe(h, wg, bg, we, be, w_gate, w_up, w_down):
    B, S, D = h.shape
    T = B * S
    A = T * TOP_K
    ht = h.reshape(T, D)
    group_prob = jax.nn.softmax((ht @ wg).astype(jnp.float32) + bg.astype(jnp.float32), axis=-1)
    group_p, group_idx = lax.top_k(group_prob, 1)
    expert_logits = ((ht @ we).astype(jnp.float32) + be.astype(jnp.float32)).reshape(T, N_GROUPS, EXPERTS_PER_GROUP)
    in_group = jnp.take_along_axis(expert_logits, group_idx[:, :, None], axis=1)[:, 0]
    top_val, top_idx = lax.top_k(in_group, TOP_K)
    weights = group_p * jax.nn.softmax(top_val, axis=-1)
    expert_id = group_idx * EXPERTS_PER_GROUP + top_idx
    e_flat = expert_id.reshape(A)
    tok_flat = jnp.repeat(jnp.arange(T, dtype=jnp.int32), TOP_K)
    w_flat = weights.reshape(A)
    order = jnp.argsort(e_flat)
    e_sorted = e_flat[order]
    counts = jnp.bincount(e_flat, length=N_EXPERTS)
    padded = (counts + MOE_BLOCK - 1) // MOE_BLOCK * MOE_BLOCK
    starts = jnp.cumsum(counts) - counts
    padded_ends = jnp.cumsum(padded)
    padded_starts = padded_ends - padded
    dest = padded_starts[e_sorted] + jnp.arange(A, dtype=jnp.int32) - starts[e_sorted]
    cap = A + N_EXPERTS * MOE_BLOCK
    n_blocks = cap // MOE_BLOCK
    buf_tok = jnp.zeros((cap,), jnp.int32).at[dest].set(tok_flat[order])
    buf_w = jnp.zeros((cap,), h.dtype).at[dest].set(w_flat[order].astype(h.dtype))
    block_expert = jnp.clip(jnp.searchsorted(padded_ends, jnp.arange(n_blocks, dtype=jnp.int32) * MOE_BLOCK, side='right'), 0, N_EXPERTS - 1)
    xb = ht[buf_tok].reshape(n_blocks, MOE_BLOCK, D)

    def expert_block(args):
        xe, e = args
        hid = jax.nn.silu(xe @ w_gate[e]) * (xe @ w_up[e])
        return hid @ w_down[e]

    yb = lax.map(expert_block, (xb, block_expert)).reshape(cap, D)
    out = jnp.zeros((T, D), h.dtype).at[buf_tok].add(yb * buf_w[:, None])
    return out.reshape(B, S, D)


def setup_inputs(seed: int = 0) -> dict:
    key = jax.random.key(seed)
    ks = jax.random.split(key, 24)
    f32 = jnp.float32
    L = DEPTH

    def nrm(k, shape, fan_in):
        return jax.random.normal(k, shape, f32) * fan_in ** -0.5

    def gain(k, shape):
        return 1.0 + 0.05 * jax.random.normal(k, shape, f32)

    return {
        'x': jax.random.normal(ks[0], (BATCH, SEQ, D_MODEL), f32),
        'c': jax.random.normal(ks[1], (BATCH, D_MODEL), f32),
        'positions': jnp.arange(SEQ, dtype=jnp.int32)[None, :] + jax.random.randint(ks[2], (BATCH, 1), 0, 4096, dtype=jnp.int32),
        'ada_w': nrm(ks[3], (L, D_MODEL, 6 * D_MODEL), D_MODEL),
        'ada_b': 0.02 * jax.random.normal(ks[4], (L, 6 * D_MODEL), f32),
        'norm1_g': gain(ks[5], (L, D_MODEL)),
        'w_in': nrm(ks[6], (L, D_MODEL, IN_COLS), D_MODEL),
        'mla_qa_g': gain(ks[7], (L, MLA_Q_LORA)),
        'mla_wq_up': nrm(ks[8], (L, MLA_Q_LORA, MLA_HEADS * MLA_QK_DIM), MLA_Q_LORA),
        'mla_kva_g': gain(ks[9], (L, MLA_KV_LORA)),
        'mla_wkv_up': nrm(ks[10], (L, MLA_KV_LORA, MLA_HEADS * (MLA_NOPE_DIM + MLA_V_DIM)), MLA_KV_LORA),
        'mla_qn_g': gain(ks[11], (L, MLA_QK_DIM)),
        'mla_kn_g': gain(ks[12], (L, MLA_QK_DIM)),
        'hg_lb_logits': 0.1 * jax.random.normal(ks[13], (2, L + 1, HG_WIDTH), f32),
        'hg_norm_g': gain(ks[14], (L, HG_HEAD_DIM)),
        'w_out': nrm(ks[15], (L, D_MIX, D_MODEL), D_MIX),
        'norm2_g': gain(ks[16], (L, D_MODEL)),
        'router_group_w': nrm(ks[17], (L, D_MODEL, N_GROUPS), D_MODEL),
        'router_group_b': 0.01 * jax.random.normal(ks[18], (L, N_GROUPS), f32),
        'router_expert_w': nrm(ks[19], (L, D_MODEL, N_EXPERTS), D_MODEL),
        'router_expert_b': 0.01 * jax.random.normal(ks[20], (L, N_EXPERTS), f32),
        'w_gate': nrm(ks[21], (L, N_EXPERTS, D_MODEL, D_EXPERT), D_MODEL),
        'w_up': nrm(ks[22], (L, N_EXPERTS, D_MODEL, D_EXPERT), D_MODEL),
        'w_down': nrm(ks[23], (L, N_EXPERTS, D_EXPERT, D_MODEL), D_EXPERT),
    }


def reference(x, c, positions, ada_w, ada_b, norm1_g, w_in, mla_qa_g, mla_wq_up, mla_kva_g, mla_wkv_up, mla_qn_g, mla_kn_g, hg_lb_logits, hg_norm_g, w_out, norm2_g, router_group_w, router_group_b, router_expert_w, router_expert_b, w_gate, w_up, w_down):
    lower_bounds = jnp.cumsum(jax.nn.softmax(hg_lb_logits.astype(jnp.float32), axis=1), axis=1)
    split_points = [int(p) for p in np.cumsum(IN_SPLITS)[:-1]]
    c_act = jax.nn.silu(c)
    for layer in range(DEPTH):
        mod = c_act @ ada_w[layer] + ada_b[layer]
        shift1, scale1, gate1, shift2, scale2, gate2 = jnp.split(mod, 6, axis=-1)
        h = modulate(rms_norm(x, norm1_g[layer]), shift1, scale1)
        proj = h @ w_in[layer]
        q_lat, kv_lat, k_rope, hg_q, hg_f_fwd, hg_f_bwd, hg_i, hg_g = jnp.split(proj, split_points, axis=-1)
        attn_out = mla_mixer(q_lat, kv_lat, k_rope, positions, mla_qa_g[layer], mla_wq_up[layer], mla_kva_g[layer], mla_wkv_up[layer], mla_qn_g[layer], mla_kn_g[layer])
        rec_out = hgrn2_mixer(hg_q, hg_f_fwd, hg_f_bwd, hg_i, hg_g, lower_bounds[:, layer], hg_norm_g[layer])
        mixed = jnp.concatenate([attn_out, rec_out], axis=-1) @ w_out[layer]
        x = x + gate1[:, None, :] * mixed
        h2 = modulate(rms_norm(x, norm2_g[layer]), shift2, scale2)
        moe_out = hierarchical_moe(h2, router_group_w[layer], router_group_b[layer], router_expert_w[layer], router_expert_b[layer], w_gate[layer], w_up[layer], w_down[layer])
        x = x + gate2[:, None, :] * moe_out
    return x
```

```python
import numpy as np
import ml_dtypes
from contextlib import ExitStack, nullcontext
import concourse.bass as bass
import concourse.mybir as mybir
from concourse.bass_utils import run_bass_kernel_spmd

F32 = mybir.dt.float32
BF16 = mybir.dt.bfloat16
I32 = mybir.dt.int32
AF = mybir.ActivationFunctionType
ALU = mybir.AluOpType
AX = mybir.AxisListType

NCORES = 8
D = 1024
S = 2048
NT = S // 128
NSEQ = 2
EPS = 1e-6
INCOLS = 3232
NEXP = 32
BIG = 1.0e30


class Buf:
    __slots__ = ("name", "w", "r", "excl")

    def __init__(self, name, excl=False):
        self.name = name
        self.excl = excl
        self.w = None
        self.r = {}


class KB:
    ENG = ("pe", "act", "dve", "pool", "sp")

    def __init__(self, nc):
        self.nc = nc
        self.es = ExitStack()
        self.sems = {}
        self.cnt = {}
        self.waited = {e: {} for e in self.ENG}
        self.prog = {e: [] for e in self.ENG}
        for e in self.ENG:
            self.sems[e] = self.es.enter_context(nc.semaphore("s_" + e))
            self.cnt[e] = 0
        self.nbuf = 0
        self.ninst = 0
        self.nflush = 0
        self.regs = {}

    def buf(self, name=None):
        self.nbuf += 1
        return Buf(name or ("b%d" % self.nbuf))

    def pbuf(self, name=None):
        self.nbuf += 1
        return Buf(name or ("p%d" % self.nbuf), excl=True)

    def dsem(self, key):
        if key not in self.sems:
            self.sems[key] = self.es.enter_context(self.nc.semaphore("d_" + key))
            self.cnt[key] = 0
        return key

    def _waits(self, eng, reads, writes):
        need = {}

        def add(dep):
            if dep is None:
                return
            k, v, e2 = dep
            if need.get(k, 0) < v:
                need[k] = v

        for b in reads:
            add(b.w)
        strict = (eng == "pool")
        for b in writes:
            if b.w is not None and (b.w[2] != eng or strict):
                add(b.w)
            for k, (v, e2) in b.r.items():
                if e2 != eng or strict:
                    add((k, v, e2))
        out = []
        wd = self.waited[eng]
        for k, v in need.items():
            if wd.get(k, 0) < v:
                wd[k] = v
                out.append((k, v))
        return out

    def emit(self, eng, fn, reads=(), writes=(), inc=True):
        if any(b.excl for b in reads):
            writes = list(writes) + [b for b in reads if b.excl and b not in writes]
            reads = [b for b in reads if not b.excl]
        waits = self._waits(eng, reads, writes)
        val = self.cnt[eng] + 1
        if inc:
            self.cnt[eng] = val
        rec_w = (eng, val, eng)
        for b in reads:
            old = b.r.get(eng)
            if old is None or old[0] < val:
                b.r[eng] = (val, eng)
        for b in writes:
            b.w = rec_w
            b.r = {}
        self.prog[eng].append((waits, fn, eng if inc else None, 1))
        self.ninst += 1

    def dma(self, q, fn, key, reads=(), writes=()):
        self.dsem(key)
        waits = self._waits(q, reads, writes)
        val = self.cnt[key] + 16
        self.cnt[key] = val
        for b in reads:
            old = b.r.get(key)
            if old is None or old[0] < val:
                b.r[key] = (val, "dma")
        for b in writes:
            b.w = (key, val, "dma")
            b.r = {}
        self.prog[q].append((waits, fn, key, 16))
        self.ninst += 1

    def barrier(self):
        tgt = {k: v for k, v in self.cnt.items() if v > 0}
        for e in self.ENG:
            waits = []
            for k, v in tgt.items():
                if k == e:
                    continue
                if self.waited[e].get(k, 0) < v:
                    self.waited[e][k] = v
                    waits.append((k, v))
            if waits:
                self.prog[e].append((waits, None, None, 0))

    def flush(self):
        nc = self.nc
        progs = self.prog
        sems = self.sems

        def run(engname, eh):
            for waits, fn, inckey, incv in progs[engname]:
                for k, v in waits:
                    eh.wait_ge(sems[k], v)
                if fn is not None:
                    ins = fn(eh)
                    if inckey is not None:
                        ins.then_inc(sems[inckey], incv)

        with nc.Block() as block:
            @block.tensor
            def _(e):
                run("pe", e)

            @block.scalar
            def _(e):
                run("act", e)

            @block.vector
            def _(e):
                run("dve", e)

            @block.gpsimd
            def _(e):
                run("pool", e)

            @block.sync
            def _(e):
                run("sp", e)
        self.prog = {e: [] for e in self.ENG}
        self.nflush += 1

    def const_reg(self, e, val):
        key = (self.nflush, val)
        if key not in self.regs:
            self.regs[key] = e.to_reg(val)
        return self.regs[key]


def build(dbg=None, stop=None):
    nc = bass.Bass("TRN2", target_bir_lowering=False)
    kb = KB(nc)
    es = kb.es

    def din(name, shape, dt=F32):
        return nc.dram_tensor(name, list(shape), dt, kind="ExternalInput").ap()

    x_d = din("x", [NSEQ, S, D])
    pos_d = din("pos", [128, NSEQ * NT], I32)
    cT_d = din("cT", [128, 8, NSEQ])
    adaw_d = din("ada_w", [128, 8, 6 * D])
    adab_d = din("ada_b", [1, 6 * D])
    g1_d = din("g1", [128, 8])
    g2_d = din("g2", [128, 8])
    win_d = din("w_in", [128, 8, INCOLS])
    qag_d = din("qa_g", [128, 3])
    wq_d = din("wq_up", [128, 3, 768])
    kvag_d = din("kva_g", [128, 2])
    wkv_d = din("wkv_up", [128, 2, 1024])
    qng_d = din("qn_g", [1, 96])
    kng_d = din("kn_g", [1, 96])
    lbl_d = din("lb_logits", [1, 2 * 2 * 512])
    hgn_d = din("hg_norm_g", [1, 128])
    woa_d = din("w_out_a", [64, 8, D])
    wor_d = din("w_out_r", [128, 4, D])
    wr_d = din("w_router", [128, 8, 36])
    br_d = din("b_router", [1, 36])
    wg_d = din("w_gate", [NEXP, 128, 8, 512])
    wu_d = din("w_up", [NEXP, 128, 8, 512])
    wd_d = din("w_down", [NEXP, 128, 4, D])
    identb_d = din("ident_bf", [128, 128], BF16)
    consts_d = din("consts_f", [128, 1024])
    out_d = nc.dram_tensor("out", [NSEQ, S, D], F32, kind="ExternalOutput").ap()
    x1_d = nc.dram_tensor("x1_scratch", [NSEQ, S, D], F32, kind="Internal").ap()
    h2tok_d = nc.dram_tensor("h2tok_scratch", [NSEQ * S, D], BF16, kind="Internal").ap()
    mod2_d = nc.dram_tensor("mod2_scratch", [NSEQ, 2 * D], F32, kind="Internal").ap()
    NBB = 64
    xs_d = nc.dram_tensor("xs_scratch", [NBB * 256, D], BF16, kind="Internal").ap()
    ys_d = nc.dram_tensor("ys_scratch", [NBB * 256, D], F32, kind="Internal").ap()
    g2row_d = din("g2row", [1, D])
    wbf_d = nc.dram_tensor("wbf", [NEXP * 128, 3 * 4096], BF16, kind="Internal").ap()
    cstm_d = din("cstm", [128, 192])
    crow_d = din("crow", [1, 3200])
    dbg_d = {}
    if dbg:
        for k, shp in dbg.items():
            dbg_d[k] = nc.dram_tensor("dbg_" + k, list(shp), F32, kind="ExternalOutput").ap()

    uid = [0]

    def sb(name, shape, dt=F32, stack=es):
        uid[0] += 1
        return stack.enter_context(nc.sbuf_tensor("sb%d_%s" % (uid[0], name), list(shape), dt))

    def ps(name, shape, dt=F32, stack=es):
        uid[0] += 1
        return stack.enter_context(nc.psum_tensor("ps%d_%s" % (uid[0], name), list(shape), dt))

    identb = sb("identb", [128, 128], BF16)
    cst = sb("cst", [128, 1024])
    B_identb, B_cst = kb.buf("identb"), kb.buf("cst")
    kb.dma("sp", lambda e: e.dma_start(out=identb[:], in_=identb_d[:]), "ld_identb", writes=[B_identb])
    kb.dma("sp", lambda e: e.dma_start(out=cst[:], in_=consts_d[:]), "ld_cst", writes=[B_cst])
    ones_f = cst[:, 788:916]
    B_wbf = kb.buf("wbf")
    wsrc = [wg_d.rearrange("e p (h k) n -> e p h (k n)", h=2), wu_d.rearrange("e p (h k) n -> e p h (k n)", h=2),
            wd_d.rearrange("e p (h k) n -> e p h (k n)", h=2)]
    B_xsz = kb.buf("xsz")
    B_xs2 = [kb.buf("xs0"), kb.buf("xs1")]
    pending_cv = []
    if stop is None or stop == "moe":
        for ex in range(NEXP):
            for m_ in range(3):
                pending_cv.append((ex, m_))

    def issue_cv(n=1):
        for _ in range(n):
            if not pending_cv:
                return
            ex, m_ = pending_cv.pop(0)
            kb.dma("pool", lambda e, ex=ex, m_=m_: e.dma_start(
                out=wbf_d[ex * 128:(ex + 1) * 128, m_ * 4096:(m_ + 1) * 4096].rearrange("p (h c) -> p h c", h=2), in_=wsrc[m_][ex]),
                "cv_w", writes=[B_wbf])

    B_modrow = kb.buf("modrow")
    B_mod2d = kb.buf("mod2d")
    modcol = sb("modcol", [128, NSEQ, 4, 8])
    B_modcol = kb.buf("modcol")
    gatebc = sb("gatebc", [128, NSEQ, 2, D], BF16)
    B_gatebc = kb.buf("gatebc")

    pinit = ExitStack()
    onesb = sb("onesb", [128, 128], BF16)
    B_onesb = kb.buf()
    kb.emit("pool", lambda e: e.memset(onesb[:], 1.0), [], [B_onesb])
    wq = sb("wq", [128, 3, 768], BF16)
    wkv = sb("wkv", [128, 2, 1024], BF16)
    gq_bc = sb("gq_bc", [128, 8, 96])
    gk_bc = sb("gk_bc", [128, 8, 96])
    cosT = sb("cosT", [128, NSEQ * NT, 16])
    sinT = sb("sinT", [128, NSEQ * NT, 16])
    B_wq, B_wkv, B_gq, B_gk, B_cs = (kb.buf() for _ in range(5))
    with nullcontext(pinit) as pw:
        wq_f = sb("wq_f", [128, 3, 768], F32, pw)
        wkv_f = sb("wkv_f", [128, 2, 1024], F32, pw)
        qag = sb("qag", [128, 3], F32, pw)
        kvag = sb("kvag", [128, 2], F32, pw)
        g96 = sb("g96", [128, 2, 96], F32, pw)
        posi = sb("posi", [128, NSEQ * NT], I32, pw)
        posf = sb("posf", [128, NSEQ * NT], F32, pw)
        ang = sb("ang", [128, NSEQ * NT, 16], F32, pw)
        ang2 = sb("ang2", [128, NSEQ * NT, 16], F32, pw)
        B_wqf, B_wkvf, B_qag, B_kvag, B_g96, B_posi, B_posf, B_ang, B_ang2 = (kb.buf() for _ in range(9))
        kb.dma("sp", lambda e: e.dma_start(out=wq_f[:], in_=wq_d[:]), "ld_wqf", writes=[B_wqf])
        kb.dma("sp", lambda e: e.dma_start(out=wkv_f[:], in_=wkv_d[:]), "ld_wkvf", writes=[B_wkvf])
        kb.dma("sp", lambda e: e.dma_start(out=qag[:], in_=qag_d[:]), "ld_qag", writes=[B_qag])
        kb.dma("sp", lambda e: e.dma_start(out=kvag[:], in_=kvag_d[:]), "ld_kvag", writes=[B_kvag])
        kb.dma("sp", lambda e: e.dma_start(out=g96[:, 0, :], in_=qng_d.partition_broadcast(128)), "ld_g96", writes=[B_g96])
        kb.dma("sp", lambda e: e.dma_start(out=g96[:, 1, :], in_=kng_d.partition_broadcast(128)), "ld_g96", writes=[B_g96])
        kb.dma("sp", lambda e: e.dma_start(out=posi[:], in_=pos_d[:]), "ld_pos", writes=[B_posi])
        for c in range(3):
            kb.emit("dve", lambda e, c=c: e.tensor_scalar_mul(out=wq[:, c, :], in0=wq_f[:, c, :], scalar1=qag[:, c:c + 1]),
                    [B_wqf, B_qag], [B_wq])
        for c in range(2):
            kb.emit("dve", lambda e, c=c: e.tensor_scalar_mul(out=wkv[:, c, :], in0=wkv_f[:, c, :], scalar1=kvag[:, c:c + 1]),
                    [B_wkvf, B_kvag], [B_wkv])
        kb.emit("dve", lambda e: e.tensor_scalar_mul(out=gq_bc[:], in0=g96[:, 0:1, :].to_broadcast([128, 8, 96]),
                                                     scalar1=float(96 ** -0.5)), [B_g96], [B_gq])
        kb.emit("dve", lambda e: e.tensor_copy(out=gk_bc[:], in_=g96[:, 1:2, :].to_broadcast([128, 8, 96])), [B_g96], [B_gk])
        kb.emit("dve", lambda e: e.tensor_copy(out=posf[:], in_=posi[:]), [B_posi], [B_posf])
        kb.emit("dve", lambda e: e.tensor_tensor(out=ang[:], in0=posf[:].unsqueeze(2).to_broadcast([128, NSEQ * NT, 16]),
                                                 in1=cst[:, 514:530].unsqueeze(1).to_broadcast([128, NSEQ * NT, 16]),
                                                 op=ALU.mult), [B_posf, B_cst], [B_ang])
        PI = float(np.pi)
        angi = sb("angi", [128, NSEQ * NT, 16], I32, pw)
        B_angi = kb.buf()

        def sin_table(dst, shift):
            kb.emit("dve", lambda e: e.tensor_scalar(out=ang2[:], in0=ang[:], scalar1=float(1.0 / (2 * PI)), scalar2=None, op0=ALU.mult),
                    [B_ang], [B_ang2])
            kb.emit("dve", lambda e: e.tensor_copy(out=angi[:], in_=ang2[:]), [B_ang2], [B_angi])
            kb.emit("dve", lambda e: e.tensor_copy(out=ang2[:], in_=angi[:]), [B_angi], [B_ang2])
            kb.emit("dve", lambda e: e.scalar_tensor_tensor(out=ang2[:], in0=ang2[:], scalar=-2 * PI, in1=ang[:], op0=ALU.mult, op1=ALU.add),
                    [B_ang2, B_ang], [B_ang2])
            if shift != 0.0:
                kb.emit("dve", lambda e: e.tensor_scalar(out=ang2[:], in0=ang2[:], scalar1=float(shift), scalar2=None, op0=ALU.add),
                        [B_ang2], [B_ang2])
            kb.emit("dve", lambda e: e.tensor_scalar(out=angi[:].bitcast(F32), in0=ang2[:], scalar1=PI, scalar2=-2 * PI, op0=ALU.is_gt, op1=ALU.mult),
                    [B_ang2], [B_angi])
            kb.emit("dve", lambda e: e.tensor_tensor(out=ang2[:], in0=ang2[:], in1=angi[:].bitcast(F32), op=ALU.add), [B_ang2, B_angi], [B_ang2])
            kb.emit("dve", lambda e: e.tensor_scalar(out=angi[:].bitcast(F32), in0=ang2[:], scalar1=-PI, scalar2=2 * PI, op0=ALU.is_lt, op1=ALU.mult),
                    [B_ang2], [B_angi])
            kb.emit("dve", lambda e: e.tensor_tensor(out=ang2[:], in0=ang2[:], in1=angi[:].bitcast(F32), op=ALU.add), [B_ang2, B_angi], [B_ang2])
            kb.emit("act", lambda e: e.activation(out=dst[:], in_=ang2[:], func=AF.Sin), [B_ang2], [B_cs])

        sin_table(sinT, 0.0)
        sin_table(cosT, PI / 2)

    with nullcontext(pinit) as p0:
        modrow = sb("modrow", [2, 6 * D], F32, p0)
        cT = sb("cT", [128, 8, NSEQ], F32, p0)
        cact = sb("cact", [128, 8, NSEQ], F32, p0)
        adab = sb("adab", [2, 6 * D], F32, p0)
        g12 = sb("g12", [128, 2, 8], F32, p0)
        B_cT, B_cact, B_adab, B_g12 = kb.buf(), kb.buf(), kb.buf(), kb.buf()
        kb.dma("sp", lambda e: e.dma_start(out=cT[:], in_=cT_d[:]), "ld_cT", writes=[B_cT])
        kb.dma("sp", lambda e: e.dma_start(out=adab[0:1, :], in_=adab_d[:]), "ld_adab", writes=[B_adab])
        kb.dma("sp", lambda e: e.dma_start(out=adab[1:2, :], in_=adab_d[:]), "ld_adab", writes=[B_adab])
        kb.dma("sp", lambda e: e.dma_start(out=g12[:, 0, :], in_=g1_d[:]), "ld_g12", writes=[B_g12])
        kb.dma("sp", lambda e: e.dma_start(out=g12[:, 1, :], in_=g2_d[:]), "ld_g12", writes=[B_g12])
        kb.emit("act", lambda e: e.activation(out=cact[:], in_=cT[:], func=AF.Silu), [B_cT], [B_cact])
        wbuf = [sb("adaw%d" % i, [128, 8, 512], F32, p0) for i in range(2)]
        B_wbuf = [kb.buf(), kb.buf()]
        pmod = [ps("pmod%d" % i, [2, 512], F32, p0) for i in range(2)]
        B_pmod = [kb.pbuf(), kb.pbuf()]
        for n in range(12):
            j = n % 2
            kb.dma("sp", lambda e, n=n, j=j: e.dma_start(out=wbuf[j][:], in_=adaw_d[:, :, n * 512:(n + 1) * 512]),
                   "ld_adaw%d" % j, writes=[B_wbuf[j]])
            for kc in range(8):
                kb.emit("pe", lambda e, j=j, kc=kc: e.matmul(pmod[j][:], lhsT=cact[:, kc, :], rhs=wbuf[j][:, kc, :],
                                                              start=(kc == 0), stop=(kc == 7)),
                        [B_cact, B_wbuf[j]], [B_pmod[j]], inc=(kc == 7))
            kb.emit("dve", lambda e, n=n, j=j: e.tensor_tensor(out=modrow[:, n * 512:(n + 1) * 512], in0=pmod[j][:],
                                                               in1=adab[:, n * 512:(n + 1) * 512], op=ALU.add),
                    [B_pmod[j], B_adab], [B_modrow])
        pcol = ps("pcol", [128, 64], F32, p0)
        B_pcol = kb.pbuf()
        col_src = [1, 0, 4, 3]
        for s in range(NSEQ):
            for j in range(4):
                for kc in range(8):
                    c0 = col_src[j] * D + kc * 128
                    idx = (s * 4 + j) * 8 + kc
                    kb.emit("pe", lambda e, s=s, c0=c0, idx=idx: e.matmul(
                        pcol[:, idx:idx + 1], lhsT=modrow[0:2, c0:c0 + 128], rhs=cst[0:2, 530 + s:531 + s],
                        start=True, stop=True), [B_modrow, B_cst], [B_pcol], inc=(j == 3 and kc == 7 and s == NSEQ - 1))
        kb.emit("dve", lambda e: e.tensor_copy(out=modcol[:].rearrange("p s j k -> p (s j k)"), in_=pcol[:]),
                [B_pcol], [B_modcol])
        for s in range(NSEQ):
            for jj, gi in ((0, 0), (2, 1)):
                kb.emit("dve", lambda e, s=s, jj=jj, gi=gi: e.scalar_tensor_tensor(
                    out=modcol[:, s, jj, :], in0=modcol[:, s, jj, :], scalar=1.0, in1=g12[:, gi, :],
                    op0=ALU.add, op1=ALU.mult), [B_modcol, B_g12], [B_modcol])
        pbc = [ps("pbc%d" % i, [128, 512], F32, p0) for i in range(2)]
        B_pbc = [kb.pbuf(), kb.pbuf()]
        t = 0
        for s in range(NSEQ):
            for g, base in ((0, 2 * D), (1, 5 * D)):
                for hh in range(2):
                    j = t % 2
                    t += 1
                    kb.emit("pe", lambda e, s=s, base=base, hh=hh, j=j: e.matmul(
                        pbc[j][:], lhsT=cst[0:2, 532 + s * 128:532 + (s + 1) * 128],
                        rhs=modrow[0:2, base + hh * 512:base + (hh + 1) * 512], start=True, stop=True),
                        [B_modrow, B_cst], [B_pbc[j]])
                    kb.emit("act", lambda e, s=s, g=g, hh=hh, j=j: e.copy(
                        out=gatebc[:, s, g, hh * 512:(hh + 1) * 512], in_=pbc[j][:]), [B_pbc[j]], [B_gatebc])
        if dbg and "modcol" in dbg:
            kb.dma("sp", lambda e: e.dma_start(out=dbg_d["modcol"][:], in_=modcol[:].rearrange("p s j k -> p (s j k)")),
                   "st_dbg", reads=[B_modcol])
        g2r = sb("g2r", [2, D], F32, p0)
        B_g2r = kb.buf()
        for r_ in range(2):
            kb.dma("sp", lambda e, r_=r_: e.dma_start(out=g2r[r_:r_ + 1, :], in_=g2row_d[:]), "ld_g2r", writes=[B_g2r])
        kb.emit("dve", lambda e: e.scalar_tensor_tensor(out=g2r[:], in0=modrow[:, 4 * D:5 * D], scalar=1.0, in1=g2r[:], op0=ALU.add, op1=ALU.mult),
                [B_modrow, B_g2r], [B_g2r])
        kb.dma("sp", lambda e: e.dma_start(out=mod2_d[:, 0:D], in_=g2r[:]), "st_mod2", reads=[B_g2r], writes=[B_mod2d])
        kb.dma("sp", lambda e: e.dma_start(out=mod2_d[:, D:2 * D], in_=modrow[:, 3 * D:4 * D]), "st_mod2", reads=[B_modrow], writes=[B_mod2d])
        kb.barrier()
        kb.flush()
    pinit.close()

    wr = sb("wr", [128, 8, 36], BF16)
    brb = sb("brb", [128, 36])
    EIDX = sb("EIDX", [128, NSEQ * NT, 2])
    WK = sb("WK", [128, NSEQ * NT, 2])
    iota_e = sb("iota_e", [128, 32])
    B_wr, B_brb, B_iota = kb.buf(), kb.buf(), kb.buf()
    B_EW = [kb.buf() for _ in range(NSEQ * NT)]
    kb.dma("sp", lambda e: e.dma_start(out=iota_e[:], in_=cstm_d[:, 128:160]), "ld_iota", writes=[B_iota])
    kb.dma("pool", lambda e: e.dma_start(out=wr[:], in_=wr_d[:]), "ld_wr", writes=[B_wr])
    kb.dma("sp", lambda e: e.dma_start(out=brb[:], in_=br_d.partition_broadcast(128)), "ld_brb", writes=[B_brb])
    B_x1d = [[kb.buf() for _ in range(NT)] for _ in range(NSEQ)]
    B_h2d = [[kb.buf() for _ in range(NT)] for _ in range(NSEQ)]

    def dbg_store(name, ap, bufs):
        if dbg and name in dbg:
            kb.dma("sp", lambda e: e.dma_start(out=dbg_d[name][:], in_=ap), "st_dbg", reads=bufs)

    class NormT:
        def __init__(self, stack, tag, ntp=2, dve_evac=False):
            self.dve_evac = dve_evac
            self.xn = [sb("xn%s%d" % (tag, i), [128, D], BF16, stack) for i in range(2)]
            self.st = sb("st" + tag, [128, 2, 4], F32, stack)
            tps = [ps("tp%s%d" % (tag, i), [128, 8, 128], BF16, stack) for i in range(ntp)]
            btp = [kb.pbuf() for _ in range(ntp)]
            self.tp = [tps[i % ntp] for i in range(2)]
            self.B_tp = [btp[i % ntp] for i in range(2)]
            self.B_xn = [kb.buf(), kb.buf()]
            self.B_st = [kb.buf(), kb.buf()]
            self.n = 0

        def run(self, src, B_src, s, jg, dst, B_dst):
            j = self.n % 2
            self.n += 1
            issue_cv(1)
            st, xn, tp = self.st, self.xn[j], self.tp[j]
            junk = xn
            B_st, B_xn, B_tp = self.B_st[j], self.B_xn[j], self.B_tp[j]
            kb.emit("act", lambda e: e.activation(out=junk[:], in_=src, func=AF.Square, accum_out=st[:, j, 0:1]),
                    [B_src], [B_xn, B_st])
            kb.emit("act", lambda e: e.activation(out=st[:, j, 1:2], in_=st[:, j, 0:1], func=AF.Ln, scale=1.0 / D, bias=cst[:, 919:920]),
                    [B_st, B_cst], [B_st])
            kb.emit("act", lambda e: e.activation(out=st[:, j, 2:3], in_=st[:, j, 1:2], func=AF.Exp, scale=-0.5), [B_st], [B_st])
            kb.emit("dve", lambda e: e.tensor_scalar_mul(out=xn[:], in0=src, scalar1=st[:, j, 2:3]), [B_src, B_st], [B_xn])
            for kc in range(8):
                kb.emit("pe", lambda e, kc=kc: e.transpose(out=tp[:, kc, :], in_=xn[:, kc * 128:(kc + 1) * 128], identity=identb[:]),
                        [B_xn, B_identb], [B_tp], inc=(kc == 7))
            if self.dve_evac:
                kb.emit("dve", lambda e: e.tensor_tensor(out=dst, in0=tp[:], in1=modcol[:, s, jg, :].unsqueeze(2).to_broadcast([128, 8, 128]), op=ALU.mult),
                        [B_tp, B_modcol], [B_dst])
                kb.emit("pool", lambda e: e.tensor_tensor(out=dst, in0=dst, in1=modcol[:, s, jg + 1, :].unsqueeze(2).to_broadcast([128, 8, 128]), op=ALU.add),
                        [B_dst, B_modcol], [B_dst])
            else:
                for kc in range(8):
                    kb.emit("act", lambda e, kc=kc: e.activation(out=dst[:, kc, :], in_=tp[:, kc, :], func=AF.Identity,
                                                                 bias=modcol[:, s, jg + 1, kc:kc + 1], scale=modcol[:, s, jg, kc:kc + 1]),
                            [B_tp, B_modcol], [B_dst])
            return xn, B_xn

    nseq_run = NSEQ if stop is None else 1
    for s in range(nseq_run):
        with ExitStack() as sq_:
            OT = sb("OT", [64, 8, S], BF16, sq_)
            B_OT = kb.buf()

            with ExitStack() as pm:
                QT = sb("QT", [128, 8, S if stop != "hT" else 4], BF16, pm)
                KT = sb("KT", [128, 8, S if stop != "hT" else 4], BF16, pm)
                V2 = sb("V2", [128, NT, 8, 65], BF16, pm)
                rstdk = sb("rstdk", [128, NT, 8], F32, pm)
                B_QT, B_KT, B_V2, B_rk = kb.buf(), kb.buf(), kb.buf(), kb.buf()
                kb.emit("pool", lambda e: e.memset(V2[:, :, :, 64:65], 1.0), [], [B_V2])
                with ExitStack() as pb:
                    winm = sb("winm", [128, 8, 672], BF16, pb)
                    B_winm = kb.buf()
                    kb.dma("pool", lambda e: e.dma_start(out=winm[:], in_=win_d[:, :, 0:672]), "ld_winm", writes=[B_winm])
                    xt = [sb("xt0", [128, D], F32, pb), sb("xt1", [128, D], F32, pb)]
                    B_xt = [kb.buf(), kb.buf()]
                    nrm = NormT(pb, "a", ntp=1)
                    hTc = sb("hTc", [128, 8, 512], BF16, pb)
                    B_hTc = [kb.buf() for _ in range(4)]
                    latT = sb("latT", [128, 5, 512], BF16, pb)
                    sqT = sb("sqT", [128, 5, 512], BF16, pb)
                    B_latT, B_sqT = kb.buf(), kb.buf()
                    plat = [ps("plat%d" % i, [128, 512], F32, pb) for i in range(2)]
                    B_plat = [kb.pbuf(), kb.pbuf()]
                    pq = ps("pq", [128, 1024], F32, pb)
                    B_pq = kb.pbuf()
                    pkv = ps("pkv", [128, 1024], F32, pb)
                    B_pkv = kb.pbuf()
                    psm = ps("psm", [128, 64], F32, pb)
                    B_pss = kb.pbuf()
                    B_pkr = B_pss
                    ptq = nrm.tp[0]
                    B_ptq = nrm.B_tp[0]
                    rst4 = sb("rst", [128, 4, 4], F32, pb)
                    B_rst = kb.buf()
                    hstk = sb("hstk", [128, 3, 8], F32, pb)
                    rpk = sb("rpk", [128, 4, 1, 16], F32, pb)
                    B_hstk, B_rpk = kb.buf(), kb.buf()
                    qf = sb("qf", [128, 8, 96], F32, pb)
                    qsq = sb("qsq", [128, 8, 96], F32, pb)
                    qn = sb("qn", [128, 8, 96], F32, pb)
                    hst = sb("hst", [128, 3, 8], F32, pb)
                    rp_q = sb("rp", [128, 4, 8, 16], F32, pb)
                    qfin = sb("qfin", [128, 8, 96], BF16, pb)
                    kvf = sb("kvf", [128, 8, 128], F32, pb)
                    ksq = sb("ksq", [128, 8, 64], F32, pb)
                    krf = sb("krf", [128, 3, 32], F32, pb)
                    kst = sb("kst", [128, 4], F32, pb)
                    kfin = sb("kfin", [128, 8, 96], BF16, pb)
                    B_qf, B_qsq, B_qn, B_hst, B_rp_q, B_qfin, B_kvf, B_ksq, B_krf, B_kst, B_kfin = (kb.buf() for _ in range(11))

                    def rope(src3, dst3, cs_i, nh, Bsrc, Bdst, rp=None, B_rp=None):
                        if rp is None:
                            rp, B_rp = rp_q, B_rp_q
                        cb = cosT[:, cs_i:cs_i + 1, :].to_broadcast([128, nh, 16])
                        sbb = sinT[:, cs_i:cs_i + 1, :].to_broadcast([128, nh, 16])
                        x1 = src3[:, :, 0:16]
                        x2 = src3[:, :, 16:32]
                        r = rp[:, :, 0:nh, :]
                        kb.emit("dve", lambda e: e.tensor_tensor(out=r[:, 0], in0=x1, in1=cb, op=ALU.mult), [Bsrc, B_cs], [B_rp])
                        kb.emit("dve", lambda e: e.tensor_tensor(out=r[:, 1], in0=x2, in1=sbb, op=ALU.mult), [Bsrc, B_cs], [B_rp])
                        kb.emit("dve", lambda e: e.tensor_tensor(out=r[:, 2], in0=x2, in1=cb, op=ALU.mult), [Bsrc, B_cs], [B_rp])
                        kb.emit("dve", lambda e: e.tensor_tensor(out=r[:, 3], in0=x1, in1=sbb, op=ALU.mult), [Bsrc, B_cs], [B_rp])
                        kb.emit("dve", lambda e: e.tensor_tensor(out=dst3[:, :, 0:16], in0=r[:, 0], in1=r[:, 1], op=ALU.subtract),
                                [B_rp], [Bdst])
                        kb.emit("dve", lambda e: e.tensor_tensor(out=dst3[:, :, 16:32], in0=r[:, 2], in1=r[:, 3], op=ALU.add),
                                [B_rp], [Bdst])

                    ncc = 4 if stop != "hT" else 1
                    def ld_x(i):
                        j = i % 2
                        kb.dma("sp", lambda e: e.dma_start(out=xt[j][:], in_=x_d[s, i * 128:(i + 1) * 128, :]), "ld_xt%d" % j, writes=[B_xt[j]])

                    ld_x(0)
                    ld_x(1)
                    for cc in range(ncc):
                        for ti in range(4):
                            i = cc * 4 + ti
                            j = i % 2
                            nrm.run(xt[j][:], B_xt[j], s, 0, hTc[:, :, ti * 128:(ti + 1) * 128], B_hTc[ti])
                            if i + 2 < 4 * ncc:
                                ld_x(i + 2)
                        if stop == "hT":
                            break
                        for jc in range(5):
                            pj = jc % 2
                            for kc in range(8):
                                kb.emit("pe", lambda e, pj=pj, jc=jc, kc=kc: e.matmul(
                                    plat[pj][:], lhsT=winm[:, kc, jc * 128:(jc + 1) * 128], rhs=hTc[:, kc, :],
                                    start=(kc == 0), stop=(kc == 7)), [B_winm] + B_hTc, [B_plat[pj]], inc=(kc == 7))
                            kb.emit("act", lambda e, pj=pj, jc=jc: e.copy(out=latT[:, jc, :], in_=plat[pj][:]), [B_plat[pj]], [B_latT])
                            kb.emit("act", lambda e, pj=pj, jc=jc: e.activation(out=sqT[:, jc, :], in_=plat[pj][:], func=AF.Square),
                                    [B_plat[pj]], [B_sqT])
                        for ti in range(4):
                            t0 = ti * 128
                            rst = rst4[:, ti, :]
                            for jc in range(5):
                                col = 0 if jc < 3 else 1
                                kb.emit("pe", lambda e, jc=jc, col=col, t0=t0: e.matmul(
                                    psm[:, col:col + 1], lhsT=sqT[:, jc, t0:t0 + 128], rhs=onesb[:, 0:1],
                                    start=(jc in (0, 3)), stop=(jc in (2, 4))), [B_sqT, B_onesb], [B_pss], inc=(jc == 4))
                            kb.emit("dve", lambda e, rst=rst: e.tensor_tensor(out=rst[:, 0:2], in0=psm[:, 0:2], in1=cst[:, 916:918], op=ALU.mult),
                                    [B_pss, B_cst], [B_rst])
                            kb.emit("act", lambda e, rst=rst: e.activation(out=rst[:, 2:4], in_=rst[:, 0:2], func=AF.Ln, bias=cst[:, 919:920]),
                                    [B_rst, B_cst], [B_rst])
                            kb.emit("act", lambda e, rst=rst: e.activation(out=rst[:, 2:4], in_=rst[:, 2:4], func=AF.Exp, scale=-0.5), [B_rst], [B_rst])

                        def qchain(ti):
                            i = cc * 4 + ti
                            gi = s * NT + i
                            t0 = ti * 128
                            rst = rst4[:, ti, :]
                            for c in range(3):
                                kb.emit("pe", lambda e, c=c: e.matmul(pq[:, 0:512], lhsT=latT[:, c, t0:t0 + 128], rhs=wq[:, c, 0:512],
                                                                      start=(c == 0), stop=(c == 2)), [B_latT, B_wq], [B_pq], inc=False)
                            for c in range(3):
                                kb.emit("pe", lambda e, c=c: e.matmul(pq[:, 512:768], lhsT=latT[:, c, t0:t0 + 128], rhs=wq[:, c, 512:768],
                                                                      start=(c == 0), stop=(c == 2)), [B_latT, B_wq], [B_pq], inc=(c == 2))
                            yield
                            qf2 = qf[:].rearrange("p h d -> p (h d)")
                            kb.emit("act", lambda e: e.activation(out=qf2, in_=pq[:, 0:768], func=AF.Copy, scale=rst[:, 2:3]), [B_pq, B_rst], [B_qf])
                            yield
                            kb.emit("act", lambda e: e.activation(out=qsq[:], in_=qf[:], func=AF.Square), [B_qf], [B_qsq])
                            yield
                            kb.emit("dve", lambda e: e.tensor_reduce(out=hst[:, 0, :], in_=qsq[:], axis=AX.X, op=ALU.add), [B_qsq], [B_hst])
                            yield
                            kb.emit("act", lambda e: e.activation(out=hst[:, 1, :], in_=hst[:, 0, :], func=AF.Ln, scale=1.0 / 96, bias=cst[:, 919:920]),
                                    [B_hst, B_cst], [B_hst])
                            kb.emit("act", lambda e: e.activation(out=hst[:, 2, :], in_=hst[:, 1, :], func=AF.Exp, scale=-0.5), [B_hst], [B_hst])
                            yield
                            kb.emit("dve", lambda e: e.tensor_tensor(out=qn[:], in0=qf[:], in1=hst[:, 2, :].unsqueeze(2).to_broadcast([128, 8, 96]),
                                                                     op=ALU.mult), [B_qf, B_hst], [B_qn])
                            yield
                            kb.emit("dve", lambda e: e.tensor_tensor(out=qn[:], in0=qn[:], in1=gq_bc[:], op=ALU.mult), [B_qn, B_gq], [B_qn])
                            yield
                            kb.emit("act", lambda e: e.copy(out=qfin[:, :, 0:64], in_=qn[:, :, 0:64]), [B_qn], [B_qfin])
                            rope(qn[:, :, 64:96], qfin[:, :, 64:96], gi, 8, B_qn, B_qfin)
                            yield
                            for h in range(8):
                                kb.emit("pe", lambda e, h=h: e.transpose(out=ptq[0:96, h, :], in_=qfin[:, h, :], identity=identb[:]),
                                        [B_qfin, B_identb], [B_ptq], inc=(h == 7))
                            kb.emit("act", lambda e: e.copy(out=QT[0:96, :, i * 128:(i + 1) * 128], in_=ptq[0:96, :, :]), [B_ptq], [B_QT])
                            yield

                        def kchain(ti):
                            i = cc * 4 + ti
                            gi = s * NT + i
                            t0 = ti * 128
                            rst = rst4[:, ti, :]
                            hst_ = hstk
                            for hh in range(2):
                                for c in range(2):
                                    kb.emit("pe", lambda e, c=c, hh=hh: e.matmul(
                                        pkv[:, hh * 512:(hh + 1) * 512], lhsT=latT[:, 3 + c, t0:t0 + 128], rhs=wkv[:, c, hh * 512:(hh + 1) * 512],
                                        start=(c == 0), stop=(c == 1)), [B_latT, B_wkv], [B_pkv], inc=(c == 1 and hh == 1))
                            for kc in range(8):
                                kb.emit("pe", lambda e, kc=kc: e.matmul(psm[:, 32:64], lhsT=hTc[:, kc, t0:t0 + 128], rhs=winm[:, kc, 640:672],
                                                                        start=(kc == 0), stop=(kc == 7)), [B_hTc[ti], B_winm], [B_pkr], inc=(kc == 7))
                            yield
                            kvf2 = kvf[:].rearrange("p h d -> p (h d)")
                            kb.emit("act", lambda e: e.activation(out=kvf2, in_=pkv[:], func=AF.Copy, scale=rst[:, 3:4]), [B_pkv, B_rst], [B_kvf])
                            yield
                            kb.emit("pool", lambda e: e.tensor_copy(out=V2[:, i, :, 0:64], in_=kvf[:, :, 64:128]), [B_kvf], [B_V2])
                            kb.emit("act", lambda e: e.copy(out=krf[:, 0, :], in_=psm[:, 32:64]), [B_pkr], [B_krf])
                            kb.emit("act", lambda e: e.activation(out=krf[:, 1, :], in_=krf[:, 0, :], func=AF.Square, accum_out=kst[:, 0:1]),
                                    [B_krf], [B_krf, B_kst])
                            yield
                            kb.emit("act", lambda e: e.activation(out=ksq[:], in_=kvf[:, :, 0:64], func=AF.Square), [B_kvf], [B_ksq])
                            yield
                            kb.emit("dve", lambda e: e.tensor_reduce(out=hst_[:, 0, :], in_=ksq[:], axis=AX.X, op=ALU.add), [B_ksq], [B_hstk])
                            kb.emit("dve", lambda e: e.tensor_scalar(out=hst_[:, 1, :], in0=hst_[:, 0, :], scalar1=kst[:, 0:1], scalar2=1.0 / 96,
                                                                     op0=ALU.add, op1=ALU.mult), [B_hstk, B_kst], [B_hstk])
                            yield
                            kb.emit("act", lambda e: e.activation(out=hst_[:, 2, :], in_=hst_[:, 1, :], func=AF.Ln, bias=cst[:, 919:920]),
                                    [B_hstk, B_cst], [B_hstk])
                            kb.emit("act", lambda e: e.activation(out=rstdk[:, i, :], in_=hst_[:, 2, :], func=AF.Exp, scale=-0.5), [B_hstk], [B_rk])
                            yield
                            kb.emit("dve", lambda e: e.tensor_tensor(out=kfin[:, :, 0:64], in0=kvf[:, :, 0:64], in1=gk_bc[:, :, 0:64], op=ALU.mult),
                                    [B_kvf, B_gk], [B_kfin])
                            yield
                            kb.emit("dve", lambda e: e.tensor_tensor(out=krf[:, 1, :], in0=krf[:, 0, :], in1=gk_bc[:, 0, 64:96], op=ALU.mult),
                                    [B_krf, B_gk], [B_krf])
                            rope(krf[:, 1:2, :], krf[:, 2:3, :], gi, 1, B_krf, B_krf, rpk, B_rpk)
                            yield
                            kb.emit("dve", lambda e: e.tensor_copy(out=kfin[:, :, 64:96], in_=krf[:, 2:3, :].to_broadcast([128, 8, 32])),
                                    [B_krf], [B_kfin])
                            for h in range(8):
                                kb.emit("pe", lambda e, h=h: e.transpose(out=ptq[0:96, h, :], in_=kfin[:, h, :], identity=identb[:]),
                                        [B_kfin, B_identb], [B_ptq], inc=(h == 7))
                            kb.emit("act", lambda e: e.copy(out=KT[0:96, :, i * 128:(i + 1) * 128], in_=ptq[0:96, :, :]), [B_ptq], [B_KT])
                            yield

                        def run_il(gens):
                            gens = list(gens)
                            while gens:
                                for g in list(gens):
                                    try:
                                        next(g)
                                    except StopIteration:
                                        gens.remove(g)

                        run_il([qchain(0)])
                        for ti in range(4):
                            gl_ = [kchain(ti)]
                            if ti + 1 < 4:
                                gl_.append(qchain(ti + 1))
                            run_il(gl_)
                    if dbg and "hT" in dbg:
                        hTf = sb("hTf", [128, 8, 512], F32, pb)
                        B_hTf = kb.buf()
                        kb.emit("dve", lambda e: e.tensor_copy(out=hTf[:], in_=hTc[:]), B_hTc, [B_hTf])
                        dbg_store("hT", hTf[:].rearrange("p k t -> p (k t)"), [B_hTf])
                    kb.barrier()
                    kb.flush()
                if stop == "hT":
                    break
                if dbg and "QT" in dbg:
                    with ExitStack() as pd:
                        tf = sb("QTf", [128, 8, 512], F32, pd)
                        B_tf = kb.buf()
                        for nm, src, Bs in (("QT", QT, B_QT), ("KT", KT, B_KT)):
                            dv = dbg_d[nm].rearrange("p (k t) -> p k t", k=8)
                            for cc in range(4):
                                kb.emit("dve", lambda e, src=src, cc=cc: e.tensor_copy(out=tf[0:96], in_=src[0:96, :, cc * 512:(cc + 1) * 512]), [Bs], [B_tf])
                                kb.dma("sp", lambda e, dv=dv, cc=cc: e.dma_start(out=dv[:, :, cc * 512:(cc + 1) * 512], in_=tf[0:96]), "st_dbg", reads=[B_tf])
                        dbg_store("rstdk", rstdk[:].rearrange("p i h -> p (i h)"), [B_rk])
                        kb.barrier(); kb.flush()
                if stop == "QK":
                    break

                with ExitStack() as pat:
                    pst = [ps("pst%d" % i, [128, 512], F32, pat) for i in range(4)]
                    B_pst = [kb.pbuf() for _ in range(4)]
                    po = [ps("po%d" % i, [128, 512], F32, pat) for i in range(2)]
                    B_po = [kb.pbuf() for _ in range(2)]
                    pbc2 = ps("pbc2", [64, 512], F32, pat)
                    B_pbc2 = kb.pbuf()
                    pT = [sb("pT%d" % i, [128, 512], BF16, pat) for i in range(4)]
                    B_pT = [kb.buf() for _ in range(4)]
                    rec = sb("rec", [128, 512], F32, pat)
                    B_rec = kb.buf()
                    recb = sb("recb", [64, 512], F32, pat)
                    B_recb = kb.buf()
                    if s == 0 and (stop is None or stop == "moe"):
                        zt = sb("zt", [128, 2048], BF16, pat)
                        B_zt = kb.buf()
                        kb.emit("pool", lambda e: e.memset(zt[:], 0.0), [], [B_zt])
                        xs_v = xs_d.rearrange("(c p r) d -> c p (r d)", p=128, r=2)
                        for c_ in range(xs_v.shape[0]):
                            kb.dma("sp", lambda e, c_=c_: e.dma_start(out=xs_v[c_], in_=zt[:]), "zf_xs", reads=[B_zt], writes=[B_xsz])
                    units = [(h, qc) for h in range(8) for qc in range(4)]
                    steps = [(u, kt) for u in range(len(units)) for kt in range(NT)]

                    def emit_S(n):
                        u, kt = steps[n]
                        h, qc = units[u]
                        j = n % 4
                        kb.emit("pe", lambda e: e.matmul(pst[j][:], lhsT=KT[0:96, h, kt * 128:(kt + 1) * 128],
                                                         rhs=QT[0:96, h, qc * 512:(qc + 1) * 512], start=True, stop=True),
                                [B_KT, B_QT], [B_pst[j]])
                        kb.emit("act", lambda e: e.activation(out=pT[j][:], in_=pst[j][:], func=AF.Exp, scale=rstdk[:, kt, h:h + 1]),
                                [B_pst[j], B_rk], [B_pT[j]])

                    def emit_PV(n):
                        u, kt = steps[n]
                        h, qc = units[u]
                        j = n % 4
                        a = u % 2
                        kb.emit("pe", lambda e: e.matmul(po[a][0:65, :], lhsT=V2[:, kt, h, :], rhs=pT[j][:], start=(kt == 0), stop=(kt == NT - 1)),
                                [B_V2, B_pT[j]], [B_po[a]], inc=(kt == NT - 1))
                        if kt == NT - 1:
                            kb.emit("dve", lambda e: e.reciprocal(out=rec[64:65, :], in_=po[a][64:65, :]), [B_po[a]], [B_rec])
                            kb.emit("pe", lambda e: e.matmul(pbc2[:], lhsT=ones_f[64:65, 0:64], rhs=rec[64:65, :], start=True, stop=True),
                                    [B_rec, B_cst], [B_pbc2])
                            kb.emit("act", lambda e: e.copy(out=recb[:], in_=pbc2[:]), [B_pbc2], [B_recb])
                            kb.emit("dve", lambda e: e.tensor_tensor(out=OT[0:64, h, qc * 512:(qc + 1) * 512], in0=po[a][0:64, :],
                                                                     in1=recb[:], op=ALU.mult), [B_po[a], B_recb], [B_OT])

                    LA = 3
                    for n in range(len(steps) + LA):
                        if n < len(steps):
                            emit_S(n)
                        if n >= LA:
                            emit_PV(n - LA)
                    kb.barrier()
                    kb.flush()
            if dbg and "OT" in dbg:
                with ExitStack() as pd:
                    tf = sb("OTf", [64, 8, S], F32, pd)
                    B_tf = kb.buf()
                    kb.emit("dve", lambda e: e.tensor_copy(out=tf[:], in_=OT[:]), [B_OT], [B_tf])
                    dbg_store("OT", tf[:].rearrange("p k t -> p (k t)"), [B_tf])
                    kb.barrier(); kb.flush()
            if stop == "attn":
                break


            recT = sb("recT", [128, 4, S], BF16, sq_)
            B_recT = kb.buf()
            with ExitStack() as ph:
                winh = sb("winh", [128, 8, 2560], BF16, ph)
                B_winh5 = [kb.buf() for _ in range(5)]
                for cb in (0, 3, 1, 4, 2):
                    kb.dma("pool", lambda e, cb=cb: e.dma_start(out=winh[:, :, cb * 512:(cb + 1) * 512],
                                                                in_=win_d[:, :, 672 + cb * 512:672 + (cb + 1) * 512]),
                           "ld_winh%d" % cb, writes=[B_winh5[cb]])
                ofw = sb("ofw", [128, NT, 512], BF16, ph)
                B_ofw = [kb.buf() for _ in range(NT)]
                lbc = sb("lbc", [128, 2, 512], F32, ph)
                oml = sb("oml", [128, 2, 512], F32, ph)
                hgn = sb("hgn", [128, 128], F32, ph)
                B_lb, B_hgn = kb.buf(), kb.buf()
                with ExitStack() as pl:
                    lraw = sb("lraw", [128, 2, 2, 512], F32, pl)
                    B_lraw = kb.buf()
                    kb.dma("sp", lambda e: e.dma_start(out=lraw[:].rearrange("p a b n -> p (a b n)"), in_=lbl_d.partition_broadcast(128)),
                           "ld_lraw", writes=[B_lraw])
                    kb.dma("sp", lambda e: e.dma_start(out=hgn[:], in_=hgn_d.partition_broadcast(128)), "ld_hgn", writes=[B_hgn])
                    kb.emit("dve", lambda e: e.tensor_tensor(out=lbc[:], in0=lraw[:, :, 0, :], in1=lraw[:, :, 1, :], op=ALU.subtract),
                            [B_lraw], [B_lb])
                    kb.emit("act", lambda e: e.activation(out=lbc[:], in_=lbc[:], func=AF.Sigmoid), [B_lb], [B_lb])
                    kb.emit("dve", lambda e: e.tensor_scalar(out=oml[:], in0=lbc[:], scalar1=-1.0, scalar2=1.0, op0=ALU.mult, op1=ALU.add),
                            [B_lb], [B_lb])
                    kb.barrier()
                    kb.flush()
                xt0 = sb("xth", [128, D], F32, ph)
                B_xt0 = kb.buf()
                nrm = NormT(ph, "h", ntp=1, dve_evac=True)
                hTt2 = [sb("hTt%d" % i, [128, 8, 128], BF16, ph) for i in range(2)]
                B_hTt2 = [kb.buf(), kb.buf()]
                pg = [ps("pg%d" % i, [128, 512], F32, ph) for i in range(2)]
                B_pg = [kb.pbuf() for _ in range(2)]
                pgn = [0]

                def next_pg():
                    j = pgn[0] % 2
                    pgn[0] += 1
                    return pg[j], B_pg[j]

                PA = ps("hPA", [128, 4, 128], F32, ph)
                PK = ps("hPK", [128, 4, 128], F32, ph)
                PI = ps("hPI", [128, 4, 128], F32, ph)
                PAo = ps("hPAo", [128, 4, 128], F32, ph)
                PBo = ps("hPBo", [128, 4, 128], F32, ph)
                B_PA, B_PK, B_PI, B_PAo, B_PBo = (kb.pbuf() for _ in range(5))
                ptr = nrm.tp[0]
                B_ptr = nrm.B_tp[0]
                qq2 = [sb("hq_q%d" % i, [128, 512], F32, ph) for i in range(2)]
                vv = [sb("hq_v%d" % i, [128, 512], BF16, ph) for i in range(3)]
                gg = [sb("hq_g%d" % i, [128, 512], BF16, ph) for i in range(3)]
                sg = sb("hq_sg", [128, 512], F32, ph)
                ff = sb("hq_f", [128, 512], F32, ph)
                kk = sb("hq_k", [128, 512], F32, ph)
                lf = sb("hq_lf", [128, 512], F32, ph)
                eb = sb("hq_eb", [128, 512], F32, ph)
                enb = sb("hq_enb", [128, 512], F32, ph)
                er = sb("hq_er", [128, 512], F32, ph)
                qkd = sb("hq_qkd", [128, 8, 128], BF16, ph)
                kend = [sb("hq_kend%d" % i, [128, 2, 512], BF16, ph) for i in range(2)]
                qkT = [sb("hq_qkT%d" % i, [128, 8, 128], BF16, ph) for i in range(2)]
                qAB = [sb("hq_qAB%d" % i, [128, 2, 4, 128], BF16, ph) for i in range(2)]
                dec = [sb("hq_dec%d" % i, [128, 4, 2], F32, ph) for i in range(2)]
                attm = sb("hq_attm", [128, 4, 128], BF16, ph)
                Sst = sb("hq_S", [128, 4, 128], F32, ph)
                Sbf = sb("hq_Sb", [128, 4, 128], BF16, ph)
                osum = sb("hq_osum", [128, 4, 128], F32, ph)
                osq = sb("hq_osq", [128, 4, 128], F32, ph)
                ost = sb("hq_ost", [128, 3, 4], F32, ph)
                recb_ = sb("hq_rec", [128, 512], BF16, ph)
                (B_sg, B_ff, B_kk, B_lf, B_eb, B_enb, B_er, B_qkd, B_attm, B_osum, B_osq, B_ost, B_rec2) = (kb.buf() for _ in range(13))
                B_qq2 = [kb.buf(), kb.buf()]
                B_vv = [kb.buf(), kb.buf(), kb.buf()]
                B_gg = [kb.buf(), kb.buf(), kb.buf()]
                B_kend = [kb.buf(), kb.buf()]
                B_qkT = [kb.buf(), kb.buf()]
                B_qAB = [kb.buf(), kb.buf()]
                B_dec = [kb.buf(), kb.buf()]
                B_S = [kb.buf() for _ in range(4)]
                B_Sb = [kb.buf() for _ in range(4)]
                for st_ in range(2):
                    kb.emit("pool", lambda e, st_=st_: e.memset(qAB[st_][:], 0.0), [], [B_qAB[st_]])

                def proj(cb, hTt, B_hTt):
                    p, B_p = next_pg()
                    for kc in range(8):
                        kb.emit("pe", lambda e, kc=kc: e.matmul(p[:], lhsT=hTt[:, kc, :], rhs=winh[:, kc, cb * 512:(cb + 1) * 512],
                                                                start=(kc == 0), stop=(kc == 7)), [B_hTt, B_winh5[cb]], [B_p], inc=(kc == 7))
                    return p, B_p

                def stageA1(d, i, n):
                    hTt, B_hTt = hTt2[n % 2], B_hTt2[n % 2]
                    qq, B_qq = qq2[n % 2], B_qq2[n % 2]
                    s3 = n % 3
                    kb.dma("sp", lambda e: e.dma_start(out=xt0[:], in_=x_d[s, i * 128:(i + 1) * 128, :]), "ld_xth", writes=[B_xt0])
                    nrm.run(xt0[:], B_xt0, s, 0, hTt[:], B_hTt)
                    yield
                    p, B_p = proj(0, hTt, B_hTt)
                    kb.emit("act", lambda e: e.activation(out=qq[:], in_=p[:], func=AF.Silu), [B_p], [B_qq])
                    yield
                    if d == 1:
                        p3, B_p3 = proj(4, hTt, B_hTt)
                        kb.emit("act", lambda e: e.activation(out=gg[s3][:], in_=p3[:], func=AF.Silu), [B_p3], [B_gg[s3]])
                        yield
                    p2, B_p2 = proj(3, hTt, B_hTt)
                    kb.emit("act", lambda e: e.copy(out=vv[s3][:], in_=p2[:]), [B_p2], [B_vv[s3]])
                    yield

                def stageA(d, i, n):
                    st = n % 2
                    hTt, B_hTt = hTt2[n % 2], B_hTt2[n % 2]
                    qq, B_qq = qq2[n % 2], B_qq2[n % 2]
                    tri_c = cst[:, 0:128] if d == 0 else cst[:, 128:256]
                    rev_c = cst[:, 256:384] if d == 0 else cst[:, 384:512]
                    p4, B_p4 = proj(1 + d, hTt, B_hTt)
                    kb.emit("act", lambda e: e.activation(out=sg[:], in_=p4[:], func=AF.Sigmoid), [B_p4], [B_sg])
                    kb.emit("dve", lambda e: e.tensor_tensor(out=ff[:], in0=sg[:], in1=oml[:, d, :], op=ALU.mult), [B_sg, B_lb], [B_ff])
                    kb.emit("dve", lambda e: e.tensor_tensor(out=ff[:], in0=ff[:], in1=lbc[:, d, :], op=ALU.add), [B_ff, B_lb], [B_ff])
                    kb.emit("dve", lambda e: e.tensor_scalar(out=kk[:], in0=ff[:], scalar1=-1.0, scalar2=1.0, op0=ALU.mult, op1=ALU.add),
                            [B_ff], [B_kk])
                    kb.emit("act", lambda e: e.activation(out=lf[:], in_=ff[:], func=AF.Ln), [B_ff], [B_lf])
                    yield
                    pb_, B_pb = next_pg()
                    kb.emit("pe", lambda e: e.matmul(pb_[:], lhsT=tri_c, rhs=lf[:], start=True, stop=True), [B_cst, B_lf], [B_pb])
                    kb.emit("act", lambda e: e.activation(out=eb[:], in_=pb_[:], func=AF.Exp), [B_pb], [B_eb])
                    kb.emit("act", lambda e: e.activation(out=enb[:], in_=pb_[:], func=AF.Exp, scale=-1.0), [B_pb], [B_enb])
                    yield
                    pr_, B_pr = next_pg()
                    kb.emit("pe", lambda e: e.matmul(pr_[:], lhsT=rev_c, rhs=lf[:], start=True, stop=True), [B_cst, B_lf], [B_pr])
                    kb.emit("act", lambda e: e.activation(out=er[:], in_=pr_[:], func=AF.Exp), [B_pr], [B_er])
                    yield
                    pd_, B_pd = next_pg()
                    for h in range(4):
                        kb.emit("pe", lambda e, h=h: e.matmul(pd_[:, 2 * h:2 * h + 2], lhsT=lf[:, h * 128:(h + 1) * 128],
                                                              rhs=cst[:, 512:514], start=True, stop=True), [B_lf, B_cst], [B_pd], inc=(h == 3))
                    kb.emit("act", lambda e: e.activation(out=dec[st][:].rearrange("p h c -> p (h c)"), in_=pd_[:, 0:8], func=AF.Exp),
                            [B_pd], [B_dec[st]])
                    kb.emit("dve", lambda e: e.tensor_tensor(out=qkd[:, 0:4, :].rearrange("p h k -> p (h k)"), in0=qq[:], in1=eb[:], op=ALU.mult),
                            [B_qq, B_eb], [B_qkd])
                    kb.emit("dve", lambda e: e.tensor_tensor(out=qkd[:, 4:8, :].rearrange("p h k -> p (h k)"), in0=kk[:], in1=enb[:], op=ALU.mult),
                            [B_kk, B_enb], [B_qkd])
                    for c2 in range(2):
                        kb.emit("dve", lambda e, c2=c2: e.scalar_tensor_tensor(out=kend[st][:, c2, :], in0=er[:], scalar=cst[:, 512 + c2:513 + c2],
                                                                             in1=kk[:], op0=ALU.mult, op1=ALU.mult),
                                [B_kk, B_er, B_cst], [B_kend[st]])
                    yield
                    for j8 in range(8):
                        kb.emit("pe", lambda e, j8=j8: e.transpose(out=ptr[:, j8, :], in_=qkd[:, j8, :], identity=identb[:]),
                                [B_qkd, B_identb], [B_ptr], inc=(j8 == 7))
                    kb.emit("act", lambda e: e.copy(out=qkT[st][:], in_=ptr[:]), [B_ptr], [B_qkT[st]])
                    kb.emit("dve", lambda e: e.tensor_copy(out=qAB[st][:, 0, :, 0:64], in_=ptr[:, 0:4, 0:64]), [B_ptr], [B_qAB[st]])
                    kb.emit("dve", lambda e: e.tensor_copy(out=qAB[st][:, 1, :, 64:128], in_=ptr[:, 0:4, 64:128]), [B_ptr], [B_qAB[st]])
                    yield

                def stageB(d, i, n):
                    st = n % 2
                    s3 = n % 3
                    tri_c = cst[:, 0:128] if d == 0 else cst[:, 128:256]
                    corder = [0, 1] if d == 0 else [1, 0]
                    for h in range(4):
                        kb.emit("pe", lambda e, h=h: e.matmul(PA[:, h, :], lhsT=qkT[st][:, 4 + h, :], rhs=qkT[st][:, h, :], start=True, stop=True),
                                [B_qkT[st]], [B_PA], inc=(h == 3))
                    yield
                    kb.emit("dve", lambda e: e.tensor_tensor(out=attm[:], in0=PA[:], in1=tri_c.unsqueeze(1).to_broadcast([128, 4, 128]), op=ALU.mult),
                            [B_PA, B_cst], [B_attm])
                    yield
                    for ci, cidx in enumerate(corder):
                        Po_, B_Po_ = (PAo, B_PAo) if ci == 0 else (PBo, B_PBo)
                        for h in range(4):
                            hs = slice(h * 128, (h + 1) * 128)
                            if ci == 0:
                                kb.emit("pe", lambda e, h=h, hs=hs: e.matmul(PI[:, h, :], lhsT=attm[:, h, :], rhs=vv[s3][:, hs], start=True, stop=True),
                                        [B_attm, B_vv[s3]], [B_PI], inc=False)
                            kb.emit("pe", lambda e, h=h, cidx=cidx, Po_=Po_: e.matmul(Po_[:, h, :], lhsT=qAB[st][:, cidx, h, :], rhs=Sbf[:, h, :],
                                                                                    start=True, stop=True), [B_qAB[st], B_Sb[h]], [B_Po_], inc=False)
                            kb.emit("pe", lambda e, h=h, cidx=cidx, hs=hs: e.matmul(PK[:, h, :], lhsT=kend[st][:, cidx, hs], rhs=vv[s3][:, hs],
                                                                                  start=True, stop=True), [B_kend[st], B_vv[s3]], [B_PK], inc=(h == 3))
                        yield
                        for h in range(4):
                            kb.emit("dve", lambda e, h=h, cidx=cidx: e.scalar_tensor_tensor(
                                out=Sst[:, h, :], in0=Sst[:, h, :], scalar=dec[st][:, h, cidx:cidx + 1], in1=PK[:, h, :], op0=ALU.mult, op1=ALU.add),
                                [B_S[h], B_dec[st], B_PK], [B_S[h]])
                            kb.emit("pool", lambda e, h=h: e.tensor_copy(out=Sbf[:, h, :], in_=Sst[:, h, :]), [B_S[h]], [B_Sb[h]])
                        yield
                    kb.emit("act", lambda e: e.copy(out=osum[:], in_=PI[:]), [B_PI], [B_osum])
                    kb.emit("dve", lambda e: e.tensor_tensor(out=osum[:], in0=osum[:], in1=PAo[:], op=ALU.add), [B_osum, B_PAo], [B_osum])
                    if d == 0:
                        kb.emit("dve", lambda e: e.tensor_tensor(out=ofw[:, i, :], in0=osum[:].rearrange("p h v -> p (h v)"),
                                                                 in1=PBo[:].rearrange("p h v -> p (h v)"), op=ALU.add), [B_osum, B_PBo], [B_ofw[i]])
                        yield
                        return
                    kb.emit("dve", lambda e: e.tensor_tensor(out=osum[:], in0=osum[:], in1=PBo[:], op=ALU.add), [B_osum, B_PBo], [B_osum])
                    kb.emit("dve", lambda e: e.tensor_tensor(out=osum[:].rearrange("p h v -> p (h v)"), in0=osum[:].rearrange("p h v -> p (h v)"),
                                                             in1=ofw[:, i, :], op=ALU.add), [B_osum, B_ofw[i]], [B_osum])
                    yield
                    kb.emit("act", lambda e: e.activation(out=osq[:], in_=osum[:], func=AF.Square), [B_osum], [B_osq])
                    kb.emit("dve", lambda e: e.tensor_reduce(out=ost[:, 0, :], in_=osq[:], axis=AX.X, op=ALU.add), [B_osq], [B_ost])
                    kb.emit("act", lambda e: e.activation(out=ost[:, 1, :], in_=ost[:, 0, :], func=AF.Ln, scale=1.0 / 128, bias=cst[:, 919:920]),
                            [B_ost, B_cst], [B_ost])
                    kb.emit("act", lambda e: e.activation(out=ost[:, 2, :], in_=ost[:, 1, :], func=AF.Exp, scale=-0.5), [B_ost], [B_ost])
                    kb.emit("dve", lambda e: e.tensor_tensor(out=osum[:], in0=osum[:], in1=ost[:, 2, :].unsqueeze(2).to_broadcast([128, 4, 128]),
                                                             op=ALU.mult), [B_osum, B_ost], [B_osum])
                    kb.emit("dve", lambda e: e.tensor_tensor(out=osum[:], in0=osum[:], in1=hgn[:].unsqueeze(1).to_broadcast([128, 4, 128]),
                                                             op=ALU.mult), [B_osum, B_hgn], [B_osum])
                    kb.emit("dve", lambda e: e.tensor_tensor(out=recb_[:], in0=osum[:].rearrange("p h v -> p (h v)"), in1=gg[s3][:], op=ALU.mult),
                            [B_osum, B_gg[s3]], [B_rec2])
                    yield
                    for h in range(4):
                        kb.emit("pe", lambda e, h=h: e.transpose(out=ptr[:, h, :], in_=recb_[:, h * 128:(h + 1) * 128], identity=identb[:]),
                                [B_rec2, B_identb], [B_ptr], inc=(h == 3))
                    kb.emit("act", lambda e: e.copy(out=recT[:, :, i * 128:(i + 1) * 128], in_=ptr[:, 0:4, :]), [B_ptr], [B_recT])
                    yield

                def run_interleaved(gens):
                    gens = [g for g in gens if g is not None]
                    while gens:
                        for g in list(gens):
                            try:
                                next(g)
                            except StopIteration:
                                gens.remove(g)

                for d in range(2):
                    kb.emit("pool", lambda e: e.memset(Sst[:], 0.0), [], B_S)
                    kb.emit("pool", lambda e: e.memset(Sbf[:], 0.0), [], B_Sb)
                    order = list(range(NT)) if d == 0 else list(range(NT - 1, -1, -1))
                    run_interleaved([stageA1(d, order[0], 0)])
                    run_interleaved([stageA1(d, order[1], 1), stageA(d, order[0], 0)])
                    for n in range(NT):
                        g1 = stageA1(d, order[n + 2], n + 2) if n + 2 < NT else None
                        g2 = stageA(d, order[n + 1], n + 1) if n + 1 < NT else None
                        run_interleaved([g1, g2, stageB(d, order[n], n)])
                kb.barrier()
                kb.flush()
            if dbg and "recT" in dbg:
                with ExitStack() as pd:
                    tf = sb("recTf", [128, 4, S], F32, pd)
                    B_tf = kb.buf()
                    kb.emit("dve", lambda e: e.tensor_copy(out=tf[:], in_=recT[:]), [B_recT], [B_tf])
                    dbg_store("recT", tf[:].rearrange("p k t -> p (k t)"), [B_tf])
                    kb.barrier(); kb.flush()
            if stop in ("hgrn", "hgrn1"):
                break


            with ExitStack() as po_:
                woa = sb("woa", [64, 8, D], BF16, po_)
                wor = sb("wor", [128, 4, D], BF16, po_)
                B_wo = kb.buf()
                with ExitStack() as pst_:
                    stg = sb("wostg", [128, 4, D], F32, pst_)
                    B_stg = kb.buf()
                    g1b = gatebc[:, s, 0, :].unsqueeze(1).to_broadcast([128, 4, D])
                    for part in range(3):
                        if part < 2:
                            kb.dma("sp", lambda e, part=part: e.dma_start(out=stg[0:64], in_=woa_d[:, part * 4:(part + 1) * 4, :]),
                                   "ld_wostg", writes=[B_stg])
                            kb.emit("dve", lambda e, part=part: e.tensor_tensor(out=woa[:, part * 4:(part + 1) * 4, :], in0=stg[0:64],
                                                                              in1=gatebc[0:64, s, 0, :].unsqueeze(1).to_broadcast([64, 4, D]),
                                                                              op=ALU.mult), [B_stg, B_gatebc], [B_wo])
                        else:
                            kb.dma("sp", lambda e: e.dma_start(out=stg[:], in_=wor_d[:]), "ld_wostg", writes=[B_stg])
                            kb.emit("dve", lambda e: e.tensor_tensor(out=wor[:], in0=stg[:], in1=g1b, op=ALU.mult),
                                    [B_stg, B_gatebc], [B_wo])
                    kb.barrier()
                    kb.flush()
                xt0 = sb("xto", [128, D], F32, po_)
                B_xt0 = kb.buf()
                x1t = [sb("x1t%d" % i, [128, D], F32, po_) for i in range(2)]
                B_x1t = [kb.buf(), kb.buf()]
                h2t = [sb("h2t%d" % i, [128, 8, 128], BF16, po_) for i in range(2)]
                B_h2t = [kb.buf(), kb.buf()]
                m2bc = sb("m2bc", [128, 2, D], F32, po_)
                B_m2bc = kb.buf()
                kb.dma("sp", lambda e: e.dma_start(out=m2bc[:].rearrange("p a d -> p (a d)"), in_=mod2_d[s:s + 1, :].partition_broadcast(128)),
                       "ld_m2bc", reads=[B_mod2d], writes=[B_m2bc])
                h2k = [sb("h2k%d" % i, [128, D], BF16, po_) for i in range(2)]
                h2kf = sb("h2kf", [128, D], F32, po_)
                B_h2k = [kb.buf(), kb.buf()]
                B_h2kf = kb.buf()
                nrm = NormT(po_, "o", ntp=2)
                pmx = [ps("pmx%d" % i, [128, D], F32, po_) for i in range(2)]
                B_pmx = [kb.pbuf(), kb.pbuf()]
                plg = ps("plg", [128, 64], F32, po_)
                B_plg = kb.pbuf()
                lga = sb("lga", [128, NT, 36], F32, po_)
                B_lga = kb.buf()
                xt1 = sb("xto1", [128, D], F32, po_)
                xts = [xt0, xt1]
                B_xts = [B_xt0, kb.buf()]

                def op_S1(i):
                    j = i % 2
                    tsl = slice(i * 128, (i + 1) * 128)
                    for hh in range(2):
                        for h in range(8):
                            kb.emit("pe", lambda e, j=j, h=h, hh=hh, tsl=tsl: e.matmul(
                                pmx[j][:, hh * 512:(hh + 1) * 512], lhsT=OT[0:64, h, tsl], rhs=woa[0:64, h, hh * 512:(hh + 1) * 512],
                                start=(h == 0), stop=False), [B_OT, B_wo], [B_pmx[j]], inc=False)
                        for c in range(4):
                            kb.emit("pe", lambda e, j=j, c=c, hh=hh, tsl=tsl: e.matmul(
                                pmx[j][:, hh * 512:(hh + 1) * 512], lhsT=recT[:, c, tsl], rhs=wor[:, c, hh * 512:(hh + 1) * 512],
                                start=False, stop=(c == 3)), [B_recT, B_wo], [B_pmx[j]], inc=(c == 3 and hh == 1))
                    kb.dma("sp", lambda e, i=i, j=j: e.dma_start(out=xts[j][:], in_=x_d[s, i * 128:(i + 1) * 128, :]), "ld_xto%d" % j, writes=[B_xts[j]])
                    kb.emit("dve", lambda e, j=j: e.tensor_tensor(out=x1t[j][:], in0=pmx[j][:], in1=xts[j][:], op=ALU.add),
                            [B_pmx[j], B_xts[j]], [B_x1t[j]])
                    kb.dma("sp", lambda e, i=i, j=j: e.dma_start(out=x1_d[s, i * 128:(i + 1) * 128, :], in_=x1t[j][:]), "st_x1_%d" % j,
                           reads=[B_x1t[j]], writes=[B_x1d[s][i]])

                def op_S2(i):
                    j = i % 2
                    gi = s * NT + i
                    xn_, B_xn_ = nrm.run(x1t[j][:], B_x1t[j], s, 2, h2t[j][:], B_h2t[j])
                    kb.emit("pool", lambda e, xn_=xn_: e.tensor_tensor(out=h2kf[:], in0=xn_[:], in1=m2bc[:, 0, :], op=ALU.mult),
                            [B_xn_, B_m2bc], [B_h2kf])
                    kb.emit("pool", lambda e, j=j: e.tensor_tensor(out=h2k[j][:], in0=h2kf[:], in1=m2bc[:, 1, :], op=ALU.add),
                            [B_h2kf, B_m2bc], [B_h2k[j]])
                    kb.dma("sp", lambda e, gi=gi, j=j: e.dma_start(out=h2tok_d[gi * 128:(gi + 1) * 128, :], in_=h2k[j][:]), "st_h2_%d" % j,
                           reads=[B_h2k[j]], writes=[B_h2d[s][i]])
                    for kc in range(8):
                        kb.emit("pe", lambda e, j=j, kc=kc: e.matmul(plg[:, 0:36], lhsT=h2t[j][:, kc, :], rhs=wr[:, kc, :],
                                                                     start=(kc == 0), stop=(kc == 7)), [B_h2t[j], B_wr], [B_plg], inc=(kc == 7))
                    kb.emit("dve", lambda e, i=i: e.tensor_tensor(out=lga[:, i, :], in0=plg[:, 0:36], in1=brb[:], op=ALU.add), [B_plg, B_brb], [B_lga])

                op_S1(0)
                for i in range(NT):
                    if i + 1 < NT:
                        op_S1(i + 1)
                    op_S2(i)
                g0 = s * NT
                gsel = sb("gsel", [128, 3, NT, 4], F32, po_)
                tkb = sb("tkb", [128, 8, NT], F32, po_)
                elm = sb("elm", [128, 4, NT, 32], F32, po_)
                B_gsel, B_tkb, B_elm = kb.buf(), kb.buf(), kb.buf()
                gl = lga[:, :, 0:4]
                el = lga[:, :, 4:36]
                bc4 = lambda ap: ap.unsqueeze(2).to_broadcast([128, NT, 4])
                bc32 = lambda ap: ap.unsqueeze(2).to_broadcast([128, NT, 32])
                kb.emit("dve", lambda e: e.tensor_reduce(out=tkb[:, 0, :], in_=gl, axis=AX.X, op=ALU.max), [B_lga], [B_tkb])
                kb.emit("dve", lambda e: e.tensor_tensor(out=gsel[:, 0], in0=gl, in1=bc4(tkb[:, 0, :]), op=ALU.is_ge), [B_lga, B_tkb], [B_gsel])
                kb.emit("dve", lambda e: e.tensor_tensor(out=gsel[:, 2], in0=gl, in1=bc4(tkb[:, 0, :]), op=ALU.subtract), [B_lga, B_tkb], [B_gsel])
                kb.emit("act", lambda e: e.activation(out=gsel[:, 2], in_=gsel[:, 2], func=AF.Exp), [B_gsel], [B_gsel])
                kb.emit("dve", lambda e: e.tensor_reduce(out=tkb[:, 1, :], in_=gsel[:, 2], axis=AX.X, op=ALU.add), [B_gsel, B_tkb], [B_tkb])
                kb.emit("dve", lambda e: e.reciprocal(out=tkb[:, 2, :], in_=tkb[:, 1, :]), [B_tkb], [B_tkb])
                kb.emit("dve", lambda e: e.tensor_scalar(out=gsel[:, 1], in0=gsel[:, 0], scalar1=-1.0, scalar2=BIG, op0=ALU.add, op1=ALU.mult),
                        [B_gsel], [B_gsel])
                kb.emit("dve", lambda e: e.tensor_tensor(out=elm[:, 0].rearrange("p t (g e) -> p t g e", g=4),
                                                         in0=lga[:, :, 4:36].rearrange("p t (g e) -> p t g e", g=4),
                                                         in1=gsel[:, 1].unsqueeze(3).to_broadcast([128, NT, 4, 8]), op=ALU.add),
                        [B_lga, B_gsel], [B_elm])
                kb.emit("dve", lambda e: e.tensor_reduce(out=tkb[:, 3, :], in_=elm[:, 0], axis=AX.X, op=ALU.max), [B_elm, B_tkb], [B_tkb])
                kb.emit("dve", lambda e: e.tensor_tensor(out=elm[:, 1], in0=elm[:, 0], in1=bc32(tkb[:, 3, :]), op=ALU.is_ge), [B_elm, B_tkb], [B_elm])
                kb.emit("dve", lambda e: e.scalar_tensor_tensor(out=elm[:, 2], in0=elm[:, 1], scalar=-BIG, in1=elm[:, 0], op0=ALU.mult, op1=ALU.add),
                        [B_elm], [B_elm])
                kb.emit("dve", lambda e: e.tensor_reduce(out=tkb[:, 4, :], in_=elm[:, 2], axis=AX.X, op=ALU.max), [B_elm, B_tkb], [B_tkb])
                kb.emit("dve", lambda e: e.tensor_tensor(out=elm[:, 3], in0=elm[:, 2], in1=bc32(tkb[:, 4, :]), op=ALU.is_ge), [B_elm, B_tkb], [B_elm])
                kb.emit("dve", lambda e: e.tensor_tensor(out=tkb[:, 5, :], in0=tkb[:, 4, :], in1=tkb[:, 3, :], op=ALU.subtract), [B_tkb], [B_tkb])
                kb.emit("act", lambda e: e.activation(out=tkb[:, 5, :], in_=tkb[:, 5, :], func=AF.Exp), [B_tkb], [B_tkb])
                kb.emit("dve", lambda e: e.tensor_scalar(out=tkb[:, 6, :], in0=tkb[:, 5, :], scalar1=1.0, scalar2=None, op0=ALU.add), [B_tkb], [B_tkb])
                kb.emit("dve", lambda e: e.reciprocal(out=tkb[:, 6, :], in_=tkb[:, 6, :]), [B_tkb], [B_tkb])
                kb.emit("dve", lambda e: e.tensor_tensor(out=WK[:, g0:g0 + NT, 0], in0=tkb[:, 6, :], in1=tkb[:, 2, :], op=ALU.mult), [B_tkb], B_EW[g0:g0 + NT])
                kb.emit("dve", lambda e: e.tensor_tensor(out=WK[:, g0:g0 + NT, 1], in0=WK[:, g0:g0 + NT, 0], in1=tkb[:, 5, :], op=ALU.mult),
                        [B_tkb] + B_EW[g0:g0 + NT], B_EW[g0:g0 + NT])
                for k2 in range(2):
                    kb.emit("dve", lambda e, k2=k2: e.tensor_tensor(out=elm[:, 0], in0=elm[:, 1 + 2 * k2], in1=iota_e[:].unsqueeze(1).to_broadcast([128, NT, 32]),
                                                                    op=ALU.mult), [B_elm, B_iota], [B_elm])
                    kb.emit("dve", lambda e, k2=k2: e.tensor_reduce(out=EIDX[:, g0:g0 + NT, k2], in_=elm[:, 0], axis=AX.X, op=ALU.add),
                            [B_elm] + B_EW[g0:g0 + NT], B_EW[g0:g0 + NT])
                kb.barrier()
                kb.flush()
            if stop == "mix":
                break

    if stop == "mix":
        if dbg and "EIDX" in dbg:
            dbg_store("EIDX", EIDX[:].rearrange("p i e -> p (i e)"), B_EW)
            dbg_store("WK", WK[:].rearrange("p i e -> p (i e)"), B_EW)
            kb.barrier()
            kb.flush()

    issue_cv(1000)
    if stop is None or stop == "moe":
        NG = nseq_run * NT
        with ExitStack() as pe_:
            cstm = sb("cstm", [128, 192], F32, pe_)
            crow = sb("crow", [1, 3200], F32, pe_)
            B_cstm, B_crow = kb.buf(), kb.buf()
            kb.dma("sp", lambda e: e.dma_start(out=cstm[:], in_=cstm_d[:]), "ld_cstm", writes=[B_cstm])
            kb.dma("sp", lambda e: e.dma_start(out=crow[:], in_=crow_d[:]), "ld_crow", writes=[B_crow])
            sltb = sb("sltb", [128, 128], BF16, pe_)
            B_sltb = kb.buf()
            kb.emit("dve", lambda e: e.tensor_copy(out=sltb[:], in_=cstm[:, 0:128]), [B_cstm], [B_sltb])
            NGT = NSEQ * NT
            Mb = sb("Mb", [128, NGT, 32], BF16, pe_)
            CS = sb("CS", [128, NGT + 1, 32], F32, pe_)
            RK = sb("RK", [128, NGT, 32], F32, pe_)
            oh = sb("oh", [128, NGT, 2, 32], F32, pe_)
            ohr = sb("ohr", [128, NGT, 32], F32, pe_)
            B_Mb, B_CS, B_RK, B_oh, B_ohr = (kb.buf() for _ in range(5))
            SLOTF = sb("SLOTF", [128, NGT, 2], F32, pe_)
            SLOT = sb("SLOT", [128, NGT, 2], I32, pe_)
            B_slotf, B_slot = kb.buf(), kb.buf()
            IDXW = sb("IDXW", [128, NBB, 2], I32, pe_)
            B_idxw = kb.buf()
            with ExitStack() as pr_:
                pcs = ps("pcs", [128, 1024], F32, pr_)
                B_pcs = kb.pbuf()
                prk = ps("prk", [128, 1024], F32, pr_)
                B_prk = kb.pbuf()
                pbcr = ps("pbcr", [128, 96], F32, pr_)
                B_pbcr = kb.pbuf()
                kb.emit("dve", lambda e: e.tensor_tensor(out=oh[:].rearrange("p g k e -> p (g k) e"),
                                                         in0=iota_e[:].unsqueeze(1).to_broadcast([128, NGT * 2, 32]),
                                                         in1=EIDX[:].rearrange("p g k -> p (g k)").unsqueeze(2).to_broadcast([128, NGT * 2, 32]),
                                                         op=ALU.is_equal), [B_iota] + B_EW, [B_oh])
                kb.emit("dve", lambda e: e.tensor_tensor(out=Mb[:], in0=oh[:, :, 0, :], in1=oh[:, :, 1, :], op=ALU.add), [B_oh], [B_Mb])
                Mbf = Mb[:].rearrange("p g e -> p (g e)")
                for hh in range(2):
                    kb.emit("pe", lambda e, hh=hh: e.matmul(pcs[:, hh * 512:(hh + 1) * 512], lhsT=onesb[:], rhs=Mbf[:, hh * 512:(hh + 1) * 512], start=True, stop=True),
                            [B_onesb, B_Mb], [B_pcs], inc=(hh == 1))
                for hh in range(2):
                    kb.emit("pe", lambda e, hh=hh: e.matmul(prk[:, hh * 512:(hh + 1) * 512], lhsT=sltb[:], rhs=Mbf[:, hh * 512:(hh + 1) * 512], start=True, stop=True),
                            [B_sltb, B_Mb], [B_prk], inc=(hh == 1))
                kb.emit("pool", lambda e: e.memset(CS[:, 0, :], 0.0), [], [B_CS])
                for g in range(NGT):
                    kb.emit("dve", lambda e, g=g: e.tensor_tensor(out=CS[:, g + 1, :], in0=CS[:, g, :], in1=pcs[:, g * 32:(g + 1) * 32], op=ALU.add),
                            [B_CS, B_pcs], [B_CS])
                kb.emit("dve", lambda e: e.tensor_tensor(out=RK[:].rearrange("p g e -> p (g e)"), in0=prk[:], in1=CS[:, 0:NGT, :].rearrange("p g e -> p (g e)"), op=ALU.add),
                        [B_prk, B_CS], [B_RK])
                rw = sb("rw", [1, 8, 64], F32, pr_)
                g1 = sb("g1", [1, 2048], F32, pr_)
                B_rw, B_g1 = kb.buf(), kb.buf()
                kb.emit("pool", lambda e: e.memset(rw[:], 0.0), [], [B_rw])
                kb.emit("dve", lambda e: e.tensor_copy(out=rw[:, 0, 0:32], in_=CS[0:1, NGT, :]), [B_CS, B_rw], [B_rw])
                kb.emit("dve", lambda e: e.tensor_tensor(out=g1[:, 0:512].rearrange("p (a b) -> p a b", a=32),
                                                         in0=rw[:, 0, 0:32].unsqueeze(2).to_broadcast([1, 32, 16]),
                                                         in1=crow[:, 0:16].unsqueeze(1).to_broadcast([1, 32, 16]), op=ALU.is_gt),
                        [B_rw, B_crow], [B_g1])
                kb.emit("dve", lambda e: e.tensor_reduce(out=rw[:, 1, 0:32], in_=g1[:, 0:512].rearrange("p (a b) -> p a b", a=32), axis=AX.X, op=ALU.add),
                        [B_g1, B_rw], [B_rw])
                kb.emit("dve", lambda e: e.tensor_tensor(out=g1[:, 0:1024].rearrange("p (a b) -> p a b", a=32),
                                                         in0=rw[:, 1, 0:32].unsqueeze(1).to_broadcast([1, 32, 32]),
                                                         in1=crow[:, 16:1040].rearrange("p (a b) -> p a b", a=32), op=ALU.mult),
                        [B_rw, B_crow], [B_g1])
                kb.emit("dve", lambda e: e.tensor_reduce(out=rw[:, 2, 0:32], in_=g1[:, 0:1024].rearrange("p (a b) -> p a b", a=32), axis=AX.X, op=ALU.add),
                        [B_g1, B_rw], [B_rw])
                kb.emit("dve", lambda e: e.tensor_tensor(out=rw[:, 3, 0:32], in0=rw[:, 2, 0:32], in1=rw[:, 1, 0:32], op=ALU.subtract), [B_rw], [B_rw])
                kb.emit("dve", lambda e: e.tensor_scalar(out=rw[:, 3, 0:32], in0=rw[:, 3, 0:32], scalar1=256.0, scalar2=None, op0=ALU.mult), [B_rw], [B_rw])
                kb.emit("dve", lambda e: e.tensor_tensor(out=g1[:].rearrange("p (a b) -> p a b", a=64),
                                                         in0=rw[:, 2, 0:32].unsqueeze(1).to_broadcast([1, 64, 32]),
                                                         in1=crow[:, 1040:3088].rearrange("p (a b) -> p a b", a=64), op=ALU.is_le),
                        [B_rw, B_crow], [B_g1])
                kb.emit("dve", lambda e: e.tensor_reduce(out=rw[:, 4, :], in_=g1[:].rearrange("p (a b) -> p a b", a=64), axis=AX.X, op=ALU.add),
                        [B_g1, B_rw], [B_rw])
                kb.emit("dve", lambda e: e.tensor_scalar(out=rw[:, 4, :], in0=rw[:, 4, :], scalar1=31.0, scalar2=None, op0=ALU.min), [B_rw], [B_rw])
                kb.emit("dve", lambda e: e.tensor_tensor(out=rw[:, 5, 2:64], in0=rw[:, 4, 2:64], in1=rw[:, 4, 0:62], op=ALU.is_equal), [B_rw], [B_rw])
                kb.emit("dve", lambda e: e.tensor_scalar(out=rw[:, 5, :], in0=rw[:, 5, :], scalar1=5.0e8, scalar2=None, op0=ALU.mult), [B_rw], [B_rw])
                kb.emit("dve", lambda e: e.scalar_tensor_tensor(out=rw[:, 6, :], in0=rw[:, 4, :], scalar=128.0, in1=rw[:, 5, :], op0=ALU.mult, op1=ALU.add),
                        [B_rw], [B_rw])
                kb.emit("pe", lambda e: e.matmul(pbcr[:, 0:64], lhsT=ones_f[0:1, 0:128], rhs=rw[:, 6, :], start=True, stop=True), [B_cst, B_rw], [B_pbcr], inc=False)
                kb.emit("pe", lambda e: e.matmul(pbcr[:, 64:96], lhsT=ones_f[0:1, 0:128], rhs=rw[:, 3, 0:32], start=True, stop=True), [B_cst, B_rw], [B_pbcr])
                bcs = sb("bcs", [128, 96], F32, pr_)
                idf = sb("idf", [128, NBB, 2], F32, pr_)
                B_bcs, B_idf = kb.buf(), kb.buf()
                kb.emit("dve", lambda e: e.tensor_copy(out=bcs[:], in_=pbcr[:]), [B_pbcr], [B_bcs])
                kb.emit("dve", lambda e: e.tensor_scalar(out=idf[:, :, 0], in0=bcs[:, 0:64], scalar1=cstm[:, 160:161], scalar2=None, op0=ALU.add),
                        [B_bcs, B_cstm], [B_idf])
                kb.emit("dve", lambda e: e.tensor_scalar(out=idf[:, :, 1], in0=idf[:, :, 0], scalar1=1.0, scalar2=None, op0=ALU.add), [B_idf], [B_idf])
                kb.emit("dve", lambda e: e.tensor_copy(out=IDXW[:], in_=idf[:]), [B_idf], [B_idxw])
                kb.emit("dve", lambda e: e.tensor_tensor(out=RK[:], in0=RK[:], in1=bcs[:, 64:96].unsqueeze(1).to_broadcast([128, NGT, 32]), op=ALU.add),
                        [B_RK, B_bcs], [B_RK])
                for k2 in range(2):
                    kb.emit("dve", lambda e, k2=k2: e.tensor_tensor(out=ohr[:], in0=oh[:, :, k2, :], in1=RK[:], op=ALU.mult), [B_oh, B_RK], [B_ohr])
                    kb.emit("dve", lambda e, k2=k2: e.tensor_reduce(out=SLOTF[:, :, k2], in_=ohr[:], axis=AX.X, op=ALU.add), [B_ohr, B_slotf], [B_slotf])
                kb.emit("dve", lambda e: e.tensor_copy(out=SLOT[:], in_=SLOTF[:]), [B_slotf], [B_slot])
                if dbg and "SLOT" in dbg:
                    dbg_store("SLOT", SLOTF[:].rearrange("p i e -> p (i e)"), [B_slotf])
                    dbg_store("BE", rw[:].rearrange("p a b -> p (a b)"), [B_rw])
                kb.barrier()
                kb.flush()
            with nullcontext(pe_) as pd_:
                hk = [sb("hk%d" % i, [128, D], BF16, pd_) for i in range(2)]
                B_hk = [kb.buf(), kb.buf()]
                for gi in range(NG):
                    j = gi % 2
                    s_, i_ = gi // NT, gi % NT
                    kb.dma("sp", lambda e, gi=gi, j=j: e.dma_start(out=hk[j][:], in_=h2tok_d[gi * 128:(gi + 1) * 128, :]), "ld_hk%d" % j,
                           reads=[B_h2d[s_][i_]], writes=[B_hk[j]])
                    for k2 in range(2):
                        kb.dma("pool", lambda e, gi=gi, j=j, k2=k2: e.indirect_dma_start(
                            out=xs_d[:, :], out_offset=bass.IndirectOffsetOnAxis(ap=SLOT[:, gi, k2:k2 + 1], axis=0), in_=hk[j][:], in_offset=None),
                            "sc_xs%d" % j, reads=[B_hk[j], B_slot, B_xsz], writes=[B_xs2[j]])
            B_ys2 = [kb.buf("ys0"), kb.buf("ys1")]
            with nullcontext(pe_) as px_:
                wall = [sb("wall%d" % i, [128, 3 * 4096], BF16, px_) for i in range(2)]
                wge = [wall[i][:, 0:4096].rearrange("p (k n) -> p k n", k=8) for i in range(2)]
                wue = [wall[i][:, 4096:8192].rearrange("p (k n) -> p k n", k=8) for i in range(2)]
                wde = [wall[i][:, 8192:12288].rearrange("p (k n) -> p k n", k=4) for i in range(2)]
                B_we = [kb.buf(), kb.buf()]
                xb = [sb("xb%d" % i, [128, 2, D], BF16, px_) for i in range(2)]
                B_xb = [kb.buf(), kb.buf()]
                xsT = [sb("xsT%d" % i, [128, 8, 256], BF16, px_) for i in range(2)]
                B_xsT = [kb.buf(), kb.buf()]
                hid = [sb("hid%d" % i, [128, 4, 256], BF16, px_) for i in range(2)]
                B_hid = [kb.buf(), kb.buf()]
                sgt = [sb("sgt%d" % i, [128, 256], F32, px_) for i in range(2)]
                B_sgt = [kb.buf(), kb.buf()]
                ysb = [sb("ysb%d" % i, [128, D], F32, px_) for i in range(2)]
                B_ysb = [kb.buf(), kb.buf()]
                ptx = [ps("ptx%d" % i, [128, 8, 128], BF16, px_) for i in range(2)]
                B_ptx = [kb.pbuf(), kb.pbuf()]
                pgu = [ps("pgu%d" % i, [128, 256], F32, px_) for i in range(4)]
                B_pgu = [kb.pbuf() for _ in range(4)]
                py = ps("py", [128, D], F32, px_)
                B_py = kb.pbuf()
                nbb_run = NBB if stop is None else 8
                cnt_g = 0
                cnt_t = 0
                cnt_y = 0
                for b in range(nbb_run):
                    wj = b % 2
                    kb.dma("pool", lambda e, b=b, wj=wj: e.indirect_dma_start(
                        out=wall[wj][:, :], out_offset=None, in_=wbf_d[:, :],
                        in_offset=bass.IndirectOffsetOnAxis(ap=IDXW[:, b, 0:1], axis=0),
                        bounds_check=kb.const_reg(e, NEXP * 128 - 1), oob_is_err=False), "ld_we%d" % wj, reads=[B_idxw, B_wbf], writes=[B_we[wj]])
                    xj = b % 2
                    for bb_ in ([0, 1] if b == 0 else [b + 1]):
                        if bb_ < nbb_run:
                            kb.dma("sp", lambda e, bb_=bb_: e.dma_start(out=xb[bb_ % 2][:], in_=xs_d[bb_ * 256:(bb_ + 1) * 256, :].rearrange("(t p) d -> p t d", p=128)),
                                   "ld_xb%d" % (bb_ % 2), reads=B_xs2, writes=[B_xb[bb_ % 2]])
                    for t2 in range(2):
                        tj = cnt_t % 2
                        cnt_t += 1
                        for kc in range(8):
                            kb.emit("pe", lambda e, xj=xj, t2=t2, kc=kc, tj=tj: e.transpose(out=ptx[tj][:, kc, :], in_=xb[xj][:, t2, kc * 128:(kc + 1) * 128],
                                                                                       identity=identb[:]), [B_xb[xj], B_identb], [B_ptx[tj]], inc=(kc == 7))
                        kb.emit("act", lambda e, xj=xj, t2=t2, tj=tj: e.copy(out=xsT[xj][:, :, t2 * 128:(t2 + 1) * 128], in_=ptx[tj][:]), [B_ptx[tj]], [B_xsT[xj]])
                    hj = b % 2
                    for m in range(4):
                        pg_i = (cnt_g % 2) * 2
                        cnt_g += 1
                        for kc in range(8):
                            kb.emit("pe", lambda e, wj=wj, m=m, kc=kc, xj=xj, pg_i=pg_i: e.matmul(
                                pgu[pg_i][:], lhsT=wge[wj][:, kc, m * 128:(m + 1) * 128], rhs=xsT[xj][:, kc, :],
                                start=(kc == 0), stop=(kc == 7)), [B_we[wj], B_xsT[xj]], [B_pgu[pg_i]], inc=(kc == 7))
                        for kc in range(8):
                            kb.emit("pe", lambda e, wj=wj, m=m, kc=kc, xj=xj, pg_i=pg_i: e.matmul(
                                pgu[pg_i + 1][:], lhsT=wue[wj][:, kc, m * 128:(m + 1) * 128], rhs=xsT[xj][:, kc, :],
                                start=(kc == 0), stop=(kc == 7)), [B_we[wj], B_xsT[xj]], [B_pgu[pg_i + 1]], inc=(kc == 7))
                        sj = m % 2
                        kb.emit("act", lambda e, pg_i=pg_i, sj=sj: e.activation(out=sgt[sj][:], in_=pgu[pg_i][:], func=AF.Silu), [B_pgu[pg_i]], [B_sgt[sj]])
                        kb.emit("dve", lambda e, pg_i=pg_i, sj=sj, hj=hj, m=m: e.tensor_tensor(out=hid[hj][:, m, :], in0=sgt[sj][:], in1=pgu[pg_i + 1][:], op=ALU.mult),
                                [B_sgt[sj], B_pgu[pg_i + 1]], [B_hid[hj]])
                    for t2 in range(2):
                        yj = cnt_y % 2
                        cnt_y += 1
                        for hh in range(2):
                            for m in range(4):
                                kb.emit("pe", lambda e, wj=wj, m=m, hh=hh, hj=hj, t2=t2: e.matmul(
                                    py[:, hh * 512:(hh + 1) * 512], lhsT=hid[hj][:, m, t2 * 128:(t2 + 1) * 128],
                                    rhs=wde[wj][:, m, hh * 512:(hh + 1) * 512], start=(m == 0), stop=(m == 3)),
                                    [B_hid[hj], B_we[wj]], [B_py], inc=(m == 3 and hh == 1))
                        kb.emit("act", lambda e, yj=yj: e.copy(out=ysb[yj][:], in_=py[:]), [B_py], [B_ysb[yj]])
                        kb.dma("sp", lambda e, b=b, t2=t2, yj=yj: e.dma_start(out=ys_d[b * 256 + t2 * 128:b * 256 + (t2 + 1) * 128, :], in_=ysb[yj][:]),
                               "st_ys%d" % yj, reads=[B_ysb[yj]], writes=[B_ys2[yj]])
            with nullcontext(pe_) as pc_:
                xr = [sb("xr%d" % i, [128, D], F32, pc_) for i in range(2)]
                yg = [sb("yg%d" % i, [128, 2, D], F32, pc_) for i in range(2)]
                B_xr = [kb.buf(), kb.buf()]
                B_yg = [kb.buf(), kb.buf()]
                for gi in range(NG):
                    j = gi % 2
                    s_, i_ = gi // NT, gi % NT
                    kb.dma("sp", lambda e, s_=s_, i_=i_, j=j: e.dma_start(out=xr[j][:], in_=x1_d[s_, i_ * 128:(i_ + 1) * 128, :]), "ld_xr%d" % j,
                           reads=[B_x1d[s_][i_]], writes=[B_xr[j]])
                    for k2 in range(2):
                        kb.dma("pool", lambda e, gi=gi, j=j, k2=k2: e.indirect_dma_start(
                            out=yg[j][:, k2, :], out_offset=None, in_=ys_d[:, :],
                            in_offset=bass.IndirectOffsetOnAxis(ap=SLOT[:, gi, k2:k2 + 1], axis=0)), "ld_yg%d" % j,
                            reads=B_ys2 + [B_slot], writes=[B_yg[j]])
                    kb.emit("dve", lambda e, gi=gi, j=j: e.tensor_scalar(out=yg[j][:, 0, :], in0=yg[j][:, 0, :], scalar1=WK[:, gi, 0:1], scalar2=None, op0=ALU.mult),
                            [B_yg[j], B_EW[gi]], [B_yg[j]])
                    kb.emit("dve", lambda e, gi=gi, j=j: e.scalar_tensor_tensor(out=yg[j][:, 0, :], in0=yg[j][:, 1, :], scalar=WK[:, gi, 1:2], in1=yg[j][:, 0, :],
                                                                              op0=ALU.mult, op1=ALU.add), [B_yg[j], B_EW[gi]], [B_yg[j]])
                    kb.emit("dve", lambda e, s_=s_, j=j: e.tensor_tensor(out=yg[j][:, 0, :], in0=yg[j][:, 0, :], in1=gatebc[:, s_, 1, :], op=ALU.mult),
                            [B_yg[j], B_gatebc], [B_yg[j]])
                    kb.emit("dve", lambda e, j=j: e.tensor_tensor(out=xr[j][:], in0=xr[j][:], in1=yg[j][:, 0, :], op=ALU.add), [B_xr[j], B_yg[j]], [B_xr[j]])
                    kb.dma("sp", lambda e, s_=s_, i_=i_, j=j: e.dma_start(out=out_d[s_, i_ * 128:(i_ + 1) * 128, :], in_=xr[j][:]), "st_out%d" % j,
                           reads=[B_xr[j]])
                kb.barrier()
                kb.flush()

    kb.barrier()
    kb.flush()
    kb.es.close()
    return nc


def _consts():
    c = np.zeros((128, 1024), np.float32)
    idx = np.arange(128)
    same = (idx[:, None] // 64) == (idx[None, :] // 64)
    triF = ((idx[:, None] <= idx[None, :]) & same).astype(np.float32)
    triB = triF.T.copy()
    c[:, 0:128] = triF
    c[:, 128:256] = triB
    c[:, 256:384] = triB - np.eye(128, dtype=np.float32)
    c[:, 384:512] = triF - np.eye(128, dtype=np.float32)
    c[:, 512] = (idx < 64)
    c[:, 513] = (idx >= 64)
    half = 16
    inv_freq = (10000.0 ** (-np.arange(half, dtype=np.float32) / half)).astype(np.float32)
    c[:, 514:530] = inv_freq[None, :]
    c[0, 530] = 1.0
    c[1, 531] = 1.0
    c[0, 532:660] = 1.0
    c[1, 660:788] = 1.0
    c[:, 788:916] = 1.0
    c[:, 916] = 1.0 / 384
    c[:, 917] = 1.0 / 256
    c[:, 918] = -np.pi
    c[:, 919] = 1e-6
    return c


def _cstm():
    c = np.zeros((128, 192), np.float32)
    idx = np.arange(128)
    c[:, 0:128] = (idx[:, None] < idx[None, :]).astype(np.float32)
    c[:, 128:160] = np.arange(32, dtype=np.float32)[None, :]
    c[:, 160] = idx
    return c


def _crow():
    r = np.zeros((1, 3200), np.float32)
    r[0, 0:16] = 256.0 * np.arange(16)
    e = np.arange(32)
    r[0, 16:1040] = (e[None, :] <= e[:, None]).astype(np.float32).reshape(-1)
    r[0, 1040:3088] = np.repeat(np.arange(64, dtype=np.float32), 32)
    return r


def _kc(w):
    K, N = w.shape
    return np.ascontiguousarray(w.reshape(K // 128, 128, N).transpose(1, 0, 2))


def make_in_maps(inp):
    f = lambda a: np.ascontiguousarray(np.asarray(a, dtype=np.float32))
    x = f(inp["x"]); c = f(inp["c"]); pos = np.asarray(inp["positions"]).astype(np.int32)
    shared = {
        "ada_w": _kc(f(inp["ada_w"])[0]),
        "ada_b": f(inp["ada_b"])[0][None, :],
        "g1": np.ascontiguousarray(f(inp["norm1_g"])[0].reshape(8, 128).T),
        "g2": np.ascontiguousarray(f(inp["norm2_g"])[0].reshape(8, 128).T),
        "w_in": _kc(f(inp["w_in"])[0]),
        "qa_g": np.ascontiguousarray(f(inp["mla_qa_g"])[0].reshape(3, 128).T),
        "wq_up": _kc(f(inp["mla_wq_up"])[0]),
        "kva_g": np.ascontiguousarray(f(inp["mla_kva_g"])[0].reshape(2, 128).T),
        "wkv_up": _kc(f(inp["mla_wkv_up"])[0]),
        "qn_g": f(inp["mla_qn_g"])[0][None, :],
        "kn_g": f(inp["mla_kn_g"])[0][None, :],
        "lb_logits": f(inp["hg_lb_logits"]).reshape(1, -1),
        "hg_norm_g": f(inp["hg_norm_g"])[0][None, :],
        "w_out_a": np.ascontiguousarray(f(inp["w_out"])[0][:512].reshape(8, 64, D).transpose(1, 0, 2)),
        "w_out_r": _kc(f(inp["w_out"])[0][512:]),
        "w_router": _kc(np.concatenate([f(inp["router_group_w"])[0], f(inp["router_expert_w"])[0]], axis=1)),
        "b_router": np.concatenate([f(inp["router_group_b"])[0], f(inp["router_expert_b"])[0]])[None, :],
        "w_gate": np.ascontiguousarray(f(inp["w_gate"])[0].reshape(NEXP, 8, 128, 512).transpose(0, 2, 1, 3)),
        "w_up": np.ascontiguousarray(f(inp["w_up"])[0].reshape(NEXP, 8, 128, 512).transpose(0, 2, 1, 3)),
        "w_down": np.ascontiguousarray(f(inp["w_down"])[0].reshape(NEXP, 4, 128, D).transpose(0, 2, 1, 3)),
        "ident_bf": np.eye(128, dtype=np.float32).astype(ml_dtypes.bfloat16),
        "consts_f": _consts(),
        "cstm": _cstm(),
        "crow": _crow(),
        "g2row": f(inp["norm2_g"])[0][None, :],
    }
    maps = []
    for i in range(NCORES):
        b0 = NSEQ * i
        m = dict(shared)
        m["x"] = np.ascontiguousarray(x[b0:b0 + NSEQ])
        p = pos[b0:b0 + NSEQ].reshape(NSEQ, NT, 128)
        m["pos"] = np.ascontiguousarray(p.transpose(2, 0, 1).reshape(128, NSEQ * NT))
        m["cT"] = np.ascontiguousarray(c[b0:b0 + NSEQ].reshape(NSEQ, 8, 128).transpose(2, 1, 0))
        maps.append(m)
    return maps


def kernel(**inputs):
    nc = build()
    in_maps = make_in_maps(inputs)
    res = run_bass_kernel_spmd(nc, in_maps, core_ids=list(range(NCORES)))
    outs = [np.asarray(r["out"]).reshape(NSEQ, S, D) for r in res.results]
    return np.concatenate(outs, axis=0).astype(np.float32)
```

```python
import numpy as np
import ml_dtypes
from contextlib import ExitStack, nullcontext
import concourse.bass as bass
import concourse.mybir as mybir
from concourse.bass_utils import run_bass_kernel_spmd

F32 = mybir.dt.float32
BF16 = mybir.dt.bfloat16
I32 = mybir.dt.int32
AF = mybir.ActivationFunctionType
ALU = mybir.AluOpType
AX = mybir.AxisListType

NCORES = 8
D = 1024
S = 2048
NT = S // 128
NSEQ = 2
EPS = 1e-6
INCOLS = 3232
NEXP = 32
BIG = 1.0e30


class Buf:
    __slots__ = ("name", "w", "r", "excl")

    def __init__(self, name, excl=False):
        self.name = name
        self.excl = excl
        self.w = None
        self.r = {}


class KB:
    ENG = ("pe", "act", "dve", "pool", "sp")

    def __init__(self, nc):
        self.nc = nc
        self.es = ExitStack()
        self.sems = {}
        self.cnt = {}
        self.waited = {e: {} for e in self.ENG}
        self.prog = {e: [] for e in self.ENG}
        for e in self.ENG:
            self.sems[e] = self.es.enter_context(nc.semaphore("s_" + e))
            self.cnt[e] = 0
        self.nbuf = 0
        self.ninst = 0
        self.nflush = 0
        self.regs = {}

    def buf(self, name=None):
        self.nbuf += 1
        return Buf(name or ("b%d" % self.nbuf))

    def pbuf(self, name=None):
        self.nbuf += 1
        return Buf(name or ("p%d" % self.nbuf), excl=True)

    def dsem(self, key):
        if key not in self.sems:
            self.sems[key] = self.es.enter_context(self.nc.semaphore("d_" + key))
            self.cnt[key] = 0
        return key

    def _waits(self, eng, reads, writes):
        need = {}

        def add(dep):
            if dep is None:
                return
            k, v, e2 = dep
            if need.get(k, 0) < v:
                need[k] = v

        for b in reads:
            add(b.w)
        strict = (eng == "pool")
        for b in writes:
            if b.w is not None and (b.w[2] != eng or strict):
                add(b.w)
            for k, (v, e2) in b.r.items():
                if e2 != eng or strict:
                    add((k, v, e2))
        out = []
        wd = self.waited[eng]
        for k, v in need.items():
            if wd.get(k, 0) < v:
                wd[k] = v
                out.append((k, v))
        return out

    def emit(self, eng, fn, reads=(), writes=(), inc=True):
        if any(b.excl for b in reads):
            writes = list(writes) + [b for b in reads if b.excl and b not in writes]
            reads = [b for b in reads if not b.excl]
        waits = self._waits(eng, reads, writes)
        val = self.cnt[eng] + 1
        if inc:
            self.cnt[eng] = val
        rec_w = (eng, val, eng)
        for b in reads:
            old = b.r.get(eng)
            if old is None or old[0] < val:
                b.r[eng] = (val, eng)
        for b in writes:
            b.w = rec_w
            b.r = {}
        self.prog[eng].append((waits, fn, eng if inc else None, 1))
        self.ninst += 1

    def dma(self, q, fn, key, reads=(), writes=()):
        self.dsem(key)
        waits = self._waits(q, reads, writes)
        val = self.cnt[key] + 16
        self.cnt[key] = val
        for b in reads:
            old = b.r.get(key)
            if old is None or old[0] < val:
                b.r[key] = (val, "dma")
        for b in writes:
            b.w = (key, val, "dma")
            b.r = {}
        self.prog[q].append((waits, fn, key, 16))
        self.ninst += 1

    def barrier(self):
        tgt = {k: v for k, v in self.cnt.items() if v > 0}
        for e in self.ENG:
            waits = []
            for k, v in tgt.items():
                if k == e:
                    continue
                if self.waited[e].get(k, 0) < v:
                    self.waited[e][k] = v
                    waits.append((k, v))
            if waits:
                self.prog[e].append((waits, None, None, 0))

    def flush(self):
        nc = self.nc
        progs = self.prog
        sems = self.sems

        def run(engname, eh):
            for waits, fn, inckey, incv in progs[engname]:
                for k, v in waits:
                    eh.wait_ge(sems[k], v)
                if fn is not None:
                    ins = fn(eh)
                    if inckey is not None:
                        ins.then_inc(sems[inckey], incv)

        with nc.Block() as block:
            @block.tensor
            def _(e):
                run("pe", e)

            @block.scalar
            def _(e):
                run("act", e)

            @block.vector
            def _(e):
                run("dve", e)

            @block.gpsimd
            def _(e):
                run("pool", e)

            @block.sync
            def _(e):
                run("sp", e)
        self.prog = {e: [] for e in self.ENG}
        self.nflush += 1

    def const_reg(self, e, val):
        key = (self.nflush, val)
        if key not in self.regs:
            self.regs[key] = e.to_reg(val)
        return self.regs[key]


def build(dbg=None, stop=None):
    nc = bass.Bass("TRN2", target_bir_lowering=False)
    kb = KB(nc)
    es = kb.es

    def din(name, shape, dt=F32):
        return nc.dram_tensor(name, list(shape), dt, kind="ExternalInput").ap()

    x_d = din("x", [NSEQ, S, D])
    pos_d = din("pos", [128, NSEQ * NT], I32)
    cT_d = din("cT", [128, 8, NSEQ])
    adaw_d = din("ada_w", [128, 8, 6 * D])
    adab_d = din("ada_b", [1, 6 * D])
    g1_d = din("g1", [128, 8])
    g2_d = din("g2", [128, 8])
    win_d = din("w_in", [128, 8, INCOLS])
    qag_d = din("qa_g", [128, 3])
    wq_d = din("wq_up", [128, 3, 768])
    kvag_d = din("kva_g", [128, 2])
    wkv_d = din("wkv_up", [128, 2, 1024])
    qng_d = din("qn_g", [1, 96])
    kng_d = din("kn_g", [1, 96])
    lbl_d = din("lb_logits", [1, 2 * 2 * 512])
    hgn_d = din("hg_norm_g", [1, 128])
    woa_d = din("w_out_a", [64, 8, D])
    wor_d = din("w_out_r", [128, 4, D])
    wr_d = din("w_router", [128, 8, 36])
    br_d = din("b_router", [1, 36])
    wg_d = din("w_gate", [NEXP, 128, 8, 512])
    wu_d = din("w_up", [NEXP, 128, 8, 512])
    wd_d = din("w_down", [NEXP, 128, 4, D])
    identb_d = din("ident_bf", [128, 128], BF16)
    consts_d = din("consts_f", [128, 1024])
    out_d = nc.dram_tensor("out", [NSEQ, S, D], F32, kind="ExternalOutput").ap()
    x1_d = nc.dram_tensor("x1_scratch", [NSEQ, S, D], F32, kind="Internal").ap()
    h2tok_d = nc.dram_tensor("h2tok_scratch", [NSEQ * S, D], BF16, kind="Internal").ap()
    mod2_d = nc.dram_tensor("mod2_scratch", [NSEQ, 2 * D], F32, kind="Internal").ap()
    NBB = 64
    xs_d = nc.dram_tensor("xs_scratch", [NBB * 256, D], BF16, kind="Internal").ap()
    ys_d = nc.dram_tensor("ys_scratch", [NBB * 256, D], F32, kind="Internal").ap()
    g2row_d = din("g2row", [1, D])
    wbf_d = nc.dram_tensor("wbf", [NEXP * 128, 3 * 4096], BF16, kind="Internal").ap()
    cstm_d = din("cstm", [128, 192])
    crow_d = din("crow", [1, 3200])
    dbg_d = {}
    if dbg:
        for k, shp in dbg.items():
            dbg_d[k] = nc.dram_tensor("dbg_" + k, list(shp), F32, kind="ExternalOutput").ap()

    uid = [0]

    def sb(name, shape, dt=F32, stack=es):
        uid[0] += 1
        return stack.enter_context(nc.sbuf_tensor("sb%d_%s" % (uid[0], name), list(shape), dt))

    def ps(name, shape, dt=F32, stack=es):
        uid[0] += 1
        return stack.enter_context(nc.psum_tensor("ps%d_%s" % (uid[0], name), list(shape), dt))

    identb = sb("identb", [128, 128], BF16)
    cst = sb("cst", [128, 1024])
    B_identb, B_cst = kb.buf("identb"), kb.buf("cst")
    kb.dma("sp", lambda e: e.dma_start(out=identb[:], in_=identb_d[:]), "ld_identb", writes=[B_identb])
    kb.dma("sp", lambda e: e.dma_start(out=cst[:], in_=consts_d[:]), "ld_cst", writes=[B_cst])
    ones_f = cst[:, 788:916]
    B_wbf = kb.buf("wbf")
    wsrc = [wg_d.rearrange("e p (h k) n -> e p h (k n)", h=2), wu_d.rearrange("e p (h k) n -> e p h (k n)", h=2),
            wd_d.rearrange("e p (h k) n -> e p h (k n)", h=2)]
    B_xsz = kb.buf("xsz")
    B_xs2 = [kb.buf("xs0"), kb.buf("xs1")]
    pending_cv = []
    if stop is None or stop == "moe":
        for ex in range(NEXP):
            for m_ in range(3):
                pending_cv.append((ex, m_))

    def issue_cv(n=1):
        for _ in range(n):
            if not pending_cv:
                return
            ex, m_ = pending_cv.pop(0)
            kb.dma("pool", lambda e, ex=ex, m_=m_: e.dma_start(
                out=wbf_d[ex * 128:(ex + 1) * 128, m_ * 4096:(m_ + 1) * 4096].rearrange("p (h c) -> p h c", h=2), in_=wsrc[m_][ex]),
                "cv_w", writes=[B_wbf])

    B_modrow = kb.buf("modrow")
    B_mod2d = kb.buf("mod2d")
    modcol = sb("modcol", [128, NSEQ, 4, 8])
    B_modcol = kb.buf("modcol")
    gatebc = sb("gatebc", [128, NSEQ, 2, D], BF16)
    B_gatebc = kb.buf("gatebc")

    with ExitStack() as p0:
        modrow = sb("modrow", [2, 6 * D], F32, p0)
        cT = sb("cT", [128, 8, NSEQ], F32, p0)
        cact = sb("cact", [128, 8, NSEQ], F32, p0)
        adab = sb("adab", [2, 6 * D], F32, p0)
        g12 = sb("g12", [128, 2, 8], F32, p0)
        B_cT, B_cact, B_adab, B_g12 = kb.buf(), kb.buf(), kb.buf(), kb.buf()
        kb.dma("sp", lambda e: e.dma_start(out=cT[:], in_=cT_d[:]), "ld_cT", writes=[B_cT])
        kb.dma("sp", lambda e: e.dma_start(out=adab[0:1, :], in_=adab_d[:]), "ld_adab", writes=[B_adab])
        kb.dma("sp", lambda e: e.dma_start(out=adab[1:2, :], in_=adab_d[:]), "ld_adab", writes=[B_adab])
        kb.dma("sp", lambda e: e.dma_start(out=g12[:, 0, :], in_=g1_d[:]), "ld_g12", writes=[B_g12])
        kb.dma("sp", lambda e: e.dma_start(out=g12[:, 1, :], in_=g2_d[:]), "ld_g12", writes=[B_g12])
        kb.emit("act", lambda e: e.activation(out=cact[:], in_=cT[:], func=AF.Silu), [B_cT], [B_cact])
        wbuf = [sb("adaw%d" % i, [128, 8, 512], F32, p0) for i in range(2)]
        B_wbuf = [kb.buf(), kb.buf()]
        pmod = [ps("pmod%d" % i, [2, 512], F32, p0) for i in range(2)]
        B_pmod = [kb.pbuf(), kb.pbuf()]
        for n in range(12):
            j = n % 2
            kb.dma("sp", lambda e, n=n, j=j: e.dma_start(out=wbuf[j][:], in_=adaw_d[:, :, n * 512:(n + 1) * 512]),
                   "ld_adaw%d" % j, writes=[B_wbuf[j]])
            for kc in range(8):
                kb.emit("pe", lambda e, j=j, kc=kc: e.matmul(pmod[j][:], lhsT=cact[:, kc, :], rhs=wbuf[j][:, kc, :],
                                                              start=(kc == 0), stop=(kc == 7)),
                        [B_cact, B_wbuf[j]], [B_pmod[j]], inc=(kc == 7))
            kb.emit("dve", lambda e, n=n, j=j: e.tensor_tensor(out=modrow[:, n * 512:(n + 1) * 512], in0=pmod[j][:],
                                                               in1=adab[:, n * 512:(n + 1) * 512], op=ALU.add),
                    [B_pmod[j], B_adab], [B_modrow])
        pcol = ps("pcol", [128, 64], F32, p0)
        B_pcol = kb.pbuf()
        col_src = [1, 0, 4, 3]
        for s in range(NSEQ):
            for j in range(4):
                for kc in range(8):
                    c0 = col_src[j] * D + kc * 128
                    idx = (s * 4 + j) * 8 + kc
                    kb.emit("pe", lambda e, s=s, c0=c0, idx=idx: e.matmul(
                        pcol[:, idx:idx + 1], lhsT=modrow[0:2, c0:c0 + 128], rhs=cst[0:2, 530 + s:531 + s],
                        start=True, stop=True), [B_modrow, B_cst], [B_pcol], inc=(j == 3 and kc == 7 and s == NSEQ - 1))
        kb.emit("dve", lambda e: e.tensor_copy(out=modcol[:].rearrange("p s j k -> p (s j k)"), in_=pcol[:]),
                [B_pcol], [B_modcol])
        for s in range(NSEQ):
            for jj, gi in ((0, 0), (2, 1)):
                kb.emit("dve", lambda e, s=s, jj=jj, gi=gi: e.scalar_tensor_tensor(
                    out=modcol[:, s, jj, :], in0=modcol[:, s, jj, :], scalar=1.0, in1=g12[:, gi, :],
                    op0=ALU.add, op1=ALU.mult), [B_modcol, B_g12], [B_modcol])
        pbc = [ps("pbc%d" % i, [128, 512], F32, p0) for i in range(2)]
        B_pbc = [kb.pbuf(), kb.pbuf()]
        t = 0
        for s in range(NSEQ):
            for g, base in ((0, 2 * D), (1, 5 * D)):
                for hh in range(2):
                    j = t % 2
                    t += 1
                    kb.emit("pe", lambda e, s=s, base=base, hh=hh, j=j: e.matmul(
                        pbc[j][:], lhsT=cst[0:2, 532 + s * 128:532 + (s + 1) * 128],
                        rhs=modrow[0:2, base + hh * 512:base + (hh + 1) * 512], start=True, stop=True),
                        [B_modrow, B_cst], [B_pbc[j]])
                    kb.emit("act", lambda e, s=s, g=g, hh=hh, j=j: e.copy(
                        out=gatebc[:, s, g, hh * 512:(hh + 1) * 512], in_=pbc[j][:]), [B_pbc[j]], [B_gatebc])
        if dbg and "modcol" in dbg:
            kb.dma("sp", lambda e: e.dma_start(out=dbg_d["modcol"][:], in_=modcol[:].rearrange("p s j k -> p (s j k)")),
                   "st_dbg", reads=[B_modcol])
        g2r = sb("g2r", [2, D], F32, p0)
        B_g2r = kb.buf()
        for r_ in range(2):
            kb.dma("sp", lambda e, r_=r_: e.dma_start(out=g2r[r_:r_ + 1, :], in_=g2row_d[:]), "ld_g2r", writes=[B_g2r])
        kb.emit("dve", lambda e: e.scalar_tensor_tensor(out=g2r[:], in0=modrow[:, 4 * D:5 * D], scalar=1.0, in1=g2r[:], op0=ALU.add, op1=ALU.mult),
                [B_modrow, B_g2r], [B_g2r])
        kb.dma("sp", lambda e: e.dma_start(out=mod2_d[:, 0:D], in_=g2r[:]), "st_mod2", reads=[B_g2r], writes=[B_mod2d])
        kb.dma("sp", lambda e: e.dma_start(out=mod2_d[:, D:2 * D], in_=modrow[:, 3 * D:4 * D]), "st_mod2", reads=[B_modrow], writes=[B_mod2d])
        kb.barrier()
        kb.flush()

    onesb = sb("onesb", [128, 128], BF16)
    B_onesb = kb.buf()
    kb.emit("pool", lambda e: e.memset(onesb[:], 1.0), [], [B_onesb])
    wq = sb("wq", [128, 3, 768], BF16)
    wkv = sb("wkv", [128, 2, 1024], BF16)
    gq_bc = sb("gq_bc", [128, 8, 96])
    gk_bc = sb("gk_bc", [128, 8, 96])
    cosT = sb("cosT", [128, NSEQ * NT, 16])
    sinT = sb("sinT", [128, NSEQ * NT, 16])
    B_wq, B_wkv, B_gq, B_gk, B_cs = (kb.buf() for _ in range(5))
    with ExitStack() as pw:
        wq_f = sb("wq_f", [128, 3, 768], F32, pw)
        wkv_f = sb("wkv_f", [128, 2, 1024], F32, pw)
        qag = sb("qag", [128, 3], F32, pw)
        kvag = sb("kvag", [128, 2], F32, pw)
        g96 = sb("g96", [128, 2, 96], F32, pw)
        posi = sb("posi", [128, NSEQ * NT], I32, pw)
        posf = sb("posf", [128, NSEQ * NT], F32, pw)
        ang = sb("ang", [128, NSEQ * NT, 16], F32, pw)
        ang2 = sb("ang2", [128, NSEQ * NT, 16], F32, pw)
        B_wqf, B_wkvf, B_qag, B_kvag, B_g96, B_posi, B_posf, B_ang, B_ang2 = (kb.buf() for _ in range(9))
        kb.dma("sp", lambda e: e.dma_start(out=wq_f[:], in_=wq_d[:]), "ld_wqf", writes=[B_wqf])
        kb.dma("sp", lambda e: e.dma_start(out=wkv_f[:], in_=wkv_d[:]), "ld_wkvf", writes=[B_wkvf])
        kb.dma("sp", lambda e: e.dma_start(out=qag[:], in_=qag_d[:]), "ld_qag", writes=[B_qag])
        kb.dma("sp", lambda e: e.dma_start(out=kvag[:], in_=kvag_d[:]), "ld_kvag", writes=[B_kvag])
        kb.dma("sp", lambda e: e.dma_start(out=g96[:, 0, :], in_=qng_d.partition_broadcast(128)), "ld_g96", writes=[B_g96])
        kb.dma("sp", lambda e: e.dma_start(out=g96[:, 1, :], in_=kng_d.partition_broadcast(128)), "ld_g96", writes=[B_g96])
        kb.dma("sp", lambda e: e.dma_start(out=posi[:], in_=pos_d[:]), "ld_pos", writes=[B_posi])
        for c in range(3):
            kb.emit("dve", lambda e, c=c: e.tensor_scalar_mul(out=wq[:, c, :], in0=wq_f[:, c, :], scalar1=qag[:, c:c + 1]),
                    [B_wqf, B_qag], [B_wq])
        for c in range(2):
            kb.emit("dve", lambda e, c=c: e.tensor_scalar_mul(out=wkv[:, c, :], in0=wkv_f[:, c, :], scalar1=kvag[:, c:c + 1]),
                    [B_wkvf, B_kvag], [B_wkv])
        kb.emit("dve", lambda e: e.tensor_scalar_mul(out=gq_bc[:], in0=g96[:, 0:1, :].to_broadcast([128, 8, 96]),
                                                     scalar1=float(96 ** -0.5)), [B_g96], [B_gq])
        kb.emit("dve", lambda e: e.tensor_copy(out=gk_bc[:], in_=g96[:, 1:2, :].to_broadcast([128, 8, 96])), [B_g96], [B_gk])
        kb.emit("dve", lambda e: e.tensor_copy(out=posf[:], in_=posi[:]), [B_posi], [B_posf])
        kb.emit("dve", lambda e: e.tensor_tensor(out=ang[:], in0=posf[:].unsqueeze(2).to_broadcast([128, NSEQ * NT, 16]),
                                                 in1=cst[:, 514:530].unsqueeze(1).to_broadcast([128, NSEQ * NT, 16]),
                                                 op=ALU.mult), [B_posf, B_cst], [B_ang])
        PI = float(np.pi)
        angi = sb("angi", [128, NSEQ * NT, 16], I32, pw)
        B_angi = kb.buf()

        def sin_table(dst, shift):
            kb.emit("dve", lambda e: e.tensor_scalar(out=ang2[:], in0=ang[:], scalar1=float(1.0 / (2 * PI)), scalar2=None, op0=ALU.mult),
                    [B_ang], [B_ang2])
            kb.emit("dve", lambda e: e.tensor_copy(out=angi[:], in_=ang2[:]), [B_ang2], [B_angi])
            kb.emit("dve", lambda e: e.tensor_copy(out=ang2[:], in_=angi[:]), [B_angi], [B_ang2])
            kb.emit("dve", lambda e: e.scalar_tensor_tensor(out=ang2[:], in0=ang2[:], scalar=-2 * PI, in1=ang[:], op0=ALU.mult, op1=ALU.add),
                    [B_ang2, B_ang], [B_ang2])
            if shift != 0.0:
                kb.emit("dve", lambda e: e.tensor_scalar(out=ang2[:], in0=ang2[:], scalar1=float(shift), scalar2=None, op0=ALU.add),
                        [B_ang2], [B_ang2])
            kb.emit("dve", lambda e: e.tensor_scalar(out=angi[:].bitcast(F32), in0=ang2[:], scalar1=PI, scalar2=-2 * PI, op0=ALU.is_gt, op1=ALU.mult),
                    [B_ang2], [B_angi])
            kb.emit("dve", lambda e: e.tensor_tensor(out=ang2[:], in0=ang2[:], in1=angi[:].bitcast(F32), op=ALU.add), [B_ang2, B_angi], [B_ang2])
            kb.emit("dve", lambda e: e.tensor_scalar(out=angi[:].bitcast(F32), in0=ang2[:], scalar1=-PI, scalar2=2 * PI, op0=ALU.is_lt, op1=ALU.mult),
                    [B_ang2], [B_angi])
            kb.emit("dve", lambda e: e.tensor_tensor(out=ang2[:], in0=ang2[:], in1=angi[:].bitcast(F32), op=ALU.add), [B_ang2, B_angi], [B_ang2])
            kb.emit("act", lambda e: e.activation(out=dst[:], in_=ang2[:], func=AF.Sin), [B_ang2], [B_cs])

        sin_table(sinT, 0.0)
        sin_table(cosT, PI / 2)
        kb.barrier()
        kb.flush()

    wr = sb("wr", [128, 8, 36], BF16)
    brb = sb("brb", [128, 36])
    EIDX = sb("EIDX", [128, NSEQ * NT, 2])
    WK = sb("WK", [128, NSEQ * NT, 2])
    iota_e = sb("iota_e", [128, 32])
    B_wr, B_brb, B_iota = kb.buf(), kb.buf(), kb.buf()
    B_EW = [kb.buf() for _ in range(NSEQ * NT)]
    kb.dma("sp", lambda e: e.dma_start(out=iota_e[:], in_=cstm_d[:, 128:160]), "ld_iota", writes=[B_iota])
    kb.dma("pool", lambda e: e.dma_start(out=wr[:], in_=wr_d[:]), "ld_wr", writes=[B_wr])
    kb.dma("sp", lambda e: e.dma_start(out=brb[:], in_=br_d.partition_broadcast(128)), "ld_brb", writes=[B_brb])
    B_x1d = [[kb.buf() for _ in range(NT)] for _ in range(NSEQ)]
    B_h2d = [[kb.buf() for _ in range(NT)] for _ in range(NSEQ)]

    def dbg_store(name, ap, bufs):
        if dbg and name in dbg:
            kb.dma("sp", lambda e: e.dma_start(out=dbg_d[name][:], in_=ap), "st_dbg", reads=bufs)

    class NormT:
        def __init__(self, stack, tag, ntp=2, dve_evac=False, ncv=1):
            self.dve_evac = dve_evac
            self.ncv = ncv
            self.xn = [sb("xn%s%d" % (tag, i), [128, D], BF16, stack) for i in range(2)]
            self.st = sb("st" + tag, [128, 2, 4], F32, stack)
            tps = [ps("tp%s%d" % (tag, i), [128, 8, 128], BF16, stack) for i in range(ntp)]
            btp = [kb.pbuf() for _ in range(ntp)]
            self.tp = [tps[i % ntp] for i in range(2)]
            self.B_tp = [btp[i % ntp] for i in range(2)]
            self.B_xn = [kb.buf(), kb.buf()]
            self.B_st = [kb.buf(), kb.buf()]
            self.n = 0

        def run(self, src, B_src, s, jg, dst, B_dst):
            j = self.n % 2
            self.n += 1
            issue_cv(self.ncv)
            st, xn, tp = self.st, self.xn[j], self.tp[j]
            junk = xn
            B_st, B_xn, B_tp = self.B_st[j], self.B_xn[j], self.B_tp[j]
            kb.emit("act", lambda e: e.activation(out=junk[:], in_=src, func=AF.Square, accum_out=st[:, j, 0:1]),
                    [B_src], [B_xn, B_st])
            kb.emit("act", lambda e: e.activation(out=st[:, j, 1:2], in_=st[:, j, 0:1], func=AF.Ln, scale=1.0 / D, bias=cst[:, 919:920]),
                    [B_st, B_cst], [B_st])
            kb.emit("act", lambda e: e.activation(out=st[:, j, 2:3], in_=st[:, j, 1:2], func=AF.Exp, scale=-0.5), [B_st], [B_st])
            kb.emit("dve", lambda e: e.tensor_scalar_mul(out=xn[:], in0=src, scalar1=st[:, j, 2:3]), [B_src, B_st], [B_xn])
            for kc in range(8):
                kb.emit("pe", lambda e, kc=kc: e.transpose(out=tp[:, kc, :], in_=xn[:, kc * 128:(kc + 1) * 128], identity=identb[:]),
                        [B_xn, B_identb], [B_tp], inc=(kc == 7))
            if self.dve_evac:
                kb.emit("dve", lambda e: e.tensor_tensor(out=dst, in0=tp[:], in1=modcol[:, s, jg, :].unsqueeze(2).to_broadcast([128, 8, 128]), op=ALU.mult),
                        [B_tp, B_modcol], [B_dst])
                kb.emit("pool", lambda e: e.tensor_tensor(out=dst, in0=dst, in1=modcol[:, s, jg + 1, :].unsqueeze(2).to_broadcast([128, 8, 128]), op=ALU.add),
                        [B_dst, B_modcol], [B_dst])
            else:
                for kc in range(8):
                    kb.emit("act", lambda e, kc=kc: e.activation(out=dst[:, kc, :], in_=tp[:, kc, :], func=AF.Identity,
                                                                 bias=modcol[:, s, jg + 1, kc:kc + 1], scale=modcol[:, s, jg, kc:kc + 1]),
                            [B_tp, B_modcol], [B_dst])
            return xn, B_xn

    nseq_run = NSEQ if stop is None else 1
    for s in range(nseq_run):
        with ExitStack() as sq_:
            OT = sb("OT", [64, 8, S], BF16, sq_)
            B_OT = kb.buf()

            with ExitStack() as pm:
                QT = sb("QT", [128, 8, S if stop != "hT" else 4], BF16, pm)
                KT = sb("KT", [128, 8, S if stop != "hT" else 4], BF16, pm)
                V2 = sb("V2", [128, NT, 8, 65], BF16, pm)
                rstdk = sb("rstdk", [128, NT, 8], F32, pm)
                B_QT, B_KT, B_V2, B_rk = kb.buf(), kb.buf(), kb.buf(), kb.buf()
                kb.emit("pool", lambda e: e.memset(V2[:, :, :, 64:65], 1.0), [], [B_V2])
                with ExitStack() as pb:
                    winm = sb("winm", [128, 8, 672], BF16, pb)
                    B_winm = kb.buf()
                    kb.dma("pool", lambda e: e.dma_start(out=winm[:], in_=win_d[:, :, 0:672]), "ld_winm", writes=[B_winm])
                    xt = [sb("xt0", [128, D], F32, pb), sb("xt1", [128, D], F32, pb)]
                    B_xt = [kb.buf(), kb.buf()]
                    nrm = NormT(pb, "a", ntp=1, ncv=2)
                    hTc = sb("hTc", [128, 8, 512], BF16, pb)
                    B_hTc = [kb.buf() for _ in range(4)]
                    latT = sb("latT", [128, 5, 512], BF16, pb)
                    sqT = sb("sqT", [128, 5, 512], BF16, pb)
                    B_latT, B_sqT = kb.buf(), kb.buf()
                    plat = [ps("plat%d" % i, [128, 512], F32, pb) for i in range(2)]
                    B_plat = [kb.pbuf(), kb.pbuf()]
                    pq = ps("pq", [128, 1024], F32, pb)
                    B_pq = kb.pbuf()
                    pkv = ps("pkv", [128, 1024], F32, pb)
                    B_pkv = kb.pbuf()
                    psm = ps("psm", [128, 64], F32, pb)
                    B_pss = kb.pbuf()
                    B_pkr = B_pss
                    ptq = nrm.tp[0]
                    B_ptq = nrm.B_tp[0]
                    rst4 = sb("rst", [128, 4, 4], F32, pb)
                    B_rst = kb.buf()
                    hstk = sb("hstk", [128, 3, 8], F32, pb)
                    rpk = sb("rpk", [128, 4, 1, 16], F32, pb)
                    B_hstk, B_rpk = kb.buf(), kb.buf()
                    qf = sb("qf", [128, 8, 96], F32, pb)
                    qsq = sb("qsq", [128, 8, 96], F32, pb)
                    qn = sb("qn", [128, 8, 96], F32, pb)
                    hst = sb("hst", [128, 3, 8], F32, pb)
                    rp_q = sb("rp", [128, 4, 8, 16], F32, pb)
                    qfin = sb("qfin", [128, 8, 96], BF16, pb)
                    kvf = sb("kvf", [128, 8, 128], F32, pb)
                    ksq = sb("ksq", [128, 8, 64], F32, pb)
                    krf = sb("krf", [128, 3, 32], F32, pb)
                    kst = sb("kst", [128, 4], F32, pb)
                    kfin = sb("kfin", [128, 8, 96], BF16, pb)
                    B_qf, B_qsq, B_qn, B_hst, B_rp_q, B_qfin, B_kvf, B_ksq, B_krf, B_kst, B_kfin = (kb.buf() for _ in range(11))

                    def rope(src3, dst3, cs_i, nh, Bsrc, Bdst, rp=None, B_rp=None):
                        if rp is None:
                            rp, B_rp = rp_q, B_rp_q
                        cb = cosT[:, cs_i:cs_i + 1, :].to_broadcast([128, nh, 16])
                        sbb = sinT[:, cs_i:cs_i + 1, :].to_broadcast([128, nh, 16])
                        x1 = src3[:, :, 0:16]
                        x2 = src3[:, :, 16:32]
                        r = rp[:, :, 0:nh, :]
                        kb.emit("dve", lambda e: e.tensor_tensor(out=r[:, 0], in0=x1, in1=cb, op=ALU.mult), [Bsrc, B_cs], [B_rp])
                        kb.emit("dve", lambda e: e.tensor_tensor(out=r[:, 1], in0=x2, in1=sbb, op=ALU.mult), [Bsrc, B_cs], [B_rp])
                        kb.emit("dve", lambda e: e.tensor_tensor(out=r[:, 2], in0=x2, in1=cb, op=ALU.mult), [Bsrc, B_cs], [B_rp])
                        kb.emit("dve", lambda e: e.tensor_tensor(out=r[:, 3], in0=x1, in1=sbb, op=ALU.mult), [Bsrc, B_cs], [B_rp])
                        kb.emit("dve", lambda e: e.tensor_tensor(out=dst3[:, :, 0:16], in0=r[:, 0], in1=r[:, 1], op=ALU.subtract),
                                [B_rp], [Bdst])
                        kb.emit("dve", lambda e: e.tensor_tensor(out=dst3[:, :, 16:32], in0=r[:, 2], in1=r[:, 3], op=ALU.add),
                                [B_rp], [Bdst])

                    ncc = 4 if stop != "hT" else 1
                    def ld_x(i):
                        j = i % 2
                        kb.dma("sp", lambda e: e.dma_start(out=xt[j][:], in_=x_d[s, i * 128:(i + 1) * 128, :]), "ld_xt%d" % j, writes=[B_xt[j]])

                    ld_x(0)
                    ld_x(1)
                    for cc in range(ncc):
                        for ti in range(4):
                            i = cc * 4 + ti
                            j = i % 2
                            nrm.run(xt[j][:], B_xt[j], s, 0, hTc[:, :, ti * 128:(ti + 1) * 128], B_hTc[ti])
                            if i + 2 < 4 * ncc:
                                ld_x(i + 2)
                        if stop == "hT":
                            break
                        for jc in range(5):
                            pj = jc % 2
                            for kc in range(8):
                                kb.emit("pe", lambda e, pj=pj, jc=jc, kc=kc: e.matmul(
                                    plat[pj][:], lhsT=winm[:, kc, jc * 128:(jc + 1) * 128], rhs=hTc[:, kc, :],
                                    start=(kc == 0), stop=(kc == 7)), [B_winm] + B_hTc, [B_plat[pj]], inc=(kc == 7))
                            kb.emit("act", lambda e, pj=pj, jc=jc: e.copy(out=latT[:, jc, :], in_=plat[pj][:]), [B_plat[pj]], [B_latT])
                            kb.emit("act", lambda e, pj=pj, jc=jc: e.activation(out=sqT[:, jc, :], in_=plat[pj][:], func=AF.Square),
                                    [B_plat[pj]], [B_sqT])
                        for ti in range(4):
                            t0 = ti * 128
                            rst = rst4[:, ti, :]
                            for jc in range(5):
                                col = 0 if jc < 3 else 1
                                kb.emit("pe", lambda e, jc=jc, col=col, t0=t0: e.matmul(
                                    psm[:, col:col + 1], lhsT=sqT[:, jc, t0:t0 + 128], rhs=onesb[:, 0:1],
                                    start=(jc in (0, 3)), stop=(jc in (2, 4))), [B_sqT, B_onesb], [B_pss], inc=(jc == 4))
                            kb.emit("dve", lambda e, rst=rst: e.tensor_tensor(out=rst[:, 0:2], in0=psm[:, 0:2], in1=cst[:, 916:918], op=ALU.mult),
                                    [B_pss, B_cst], [B_rst])
                            kb.emit("act", lambda e, rst=rst: e.activation(out=rst[:, 2:4], in_=rst[:, 0:2], func=AF.Ln, bias=cst[:, 919:920]),
                                    [B_rst, B_cst], [B_rst])
                            kb.emit("act", lambda e, rst=rst: e.activation(out=rst[:, 2:4], in_=rst[:, 2:4], func=AF.Exp, scale=-0.5), [B_rst], [B_rst])

                        def qchain(ti):
                            i = cc * 4 + ti
                            gi = s * NT + i
                            t0 = ti * 128
                            rst = rst4[:, ti, :]
                            for c in range(3):
                                kb.emit("pe", lambda e, c=c: e.matmul(pq[:, 0:512], lhsT=latT[:, c, t0:t0 + 128], rhs=wq[:, c, 0:512],
                                                                      start=(c == 0), stop=(c == 2)), [B_latT, B_wq], [B_pq], inc=False)
                            for c in range(3):
                                kb.emit("pe", lambda e, c=c: e.matmul(pq[:, 512:768], lhsT=latT[:, c, t0:t0 + 128], rhs=wq[:, c, 512:768],
                                                                      start=(c == 0), stop=(c == 2)), [B_latT, B_wq], [B_pq], inc=(c == 2))
                            yield
                            qf2 = qf[:].rearrange("p h d -> p (h d)")
                            kb.emit("act", lambda e: e.activation(out=qf2, in_=pq[:, 0:768], func=AF.Copy, scale=rst[:, 2:3]), [B_pq, B_rst], [B_qf])
                            yield
                            kb.emit("act", lambda e: e.activation(out=qsq[:], in_=qf[:], func=AF.Square), [B_qf], [B_qsq])
                            yield
                            kb.emit("dve", lambda e: e.tensor_reduce(out=hst[:, 0, :], in_=qsq[:], axis=AX.X, op=ALU.add), [B_qsq], [B_hst])
                            yield
                            kb.emit("act", lambda e: e.activation(out=hst[:, 1, :], in_=hst[:, 0, :], func=AF.Ln, scale=1.0 / 96, bias=cst[:, 919:920]),
                                    [B_hst, B_cst], [B_hst])
                            kb.emit("act", lambda e: e.activation(out=hst[:, 2, :], in_=hst[:, 1, :], func=AF.Exp, scale=-0.5), [B_hst], [B_hst])
                            yield
                            kb.emit("dve", lambda e: e.tensor_tensor(out=qn[:], in0=qf[:], in1=hst[:, 2, :].unsqueeze(2).to_broadcast([128, 8, 96]),
                                                                     op=ALU.mult), [B_qf, B_hst], [B_qn])
                            yield
                            kb.emit("dve", lambda e: e.tensor_tensor(out=qn[:], in0=qn[:], in1=gq_bc[:], op=ALU.mult), [B_qn, B_gq], [B_qn])
                            yield
                            kb.emit("act", lambda e: e.copy(out=qfin[:, :, 0:64], in_=qn[:, :, 0:64]), [B_qn], [B_qfin])
                            rope(qn[:, :, 64:96], qfin[:, :, 64:96], gi, 8, B_qn, B_qfin)
                            yield
                            for h in range(8):
                                kb.emit("pe", lambda e, h=h: e.transpose(out=ptq[0:96, h, :], in_=qfin[:, h, :], identity=identb[:]),
                                        [B_qfin, B_identb], [B_ptq], inc=(h == 7))
                            kb.emit("act", lambda e: e.copy(out=QT[0:96, :, i * 128:(i + 1) * 128], in_=ptq[0:96, :, :]), [B_ptq], [B_QT])
                            yield

                        def kchain(ti):
                            i = cc * 4 + ti
                            gi = s * NT + i
                            t0 = ti * 128
                            rst = rst4[:, ti, :]
                            hst_ = hstk
                            for hh in range(2):
                                for c in range(2):
                                    kb.emit("pe", lambda e, c=c, hh=hh: e.matmul(
                                        pkv[:, hh * 512:(hh + 1) * 512], lhsT=latT[:, 3 + c, t0:t0 + 128], rhs=wkv[:, c, hh * 512:(hh + 1) * 512],
                                        start=(c == 0), stop=(c == 1)), [B_latT, B_wkv], [B_pkv], inc=(c == 1 and hh == 1))
                            for kc in range(8):
                                kb.emit("pe", lambda e, kc=kc: e.matmul(psm[:, 32:64], lhsT=hTc[:, kc, t0:t0 + 128], rhs=winm[:, kc, 640:672],
                                                                        start=(kc == 0), stop=(kc == 7)), [B_hTc[ti], B_winm], [B_pkr], inc=(kc == 7))
                            yield
                            kvf2 = kvf[:].rearrange("p h d -> p (h d)")
                            kb.emit("act", lambda e: e.activation(out=kvf2, in_=pkv[:], func=AF.Copy, scale=rst[:, 3:4]), [B_pkv, B_rst], [B_kvf])
                            yield
                            kb.emit("pool", lambda e: e.tensor_copy(out=V2[:, i, :, 0:64], in_=kvf[:, :, 64:128]), [B_kvf], [B_V2])
                            kb.emit("act", lambda e: e.copy(out=krf[:, 0, :], in_=psm[:, 32:64]), [B_pkr], [B_krf])
                            kb.emit("act", lambda e: e.activation(out=krf[:, 1, :], in_=krf[:, 0, :], func=AF.Square, accum_out=kst[:, 0:1]),
                                    [B_krf], [B_krf, B_kst])
                            yield
                            kb.emit("act", lambda e: e.activation(out=ksq[:], in_=kvf[:, :, 0:64], func=AF.Square), [B_kvf], [B_ksq])
                            yield
                            kb.emit("dve", lambda e: e.tensor_reduce(out=hst_[:, 0, :], in_=ksq[:], axis=AX.X, op=ALU.add), [B_ksq], [B_hstk])
                            kb.emit("dve", lambda e: e.tensor_scalar(out=hst_[:, 1, :], in0=hst_[:, 0, :], scalar1=kst[:, 0:1], scalar2=1.0 / 96,
                                                                     op0=ALU.add, op1=ALU.mult), [B_hstk, B_kst], [B_hstk])
                            yield
                            kb.emit("act", lambda e: e.activation(out=hst_[:, 2, :], in_=hst_[:, 1, :], func=AF.Ln, bias=cst[:, 919:920]),
                                    [B_hstk, B_cst], [B_hstk])
                            kb.emit("act", lambda e: e.activation(out=rstdk[:, i, :], in_=hst_[:, 2, :], func=AF.Exp, scale=-0.5), [B_hstk], [B_rk])
                            yield
                            kb.emit("dve", lambda e: e.tensor_tensor(out=kfin[:, :, 0:64], in0=kvf[:, :, 0:64], in1=gk_bc[:, :, 0:64], op=ALU.mult),
                                    [B_kvf, B_gk], [B_kfin])
                            yield
                            kb.emit("dve", lambda e: e.tensor_tensor(out=krf[:, 1, :], in0=krf[:, 0, :], in1=gk_bc[:, 0, 64:96], op=ALU.mult),
                                    [B_krf, B_gk], [B_krf])
                            rope(krf[:, 1:2, :], krf[:, 2:3, :], gi, 1, B_krf, B_krf, rpk, B_rpk)
                            yield
                            kb.emit("dve", lambda e: e.tensor_copy(out=kfin[:, :, 64:96], in_=krf[:, 2:3, :].to_broadcast([128, 8, 32])),
                                    [B_krf], [B_kfin])
                            for h in range(8):
                                kb.emit("pe", lambda e, h=h: e.transpose(out=ptq[0:96, h, :], in_=kfin[:, h, :], identity=identb[:]),
                                        [B_kfin, B_identb], [B_ptq], inc=(h == 7))
                            kb.emit("act", lambda e: e.copy(out=KT[0:96, :, i * 128:(i + 1) * 128], in_=ptq[0:96, :, :]), [B_ptq], [B_KT])
                            yield

                        def run_il(gens):
                            gens = list(gens)
                            while gens:
                                for g in list(gens):
                                    try:
                                        next(g)
                                    except StopIteration:
                                        gens.remove(g)

                        run_il([qchain(0)])
                        for ti in range(4):
                            gl_ = [kchain(ti)]
                            if ti + 1 < 4:
                                gl_.append(qchain(ti + 1))
                            run_il(gl_)
                    if dbg and "hT" in dbg:
                        hTf = sb("hTf", [128, 8, 512], F32, pb)
                        B_hTf = kb.buf()
                        kb.emit("dve", lambda e: e.tensor_copy(out=hTf[:], in_=hTc[:]), B_hTc, [B_hTf])
                        dbg_store("hT", hTf[:].rearrange("p k t -> p (k t)"), [B_hTf])
                    kb.barrier()
                    kb.flush()
                if stop == "hT":
                    break
                if dbg and "QT" in dbg:
                    with ExitStack() as pd:
                        tf = sb("QTf", [128, 8, 512], F32, pd)
                        B_tf = kb.buf()
                        for nm, src, Bs in (("QT", QT, B_QT), ("KT", KT, B_KT)):
                            dv = dbg_d[nm].rearrange("p (k t) -> p k t", k=8)
                            for cc in range(4):
                                kb.emit("dve", lambda e, src=src, cc=cc: e.tensor_copy(out=tf[0:96], in_=src[0:96, :, cc * 512:(cc + 1) * 512]), [Bs], [B_tf])
                                kb.dma("sp", lambda e, dv=dv, cc=cc: e.dma_start(out=dv[:, :, cc * 512:(cc + 1) * 512], in_=tf[0:96]), "st_dbg", reads=[B_tf])
                        dbg_store("rstdk", rstdk[:].rearrange("p i h -> p (i h)"), [B_rk])
                        kb.barrier(); kb.flush()
                if stop == "QK":
                    break

                with ExitStack() as pat:
                    pst = [ps("pst%d" % i, [128, 512], F32, pat) for i in range(4)]
                    B_pst = [kb.pbuf() for _ in range(4)]
                    po = [ps("po%d" % i, [128, 512], F32, pat) for i in range(2)]
                    B_po = [kb.pbuf() for _ in range(2)]
                    pbc2 = ps("pbc2", [64, 512], F32, pat)
                    B_pbc2 = kb.pbuf()
                    pT = [sb("pT%d" % i, [128, 512], BF16, pat) for i in range(4)]
                    B_pT = [kb.buf() for _ in range(4)]
                    rec = sb("rec", [128, 512], F32, pat)
                    B_rec = kb.buf()
                    recb = sb("recb", [64, 512], F32, pat)
                    B_recb = kb.buf()
                    if s == 0 and (stop is None or stop == "moe"):
                        zt = sb("zt", [128, 2048], BF16, pat)
                        B_zt = kb.buf()
                        kb.emit("pool", lambda e: e.memset(zt[:], 0.0), [], [B_zt])
                        xs_v = xs_d.rearrange("(c p r) d -> c p (r d)", p=128, r=2)
                        for c_ in range(xs_v.shape[0]):
                            kb.dma("sp", lambda e, c_=c_: e.dma_start(out=xs_v[c_], in_=zt[:]), "zf_xs", reads=[B_zt], writes=[B_xsz])
                    units = [(h, qc) for h in range(8) for qc in range(4)]
                    steps = [(u, kt) for u in range(len(units)) for kt in range(NT)]

                    def emit_S(n):
                        u, kt = steps[n]
                        h, qc = units[u]
                        j = n % 4
                        kb.emit("pe", lambda e: e.matmul(pst[j][:], lhsT=KT[0:96, h, kt * 128:(kt + 1) * 128],
                                                         rhs=QT[0:96, h, qc * 512:(qc + 1) * 512], start=True, stop=True),
                                [B_KT, B_QT], [B_pst[j]])
                        kb.emit("act", lambda e: e.activation(out=pT[j][:], in_=pst[j][:], func=AF.Exp, scale=rstdk[:, kt, h:h + 1]),
                                [B_pst[j], B_rk], [B_pT[j]])

                    def emit_PV(n):
                        u, kt = steps[n]
                        h, qc = units[u]
                        j = n % 4
                        a = u % 2
                        kb.emit("pe", lambda e: e.matmul(po[a][0:65, :], lhsT=V2[:, kt, h, :], rhs=pT[j][:], start=(kt == 0), stop=(kt == NT - 1)),
                                [B_V2, B_pT[j]], [B_po[a]], inc=(kt == NT - 1))
                        if kt == NT - 1:
                            kb.emit("dve", lambda e: e.reciprocal(out=rec[64:65, :], in_=po[a][64:65, :]), [B_po[a]], [B_rec])
                            kb.emit("pe", lambda e: e.matmul(pbc2[:], lhsT=ones_f[64:65, 0:64], rhs=rec[64:65, :], start=True, stop=True),
                                    [B_rec, B_cst], [B_pbc2])
                            kb.emit("act", lambda e: e.copy(out=recb[:], in_=pbc2[:]), [B_pbc2], [B_recb])
                            kb.emit("dve", lambda e: e.tensor_tensor(out=OT[0:64, h, qc * 512:(qc + 1) * 512], in0=po[a][0:64, :],
                                                                     in1=recb[:], op=ALU.mult), [B_po[a], B_recb], [B_OT])

                    LA = 3
                    for n in range(len(steps) + LA):
                        if n % 16 == 0:
                            issue_cv(1)
                        if n < len(steps):
                            emit_S(n)
                        if n >= LA:
                            emit_PV(n - LA)
                    kb.barrier()
                    kb.flush()
            if dbg and "OT" in dbg:
                with ExitStack() as pd:
                    tf = sb("OTf", [64, 8, S], F32, pd)
                    B_tf = kb.buf()
                    kb.emit("dve", lambda e: e.tensor_copy(out=tf[:], in_=OT[:]), [B_OT], [B_tf])
                    dbg_store("OT", tf[:].rearrange("p k t -> p (k t)"), [B_tf])
                    kb.barrier(); kb.flush()
            if stop == "attn":
                break


            recT = sb("recT", [128, 4, S], BF16, sq_)
            B_recT = kb.buf()
            with ExitStack() as ph:
                winh = sb("winh", [128, 8, 2560], BF16, ph)
                B_winh5 = [kb.buf() for _ in range(5)]
                for cb in (0, 3, 1, 4, 2):
                    kb.dma("pool", lambda e, cb=cb: e.dma_start(out=winh[:, :, cb * 512:(cb + 1) * 512],
                                                                in_=win_d[:, :, 672 + cb * 512:672 + (cb + 1) * 512]),
                           "ld_winh%d" % cb, writes=[B_winh5[cb]])
                ofw = sb("ofw", [128, NT, 512], BF16, ph)
                B_ofw = [kb.buf() for _ in range(NT)]
                lbc = sb("lbc", [128, 2, 512], F32, ph)
                oml = sb("oml", [128, 2, 512], F32, ph)
                hgn = sb("hgn", [128, 128], F32, ph)
                B_lb, B_hgn = kb.buf(), kb.buf()
                with ExitStack() as pl:
                    lraw = sb("lraw", [128, 2, 2, 512], F32, pl)
                    B_lraw = kb.buf()
                    kb.dma("sp", lambda e: e.dma_start(out=lraw[:].rearrange("p a b n -> p (a b n)"), in_=lbl_d.partition_broadcast(128)),
                           "ld_lraw", writes=[B_lraw])
                    kb.dma("sp", lambda e: e.dma_start(out=hgn[:], in_=hgn_d.partition_broadcast(128)), "ld_hgn", writes=[B_hgn])
                    kb.emit("dve", lambda e: e.tensor_tensor(out=lbc[:], in0=lraw[:, :, 0, :], in1=lraw[:, :, 1, :], op=ALU.subtract),
                            [B_lraw], [B_lb])
                    kb.emit("act", lambda e: e.activation(out=lbc[:], in_=lbc[:], func=AF.Sigmoid), [B_lb], [B_lb])
                    kb.emit("dve", lambda e: e.tensor_scalar(out=oml[:], in0=lbc[:], scalar1=-1.0, scalar2=1.0, op0=ALU.mult, op1=ALU.add),
                            [B_lb], [B_lb])
                    kb.barrier()
                    kb.flush()
                xt0 = sb("xth", [128, D], F32, ph)
                B_xt0 = kb.buf()
                nrm = NormT(ph, "h", ntp=1, dve_evac=True, ncv=0)
                hTt2 = [sb("hTt%d" % i, [128, 8, 128], BF16, ph) for i in range(2)]
                B_hTt2 = [kb.buf(), kb.buf()]
                pg = [ps("pg%d" % i, [128, 512], F32, ph) for i in range(2)]
                B_pg = [kb.pbuf() for _ in range(2)]
                pgn = [0]

                def next_pg():
                    j = pgn[0] % 2
                    pgn[0] += 1
                    return pg[j], B_pg[j]

                PA = ps("hPA", [128, 4, 128], F32, ph)
                PK = ps("hPK", [128, 4, 128], F32, ph)
                PI = ps("hPI", [128, 4, 128], F32, ph)
                PAo = ps("hPAo", [128, 4, 128], F32, ph)
                PBo = ps("hPBo", [128, 4, 128], F32, ph)
                B_PA, B_PK, B_PI, B_PAo, B_PBo = (kb.pbuf() for _ in range(5))
                ptr = nrm.tp[0]
                B_ptr = nrm.B_tp[0]
                qq2 = [sb("hq_q%d" % i, [128, 512], F32, ph) for i in range(2)]
                vv = [sb("hq_v%d" % i, [128, 512], BF16, ph) for i in range(3)]
                gg = [sb("hq_g%d" % i, [128, 512], BF16, ph) for i in range(3)]
                sg = sb("hq_sg", [128, 512], F32, ph)
                ff = sb("hq_f", [128, 512], F32, ph)
                kk = sb("hq_k", [128, 512], F32, ph)
                lf = sb("hq_lf", [128, 512], F32, ph)
                eb = sb("hq_eb", [128, 512], F32, ph)
                enb = sb("hq_enb", [128, 512], F32, ph)
                er = sb("hq_er", [128, 512], F32, ph)
                qkd = sb("hq_qkd", [128, 8, 128], BF16, ph)
                kend = [sb("hq_kend%d" % i, [128, 2, 512], BF16, ph) for i in range(2)]
                qkT = [sb("hq_qkT%d" % i, [128, 8, 128], BF16, ph) for i in range(2)]
                qAB = [sb("hq_qAB%d" % i, [128, 2, 4, 128], BF16, ph) for i in range(2)]
                dec = [sb("hq_dec%d" % i, [128, 4, 2], F32, ph) for i in range(2)]
                attm = sb("hq_attm", [128, 4, 128], BF16, ph)
                Sst = sb("hq_S", [128, 4, 128], F32, ph)
                Sbf = sb("hq_Sb", [128, 4, 128], BF16, ph)
                osum = sb("hq_osum", [128, 4, 128], F32, ph)
                osq = sb("hq_osq", [128, 4, 128], F32, ph)
                ost = sb("hq_ost", [128, 3, 4], F32, ph)
                recb_ = sb("hq_rec", [128, 512], BF16, ph)
                (B_sg, B_ff, B_kk, B_lf, B_eb, B_enb, B_er, B_qkd, B_attm, B_osum, B_osq, B_ost, B_rec2) = (kb.buf() for _ in range(13))
                B_qq2 = [kb.buf(), kb.buf()]
                B_vv = [kb.buf(), kb.buf(), kb.buf()]
                B_gg = [kb.buf(), kb.buf(), kb.buf()]
                B_kend = [kb.buf(), kb.buf()]
                B_qkT = [kb.buf(), kb.buf()]
                B_qAB = [kb.buf(), kb.buf()]
                B_dec = [kb.buf(), kb.buf()]
                B_S = [kb.buf() for _ in range(4)]
                B_Sb = [kb.buf() for _ in range(4)]
                for st_ in range(2):
                    kb.emit("pool", lambda e, st_=st_: e.memset(qAB[st_][:], 0.0), [], [B_qAB[st_]])

                def proj(cb, hTt, B_hTt):
                    p, B_p = next_pg()
                    for kc in range(8):
                        kb.emit("pe", lambda e, kc=kc: e.matmul(p[:], lhsT=hTt[:, kc, :], rhs=winh[:, kc, cb * 512:(cb + 1) * 512],
                                                                start=(kc == 0), stop=(kc == 7)), [B_hTt, B_winh5[cb]], [B_p], inc=(kc == 7))
                    return p, B_p

                def stageA1(d, i, n):
                    hTt, B_hTt = hTt2[n % 2], B_hTt2[n % 2]
                    qq, B_qq = qq2[n % 2], B_qq2[n % 2]
                    s3 = n % 3
                    kb.dma("sp", lambda e: e.dma_start(out=xt0[:], in_=x_d[s, i * 128:(i + 1) * 128, :]), "ld_xth", writes=[B_xt0])
                    nrm.run(xt0[:], B_xt0, s, 0, hTt[:], B_hTt)
                    yield
                    p, B_p = proj(0, hTt, B_hTt)
                    kb.emit("act", lambda e: e.activation(out=qq[:], in_=p[:], func=AF.Silu), [B_p], [B_qq])
                    yield
                    if d == 1:
                        p3, B_p3 = proj(4, hTt, B_hTt)
                        kb.emit("act", lambda e: e.activation(out=gg[s3][:], in_=p3[:], func=AF.Silu), [B_p3], [B_gg[s3]])
                        yield
                    p2, B_p2 = proj(3, hTt, B_hTt)
                    kb.emit("act", lambda e: e.copy(out=vv[s3][:], in_=p2[:]), [B_p2], [B_vv[s3]])
                    yield

                def stageA(d, i, n):
                    st = n % 2
                    hTt, B_hTt = hTt2[n % 2], B_hTt2[n % 2]
                    qq, B_qq = qq2[n % 2], B_qq2[n % 2]
                    tri_c = cst[:, 0:128] if d == 0 else cst[:, 128:256]
                    rev_c = cst[:, 256:384] if d == 0 else cst[:, 384:512]
                    p4, B_p4 = proj(1 + d, hTt, B_hTt)
                    kb.emit("act", lambda e: e.activation(out=sg[:], in_=p4[:], func=AF.Sigmoid), [B_p4], [B_sg])
                    kb.emit("dve", lambda e: e.tensor_tensor(out=ff[:], in0=sg[:], in1=oml[:, d, :], op=ALU.mult), [B_sg, B_lb], [B_ff])
                    kb.emit("dve", lambda e: e.tensor_tensor(out=ff[:], in0=ff[:], in1=lbc[:, d, :], op=ALU.add), [B_ff, B_lb], [B_ff])
                    kb.emit("dve", lambda e: e.tensor_scalar(out=kk[:], in0=ff[:], scalar1=-1.0, scalar2=1.0, op0=ALU.mult, op1=ALU.add),
                            [B_ff], [B_kk])
                    kb.emit("act", lambda e: e.activation(out=lf[:], in_=ff[:], func=AF.Ln), [B_ff], [B_lf])
                    yield
                    pb_, B_pb = next_pg()
                    kb.emit("pe", lambda e: e.matmul(pb_[:], lhsT=tri_c, rhs=lf[:], start=True, stop=True), [B_cst, B_lf], [B_pb])
                    kb.emit("act", lambda e: e.activation(out=eb[:], in_=pb_[:], func=AF.Exp), [B_pb], [B_eb])
                    kb.emit("act", lambda e: e.activation(out=enb[:], in_=pb_[:], func=AF.Exp, scale=-1.0), [B_pb], [B_enb])
                    yield
                    pr_, B_pr = next_pg()
                    kb.emit("pe", lambda e: e.matmul(pr_[:], lhsT=rev_c, rhs=lf[:], start=True, stop=True), [B_cst, B_lf], [B_pr])
                    kb.emit("act", lambda e: e.activation(out=er[:], in_=pr_[:], func=AF.Exp), [B_pr], [B_er])
                    yield
                    pd_, B_pd = next_pg()
                    for h in range(4):
                        kb.emit("pe", lambda e, h=h: e.matmul(pd_[:, 2 * h:2 * h + 2], lhsT=lf[:, h * 128:(h + 1) * 128],
                                                              rhs=cst[:, 512:514], start=True, stop=True), [B_lf, B_cst], [B_pd], inc=(h == 3))
                    kb.emit("act", lambda e: e.activation(out=dec[st][:].rearrange("p h c -> p (h c)"), in_=pd_[:, 0:8], func=AF.Exp),
                            [B_pd], [B_dec[st]])
                    kb.emit("dve", lambda e: e.tensor_tensor(out=qkd[:, 0:4, :].rearrange("p h k -> p (h k)"), in0=qq[:], in1=eb[:], op=ALU.mult),
                            [B_qq, B_eb], [B_qkd])
                    kb.emit("dve", lambda e: e.tensor_tensor(out=qkd[:, 4:8, :].rearrange("p h k -> p (h k)"), in0=kk[:], in1=enb[:], op=ALU.mult),
                            [B_kk, B_enb], [B_qkd])
                    for c2 in range(2):
                        kb.emit("dve", lambda e, c2=c2: e.scalar_tensor_tensor(out=kend[st][:, c2, :], in0=er[:], scalar=cst[:, 512 + c2:513 + c2],
                                                                             in1=kk[:], op0=ALU.mult, op1=ALU.mult),
                                [B_kk, B_er, B_cst], [B_kend[st]])
                    yield
                    for j8 in range(8):
                        kb.emit("pe", lambda e, j8=j8: e.transpose(out=ptr[:, j8, :], in_=qkd[:, j8, :], identity=identb[:]),
                                [B_qkd, B_identb], [B_ptr], inc=(j8 == 7))
                    kb.emit("act", lambda e: e.copy(out=qkT[st][:], in_=ptr[:]), [B_ptr], [B_qkT[st]])
                    kb.emit("dve", lambda e: e.tensor_copy(out=qAB[st][:, 0, :, 0:64], in_=ptr[:, 0:4, 0:64]), [B_ptr], [B_qAB[st]])
                    kb.emit("dve", lambda e: e.tensor_copy(out=qAB[st][:, 1, :, 64:128], in_=ptr[:, 0:4, 64:128]), [B_ptr], [B_qAB[st]])
                    yield

                def stageB(d, i, n):
                    st = n % 2
                    s3 = n % 3
                    tri_c = cst[:, 0:128] if d == 0 else cst[:, 128:256]
                    corder = [0, 1] if d == 0 else [1, 0]
                    for h in range(4):
                        kb.emit("pe", lambda e, h=h: e.matmul(PA[:, h, :], lhsT=qkT[st][:, 4 + h, :], rhs=qkT[st][:, h, :], start=True, stop=True),
                                [B_qkT[st]], [B_PA], inc=(h == 3))
                    yield
                    kb.emit("dve", lambda e: e.tensor_tensor(out=attm[:], in0=PA[:], in1=tri_c.unsqueeze(1).to_broadcast([128, 4, 128]), op=ALU.mult),
                            [B_PA, B_cst], [B_attm])
                    yield
                    for ci, cidx in enumerate(corder):
                        Po_, B_Po_ = (PAo, B_PAo) if ci == 0 else (PBo, B_PBo)
                        for h in range(4):
                            hs = slice(h * 128, (h + 1) * 128)
                            if ci == 0:
                                kb.emit("pe", lambda e, h=h, hs=hs: e.matmul(PI[:, h, :], lhsT=attm[:, h, :], rhs=vv[s3][:, hs], start=True, stop=True),
                                        [B_attm, B_vv[s3]], [B_PI], inc=False)
                            kb.emit("pe", lambda e, h=h, cidx=cidx, Po_=Po_: e.matmul(Po_[:, h, :], lhsT=qAB[st][:, cidx, h, :], rhs=Sbf[:, h, :],
                                                                                    start=True, stop=True), [B_qAB[st], B_Sb[h]], [B_Po_], inc=False)
                            kb.emit("pe", lambda e, h=h, cidx=cidx, hs=hs: e.matmul(PK[:, h, :], lhsT=kend[st][:, cidx, hs], rhs=vv[s3][:, hs],
                                                                                  start=True, stop=True), [B_kend[st], B_vv[s3]], [B_PK], inc=(h == 3))
                        yield
                        for h in range(4):
                            kb.emit("dve", lambda e, h=h, cidx=cidx: e.scalar_tensor_tensor(
                                out=Sst[:, h, :], in0=Sst[:, h, :], scalar=dec[st][:, h, cidx:cidx + 1], in1=PK[:, h, :], op0=ALU.mult, op1=ALU.add),
                                [B_S[h], B_dec[st], B_PK], [B_S[h]])
                            kb.emit("pool", lambda e, h=h: e.tensor_copy(out=Sbf[:, h, :], in_=Sst[:, h, :]), [B_S[h]], [B_Sb[h]])
                        yield
                    kb.emit("act", lambda e: e.copy(out=osum[:], in_=PI[:]), [B_PI], [B_osum])
                    kb.emit("dve", lambda e: e.tensor_tensor(out=osum[:], in0=osum[:], in1=PAo[:], op=ALU.add), [B_osum, B_PAo], [B_osum])
                    if d == 0:
                        kb.emit("dve", lambda e: e.tensor_tensor(out=ofw[:, i, :], in0=osum[:].rearrange("p h v -> p (h v)"),
                                                                 in1=PBo[:].rearrange("p h v -> p (h v)"), op=ALU.add), [B_osum, B_PBo], [B_ofw[i]])
                        yield
                        return
                    kb.emit("dve", lambda e: e.tensor_tensor(out=osum[:], in0=osum[:], in1=PBo[:], op=ALU.add), [B_osum, B_PBo], [B_osum])
                    kb.emit("dve", lambda e: e.tensor_tensor(out=osum[:].rearrange("p h v -> p (h v)"), in0=osum[:].rearrange("p h v -> p (h v)"),
                                                             in1=ofw[:, i, :], op=ALU.add), [B_osum, B_ofw[i]], [B_osum])
                    yield
                    kb.emit("act", lambda e: e.activation(out=osq[:], in_=osum[:], func=AF.Square), [B_osum], [B_osq])
                    kb.emit("dve", lambda e: e.tensor_reduce(out=ost[:, 0, :], in_=osq[:], axis=AX.X, op=ALU.add), [B_osq], [B_ost])
                    kb.emit("act", lambda e: e.activation(out=ost[:, 1, :], in_=ost[:, 0, :], func=AF.Ln, scale=1.0 / 128, bias=cst[:, 919:920]),
                            [B_ost, B_cst], [B_ost])
                    kb.emit("act", lambda e: e.activation(out=ost[:, 2, :], in_=ost[:, 1, :], func=AF.Exp, scale=-0.5), [B_ost], [B_ost])
                    kb.emit("dve", lambda e: e.tensor_tensor(out=osum[:], in0=osum[:], in1=ost[:, 2, :].unsqueeze(2).to_broadcast([128, 4, 128]),
                                                             op=ALU.mult), [B_osum, B_ost], [B_osum])
                    kb.emit("dve", lambda e: e.tensor_tensor(out=osum[:], in0=osum[:], in1=hgn[:].unsqueeze(1).to_broadcast([128, 4, 128]),
                                                             op=ALU.mult), [B_osum, B_hgn], [B_osum])
                    kb.emit("dve", lambda e: e.tensor_tensor(out=recb_[:], in0=osum[:].rearrange("p h v -> p (h v)"), in1=gg[s3][:], op=ALU.mult),
                            [B_osum, B_gg[s3]], [B_rec2])
                    yield
                    for h in range(4):
                        kb.emit("pe", lambda e, h=h: e.transpose(out=ptr[:, h, :], in_=recb_[:, h * 128:(h + 1) * 128], identity=identb[:]),
                                [B_rec2, B_identb], [B_ptr], inc=(h == 3))
                    kb.emit("act", lambda e: e.copy(out=recT[:, :, i * 128:(i + 1) * 128], in_=ptr[:, 0:4, :]), [B_ptr], [B_recT])
                    yield

                def run_interleaved(gens):
                    gens = [g for g in gens if g is not None]
                    while gens:
                        for g in list(gens):
                            try:
                                next(g)
                            except StopIteration:
                                gens.remove(g)

                for d in range(2):
                    kb.emit("pool", lambda e: e.memset(Sst[:], 0.0), [], B_S)
                    kb.emit("pool", lambda e: e.memset(Sbf[:], 0.0), [], B_Sb)
                    order = list(range(NT)) if d == 0 else list(range(NT - 1, -1, -1))
                    run_interleaved([stageA1(d, order[0], 0)])
                    run_interleaved([stageA1(d, order[1], 1), stageA(d, order[0], 0)])
                    for n in range(NT):
                        g1 = stageA1(d, order[n + 2], n + 2) if n + 2 < NT else None
                        g2 = stageA(d, order[n + 1], n + 1) if n + 1 < NT else None
                        run_interleaved([g1, g2, stageB(d, order[n], n)])
                kb.barrier()
                kb.flush()
            if dbg and "recT" in dbg:
                with ExitStack() as pd:
                    tf = sb("recTf", [128, 4, S], F32, pd)
                    B_tf = kb.buf()
                    kb.emit("dve", lambda e: e.tensor_copy(out=tf[:], in_=recT[:]), [B_recT], [B_tf])
                    dbg_store("recT", tf[:].rearrange("p k t -> p (k t)"), [B_tf])
                    kb.barrier(); kb.flush()
            if stop in ("hgrn", "hgrn1"):
                break


            with ExitStack() as po_:
                woa = sb("woa", [64, 8, D], BF16, po_)
                wor = sb("wor", [128, 4, D], BF16, po_)
                B_wo = kb.buf()
                with ExitStack() as pst_:
                    stg = sb("wostg", [128, 4, D], F32, pst_)
                    B_stg = kb.buf()
                    g1b = gatebc[:, s, 0, :].unsqueeze(1).to_broadcast([128, 4, D])
                    for part in range(3):
                        if part < 2:
                            kb.dma("sp", lambda e, part=part: e.dma_start(out=stg[0:64], in_=woa_d[:, part * 4:(part + 1) * 4, :]),
                                   "ld_wostg", writes=[B_stg])
                            kb.emit("dve", lambda e, part=part: e.tensor_tensor(out=woa[:, part * 4:(part + 1) * 4, :], in0=stg[0:64],
                                                                              in1=gatebc[0:64, s, 0, :].unsqueeze(1).to_broadcast([64, 4, D]),
                                                                              op=ALU.mult), [B_stg, B_gatebc], [B_wo])
                        else:
                            kb.dma("sp", lambda e: e.dma_start(out=stg[:], in_=wor_d[:]), "ld_wostg", writes=[B_stg])
                            kb.emit("dve", lambda e: e.tensor_tensor(out=wor[:], in0=stg[:], in1=g1b, op=ALU.mult),
                                    [B_stg, B_gatebc], [B_wo])
                    kb.barrier()
                    kb.flush()
                xt0 = sb("xto", [128, D], F32, po_)
                B_xt0 = kb.buf()
                x1t = [sb("x1t%d" % i, [128, D], F32, po_) for i in range(2)]
                B_x1t = [kb.buf(), kb.buf()]
                h2t = [sb("h2t%d" % i, [128, 8, 128], BF16, po_) for i in range(2)]
                B_h2t = [kb.buf(), kb.buf()]
                m2bc = sb("m2bc", [128, 2, D], F32, po_)
                B_m2bc = kb.buf()
                kb.dma("sp", lambda e: e.dma_start(out=m2bc[:].rearrange("p a d -> p (a d)"), in_=mod2_d[s:s + 1, :].partition_broadcast(128)),
                       "ld_m2bc", reads=[B_mod2d], writes=[B_m2bc])
                h2k = [sb("h2k%d" % i, [128, D], BF16, po_) for i in range(2)]
                h2kf = sb("h2kf", [128, D], F32, po_)
                B_h2k = [kb.buf(), kb.buf()]
                B_h2kf = kb.buf()
                nrm = NormT(po_, "o", ntp=2, ncv=2)
                pmx = [ps("pmx%d" % i, [128, D], F32, po_) for i in range(2)]
                B_pmx = [kb.pbuf(), kb.pbuf()]
                plg = ps("plg", [128, 64], F32, po_)
                B_plg = kb.pbuf()
                lga = sb("lga", [128, NT, 36], F32, po_)
                B_lga = kb.buf()
                xt1 = sb("xto1", [128, D], F32, po_)
                xts = [xt0, xt1]
                B_xts = [B_xt0, kb.buf()]

                def op_S1(i):
                    j = i % 2
                    tsl = slice(i * 128, (i + 1) * 128)
                    for hh in range(2):
                        for h in range(8):
                            kb.emit("pe", lambda e, j=j, h=h, hh=hh, tsl=tsl: e.matmul(
                                pmx[j][:, hh * 512:(hh + 1) * 512], lhsT=OT[0:64, h, tsl], rhs=woa[0:64, h, hh * 512:(hh + 1) * 512],
                                start=(h == 0), stop=False), [B_OT, B_wo], [B_pmx[j]], inc=False)
                        for c in range(4):
                            kb.emit("pe", lambda e, j=j, c=c, hh=hh, tsl=tsl: e.matmul(
                                pmx[j][:, hh * 512:(hh + 1) * 512], lhsT=recT[:, c, tsl], rhs=wor[:, c, hh * 512:(hh + 1) * 512],
                                start=False, stop=(c == 3)), [B_recT, B_wo], [B_pmx[j]], inc=(c == 3 and hh == 1))
                    kb.dma("sp", lambda e, i=i, j=j: e.dma_start(out=xts[j][:], in_=x_d[s, i * 128:(i + 1) * 128, :]), "ld_xto%d" % j, writes=[B_xts[j]])
                    kb.emit("dve", lambda e, j=j: e.tensor_tensor(out=x1t[j][:], in0=pmx[j][:], in1=xts[j][:], op=ALU.add),
                            [B_pmx[j], B_xts[j]], [B_x1t[j]])
                    kb.dma("sp", lambda e, i=i, j=j: e.dma_start(out=x1_d[s, i * 128:(i + 1) * 128, :], in_=x1t[j][:]), "st_x1_%d" % j,
                           reads=[B_x1t[j]], writes=[B_x1d[s][i]])

                def op_S2(i):
                    j = i % 2
                    gi = s * NT + i
                    xn_, B_xn_ = nrm.run(x1t[j][:], B_x1t[j], s, 2, h2t[j][:], B_h2t[j])
                    kb.emit("pool", lambda e, xn_=xn_: e.tensor_tensor(out=h2kf[:], in0=xn_[:], in1=m2bc[:, 0, :], op=ALU.mult),
                            [B_xn_, B_m2bc], [B_h2kf])
                    kb.emit("pool", lambda e, j=j: e.tensor_tensor(out=h2k[j][:], in0=h2kf[:], in1=m2bc[:, 1, :], op=ALU.add),
                            [B_h2kf, B_m2bc], [B_h2k[j]])
                    kb.dma("sp", lambda e, gi=gi, j=j: e.dma_start(out=h2tok_d[gi * 128:(gi + 1) * 128, :], in_=h2k[j][:]), "st_h2_%d" % j,
                           reads=[B_h2k[j]], writes=[B_h2d[s][i]])
                    for kc in range(8):
                        kb.emit("pe", lambda e, j=j, kc=kc: e.matmul(plg[:, 0:36], lhsT=h2t[j][:, kc, :], rhs=wr[:, kc, :],
                                                                     start=(kc == 0), stop=(kc == 7)), [B_h2t[j], B_wr], [B_plg], inc=(kc == 7))
                    kb.emit("dve", lambda e, i=i: e.tensor_tensor(out=lga[:, i, :], in0=plg[:, 0:36], in1=brb[:], op=ALU.add), [B_plg, B_brb], [B_lga])

                op_S1(0)
                for i in range(NT):
                    if i + 1 < NT:
                        op_S1(i + 1)
                    op_S2(i)
                g0 = s * NT
                gsel = sb("gsel", [128, 3, NT, 4], F32, po_)
                tkb = sb("tkb", [128, 8, NT], F32, po_)
                elm = sb("elm", [128, 4, NT, 32], F32, po_)
                B_gsel, B_tkb, B_elm = kb.buf(), kb.buf(), kb.buf()
                gl = lga[:, :, 0:4]
                el = lga[:, :, 4:36]
                bc4 = lambda ap: ap.unsqueeze(2).to_broadcast([128, NT, 4])
                bc32 = lambda ap: ap.unsqueeze(2).to_broadcast([128, NT, 32])
                kb.emit("dve", lambda e: e.tensor_reduce(out=tkb[:, 0, :], in_=gl, axis=AX.X, op=ALU.max), [B_lga], [B_tkb])
                kb.emit("dve", lambda e: e.tensor_tensor(out=gsel[:, 0], in0=gl, in1=bc4(tkb[:, 0, :]), op=ALU.is_ge), [B_lga, B_tkb], [B_gsel])
                kb.emit("dve", lambda e: e.tensor_tensor(out=gsel[:, 2], in0=gl, in1=bc4(tkb[:, 0, :]), op=ALU.subtract), [B_lga, B_tkb], [B_gsel])
                kb.emit("act", lambda e: e.activation(out=gsel[:, 2], in_=gsel[:, 2], func=AF.Exp), [B_gsel], [B_gsel])
                kb.emit("dve", lambda e: e.tensor_reduce(out=tkb[:, 1, :], in_=gsel[:, 2], axis=AX.X, op=ALU.add), [B_gsel, B_tkb], [B_tkb])
                kb.emit("dve", lambda e: e.reciprocal(out=tkb[:, 2, :], in_=tkb[:, 1, :]), [B_tkb], [B_tkb])
                kb.emit("dve", lambda e: e.tensor_scalar(out=gsel[:, 1], in0=gsel[:, 0], scalar1=-1.0, scalar2=BIG, op0=ALU.add, op1=ALU.mult),
                        [B_gsel], [B_gsel])
                kb.emit("dve", lambda e: e.tensor_tensor(out=elm[:, 0].rearrange("p t (g e) -> p t g e", g=4),
                                                         in0=lga[:, :, 4:36].rearrange("p t (g e) -> p t g e", g=4),
                                                         in1=gsel[:, 1].unsqueeze(3).to_broadcast([128, NT, 4, 8]), op=ALU.add),
                        [B_lga, B_gsel], [B_elm])
                kb.emit("dve", lambda e: e.tensor_reduce(out=tkb[:, 3, :], in_=elm[:, 0], axis=AX.X, op=ALU.max), [B_elm, B_tkb], [B_tkb])
                kb.emit("dve", lambda e: e.tensor_tensor(out=elm[:, 1], in0=elm[:, 0], in1=bc32(tkb[:, 3, :]), op=ALU.is_ge), [B_elm, B_tkb], [B_elm])
                kb.emit("dve", lambda e: e.scalar_tensor_tensor(out=elm[:, 2], in0=elm[:, 1], scalar=-BIG, in1=elm[:, 0], op0=ALU.mult, op1=ALU.add),
                        [B_elm], [B_elm])
                kb.emit("dve", lambda e: e.tensor_reduce(out=tkb[:, 4, :], in_=elm[:, 2], axis=AX.X, op=ALU.max), [B_elm, B_tkb], [B_tkb])
                kb.emit("dve", lambda e: e.tensor_tensor(out=elm[:, 3], in0=elm[:, 2], in1=bc32(tkb[:, 4, :]), op=ALU.is_ge), [B_elm, B_tkb], [B_elm])
                kb.emit("dve", lambda e: e.tensor_tensor(out=tkb[:, 5, :], in0=tkb[:, 4, :], in1=tkb[:, 3, :], op=ALU.subtract), [B_tkb], [B_tkb])
                kb.emit("act", lambda e: e.activation(out=tkb[:, 5, :], in_=tkb[:, 5, :], func=AF.Exp), [B_tkb], [B_tkb])
                kb.emit("dve", lambda e: e.tensor_scalar(out=tkb[:, 6, :], in0=tkb[:, 5, :], scalar1=1.0, scalar2=None, op0=ALU.add), [B_tkb], [B_tkb])
                kb.emit("dve", lambda e: e.reciprocal(out=tkb[:, 6, :], in_=tkb[:, 6, :]), [B_tkb], [B_tkb])
                kb.emit("dve", lambda e: e.tensor_tensor(out=WK[:, g0:g0 + NT, 0], in0=tkb[:, 6, :], in1=tkb[:, 2, :], op=ALU.mult), [B_tkb], B_EW[g0:g0 + NT])
                kb.emit("dve", lambda e: e.tensor_tensor(out=WK[:, g0:g0 + NT, 1], in0=WK[:, g0:g0 + NT, 0], in1=tkb[:, 5, :], op=ALU.mult),
                        [B_tkb] + B_EW[g0:g0 + NT], B_EW[g0:g0 + NT])
                for k2 in range(2):
                    kb.emit("dve", lambda e, k2=k2: e.tensor_tensor(out=elm[:, 0], in0=elm[:, 1 + 2 * k2], in1=iota_e[:].unsqueeze(1).to_broadcast([128, NT, 32]),
                                                                    op=ALU.mult), [B_elm, B_iota], [B_elm])
                    kb.emit("dve", lambda e, k2=k2: e.tensor_reduce(out=EIDX[:, g0:g0 + NT, k2], in_=elm[:, 0], axis=AX.X, op=ALU.add),
                            [B_elm] + B_EW[g0:g0 + NT], B_EW[g0:g0 + NT])
                kb.barrier()
                kb.flush()
            if stop == "mix":
                break

    if stop == "mix":
        if dbg and "EIDX" in dbg:
            dbg_store("EIDX", EIDX[:].rearrange("p i e -> p (i e)"), B_EW)
            dbg_store("WK", WK[:].rearrange("p i e -> p (i e)"), B_EW)
            kb.barrier()
            kb.flush()

    issue_cv(1000)
    if stop is None or stop == "moe":
        NG = nseq_run * NT
        with ExitStack() as pe_:
            cstm = sb("cstm", [128, 192], F32, pe_)
            crow = sb("crow", [1, 3200], F32, pe_)
            B_cstm, B_crow = kb.buf(), kb.buf()
            kb.dma("sp", lambda e: e.dma_start(out=cstm[:], in_=cstm_d[:]), "ld_cstm", writes=[B_cstm])
            kb.dma("sp", lambda e: e.dma_start(out=crow[:], in_=crow_d[:]), "ld_crow", writes=[B_crow])
            sltb = sb("sltb", [128, 128], BF16, pe_)
            B_sltb = kb.buf()
            kb.emit("dve", lambda e: e.tensor_copy(out=sltb[:], in_=cstm[:, 0:128]), [B_cstm], [B_sltb])
            NGT = NSEQ * NT
            Mb = sb("Mb", [128, NGT, 32], BF16, pe_)
            CS = sb("CS", [128, NGT + 1, 32], F32, pe_)
            RK = sb("RK", [128, NGT, 32], F32, pe_)
            oh = sb("oh", [128, NGT, 2, 32], F32, pe_)
            ohr = sb("ohr", [128, NGT, 32], F32, pe_)
            B_Mb, B_CS, B_RK, B_oh, B_ohr = (kb.buf() for _ in range(5))
            SLOTF = sb("SLOTF", [128, NGT, 2], F32, pe_)
            SLOT = sb("SLOT", [128, NGT, 2], I32, pe_)
            B_slotf, B_slot = kb.buf(), kb.buf()
            IDXW = sb("IDXW", [128, NBB, 2], I32, pe_)
            B_idxw = kb.buf()
            with ExitStack() as pr_:
                pcs = ps("pcs", [128, 1024], F32, pr_)
                B_pcs = kb.pbuf()
                prk = ps("prk", [128, 1024], F32, pr_)
                B_prk = kb.pbuf()
                pbcr = ps("pbcr", [128, 96], F32, pr_)
                B_pbcr = kb.pbuf()
                kb.emit("dve", lambda e: e.tensor_tensor(out=oh[:].rearrange("p g k e -> p (g k) e"),
                                                         in0=iota_e[:].unsqueeze(1).to_broadcast([128, NGT * 2, 32]),
                                                         in1=EIDX[:].rearrange("p g k -> p (g k)").unsqueeze(2).to_broadcast([128, NGT * 2, 32]),
                                                         op=ALU.is_equal), [B_iota] + B_EW, [B_oh])
                kb.emit("dve", lambda e: e.tensor_tensor(out=Mb[:], in0=oh[:, :, 0, :], in1=oh[:, :, 1, :], op=ALU.add), [B_oh], [B_Mb])
                Mbf = Mb[:].rearrange("p g e -> p (g e)")
                for hh in range(2):
                    kb.emit("pe", lambda e, hh=hh: e.matmul(pcs[:, hh * 512:(hh + 1) * 512], lhsT=onesb[:], rhs=Mbf[:, hh * 512:(hh + 1) * 512], start=True, stop=True),
                            [B_onesb, B_Mb], [B_pcs], inc=(hh == 1))
                for hh in range(2):
                    kb.emit("pe", lambda e, hh=hh: e.matmul(prk[:, hh * 512:(hh + 1) * 512], lhsT=sltb[:], rhs=Mbf[:, hh * 512:(hh + 1) * 512], start=True, stop=True),
                            [B_sltb, B_Mb], [B_prk], inc=(hh == 1))
                kb.emit("pool", lambda e: e.memset(CS[:, 0, :], 0.0), [], [B_CS])
                for g in range(NGT):
                    kb.emit("dve", lambda e, g=g: e.tensor_tensor(out=CS[:, g + 1, :], in0=CS[:, g, :], in1=pcs[:, g * 32:(g + 1) * 32], op=ALU.add),
                            [B_CS, B_pcs], [B_CS])
                kb.emit("dve", lambda e: e.tensor_tensor(out=RK[:].rearrange("p g e -> p (g e)"), in0=prk[:], in1=CS[:, 0:NGT, :].rearrange("p g e -> p (g e)"), op=ALU.add),
                        [B_prk, B_CS], [B_RK])
                rw = sb("rw", [1, 8, 64], F32, pr_)
                g1 = sb("g1", [1, 2048], F32, pr_)
                B_rw, B_g1 = kb.buf(), kb.buf()
                kb.emit("pool", lambda e: e.memset(rw[:], 0.0), [], [B_rw])
                kb.emit("dve", lambda e: e.tensor_copy(out=rw[:, 0, 0:32], in_=CS[0:1, NGT, :]), [B_CS, B_rw], [B_rw])
                kb.emit("dve", lambda e: e.tensor_tensor(out=g1[:, 0:512].rearrange("p (a b) -> p a b", a=32),
                                                         in0=rw[:, 0, 0:32].unsqueeze(2).to_broadcast([1, 32, 16]),
                                                         in1=crow[:, 0:16].unsqueeze(1).to_broadcast([1, 32, 16]), op=ALU.is_gt),
                        [B_rw, B_crow], [B_g1])
                kb.emit("dve", lambda e: e.tensor_reduce(out=rw[:, 1, 0:32], in_=g1[:, 0:512].rearrange("p (a b) -> p a b", a=32), axis=AX.X, op=ALU.add),
                        [B_g1, B_rw], [B_rw])
                kb.emit("dve", lambda e: e.tensor_tensor(out=g1[:, 0:1024].rearrange("p (a b) -> p a b", a=32),
                                                         in0=rw[:, 1, 0:32].unsqueeze(1).to_broadcast([1, 32, 32]),
                                                         in1=crow[:, 16:1040].rearrange("p (a b) -> p a b", a=32), op=ALU.mult),
                        [B_rw, B_crow], [B_g1])
                kb.emit("dve", lambda e: e.tensor_reduce(out=rw[:, 2, 0:32], in_=g1[:, 0:1024].rearrange("p (a b) -> p a b", a=32), axis=AX.X, op=ALU.add),
                        [B_g1, B_rw], [B_rw])
                kb.emit("dve", lambda e: e.tensor_tensor(out=rw[:, 3, 0:32], in0=rw[:, 2, 0:32], in1=rw[:, 1, 0:32], op=ALU.subtract), [B_rw], [B_rw])
                kb.emit("dve", lambda e: e.tensor_scalar(out=rw[:, 3, 0:32], in0=rw[:, 3, 0:32], scalar1=256.0, scalar2=None, op0=ALU.mult), [B_rw], [B_rw])
                kb.emit("dve", lambda e: e.tensor_tensor(out=g1[:].rearrange("p (a b) -> p a b", a=64),
                                                         in0=rw[:, 2, 0:32].unsqueeze(1).to_broadcast([1, 64, 32]),
                                                         in1=crow[:, 1040:3088].rearrange("p (a b) -> p a b", a=64), op=ALU.is_le),
                        [B_rw, B_crow], [B_g1])
                kb.emit("dve", lambda e: e.tensor_reduce(out=rw[:, 4, :], in_=g1[:].rearrange("p (a b) -> p a b", a=64), axis=AX.X, op=ALU.add),
                        [B_g1, B_rw], [B_rw])
                kb.emit("dve", lambda e: e.tensor_scalar(out=rw[:, 4, :], in0=rw[:, 4, :], scalar1=31.0, scalar2=None, op0=ALU.min), [B_rw], [B_rw])
                kb.emit("dve", lambda e: e.tensor_tensor(out=rw[:, 5, 2:64], in0=rw[:, 4, 2:64], in1=rw[:, 4, 0:62], op=ALU.is_equal), [B_rw], [B_rw])
                kb.emit("dve", lambda e: e.tensor_scalar(out=rw[:, 5, :], in0=rw[:, 5, :], scalar1=5.0e8, scalar2=None, op0=ALU.mult), [B_rw], [B_rw])
                kb.emit("dve", lambda e: e.scalar_tensor_tensor(out=rw[:, 6, :], in0=rw[:, 4, :], scalar=128.0, in1=rw[:, 5, :], op0=ALU.mult, op1=ALU.add),
                        [B_rw], [B_rw])
                kb.emit("pe", lambda e: e.matmul(pbcr[:, 0:64], lhsT=ones_f[0:1, 0:128], rhs=rw[:, 6, :], start=True, stop=True), [B_cst, B_rw], [B_pbcr], inc=False)
                kb.emit("pe", lambda e: e.matmul(pbcr[:, 64:96], lhsT=ones_f[0:1, 0:128], rhs=rw[:, 3, 0:32], start=True, stop=True), [B_cst, B_rw], [B_pbcr])
                bcs = sb("bcs", [128, 96], F32, pr_)
                idf = sb("idf", [128, NBB, 2], F32, pr_)
                B_bcs, B_idf = kb.buf(), kb.buf()
                kb.emit("dve", lambda e: e.tensor_copy(out=bcs[:], in_=pbcr[:]), [B_pbcr], [B_bcs])
                kb.emit("dve", lambda e: e.tensor_scalar(out=idf[:, :, 0], in0=bcs[:, 0:64], scalar1=cstm[:, 160:161], scalar2=None, op0=ALU.add),
                        [B_bcs, B_cstm], [B_idf])
                kb.emit("dve", lambda e: e.tensor_scalar(out=idf[:, :, 1], in0=idf[:, :, 0], scalar1=1.0, scalar2=None, op0=ALU.add), [B_idf], [B_idf])
                kb.emit("dve", lambda e: e.tensor_copy(out=IDXW[:], in_=idf[:]), [B_idf], [B_idxw])
                kb.emit("dve", lambda e: e.tensor_tensor(out=RK[:], in0=RK[:], in1=bcs[:, 64:96].unsqueeze(1).to_broadcast([128, NGT, 32]), op=ALU.add),
                        [B_RK, B_bcs], [B_RK])
                for k2 in range(2):
                    kb.emit("dve", lambda e, k2=k2: e.tensor_tensor(out=ohr[:], in0=oh[:, :, k2, :], in1=RK[:], op=ALU.mult), [B_oh, B_RK], [B_ohr])
                    kb.emit("dve", lambda e, k2=k2: e.tensor_reduce(out=SLOTF[:, :, k2], in_=ohr[:], axis=AX.X, op=ALU.add), [B_ohr, B_slotf], [B_slotf])
                kb.emit("dve", lambda e: e.tensor_copy(out=SLOT[:], in_=SLOTF[:]), [B_slotf], [B_slot])
                if dbg and "SLOT" in dbg:
                    dbg_store("SLOT", SLOTF[:].rearrange("p i e -> p (i e)"), [B_slotf])
                    dbg_store("BE", rw[:].rearrange("p a b -> p (a b)"), [B_rw])
                kb.barrier()
                kb.flush()
            with nullcontext(pe_) as pd_:
                hk = [sb("hk%d" % i, [128, D], BF16, pd_) for i in range(2)]
                B_hk = [kb.buf(), kb.buf()]
                for gi in range(NG):
                    j = gi % 2
                    s_, i_ = gi // NT, gi % NT
                    kb.dma("sp", lambda e, gi=gi, j=j: e.dma_start(out=hk[j][:], in_=h2tok_d[gi * 128:(gi + 1) * 128, :]), "ld_hk%d" % j,
                           reads=[B_h2d[s_][i_]], writes=[B_hk[j]])
                    for k2 in range(2):
                        kb.dma("pool", lambda e, gi=gi, j=j, k2=k2: e.indirect_dma_start(
                            out=xs_d[:, :], out_offset=bass.IndirectOffsetOnAxis(ap=SLOT[:, gi, k2:k2 + 1], axis=0), in_=hk[j][:], in_offset=None),
                            "sc_xs%d" % j, reads=[B_hk[j], B_slot, B_xsz], writes=[B_xs2[j]])
            B_ys2 = [kb.buf("ys0"), kb.buf("ys1")]
            with nullcontext(pe_) as px_:
                wall = [sb("wall%d" % i, [128, 3 * 4096], BF16, px_) for i in range(2)]
                wge = [wall[i][:, 0:4096].rearrange("p (k n) -> p k n", k=8) for i in range(2)]
                wue = [wall[i][:, 4096:8192].rearrange("p (k n) -> p k n", k=8) for i in range(2)]
                wde = [wall[i][:, 8192:12288].rearrange("p (k n) -> p k n", k=4) for i in range(2)]
                B_we = [kb.buf(), kb.buf()]
                xb = [sb("xb%d" % i, [128, 2, D], BF16, px_) for i in range(2)]
                B_xb = [kb.buf(), kb.buf()]
                xsT = [sb("xsT%d" % i, [128, 8, 256], BF16, px_) for i in range(2)]
                B_xsT = [kb.buf(), kb.buf()]
                hid = [sb("hid%d" % i, [128, 4, 256], BF16, px_) for i in range(2)]
                B_hid = [kb.buf(), kb.buf()]
                sgt = [sb("sgt%d" % i, [128, 256], F32, px_) for i in range(2)]
                B_sgt = [kb.buf(), kb.buf()]
                ysb = [sb("ysb%d" % i, [128, D], F32, px_) for i in range(2)]
                B_ysb = [kb.buf(), kb.buf()]
                ptx = [ps("ptx%d" % i, [128, 8, 128], BF16, px_) for i in range(2)]
                B_ptx = [kb.pbuf(), kb.pbuf()]
                pgu = [ps("pgu%d" % i, [128, 256], F32, px_) for i in range(4)]
                B_pgu = [kb.pbuf() for _ in range(4)]
                py = ps("py", [128, D], F32, px_)
                B_py = kb.pbuf()
                nbb_run = NBB if stop is None else 8
                cnt_g = 0
                cnt_t = 0
                cnt_y = 0
                for b in range(nbb_run):
                    wj = b % 2
                    kb.dma("pool", lambda e, b=b, wj=wj: e.indirect_dma_start(
                        out=wall[wj][:, :], out_offset=None, in_=wbf_d[:, :],
                        in_offset=bass.IndirectOffsetOnAxis(ap=IDXW[:, b, 0:1], axis=0),
                        bounds_check=kb.const_reg(e, NEXP * 128 - 1), oob_is_err=False), "ld_we%d" % wj, reads=[B_idxw, B_wbf], writes=[B_we[wj]])
                    xj = b % 2
                    for bb_ in ([0, 1] if b == 0 else [b + 1]):
                        if bb_ < nbb_run:
                            kb.dma("sp", lambda e, bb_=bb_: e.dma_start(out=xb[bb_ % 2][:], in_=xs_d[bb_ * 256:(bb_ + 1) * 256, :].rearrange("(t p) d -> p t d", p=128)),
                                   "ld_xb%d" % (bb_ % 2), reads=B_xs2, writes=[B_xb[bb_ % 2]])
                    for t2 in range(2):
                        tj = cnt_t % 2
                        cnt_t += 1
                        for kc in range(8):
                            kb.emit("pe", lambda e, xj=xj, t2=t2, kc=kc, tj=tj: e.transpose(out=ptx[tj][:, kc, :], in_=xb[xj][:, t2, kc * 128:(kc + 1) * 128],
                                                                                       identity=identb[:]), [B_xb[xj], B_identb], [B_ptx[tj]], inc=(kc == 7))
                        kb.emit("act", lambda e, xj=xj, t2=t2, tj=tj: e.copy(out=xsT[xj][:, :, t2 * 128:(t2 + 1) * 128], in_=ptx[tj][:]), [B_ptx[tj]], [B_xsT[xj]])
                    hj = b % 2
                    for m in range(4):
                        pg_i = (cnt_g % 2) * 2
                        cnt_g += 1
                        for kc in range(8):
                            kb.emit("pe", lambda e, wj=wj, m=m, kc=kc, xj=xj, pg_i=pg_i: e.matmul(
                                pgu[pg_i][:], lhsT=wge[wj][:, kc, m * 128:(m + 1) * 128], rhs=xsT[xj][:, kc, :],
                                start=(kc == 0), stop=(kc == 7)), [B_we[wj], B_xsT[xj]], [B_pgu[pg_i]], inc=(kc == 7))
                        for kc in range(8):
                            kb.emit("pe", lambda e, wj=wj, m=m, kc=kc, xj=xj, pg_i=pg_i: e.matmul(
                                pgu[pg_i + 1][:], lhsT=wue[wj][:, kc, m * 128:(m + 1) * 128], rhs=xsT[xj][:, kc, :],
                                start=(kc == 0), stop=(kc == 7)), [B_we[wj], B_xsT[xj]], [B_pgu[pg_i + 1]], inc=(kc == 7))
                        sj = m % 2
                        kb.emit("act", lambda e, pg_i=pg_i, sj=sj: e.activation(out=sgt[sj][:], in_=pgu[pg_i][:], func=AF.Silu), [B_pgu[pg_i]], [B_sgt[sj]])
                        kb.emit("dve", lambda e, pg_i=pg_i, sj=sj, hj=hj, m=m: e.tensor_tensor(out=hid[hj][:, m, :], in0=sgt[sj][:], in1=pgu[pg_i + 1][:], op=ALU.mult),
                                [B_sgt[sj], B_pgu[pg_i + 1]], [B_hid[hj]])
                    for t2 in range(2):
                        yj = cnt_y % 2
                        cnt_y += 1
                        for hh in range(2):
                            for m in range(4):
                                kb.emit("pe", lambda e, wj=wj, m=m, hh=hh, hj=hj, t2=t2: e.matmul(
                                    py[:, hh * 512:(hh + 1) * 512], lhsT=hid[hj][:, m, t2 * 128:(t2 + 1) * 128],
                                    rhs=wde[wj][:, m, hh * 512:(hh + 1) * 512], start=(m == 0), stop=(m == 3)),
                                    [B_hid[hj], B_we[wj]], [B_py], inc=(m == 3 and hh == 1))
                        kb.emit("act", lambda e, yj=yj: e.copy(out=ysb[yj][:], in_=py[:]), [B_py], [B_ysb[yj]])
                        kb.dma("sp", lambda e, b=b, t2=t2, yj=yj: e.dma_start(out=ys_d[b * 256 + t2 * 128:b * 256 + (t2 + 1) * 128, :], in_=ysb[yj][:]),
                               "st_ys%d" % yj, reads=[B_ysb[yj]], writes=[B_ys2[yj]])
            with nullcontext(pe_) as pc_:
                xr = [sb("xr%d" % i, [128, D], F32, pc_) for i in range(2)]
                yg = [sb("yg%d" % i, [128, 2, D], F32, pc_) for i in range(2)]
                B_xr = [kb.buf(), kb.buf()]
                B_yg = [kb.buf(), kb.buf()]
                for gi in range(NG):
                    j = gi % 2
                    s_, i_ = gi // NT, gi % NT
                    kb.dma("sp", lambda e, s_=s_, i_=i_, j=j: e.dma_start(out=xr[j][:], in_=x1_d[s_, i_ * 128:(i_ + 1) * 128, :]), "ld_xr%d" % j,
                           reads=[B_x1d[s_][i_]], writes=[B_xr[j]])
                    for k2 in range(2):
                        kb.dma("pool", lambda e, gi=gi, j=j, k2=k2: e.indirect_dma_start(
                            out=yg[j][:, k2, :], out_offset=None, in_=ys_d[:, :],
                            in_offset=bass.IndirectOffsetOnAxis(ap=SLOT[:, gi, k2:k2 + 1], axis=0)), "ld_yg%d" % j,
                            reads=B_ys2 + [B_slot], writes=[B_yg[j]])
                    kb.emit("dve", lambda e, gi=gi, j=j: e.tensor_scalar(out=yg[j][:, 0, :], in0=yg[j][:, 0, :], scalar1=WK[:, gi, 0:1], scalar2=None, op0=ALU.mult),
                            [B_yg[j], B_EW[gi]], [B_yg[j]])
                    kb.emit("dve", lambda e, gi=gi, j=j: e.scalar_tensor_tensor(out=yg[j][:, 0, :], in0=yg[j][:, 1, :], scalar=WK[:, gi, 1:2], in1=yg[j][:, 0, :],
                                                                              op0=ALU.mult, op1=ALU.add), [B_yg[j], B_EW[gi]], [B_yg[j]])
                    kb.emit("dve", lambda e, s_=s_, j=j: e.tensor_tensor(out=yg[j][:, 0, :], in0=yg[j][:, 0, :], in1=gatebc[:, s_, 1, :], op=ALU.mult),
                            [B_yg[j], B_gatebc], [B_yg[j]])
                    kb.emit("dve", lambda e, j=j: e.tensor_tensor(out=xr[j][:], in0=xr[j][:], in1=yg[j][:, 0, :], op=ALU.add), [B_xr[j], B_yg[j]], [B_xr[j]])
                    kb.dma("sp", lambda e, s_=s_, i_=i_, j=j: e.dma_start(out=out_d[s_, i_ * 128:(i_ + 1) * 128, :], in_=xr[j][:]), "st_out%d" % j,
                           reads=[B_xr[j]])
                kb.barrier()
                kb.flush()

    kb.barrier()
    kb.flush()
    kb.es.close()
    return nc


def _consts():
    c = np.zeros((128, 1024), np.float32)
    idx = np.arange(128)
    same = (idx[:, None] // 64) == (idx[None, :] // 64)
    triF = ((idx[:, None] <= idx[None, :]) & same).astype(np.float32)
    triB = triF.T.copy()
    c[:, 0:128] = triF
    c[:, 128:256] = triB
    c[:, 256:384] = triB - np.eye(128, dtype=np.float32)
    c[:, 384:512] = triF - np.eye(128, dtype=np.float32)
    c[:, 512] = (idx < 64)
    c[:, 513] = (idx >= 64)
    half = 16
    inv_freq = (10000.0 ** (-np.arange(half, dtype=np.float32) / half)).astype(np.float32)
    c[:, 514:530] = inv_freq[None, :]
    c[0, 530] = 1.0
    c[1, 531] = 1.0
    c[0, 532:660] = 1.0
    c[1, 660:788] = 1.0
    c[:, 788:916] = 1.0
    c[:, 916] = 1.0 / 384
    c[:, 917] = 1.0 / 256
    c[:, 918] = -np.pi
    c[:, 919] = 1e-6
    return c


def _cstm():
    c = np.zeros((128, 192), np.float32)
    idx = np.arange(128)
    c[:, 0:128] = (idx[:, None] < idx[None, :]).astype(np.float32)
    c[:, 128:160] = np.arange(32, dtype=np.float32)[None, :]
    c[:, 160] = idx
    return c


def _crow():
    r = np.zeros((1, 3200), np.float32)
    r[0, 0:16] = 256.0 * np.arange(16)
    e = np.arange(32)
    r[0, 16:1040] = (e[None, :] <= e[:, None]).astype(np.float32).reshape(-1)
    r[0, 1040:3088] = np.repeat(np.arange(64, dtype=np.float32), 32)
    return r


def _kc(w):
    K, N = w.shape
    return np.ascontiguousarray(w.reshape(K // 128, 128, N).transpose(1, 0, 2))


def make_in_maps(inp):
    f = lambda a: np.ascontiguousarray(np.asarray(a, dtype=np.float32))
    x = f(inp["x"]); c = f(inp["c"]); pos = np.asarray(inp["positions"]).astype(np.int32)
    shared = {
        "ada_w": _kc(f(inp["ada_w"])[0]),
        "ada_b": f(inp["ada_b"])[0][None, :],
        "g1": np.ascontiguousarray(f(inp["norm1_g"])[0].reshape(8, 128).T),
        "g2": np.ascontiguousarray(f(inp["norm2_g"])[0].reshape(8, 128).T),
        "w_in": _kc(f(inp["w_in"])[0]),
        "qa_g": np.ascontiguousarray(f(inp["mla_qa_g"])[0].reshape(3, 128).T),
        "wq_up": _kc(f(inp["mla_wq_up"])[0]),
        "kva_g": np.ascontiguousarray(f(inp["mla_kva_g"])[0].reshape(2, 128).T),
        "wkv_up": _kc(f(inp["mla_wkv_up"])[0]),
        "qn_g": f(inp["mla_qn_g"])[0][None, :],
        "kn_g": f(inp["mla_kn_g"])[0][None, :],
        "lb_logits": f(inp["hg_lb_logits"]).reshape(1, -1),
        "hg_norm_g": f(inp["hg_norm_g"])[0][None, :],
        "w_out_a": np.ascontiguousarray(f(inp["w_out"])[0][:512].reshape(8, 64, D).transpose(1, 0, 2)),
        "w_out_r": _kc(f(inp["w_out"])[0][512:]),
        "w_router": _kc(np.concatenate([f(inp["router_group_w"])[0], f(inp["router_expert_w"])[0]], axis=1)),
        "b_router": np.concatenate([f(inp["router_group_b"])[0], f(inp["router_expert_b"])[0]])[None, :],
        "w_gate": np.ascontiguousarray(f(inp["w_gate"])[0].reshape(NEXP, 8, 128, 512).transpose(0, 2, 1, 3)),
        "w_up": np.ascontiguousarray(f(inp["w_up"])[0].reshape(NEXP, 8, 128, 512).transpose(0, 2, 1, 3)),
        "w_down": np.ascontiguousarray(f(inp["w_down"])[0].reshape(NEXP, 4, 128, D).transpose(0, 2, 1, 3)),
        "ident_bf": np.eye(128, dtype=np.float32).astype(ml_dtypes.bfloat16),
        "consts_f": _consts(),
        "cstm": _cstm(),
        "crow": _crow(),
        "g2row": f(inp["norm2_g"])[0][None, :],
    }
    maps = []
    for i in range(NCORES):
        b0 = NSEQ * i
        m = dict(shared)
        m["x"] = np.ascontiguousarray(x[b0:b0 + NSEQ])
        p = pos[b0:b0 + NSEQ].reshape(NSEQ, NT, 128)
        m["pos"] = np.ascontiguousarray(p.transpose(2, 0, 1).reshape(128, NSEQ * NT))
        m["cT"] = np.ascontiguousarray(c[b0:b0 + NSEQ].reshape(NSEQ, 8, 128).transpose(2, 1, 0))
        maps.append(m)
    return maps


def kernel(**inputs):
    nc = build()
    in_maps = make_in_maps(inputs)
    res = run_bass_kernel_spmd(nc, in_maps, core_ids=list(range(NCORES)))
    outs = [np.asarray(r["out"]).reshape(NSEQ, S, D) for r in res.results]
    return np.concatenate(outs, axis=0).astype(np.float32)
```

```python
import numpy as np
import ml_dtypes
from contextlib import ExitStack, nullcontext
import concourse.bass as bass
import concourse.mybir as mybir
from concourse.bass_utils import run_bass_kernel_spmd

F32 = mybir.dt.float32
BF16 = mybir.dt.bfloat16
I32 = mybir.dt.int32
AF = mybir.ActivationFunctionType
ALU = mybir.AluOpType
AX = mybir.AxisListType

NCORES = 8
D = 1024
S = 2048
NT = S // 128
NSEQ = 2
EPS = 1e-6
INCOLS = 3232
NEXP = 32
BIG = 1.0e30


class Buf:
    __slots__ = ("name", "w", "r", "excl")

    def __init__(self, name, excl=False):
        self.name = name
        self.excl = excl
        self.w = None
        self.r = {}


class KB:
    ENG = ("pe", "act", "dve", "pool", "sp")

    def __init__(self, nc):
        self.nc = nc
        self.es = ExitStack()
        self.sems = {}
        self.cnt = {}
        self.waited = {e: {} for e in self.ENG}
        self.prog = {e: [] for e in self.ENG}
        for e in self.ENG:
            self.sems[e] = self.es.enter_context(nc.semaphore("s_" + e))
            self.cnt[e] = 0
        self.nbuf = 0
        self.ninst = 0
        self.nflush = 0
        self.regs = {}

    def buf(self, name=None):
        self.nbuf += 1
        return Buf(name or ("b%d" % self.nbuf))

    def pbuf(self, name=None):
        self.nbuf += 1
        return Buf(name or ("p%d" % self.nbuf), excl=True)

    def dsem(self, key):
        if key not in self.sems:
            self.sems[key] = self.es.enter_context(self.nc.semaphore("d_" + key))
            self.cnt[key] = 0
        return key

    def _waits(self, eng, reads, writes):
        need = {}

        def add(dep):
            if dep is None:
                return
            k, v, e2 = dep
            if need.get(k, 0) < v:
                need[k] = v

        for b in reads:
            add(b.w)
        strict = (eng == "pool")
        for b in writes:
            if b.w is not None and (b.w[2] != eng or strict):
                add(b.w)
            for k, (v, e2) in b.r.items():
                if e2 != eng or strict:
                    add((k, v, e2))
        out = []
        wd = self.waited[eng]
        for k, v in need.items():
            if wd.get(k, 0) < v:
                wd[k] = v
                out.append((k, v))
        return out

    def emit(self, eng, fn, reads=(), writes=(), inc=True):
        if any(b.excl for b in reads):
            writes = list(writes) + [b for b in reads if b.excl and b not in writes]
            reads = [b for b in reads if not b.excl]
        waits = self._waits(eng, reads, writes)
        val = self.cnt[eng] + 1
        if inc:
            self.cnt[eng] = val
        rec_w = (eng, val, eng)
        for b in reads:
            old = b.r.get(eng)
            if old is None or old[0] < val:
                b.r[eng] = (val, eng)
        for b in writes:
            b.w = rec_w
            b.r = {}
        self.prog[eng].append((waits, fn, eng if inc else None, 1))
        self.ninst += 1

    def dma(self, q, fn, key, reads=(), writes=()):
        self.dsem(key)
        waits = self._waits(q, reads, writes)
        val = self.cnt[key] + 16
        self.cnt[key] = val
        for b in reads:
            old = b.r.get(key)
            if old is None or old[0] < val:
                b.r[key] = (val, "dma")
        for b in writes:
            b.w = (key, val, "dma")
            b.r = {}
        self.prog[q].append((waits, fn, key, 16))
        self.ninst += 1

    def barrier(self):
        tgt = {k: v for k, v in self.cnt.items() if v > 0}
        for e in self.ENG:
            waits = []
            for k, v in tgt.items():
                if k == e:
                    continue
                if self.waited[e].get(k, 0) < v:
                    self.waited[e][k] = v
                    waits.append((k, v))
            if waits:
                self.prog[e].append((waits, None, None, 0))

    def flush(self):
        nc = self.nc
        progs = self.prog
        sems = self.sems

        def run(engname, eh):
            for waits, fn, inckey, incv in progs[engname]:
                for k, v in waits:
                    eh.wait_ge(sems[k], v)
                if fn is not None:
                    ins = fn(eh)
                    if inckey is not None:
                        ins.then_inc(sems[inckey], incv)

        with nc.Block() as block:
            @block.tensor
            def _(e):
                run("pe", e)

            @block.scalar
            def _(e):
                run("act", e)

            @block.vector
            def _(e):
                run("dve", e)

            @block.gpsimd
            def _(e):
                run("pool", e)

            @block.sync
            def _(e):
                run("sp", e)
        self.prog = {e: [] for e in self.ENG}
        self.nflush += 1

    def const_reg(self, e, val):
        key = (self.nflush, val)
        if key not in self.regs:
            self.regs[key] = e.to_reg(val)
        return self.regs[key]


def build(dbg=None, stop=None):
    nc = bass.Bass("TRN2", target_bir_lowering=False)
    kb = KB(nc)
    es = kb.es

    def din(name, shape, dt=F32):
        return nc.dram_tensor(name, list(shape), dt, kind="ExternalInput").ap()

    x_d = din("x", [NSEQ, S, D])
    pos_d = din("pos", [128, NSEQ * NT], I32)
    cT_d = din("cT", [128, 8, NSEQ])
    adaw_d = din("ada_w", [128, 8, 6 * D])
    adab_d = din("ada_b", [1, 6 * D])
    g1_d = din("g1", [128, 8])
    g2_d = din("g2", [128, 8])
    win_d = din("w_in", [128, 8, INCOLS])
    qag_d = din("qa_g", [128, 3])
    wq_d = din("wq_up", [128, 3, 768])
    kvag_d = din("kva_g", [128, 2])
    wkv_d = din("wkv_up", [128, 2, 1024])
    qng_d = din("qn_g", [1, 96])
    kng_d = din("kn_g", [1, 96])
    lbl_d = din("lb_logits", [1, 2 * 2 * 512])
    hgn_d = din("hg_norm_g", [1, 128])
    woa_d = din("w_out_a", [64, 8, D])
    wor_d = din("w_out_r", [128, 4, D])
    wr_d = din("w_router", [128, 8, 36])
    br_d = din("b_router", [1, 36])
    wg_d = din("w_gate", [NEXP, 128, 8, 512])
    wu_d = din("w_up", [NEXP, 128, 8, 512])
    wd_d = din("w_down", [NEXP, 128, 4, D])
    identb_d = din("ident_bf", [128, 128], BF16)
    consts_d = din("consts_f", [128, 1024])
    out_d = nc.dram_tensor("out", [NSEQ, S, D], F32, kind="ExternalOutput").ap()
    x1_d = nc.dram_tensor("x1_scratch", [NSEQ, S, D], F32, kind="Internal").ap()
    h2tok_d = nc.dram_tensor("h2tok_scratch", [NSEQ * S, D], BF16, kind="Internal").ap()
    mod2_d = nc.dram_tensor("mod2_scratch", [NSEQ, 2 * D], F32, kind="Internal").ap()
    NBB = 64
    xs_d = nc.dram_tensor("xs_scratch", [NBB * 256, D], BF16, kind="Internal").ap()
    ys_d = nc.dram_tensor("ys_scratch", [NBB * 256, D], F32, kind="Internal").ap()
    g2row_d = din("g2row", [1, D])
    wbf_d = nc.dram_tensor("wbf", [NEXP * 128, 3 * 4096], BF16, kind="Internal").ap()
    cstm_d = din("cstm", [128, 192])
    crow_d = din("crow", [1, 3200])
    dbg_d = {}
    if dbg:
        for k, shp in dbg.items():
            dbg_d[k] = nc.dram_tensor("dbg_" + k, list(shp), F32, kind="ExternalOutput").ap()

    uid = [0]

    def sb(name, shape, dt=F32, stack=es):
        uid[0] += 1
        return stack.enter_context(nc.sbuf_tensor("sb%d_%s" % (uid[0], name), list(shape), dt))

    def ps(name, shape, dt=F32, stack=es):
        uid[0] += 1
        return stack.enter_context(nc.psum_tensor("ps%d_%s" % (uid[0], name), list(shape), dt))

    identb = sb("identb", [128, 128], BF16)
    cst = sb("cst", [128, 1024])
    B_identb, B_cst = kb.buf("identb"), kb.buf("cst")
    kb.dma("sp", lambda e: e.dma_start(out=identb[:], in_=identb_d[:]), "ld_identb", writes=[B_identb])
    kb.dma("sp", lambda e: e.dma_start(out=cst[:], in_=consts_d[:]), "ld_cst", writes=[B_cst])
    ones_f = cst[:, 788:916]
    B_wbf = kb.buf("wbf")
    wsrc = [wg_d.rearrange("e p (h k) n -> e p h (k n)", h=2), wu_d.rearrange("e p (h k) n -> e p h (k n)", h=2),
            wd_d.rearrange("e p (h k) n -> e p h (k n)", h=2)]
    B_xsz = kb.buf("xsz")
    B_xs2 = [kb.buf("xs0"), kb.buf("xs1")]
    pending_cv = []
    if stop is None or stop == "moe":
        for ex in range(NEXP):
            for m_ in range(3):
                pending_cv.append((ex, m_))

    def issue_cv(n=1):
        for _ in range(n):
            if not pending_cv:
                return
            ex, m_ = pending_cv.pop(0)
            kb.dma("pool", lambda e, ex=ex, m_=m_: e.dma_start(
                out=wbf_d[ex * 128:(ex + 1) * 128, m_ * 4096:(m_ + 1) * 4096].rearrange("p (h c) -> p h c", h=2), in_=wsrc[m_][ex]),
                "cv_w", writes=[B_wbf])

    B_modrow = kb.buf("modrow")
    B_mod2d = kb.buf("mod2d")
    modcol = sb("modcol", [128, NSEQ, 4, 8])
    B_modcol = kb.buf("modcol")
    gatebc = sb("gatebc", [128, NSEQ, 2, D], BF16)
    B_gatebc = kb.buf("gatebc")

    with ExitStack() as p0:
        modrow = sb("modrow", [2, 6 * D], F32, p0)
        cT = sb("cT", [128, 8, NSEQ], F32, p0)
        cact = sb("cact", [128, 8, NSEQ], F32, p0)
        adab = sb("adab", [2, 6 * D], F32, p0)
        g12 = sb("g12", [128, 2, 8], F32, p0)
        B_cT, B_cact, B_adab, B_g12 = kb.buf(), kb.buf(), kb.buf(), kb.buf()
        kb.dma("sp", lambda e: e.dma_start(out=cT[:], in_=cT_d[:]), "ld_cT", writes=[B_cT])
        kb.dma("sp", lambda e: e.dma_start(out=adab[0:1, :], in_=adab_d[:]), "ld_adab", writes=[B_adab])
        kb.dma("sp", lambda e: e.dma_start(out=adab[1:2, :], in_=adab_d[:]), "ld_adab", writes=[B_adab])
        kb.dma("sp", lambda e: e.dma_start(out=g12[:, 0, :], in_=g1_d[:]), "ld_g12", writes=[B_g12])
        kb.dma("sp", lambda e: e.dma_start(out=g12[:, 1, :], in_=g2_d[:]), "ld_g12", writes=[B_g12])
        kb.emit("act", lambda e: e.activation(out=cact[:], in_=cT[:], func=AF.Silu), [B_cT], [B_cact])
        wbuf = [sb("adaw%d" % i, [128, 8, 512], F32, p0) for i in range(2)]
        B_wbuf = [kb.buf(), kb.buf()]
        pmod = [ps("pmod%d" % i, [2, 512], F32, p0) for i in range(2)]
        B_pmod = [kb.pbuf(), kb.pbuf()]
        for n in range(12):
            j = n % 2
            kb.dma("sp", lambda e, n=n, j=j: e.dma_start(out=wbuf[j][:], in_=adaw_d[:, :, n * 512:(n + 1) * 512]),
                   "ld_adaw%d" % j, writes=[B_wbuf[j]])
            for kc in range(8):
                kb.emit("pe", lambda e, j=j, kc=kc: e.matmul(pmod[j][:], lhsT=cact[:, kc, :], rhs=wbuf[j][:, kc, :],
                                                              start=(kc == 0), stop=(kc == 7)),
                        [B_cact, B_wbuf[j]], [B_pmod[j]], inc=(kc == 7))
            kb.emit("dve", lambda e, n=n, j=j: e.tensor_tensor(out=modrow[:, n * 512:(n + 1) * 512], in0=pmod[j][:],
                                                               in1=adab[:, n * 512:(n + 1) * 512], op=ALU.add),
                    [B_pmod[j], B_adab], [B_modrow])
        pcol = ps("pcol", [128, 64], F32, p0)
        B_pcol = kb.pbuf()
        col_src = [1, 0, 4, 3]
        for s in range(NSEQ):
            for j in range(4):
                for kc in range(8):
                    c0 = col_src[j] * D + kc * 128
                    idx = (s * 4 + j) * 8 + kc
                    kb.emit("pe", lambda e, s=s, c0=c0, idx=idx: e.matmul(
                        pcol[:, idx:idx + 1], lhsT=modrow[0:2, c0:c0 + 128], rhs=cst[0:2, 530 + s:531 + s],
                        start=True, stop=True), [B_modrow, B_cst], [B_pcol], inc=(j == 3 and kc == 7 and s == NSEQ - 1))
        kb.emit("dve", lambda e: e.tensor_copy(out=modcol[:].rearrange("p s j k -> p (s j k)"), in_=pcol[:]),
                [B_pcol], [B_modcol])
        for s in range(NSEQ):
            for jj, gi in ((0, 0), (2, 1)):
                kb.emit("dve", lambda e, s=s, jj=jj, gi=gi: e.scalar_tensor_tensor(
                    out=modcol[:, s, jj, :], in0=modcol[:, s, jj, :], scalar=1.0, in1=g12[:, gi, :],
                    op0=ALU.add, op1=ALU.mult), [B_modcol, B_g12], [B_modcol])
        pbc = [ps("pbc%d" % i, [128, 512], F32, p0) for i in range(2)]
        B_pbc = [kb.pbuf(), kb.pbuf()]
        t = 0
        for s in range(NSEQ):
            for g, base in ((0, 2 * D), (1, 5 * D)):
                for hh in range(2):
                    j = t % 2
                    t += 1
                    kb.emit("pe", lambda e, s=s, base=base, hh=hh, j=j: e.matmul(
                        pbc[j][:], lhsT=cst[0:2, 532 + s * 128:532 + (s + 1) * 128],
                        rhs=modrow[0:2, base + hh * 512:base + (hh + 1) * 512], start=True, stop=True),
                        [B_modrow, B_cst], [B_pbc[j]])
                    kb.emit("act", lambda e, s=s, g=g, hh=hh, j=j: e.copy(
                        out=gatebc[:, s, g, hh * 512:(hh + 1) * 512], in_=pbc[j][:]), [B_pbc[j]], [B_gatebc])
        if dbg and "modcol" in dbg:
            kb.dma("sp", lambda e: e.dma_start(out=dbg_d["modcol"][:], in_=modcol[:].rearrange("p s j k -> p (s j k)")),
                   "st_dbg", reads=[B_modcol])
        g2r = sb("g2r", [2, D], F32, p0)
        B_g2r = kb.buf()
        for r_ in range(2):
            kb.dma("sp", lambda e, r_=r_: e.dma_start(out=g2r[r_:r_ + 1, :], in_=g2row_d[:]), "ld_g2r", writes=[B_g2r])
        kb.emit("dve", lambda e: e.scalar_tensor_tensor(out=g2r[:], in0=modrow[:, 4 * D:5 * D], scalar=1.0, in1=g2r[:], op0=ALU.add, op1=ALU.mult),
                [B_modrow, B_g2r], [B_g2r])
        kb.dma("sp", lambda e: e.dma_start(out=mod2_d[:, 0:D], in_=g2r[:]), "st_mod2", reads=[B_g2r], writes=[B_mod2d])
        kb.dma("sp", lambda e: e.dma_start(out=mod2_d[:, D:2 * D], in_=modrow[:, 3 * D:4 * D]), "st_mod2", reads=[B_modrow], writes=[B_mod2d])
        kb.barrier()
        kb.flush()

    onesb = sb("onesb", [128, 128], BF16)
    B_onesb = kb.buf()
    kb.emit("pool", lambda e: e.memset(onesb[:], 1.0), [], [B_onesb])
    wq = sb("wq", [128, 3, 768], BF16)
    wkv = sb("wkv", [128, 2, 1024], BF16)
    gq_bc = sb("gq_bc", [128, 8, 96])
    gk_bc = sb("gk_bc", [128, 8, 96])
    cosT = sb("cosT", [128, NSEQ * NT, 16])
    sinT = sb("sinT", [128, NSEQ * NT, 16])
    B_wq, B_wkv, B_gq, B_gk, B_cs = (kb.buf() for _ in range(5))
    with ExitStack() as pw:
        wq_f = sb("wq_f", [128, 3, 768], F32, pw)
        wkv_f = sb("wkv_f", [128, 2, 1024], F32, pw)
        qag = sb("qag", [128, 3], F32, pw)
        kvag = sb("kvag", [128, 2], F32, pw)
        g96 = sb("g96", [128, 2, 96], F32, pw)
        posi = sb("posi", [128, NSEQ * NT], I32, pw)
        posf = sb("posf", [128, NSEQ * NT], F32, pw)
        ang = sb("ang", [128, NSEQ * NT, 16], F32, pw)
        ang2 = sb("ang2", [128, NSEQ * NT, 16], F32, pw)
        B_wqf, B_wkvf, B_qag, B_kvag, B_g96, B_posi, B_posf, B_ang, B_ang2 = (kb.buf() for _ in range(9))
        kb.dma("sp", lambda e: e.dma_start(out=wq_f[:], in_=wq_d[:]), "ld_wqf", writes=[B_wqf])
        kb.dma("sp", lambda e: e.dma_start(out=wkv_f[:], in_=wkv_d[:]), "ld_wkvf", writes=[B_wkvf])
        kb.dma("sp", lambda e: e.dma_start(out=qag[:], in_=qag_d[:]), "ld_qag", writes=[B_qag])
        kb.dma("sp", lambda e: e.dma_start(out=kvag[:], in_=kvag_d[:]), "ld_kvag", writes=[B_kvag])
        kb.dma("sp", lambda e: e.dma_start(out=g96[:, 0, :], in_=qng_d.partition_broadcast(128)), "ld_g96", writes=[B_g96])
        kb.dma("sp", lambda e: e.dma_start(out=g96[:, 1, :], in_=kng_d.partition_broadcast(128)), "ld_g96", writes=[B_g96])
        kb.dma("sp", lambda e: e.dma_start(out=posi[:], in_=pos_d[:]), "ld_pos", writes=[B_posi])
        for c in range(3):
            kb.emit("dve", lambda e, c=c: e.tensor_scalar_mul(out=wq[:, c, :], in0=wq_f[:, c, :], scalar1=qag[:, c:c + 1]),
                    [B_wqf, B_qag], [B_wq])
        for c in range(2):
            kb.emit("dve", lambda e, c=c: e.tensor_scalar_mul(out=wkv[:, c, :], in0=wkv_f[:, c, :], scalar1=kvag[:, c:c + 1]),
                    [B_wkvf, B_kvag], [B_wkv])
        kb.emit("dve", lambda e: e.tensor_scalar_mul(out=gq_bc[:], in0=g96[:, 0:1, :].to_broadcast([128, 8, 96]),
                                                     scalar1=float(96 ** -0.5)), [B_g96], [B_gq])
        kb.emit("dve", lambda e: e.tensor_copy(out=gk_bc[:], in_=g96[:, 1:2, :].to_broadcast([128, 8, 96])), [B_g96], [B_gk])
        kb.emit("dve", lambda e: e.tensor_copy(out=posf[:], in_=posi[:]), [B_posi], [B_posf])
        kb.emit("dve", lambda e: e.tensor_tensor(out=ang[:], in0=posf[:].unsqueeze(2).to_broadcast([128, NSEQ * NT, 16]),
                                                 in1=cst[:, 514:530].unsqueeze(1).to_broadcast([128, NSEQ * NT, 16]),
                                                 op=ALU.mult), [B_posf, B_cst], [B_ang])
        PI = float(np.pi)
        angi = sb("angi", [128, NSEQ * NT, 16], I32, pw)
        B_angi = kb.buf()

        def sin_table(dst, shift):
            kb.emit("dve", lambda e: e.tensor_scalar(out=ang2[:], in0=ang[:], scalar1=float(1.0 / (2 * PI)), scalar2=None, op0=ALU.mult),
                    [B_ang], [B_ang2])
            kb.emit("dve", lambda e: e.tensor_copy(out=angi[:], in_=ang2[:]), [B_ang2], [B_angi])
            kb.emit("dve", lambda e: e.tensor_copy(out=ang2[:], in_=angi[:]), [B_angi], [B_ang2])
            kb.emit("dve", lambda e: e.scalar_tensor_tensor(out=ang2[:], in0=ang2[:], scalar=-2 * PI, in1=ang[:], op0=ALU.mult, op1=ALU.add),
                    [B_ang2, B_ang], [B_ang2])
            if shift != 0.0:
                kb.emit("dve", lambda e: e.tensor_scalar(out=ang2[:], in0=ang2[:], scalar1=float(shift), scalar2=None, op0=ALU.add),
                        [B_ang2], [B_ang2])
            kb.emit("dve", lambda e: e.tensor_scalar(out=angi[:].bitcast(F32), in0=ang2[:], scalar1=PI, scalar2=-2 * PI, op0=ALU.is_gt, op1=ALU.mult),
                    [B_ang2], [B_angi])
            kb.emit("dve", lambda e: e.tensor_tensor(out=ang2[:], in0=ang2[:], in1=angi[:].bitcast(F32), op=ALU.add), [B_ang2, B_angi], [B_ang2])
            kb.emit("dve", lambda e: e.tensor_scalar(out=angi[:].bitcast(F32), in0=ang2[:], scalar1=-PI, scalar2=2 * PI, op0=ALU.is_lt, op1=ALU.mult),
                    [B_ang2], [B_angi])
            kb.emit("dve", lambda e: e.tensor_tensor(out=ang2[:], in0=ang2[:], in1=angi[:].bitcast(F32), op=ALU.add), [B_ang2, B_angi], [B_ang2])
            kb.emit("act", lambda e: e.activation(out=dst[:], in_=ang2[:], func=AF.Sin), [B_ang2], [B_cs])

        sin_table(sinT, 0.0)
        sin_table(cosT, PI / 2)
        kb.barrier()
        kb.flush()

    wr = sb("wr", [128, 8, 36], BF16)
    brb = sb("brb", [128, 36])
    EIDX = sb("EIDX", [128, NSEQ * NT, 2])
    WK = sb("WK", [128, NSEQ * NT, 2])
    iota_e = sb("iota_e", [128, 32])
    B_wr, B_brb, B_iota = kb.buf(), kb.buf(), kb.buf()
    B_EW = [kb.buf() for _ in range(NSEQ * NT)]
    kb.dma("sp", lambda e: e.dma_start(out=iota_e[:], in_=cstm_d[:, 128:160]), "ld_iota", writes=[B_iota])
    kb.dma("pool", lambda e: e.dma_start(out=wr[:], in_=wr_d[:]), "ld_wr", writes=[B_wr])
    kb.dma("sp", lambda e: e.dma_start(out=brb[:], in_=br_d.partition_broadcast(128)), "ld_brb", writes=[B_brb])
    B_x1d = [[kb.buf() for _ in range(NT)] for _ in range(NSEQ)]
    B_h2d = [[kb.buf() for _ in range(NT)] for _ in range(NSEQ)]

    def dbg_store(name, ap, bufs):
        if dbg and name in dbg:
            kb.dma("sp", lambda e: e.dma_start(out=dbg_d[name][:], in_=ap), "st_dbg", reads=bufs)

    class NormT:
        def __init__(self, stack, tag, ntp=2, dve_evac=False):
            self.dve_evac = dve_evac
            self.xn = [sb("xn%s%d" % (tag, i), [128, D], BF16, stack) for i in range(2)]
            self.st = sb("st" + tag, [128, 2, 4], F32, stack)
            tps = [ps("tp%s%d" % (tag, i), [128, 8, 128], BF16, stack) for i in range(ntp)]
            btp = [kb.pbuf() for _ in range(ntp)]
            self.tp = [tps[i % ntp] for i in range(2)]
            self.B_tp = [btp[i % ntp] for i in range(2)]
            self.B_xn = [kb.buf(), kb.buf()]
            self.B_st = [kb.buf(), kb.buf()]
            self.n = 0

        def run(self, src, B_src, s, jg, dst, B_dst):
            j = self.n % 2
            self.n += 1
            issue_cv(1)
            st, xn, tp = self.st, self.xn[j], self.tp[j]
            junk = xn
            B_st, B_xn, B_tp = self.B_st[j], self.B_xn[j], self.B_tp[j]
            kb.emit("act", lambda e: e.activation(out=junk[:], in_=src, func=AF.Square, accum_out=st[:, j, 0:1]),
                    [B_src], [B_xn, B_st])
            kb.emit("act", lambda e: e.activation(out=st[:, j, 1:2], in_=st[:, j, 0:1], func=AF.Ln, scale=1.0 / D, bias=cst[:, 919:920]),
                    [B_st, B_cst], [B_st])
            kb.emit("act", lambda e: e.activation(out=st[:, j, 2:3], in_=st[:, j, 1:2], func=AF.Exp, scale=-0.5), [B_st], [B_st])
            kb.emit("dve", lambda e: e.tensor_scalar_mul(out=xn[:], in0=src, scalar1=st[:, j, 2:3]), [B_src, B_st], [B_xn])
            for kc in range(8):
                kb.emit("pe", lambda e, kc=kc: e.transpose(out=tp[:, kc, :], in_=xn[:, kc * 128:(kc + 1) * 128], identity=identb[:]),
                        [B_xn, B_identb], [B_tp], inc=(kc == 7))
            if self.dve_evac:
                kb.emit("dve", lambda e: e.tensor_tensor(out=dst, in0=tp[:], in1=modcol[:, s, jg, :].unsqueeze(2).to_broadcast([128, 8, 128]), op=ALU.mult),
                        [B_tp, B_modcol], [B_dst])
                kb.emit("pool", lambda e: e.tensor_tensor(out=dst, in0=dst, in1=modcol[:, s, jg + 1, :].unsqueeze(2).to_broadcast([128, 8, 128]), op=ALU.add),
                        [B_dst, B_modcol], [B_dst])
            else:
                for kc in range(8):
                    kb.emit("act", lambda e, kc=kc: e.activation(out=dst[:, kc, :], in_=tp[:, kc, :], func=AF.Identity,
                                                                 bias=modcol[:, s, jg + 1, kc:kc + 1], scale=modcol[:, s, jg, kc:kc + 1]),
                            [B_tp, B_modcol], [B_dst])
            return xn, B_xn

    nseq_run = NSEQ if stop is None else 1
    for s in range(nseq_run):
        with ExitStack() as sq_:
            OT = sb("OT", [64, 8, S], BF16, sq_)
            B_OT = kb.buf()

            with ExitStack() as pm:
                QT = sb("QT", [128, 8, S if stop != "hT" else 4], BF16, pm)
                KT = sb("KT", [128, 8, S if stop != "hT" else 4], BF16, pm)
                V2 = sb("V2", [128, NT, 8, 65], BF16, pm)
                rstdk = sb("rstdk", [128, NT, 8], F32, pm)
                B_QT, B_KT, B_V2, B_rk = kb.buf(), kb.buf(), kb.buf(), kb.buf()
                kb.emit("pool", lambda e: e.memset(V2[:, :, :, 64:65], 1.0), [], [B_V2])
                with ExitStack() as pb:
                    winm = sb("winm", [128, 8, 672], BF16, pb)
                    B_winm = kb.buf()
                    kb.dma("pool", lambda e: e.dma_start(out=winm[:], in_=win_d[:, :, 0:672]), "ld_winm", writes=[B_winm])
                    xt = [sb("xt0", [128, D], F32, pb), sb("xt1", [128, D], F32, pb)]
                    B_xt = [kb.buf(), kb.buf()]
                    nrm = NormT(pb, "a", ntp=1)
                    hTc = sb("hTc", [128, 8, 512], BF16, pb)
                    B_hTc = [kb.buf() for _ in range(4)]
                    latT = sb("latT", [128, 5, 512], BF16, pb)
                    sqT = sb("sqT", [128, 5, 512], BF16, pb)
                    B_latT, B_sqT = kb.buf(), kb.buf()
                    plat = [ps("plat%d" % i, [128, 512], F32, pb) for i in range(2)]
                    B_plat = [kb.pbuf(), kb.pbuf()]
                    pq = ps("pq", [128, 1024], F32, pb)
                    B_pq = kb.pbuf()
                    pkv = ps("pkv", [128, 1024], F32, pb)
                    B_pkv = kb.pbuf()
                    psm = ps("psm", [128, 64], F32, pb)
                    B_pss = kb.pbuf()
                    B_pkr = B_pss
                    ptq = nrm.tp[0]
                    B_ptq = nrm.B_tp[0]
                    rst4 = sb("rst", [128, 4, 4], F32, pb)
                    B_rst = kb.buf()
                    hstk = sb("hstk", [128, 3, 8], F32, pb)
                    rpk = sb("rpk", [128, 4, 1, 16], F32, pb)
                    B_hstk, B_rpk = kb.buf(), kb.buf()
                    qf = sb("qf", [128, 8, 96], F32, pb)
                    qsq = sb("qsq", [128, 8, 96], F32, pb)
                    qn = sb("qn", [128, 8, 96], F32, pb)
                    hst = sb("hst", [128, 3, 8], F32, pb)
                    rp_q = sb("rp", [128, 4, 8, 16], F32, pb)
                    qfin = sb("qfin", [128, 8, 96], BF16, pb)
                    kvf = sb("kvf", [128, 8, 128], F32, pb)
                    ksq = sb("ksq", [128, 8, 64], F32, pb)
                    krf = sb("krf", [128, 3, 32], F32, pb)
                    kst = sb("kst", [128, 4], F32, pb)
                    kfin = sb("kfin", [128, 8, 96], BF16, pb)
                    B_qf, B_qsq, B_qn, B_hst, B_rp_q, B_qfin, B_kvf, B_ksq, B_krf, B_kst, B_kfin = (kb.buf() for _ in range(11))

                    def rope(src3, dst3, cs_i, nh, Bsrc, Bdst, rp=None, B_rp=None):
                        if rp is None:
                            rp, B_rp = rp_q, B_rp_q
                        cb = cosT[:, cs_i:cs_i + 1, :].to_broadcast([128, nh, 16])
                        sbb = sinT[:, cs_i:cs_i + 1, :].to_broadcast([128, nh, 16])
                        x1 = src3[:, :, 0:16]
                        x2 = src3[:, :, 16:32]
                        r = rp[:, :, 0:nh, :]
                        kb.emit("dve", lambda e: e.tensor_tensor(out=r[:, 0], in0=x1, in1=cb, op=ALU.mult), [Bsrc, B_cs], [B_rp])
                        kb.emit("dve", lambda e: e.tensor_tensor(out=r[:, 1], in0=x2, in1=sbb, op=ALU.mult), [Bsrc, B_cs], [B_rp])
                        kb.emit("dve", lambda e: e.tensor_tensor(out=r[:, 2], in0=x2, in1=cb, op=ALU.mult), [Bsrc, B_cs], [B_rp])
                        kb.emit("dve", lambda e: e.tensor_tensor(out=r[:, 3], in0=x1, in1=sbb, op=ALU.mult), [Bsrc, B_cs], [B_rp])
                        kb.emit("dve", lambda e: e.tensor_tensor(out=dst3[:, :, 0:16], in0=r[:, 0], in1=r[:, 1], op=ALU.subtract),
                                [B_rp], [Bdst])
                        kb.emit("dve", lambda e: e.tensor_tensor(out=dst3[:, :, 16:32], in0=r[:, 2], in1=r[:, 3], op=ALU.add),
                                [B_rp], [Bdst])

                    ncc = 4 if stop != "hT" else 1
                    def ld_x(i):
                        j = i % 2
                        kb.dma("sp", lambda e: e.dma_start(out=xt[j][:], in_=x_d[s, i * 128:(i + 1) * 128, :]), "ld_xt%d" % j, writes=[B_xt[j]])

                    ld_x(0)
                    ld_x(1)
                    for cc in range(ncc):
                        for ti in range(4):
                            i = cc * 4 + ti
                            j = i % 2
                            nrm.run(xt[j][:], B_xt[j], s, 0, hTc[:, :, ti * 128:(ti + 1) * 128], B_hTc[ti])
                            if i + 2 < 4 * ncc:
                                ld_x(i + 2)
                        if stop == "hT":
                            break
                        for jc in range(5):
                            pj = jc % 2
                            for kc in range(8):
                                kb.emit("pe", lambda e, pj=pj, jc=jc, kc=kc: e.matmul(
                                    plat[pj][:], lhsT=winm[:, kc, jc * 128:(jc + 1) * 128], rhs=hTc[:, kc, :],
                                    start=(kc == 0), stop=(kc == 7)), [B_winm] + B_hTc, [B_plat[pj]], inc=(kc == 7))
                            kb.emit("act", lambda e, pj=pj, jc=jc: e.copy(out=latT[:, jc, :], in_=plat[pj][:]), [B_plat[pj]], [B_latT])
                            kb.emit("act", lambda e, pj=pj, jc=jc: e.activation(out=sqT[:, jc, :], in_=plat[pj][:], func=AF.Square),
                                    [B_plat[pj]], [B_sqT])
                        for ti in range(4):
                            t0 = ti * 128
                            rst = rst4[:, ti, :]
                            for jc in range(5):
                                col = 0 if jc < 3 else 1
                                kb.emit("pe", lambda e, jc=jc, col=col, t0=t0: e.matmul(
                                    psm[:, col:col + 1], lhsT=sqT[:, jc, t0:t0 + 128], rhs=onesb[:, 0:1],
                                    start=(jc in (0, 3)), stop=(jc in (2, 4))), [B_sqT, B_onesb], [B_pss], inc=(jc == 4))
                            kb.emit("dve", lambda e, rst=rst: e.tensor_tensor(out=rst[:, 0:2], in0=psm[:, 0:2], in1=cst[:, 916:918], op=ALU.mult),
                                    [B_pss, B_cst], [B_rst])
                            kb.emit("act", lambda e, rst=rst: e.activation(out=rst[:, 2:4], in_=rst[:, 0:2], func=AF.Ln, bias=cst[:, 919:920]),
                                    [B_rst, B_cst], [B_rst])
                            kb.emit("act", lambda e, rst=rst: e.activation(out=rst[:, 2:4], in_=rst[:, 2:4], func=AF.Exp, scale=-0.5), [B_rst], [B_rst])

                        def qchain(ti):
                            i = cc * 4 + ti
                            gi = s * NT + i
                            t0 = ti * 128
                            rst = rst4[:, ti, :]
                            for c in range(3):
                                kb.emit("pe", lambda e, c=c: e.matmul(pq[:, 0:512], lhsT=latT[:, c, t0:t0 + 128], rhs=wq[:, c, 0:512],
                                                                      start=(c == 0), stop=(c == 2)), [B_latT, B_wq], [B_pq], inc=False)
                            for c in range(3):
                                kb.emit("pe", lambda e, c=c: e.matmul(pq[:, 512:768], lhsT=latT[:, c, t0:t0 + 128], rhs=wq[:, c, 512:768],
                                                                      start=(c == 0), stop=(c == 2)), [B_latT, B_wq], [B_pq], inc=(c == 2))
                            yield
                            qf2 = qf[:].rearrange("p h d -> p (h d)")
                            kb.emit("act", lambda e: e.activation(out=qf2, in_=pq[:, 0:768], func=AF.Copy, scale=rst[:, 2:3]), [B_pq, B_rst], [B_qf])
                            yield
                            kb.emit("act", lambda e: e.activation(out=qsq[:], in_=qf[:], func=AF.Square), [B_qf], [B_qsq])
                            yield
                            kb.emit("dve", lambda e: e.tensor_reduce(out=hst[:, 0, :], in_=qsq[:], axis=AX.X, op=ALU.add), [B_qsq], [B_hst])
                            yield
                            kb.emit("act", lambda e: e.activation(out=hst[:, 1, :], in_=hst[:, 0, :], func=AF.Ln, scale=1.0 / 96, bias=cst[:, 919:920]),
                                    [B_hst, B_cst], [B_hst])
                            kb.emit("act", lambda e: e.activation(out=hst[:, 2, :], in_=hst[:, 1, :], func=AF.Exp, scale=-0.5), [B_hst], [B_hst])
                            yield
                            kb.emit("dve", lambda e: e.tensor_tensor(out=qn[:], in0=qf[:], in1=hst[:, 2, :].unsqueeze(2).to_broadcast([128, 8, 96]),
                                                                     op=ALU.mult), [B_qf, B_hst], [B_qn])
                            yield
                            kb.emit("dve", lambda e: e.tensor_tensor(out=qn[:], in0=qn[:], in1=gq_bc[:], op=ALU.mult), [B_qn, B_gq], [B_qn])
                            yield
                            kb.emit("act", lambda e: e.copy(out=qfin[:, :, 0:64], in_=qn[:, :, 0:64]), [B_qn], [B_qfin])
                            rope(qn[:, :, 64:96], qfin[:, :, 64:96], gi, 8, B_qn, B_qfin)
                            yield
                            for h in range(8):
                                kb.emit("pe", lambda e, h=h: e.transpose(out=ptq[0:96, h, :], in_=qfin[:, h, :], identity=identb[:]),
                                        [B_qfin, B_identb], [B_ptq], inc=(h == 7))
                            kb.emit("act", lambda e: e.copy(out=QT[0:96, :, i * 128:(i + 1) * 128], in_=ptq[0:96, :, :]), [B_ptq], [B_QT])
                            yield

                        def kchain(ti):
                            i = cc * 4 + ti
                            gi = s * NT + i
                            t0 = ti * 128
                            rst = rst4[:, ti, :]
                            hst_ = hstk
                            for hh in range(2):
                                for c in range(2):
                                    kb.emit("pe", lambda e, c=c, hh=hh: e.matmul(
                                        pkv[:, hh * 512:(hh + 1) * 512], lhsT=latT[:, 3 + c, t0:t0 + 128], rhs=wkv[:, c, hh * 512:(hh + 1) * 512],
                                        start=(c == 0), stop=(c == 1)), [B_latT, B_wkv], [B_pkv], inc=(c == 1 and hh == 1))
                            for kc in range(8):
                                kb.emit("pe", lambda e, kc=kc: e.matmul(psm[:, 32:64], lhsT=hTc[:, kc, t0:t0 + 128], rhs=winm[:, kc, 640:672],
                                                                        start=(kc == 0), stop=(kc == 7)), [B_hTc[ti], B_winm], [B_pkr], inc=(kc == 7))
                            yield
                            kvf2 = kvf[:].rearrange("p h d -> p (h d)")
                            kb.emit("act", lambda e: e.activation(out=kvf2, in_=pkv[:], func=AF.Copy, scale=rst[:, 3:4]), [B_pkv, B_rst], [B_kvf])
                            yield
                            kb.emit("pool", lambda e: e.tensor_copy(out=V2[:, i, :, 0:64], in_=kvf[:, :, 64:128]), [B_kvf], [B_V2])
                            kb.emit("act", lambda e: e.copy(out=krf[:, 0, :], in_=psm[:, 32:64]), [B_pkr], [B_krf])
                            kb.emit("act", lambda e: e.activation(out=krf[:, 1, :], in_=krf[:, 0, :], func=AF.Square, accum_out=kst[:, 0:1]),
                                    [B_krf], [B_krf, B_kst])
                            yield
                            kb.emit("act", lambda e: e.activation(out=ksq[:], in_=kvf[:, :, 0:64], func=AF.Square), [B_kvf], [B_ksq])
                            yield
                            kb.emit("dve", lambda e: e.tensor_reduce(out=hst_[:, 0, :], in_=ksq[:], axis=AX.X, op=ALU.add), [B_ksq], [B_hstk])
                            kb.emit("dve", lambda e: e.tensor_scalar(out=hst_[:, 1, :], in0=hst_[:, 0, :], scalar1=kst[:, 0:1], scalar2=1.0 / 96,
                                                                     op0=ALU.add, op1=ALU.mult), [B_hstk, B_kst], [B_hstk])
                            yield
                            kb.emit("act", lambda e: e.activation(out=hst_[:, 2, :], in_=hst_[:, 1, :], func=AF.Ln, bias=cst[:, 919:920]),
                                    [B_hstk, B_cst], [B_hstk])
                            kb.emit("act", lambda e: e.activation(out=rstdk[:, i, :], in_=hst_[:, 2, :], func=AF.Exp, scale=-0.5), [B_hstk], [B_rk])
                            yield
                            kb.emit("dve", lambda e: e.tensor_tensor(out=kfin[:, :, 0:64], in0=kvf[:, :, 0:64], in1=gk_bc[:, :, 0:64], op=ALU.mult),
                                    [B_kvf, B_gk], [B_kfin])
                            yield
                            kb.emit("dve", lambda e: e.tensor_tensor(out=krf[:, 1, :], in0=krf[:, 0, :], in1=gk_bc[:, 0, 64:96], op=ALU.mult),
                                    [B_krf, B_gk], [B_krf])
                            rope(krf[:, 1:2, :], krf[:, 2:3, :], gi, 1, B_krf, B_krf, rpk, B_rpk)
                            yield
                            kb.emit("dve", lambda e: e.tensor_copy(out=kfin[:, :, 64:96], in_=krf[:, 2:3, :].to_broadcast([128, 8, 32])),
                                    [B_krf], [B_kfin])
                            for h in range(8):
                                kb.emit("pe", lambda e, h=h: e.transpose(out=ptq[0:96, h, :], in_=kfin[:, h, :], identity=identb[:]),
                                        [B_kfin, B_identb], [B_ptq], inc=(h == 7))
                            kb.emit("act", lambda e: e.copy(out=KT[0:96, :, i * 128:(i + 1) * 128], in_=ptq[0:96, :, :]), [B_ptq], [B_KT])
                            yield

                        def run_il(gens):
                            gens = list(gens)
                            while gens:
                                for g in list(gens):
                                    try:
                                        next(g)
                                    except StopIteration:
                                        gens.remove(g)

                        run_il([qchain(0)])
                        for ti in range(4):
                            gl_ = [kchain(ti)]
                            if ti + 1 < 4:
                                gl_.append(qchain(ti + 1))
                            run_il(gl_)
                    if dbg and "hT" in dbg:
                        hTf = sb("hTf", [128, 8, 512], F32, pb)
                        B_hTf = kb.buf()
                        kb.emit("dve", lambda e: e.tensor_copy(out=hTf[:], in_=hTc[:]), B_hTc, [B_hTf])
                        dbg_store("hT", hTf[:].rearrange("p k t -> p (k t)"), [B_hTf])
                    kb.barrier()
                    kb.flush()
                if stop == "hT":
                    break
                if dbg and "QT" in dbg:
                    with ExitStack() as pd:
                        tf = sb("QTf", [128, 8, 512], F32, pd)
                        B_tf = kb.buf()
                        for nm, src, Bs in (("QT", QT, B_QT), ("KT", KT, B_KT)):
                            dv = dbg_d[nm].rearrange("p (k t) -> p k t", k=8)
                            for cc in range(4):
                                kb.emit("dve", lambda e, src=src, cc=cc: e.tensor_copy(out=tf[0:96], in_=src[0:96, :, cc * 512:(cc + 1) * 512]), [Bs], [B_tf])
                                kb.dma("sp", lambda e, dv=dv, cc=cc: e.dma_start(out=dv[:, :, cc * 512:(cc + 1) * 512], in_=tf[0:96]), "st_dbg", reads=[B_tf])
                        dbg_store("rstdk", rstdk[:].rearrange("p i h -> p (i h)"), [B_rk])
                        kb.barrier(); kb.flush()
                if stop == "QK":
                    break

                with ExitStack() as pat:
                    pst = [ps("pst%d" % i, [128, 512], F32, pat) for i in range(4)]
                    B_pst = [kb.pbuf() for _ in range(4)]
                    po = [ps("po%d" % i, [128, 512], F32, pat) for i in range(2)]
                    B_po = [kb.pbuf() for _ in range(2)]
                    pbc2 = ps("pbc2", [64, 512], F32, pat)
                    B_pbc2 = kb.pbuf()
                    pT = [sb("pT%d" % i, [128, 512], BF16, pat) for i in range(4)]
                    B_pT = [kb.buf() for _ in range(4)]
                    rec = sb("rec", [128, 512], F32, pat)
                    B_rec = kb.buf()
                    recb = sb("recb", [64, 512], F32, pat)
                    B_recb = kb.buf()
                    if s == 0 and (stop is None or stop == "moe"):
                        zt = sb("zt", [128, 2048], BF16, pat)
                        B_zt = kb.buf()
                        kb.emit("pool", lambda e: e.memset(zt[:], 0.0), [], [B_zt])
                        xs_v = xs_d.rearrange("(c p r) d -> c p (r d)", p=128, r=2)
                        for c_ in range(xs_v.shape[0]):
                            kb.dma("sp", lambda e, c_=c_: e.dma_start(out=xs_v[c_], in_=zt[:]), "zf_xs", reads=[B_zt], writes=[B_xsz])
                    units = [(h, qc) for h in range(8) for qc in range(4)]
                    steps = [(u, kt) for u in range(len(units)) for kt in range(NT)]

                    def emit_S(n):
                        u, kt = steps[n]
                        h, qc = units[u]
                        j = n % 4
                        kb.emit("pe", lambda e: e.matmul(pst[j][:], lhsT=KT[0:96, h, kt * 128:(kt + 1) * 128],
                                                         rhs=QT[0:96, h, qc * 512:(qc + 1) * 512], start=True, stop=True),
                                [B_KT, B_QT], [B_pst[j]])
                        kb.emit("act", lambda e: e.activation(out=pT[j][:], in_=pst[j][:], func=AF.Exp, scale=rstdk[:, kt, h:h + 1]),
                                [B_pst[j], B_rk], [B_pT[j]])

                    def emit_PV(n):
                        u, kt = steps[n]
                        h, qc = units[u]
                        j = n % 4
                        a = u % 2
                        kb.emit("pe", lambda e: e.matmul(po[a][0:65, :], lhsT=V2[:, kt, h, :], rhs=pT[j][:], start=(kt == 0), stop=(kt == NT - 1)),
                                [B_V2, B_pT[j]], [B_po[a]], inc=(kt == NT - 1))
                        if kt == NT - 1:
                            kb.emit("dve", lambda e: e.reciprocal(out=rec[64:65, :], in_=po[a][64:65, :]), [B_po[a]], [B_rec])
                            kb.emit("pe", lambda e: e.matmul(pbc2[:], lhsT=ones_f[64:65, 0:64], rhs=rec[64:65, :], start=True, stop=True),
                                    [B_rec, B_cst], [B_pbc2])
                            kb.emit("act", lambda e: e.copy(out=recb[:], in_=pbc2[:]), [B_pbc2], [B_recb])
                            kb.emit("dve", lambda e: e.tensor_tensor(out=OT[0:64, h, qc * 512:(qc + 1) * 512], in0=po[a][0:64, :],
                                                                     in1=recb[:], op=ALU.mult), [B_po[a], B_recb], [B_OT])

                    LA = 3
                    for n in range(len(steps) + LA):
                        if n < len(steps):
                            emit_S(n)
                        if n >= LA:
                            emit_PV(n - LA)
                    kb.barrier()
                    kb.flush()
            if dbg and "OT" in dbg:
                with ExitStack() as pd:
                    tf = sb("OTf", [64, 8, S], F32, pd)
                    B_tf = kb.buf()
                    kb.emit("dve", lambda e: e.tensor_copy(out=tf[:], in_=OT[:]), [B_OT], [B_tf])
                    dbg_store("OT", tf[:].rearrange("p k t -> p (k t)"), [B_tf])
                    kb.barrier(); kb.flush()
            if stop == "attn":
                break


            recT = sb("recT", [128, 4, S], BF16, sq_)
            B_recT = kb.buf()
            with ExitStack() as ph:
                winh = sb("winh", [128, 8, 2560], BF16, ph)
                B_winh5 = [kb.buf() for _ in range(5)]
                for cb in (0, 3, 1, 4, 2):
                    kb.dma("pool", lambda e, cb=cb: e.dma_start(out=winh[:, :, cb * 512:(cb + 1) * 512],
                                                                in_=win_d[:, :, 672 + cb * 512:672 + (cb + 1) * 512]),
                           "ld_winh%d" % cb, writes=[B_winh5[cb]])
                ofw = sb("ofw", [128, NT, 512], BF16, ph)
                B_ofw = [kb.buf() for _ in range(NT)]
                lbc = sb("lbc", [128, 2, 512], F32, ph)
                oml = sb("oml", [128, 2, 512], F32, ph)
                hgn = sb("hgn", [128, 128], F32, ph)
                B_lb, B_hgn = kb.buf(), kb.buf()
                with ExitStack() as pl:
                    lraw = sb("lraw", [128, 2, 2, 512], F32, pl)
                    B_lraw = kb.buf()
                    kb.dma("sp", lambda e: e.dma_start(out=lraw[:].rearrange("p a b n -> p (a b n)"), in_=lbl_d.partition_broadcast(128)),
                           "ld_lraw", writes=[B_lraw])
                    kb.dma("sp", lambda e: e.dma_start(out=hgn[:], in_=hgn_d.partition_broadcast(128)), "ld_hgn", writes=[B_hgn])
                    kb.emit("dve", lambda e: e.tensor_tensor(out=lbc[:], in0=lraw[:, :, 0, :], in1=lraw[:, :, 1, :], op=ALU.subtract),
                            [B_lraw], [B_lb])
                    kb.emit("act", lambda e: e.activation(out=lbc[:], in_=lbc[:], func=AF.Sigmoid), [B_lb], [B_lb])
                    kb.emit("dve", lambda e: e.tensor_scalar(out=oml[:], in0=lbc[:], scalar1=-1.0, scalar2=1.0, op0=ALU.mult, op1=ALU.add),
                            [B_lb], [B_lb])
                    kb.barrier()
                    kb.flush()
                xt0 = sb("xth", [128, D], F32, ph)
                B_xt0 = kb.buf()
                nrm = NormT(ph, "h", ntp=1, dve_evac=True)
                hTt2 = [sb("hTt%d" % i, [128, 8, 128], BF16, ph) for i in range(2)]
                B_hTt2 = [kb.buf(), kb.buf()]
                pg = [ps("pg%d" % i, [128, 512], F32, ph) for i in range(2)]
                B_pg = [kb.pbuf() for _ in range(2)]
                pgn = [0]

                def next_pg():
                    j = pgn[0] % 2
                    pgn[0] += 1
                    return pg[j], B_pg[j]

                PA = ps("hPA", [128, 4, 128], F32, ph)
                PK = ps("hPK", [128, 4, 128], F32, ph)
                PI = ps("hPI", [128, 4, 128], F32, ph)
                PAo = ps("hPAo", [128, 4, 128], F32, ph)
                PBo = ps("hPBo", [128, 4, 128], F32, ph)
                B_PA, B_PK, B_PI, B_PAo, B_PBo = (kb.pbuf() for _ in range(5))
                ptr = nrm.tp[0]
                B_ptr = nrm.B_tp[0]
                qq2 = [sb("hq_q%d" % i, [128, 512], F32, ph) for i in range(2)]
                vv = [sb("hq_v%d" % i, [128, 512], BF16, ph) for i in range(3)]
                gg = [sb("hq_g%d" % i, [128, 512], BF16, ph) for i in range(3)]
                sg = sb("hq_sg", [128, 512], F32, ph)
                ff = sb("hq_f", [128, 512], F32, ph)
                kk = sb("hq_k", [128, 512], F32, ph)
                lf = sb("hq_lf", [128, 512], F32, ph)
                eb = sb("hq_eb", [128, 512], F32, ph)
                enb = sb("hq_enb", [128, 512], F32, ph)
                er = sb("hq_er", [128, 512], F32, ph)
                qkd = sb("hq_qkd", [128, 8, 128], BF16, ph)
                kend = [sb("hq_kend%d" % i, [128, 2, 512], BF16, ph) for i in range(2)]
                qkT = [sb("hq_qkT%d" % i, [128, 8, 128], BF16, ph) for i in range(2)]
                qAB = [sb("hq_qAB%d" % i, [128, 2, 4, 128], BF16, ph) for i in range(2)]
                dec = [sb("hq_dec%d" % i, [128, 4, 2], F32, ph) for i in range(2)]
                attm = sb("hq_attm", [128, 4, 128], BF16, ph)
                Sst = sb("hq_S", [128, 4, 128], F32, ph)
                Sbf = sb("hq_Sb", [128, 4, 128], BF16, ph)
                osum = sb("hq_osum", [128, 4, 128], F32, ph)
                osq = sb("hq_osq", [128, 4, 128], F32, ph)
                ost = sb("hq_ost", [128, 3, 4], F32, ph)
                recb_ = sb("hq_rec", [128, 512], BF16, ph)
                (B_sg, B_ff, B_kk, B_lf, B_eb, B_enb, B_er, B_qkd, B_attm, B_osum, B_osq, B_ost, B_rec2) = (kb.buf() for _ in range(13))
                B_qq2 = [kb.buf(), kb.buf()]
                B_vv = [kb.buf(), kb.buf(), kb.buf()]
                B_gg = [kb.buf(), kb.buf(), kb.buf()]
                B_kend = [kb.buf(), kb.buf()]
                B_qkT = [kb.buf(), kb.buf()]
                B_qAB = [kb.buf(), kb.buf()]
                B_dec = [kb.buf(), kb.buf()]
                B_S = [kb.buf() for _ in range(4)]
                B_Sb = [kb.buf() for _ in range(4)]
                for st_ in range(2):
                    kb.emit("pool", lambda e, st_=st_: e.memset(qAB[st_][:], 0.0), [], [B_qAB[st_]])

                def proj(cb, hTt, B_hTt):
                    p, B_p = next_pg()
                    for kc in range(8):
                        kb.emit("pe", lambda e, kc=kc: e.matmul(p[:], lhsT=hTt[:, kc, :], rhs=winh[:, kc, cb * 512:(cb + 1) * 512],
                                                                start=(kc == 0), stop=(kc == 7)), [B_hTt, B_winh5[cb]], [B_p], inc=(kc == 7))
                    return p, B_p

                def stageA1(d, i, n):
                    hTt, B_hTt = hTt2[n % 2], B_hTt2[n % 2]
                    qq, B_qq = qq2[n % 2], B_qq2[n % 2]
                    s3 = n % 3
                    kb.dma("sp", lambda e: e.dma_start(out=xt0[:], in_=x_d[s, i * 128:(i + 1) * 128, :]), "ld_xth", writes=[B_xt0])
                    nrm.run(xt0[:], B_xt0, s, 0, hTt[:], B_hTt)
                    yield
                    p, B_p = proj(0, hTt, B_hTt)
                    kb.emit("act", lambda e: e.activation(out=qq[:], in_=p[:], func=AF.Silu), [B_p], [B_qq])
                    yield
                    if d == 1:
                        p3, B_p3 = proj(4, hTt, B_hTt)
                        kb.emit("act", lambda e: e.activation(out=gg[s3][:], in_=p3[:], func=AF.Silu), [B_p3], [B_gg[s3]])
                        yield
                    p2, B_p2 = proj(3, hTt, B_hTt)
                    kb.emit("act", lambda e: e.copy(out=vv[s3][:], in_=p2[:]), [B_p2], [B_vv[s3]])
                    yield

                def stageA(d, i, n):
                    st = n % 2
                    hTt, B_hTt = hTt2[n % 2], B_hTt2[n % 2]
                    qq, B_qq = qq2[n % 2], B_qq2[n % 2]
                    tri_c = cst[:, 0:128] if d == 0 else cst[:, 128:256]
                    rev_c = cst[:, 256:384] if d == 0 else cst[:, 384:512]
                    p4, B_p4 = proj(1 + d, hTt, B_hTt)
                    kb.emit("act", lambda e: e.activation(out=sg[:], in_=p4[:], func=AF.Sigmoid), [B_p4], [B_sg])
                    kb.emit("dve", lambda e: e.tensor_tensor(out=ff[:], in0=sg[:], in1=oml[:, d, :], op=ALU.mult), [B_sg, B_lb], [B_ff])
                    kb.emit("dve", lambda e: e.tensor_tensor(out=ff[:], in0=ff[:], in1=lbc[:, d, :], op=ALU.add), [B_ff, B_lb], [B_ff])
                    kb.emit("dve", lambda e: e.tensor_scalar(out=kk[:], in0=ff[:], scalar1=-1.0, scalar2=1.0, op0=ALU.mult, op1=ALU.add),
                            [B_ff], [B_kk])
                    kb.emit("act", lambda e: e.activation(out=lf[:], in_=ff[:], func=AF.Ln), [B_ff], [B_lf])
                    yield
                    pb_, B_pb = next_pg()
                    kb.emit("pe", lambda e: e.matmul(pb_[:], lhsT=tri_c, rhs=lf[:], start=True, stop=True), [B_cst, B_lf], [B_pb])
                    kb.emit("act", lambda e: e.activation(out=eb[:], in_=pb_[:], func=AF.Exp), [B_pb], [B_eb])
                    kb.emit("act", lambda e: e.activation(out=enb[:], in_=pb_[:], func=AF.Exp, scale=-1.0), [B_pb], [B_enb])
                    yield
                    pr_, B_pr = next_pg()
                    kb.emit("pe", lambda e: e.matmul(pr_[:], lhsT=rev_c, rhs=lf[:], start=True, stop=True), [B_cst, B_lf], [B_pr])
                    kb.emit("act", lambda e: e.activation(out=er[:], in_=pr_[:], func=AF.Exp), [B_pr], [B_er])
                    yield
                    pd_, B_pd = next_pg()
                    for h in range(4):
                        kb.emit("pe", lambda e, h=h: e.matmul(pd_[:, 2 * h:2 * h + 2], lhsT=lf[:, h * 128:(h + 1) * 128],
                                                              rhs=cst[:, 512:514], start=True, stop=True), [B_lf, B_cst], [B_pd], inc=(h == 3))
                    kb.emit("act", lambda e: e.activation(out=dec[st][:].rearrange("p h c -> p (h c)"), in_=pd_[:, 0:8], func=AF.Exp),
                            [B_pd], [B_dec[st]])
                    kb.emit("dve", lambda e: e.tensor_tensor(out=qkd[:, 0:4, :].rearrange("p h k -> p (h k)"), in0=qq[:], in1=eb[:], op=ALU.mult),
                            [B_qq, B_eb], [B_qkd])
                    kb.emit("dve", lambda e: e.tensor_tensor(out=qkd[:, 4:8, :].rearrange("p h k -> p (h k)"), in0=kk[:], in1=enb[:], op=ALU.mult),
                            [B_kk, B_enb], [B_qkd])
                    for c2 in range(2):
                        kb.emit("dve", lambda e, c2=c2: e.scalar_tensor_tensor(out=kend[st][:, c2, :], in0=er[:], scalar=cst[:, 512 + c2:513 + c2],
                                                                             in1=kk[:], op0=ALU.mult, op1=ALU.mult),
                                [B_kk, B_er, B_cst], [B_kend[st]])
                    yield
                    for j8 in range(8):
                        kb.emit("pe", lambda e, j8=j8: e.transpose(out=ptr[:, j8, :], in_=qkd[:, j8, :], identity=identb[:]),
                                [B_qkd, B_identb], [B_ptr], inc=(j8 == 7))
                    kb.emit("act", lambda e: e.copy(out=qkT[st][:], in_=ptr[:]), [B_ptr], [B_qkT[st]])
                    kb.emit("dve", lambda e: e.tensor_copy(out=qAB[st][:, 0, :, 0:64], in_=ptr[:, 0:4, 0:64]), [B_ptr], [B_qAB[st]])
                    kb.emit("dve", lambda e: e.tensor_copy(out=qAB[st][:, 1, :, 64:128], in_=ptr[:, 0:4, 64:128]), [B_ptr], [B_qAB[st]])
                    yield

                def stageB(d, i, n):
                    st = n % 2
                    s3 = n % 3
                    tri_c = cst[:, 0:128] if d == 0 else cst[:, 128:256]
                    corder = [0, 1] if d == 0 else [1, 0]
                    for h in range(4):
                        kb.emit("pe", lambda e, h=h: e.matmul(PA[:, h, :], lhsT=qkT[st][:, 4 + h, :], rhs=qkT[st][:, h, :], start=True, stop=True),
                                [B_qkT[st]], [B_PA], inc=(h == 3))
                    yield
                    kb.emit("dve", lambda e: e.tensor_tensor(out=attm[:], in0=PA[:], in1=tri_c.unsqueeze(1).to_broadcast([128, 4, 128]), op=ALU.mult),
                            [B_PA, B_cst], [B_attm])
                    yield
                    for ci, cidx in enumerate(corder):
                        Po_, B_Po_ = (PAo, B_PAo) if ci == 0 else (PBo, B_PBo)
                        for h in range(4):
                            hs = slice(h * 128, (h + 1) * 128)
                            if ci == 0:
                                kb.emit("pe", lambda e, h=h, hs=hs: e.matmul(PI[:, h, :], lhsT=attm[:, h, :], rhs=vv[s3][:, hs], start=True, stop=True),
                                        [B_attm, B_vv[s3]], [B_PI], inc=False)
                            kb.emit("pe", lambda e, h=h, cidx=cidx, Po_=Po_: e.matmul(Po_[:, h, :], lhsT=qAB[st][:, cidx, h, :], rhs=Sbf[:, h, :],
                                                                                    start=True, stop=True), [B_qAB[st], B_Sb[h]], [B_Po_], inc=False)
                            kb.emit("pe", lambda e, h=h, cidx=cidx, hs=hs: e.matmul(PK[:, h, :], lhsT=kend[st][:, cidx, hs], rhs=vv[s3][:, hs],
                                                                                  start=True, stop=True), [B_kend[st], B_vv[s3]], [B_PK], inc=(h == 3))
                        yield
                        for h in range(4):
                            kb.emit("dve", lambda e, h=h, cidx=cidx: e.scalar_tensor_tensor(
                                out=Sst[:, h, :], in0=Sst[:, h, :], scalar=dec[st][:, h, cidx:cidx + 1], in1=PK[:, h, :], op0=ALU.mult, op1=ALU.add),
                                [B_S[h], B_dec[st], B_PK], [B_S[h]])
                            kb.emit("pool", lambda e, h=h: e.tensor_copy(out=Sbf[:, h, :], in_=Sst[:, h, :]), [B_S[h]], [B_Sb[h]])
                        yield
                    kb.emit("act", lambda e: e.copy(out=osum[:], in_=PI[:]), [B_PI], [B_osum])
                    kb.emit("dve", lambda e: e.tensor_tensor(out=osum[:], in0=osum[:], in1=PAo[:], op=ALU.add), [B_osum, B_PAo], [B_osum])
                    if d == 0:
                        kb.emit("dve", lambda e: e.tensor_tensor(out=ofw[:, i, :], in0=osum[:].rearrange("p h v -> p (h v)"),
                                                                 in1=PBo[:].rearrange("p h v -> p (h v)"), op=ALU.add), [B_osum, B_PBo], [B_ofw[i]])
                        yield
                        return
                    kb.emit("dve", lambda e: e.tensor_tensor(out=osum[:], in0=osum[:], in1=PBo[:], op=ALU.add), [B_osum, B_PBo], [B_osum])
                    kb.emit("dve", lambda e: e.tensor_tensor(out=osum[:].rearrange("p h v -> p (h v)"), in0=osum[:].rearrange("p h v -> p (h v)"),
                                                             in1=ofw[:, i, :], op=ALU.add), [B_osum, B_ofw[i]], [B_osum])
                    yield
                    kb.emit("act", lambda e: e.activation(out=osq[:], in_=osum[:], func=AF.Square), [B_osum], [B_osq])
                    kb.emit("dve", lambda e: e.tensor_reduce(out=ost[:, 0, :], in_=osq[:], axis=AX.X, op=ALU.add), [B_osq], [B_ost])
                    kb.emit("act", lambda e: e.activation(out=ost[:, 1, :], in_=ost[:, 0, :], func=AF.Ln, scale=1.0 / 128, bias=cst[:, 919:920]),
                            [B_ost, B_cst], [B_ost])
                    kb.emit("act", lambda e: e.activation(out=ost[:, 2, :], in_=ost[:, 1, :], func=AF.Exp, scale=-0.5), [B_ost], [B_ost])
                    kb.emit("dve", lambda e: e.tensor_tensor(out=osum[:], in0=osum[:], in1=ost[:, 2, :].unsqueeze(2).to_broadcast([128, 4, 128]),
                                                             op=ALU.mult), [B_osum, B_ost], [B_osum])
                    kb.emit("dve", lambda e: e.tensor_tensor(out=osum[:], in0=osum[:], in1=hgn[:].unsqueeze(1).to_broadcast([128, 4, 128]),
                                                             op=ALU.mult), [B_osum, B_hgn], [B_osum])
                    kb.emit("dve", lambda e: e.tensor_tensor(out=recb_[:], in0=osum[:].rearrange("p h v -> p (h v)"), in1=gg[s3][:], op=ALU.mult),
                            [B_osum, B_gg[s3]], [B_rec2])
                    yield
                    for h in range(4):
                        kb.emit("pe", lambda e, h=h: e.transpose(out=ptr[:, h, :], in_=recb_[:, h * 128:(h + 1) * 128], identity=identb[:]),
                                [B_rec2, B_identb], [B_ptr], inc=(h == 3))
                    kb.emit("act", lambda e: e.copy(out=recT[:, :, i * 128:(i + 1) * 128], in_=ptr[:, 0:4, :]), [B_ptr], [B_recT])
                    yield

                def run_interleaved(gens):
                    gens = [g for g in gens if g is not None]
                    while gens:
                        for g in list(gens):
                            try:
                                next(g)
                            except StopIteration:
                                gens.remove(g)

                for d in range(2):
                    kb.emit("pool", lambda e: e.memset(Sst[:], 0.0), [], B_S)
                    kb.emit("pool", lambda e: e.memset(Sbf[:], 0.0), [], B_Sb)
                    order = list(range(NT)) if d == 0 else list(range(NT - 1, -1, -1))
                    run_interleaved([stageA1(d, order[0], 0)])
                    run_interleaved([stageA1(d, order[1], 1), stageA(d, order[0], 0)])
                    for n in range(NT):
                        g1 = stageA1(d, order[n + 2], n + 2) if n + 2 < NT else None
                        g2 = stageA(d, order[n + 1], n + 1) if n + 1 < NT else None
                        run_interleaved([g1, g2, stageB(d, order[n], n)])
                kb.barrier()
                kb.flush()
            if dbg and "recT" in dbg:
                with ExitStack() as pd:
                    tf = sb("recTf", [128, 4, S], F32, pd)
                    B_tf = kb.buf()
                    kb.emit("dve", lambda e: e.tensor_copy(out=tf[:], in_=recT[:]), [B_recT], [B_tf])
                    dbg_store("recT", tf[:].rearrange("p k t -> p (k t)"), [B_tf])
                    kb.barrier(); kb.flush()
            if stop in ("hgrn", "hgrn1"):
                break


            with ExitStack() as po_:
                woa = sb("woa", [64, 8, D], BF16, po_)
                wor = sb("wor", [128, 4, D], BF16, po_)
                B_wo = kb.buf()
                with ExitStack() as pst_:
                    stg = sb("wostg", [128, 4, D], F32, pst_)
                    B_stg = kb.buf()
                    g1b = gatebc[:, s, 0, :].unsqueeze(1).to_broadcast([128, 4, D])
                    for part in range(3):
                        if part < 2:
                            kb.dma("sp", lambda e, part=part: e.dma_start(out=stg[0:64], in_=woa_d[:, part * 4:(part + 1) * 4, :]),
                                   "ld_wostg", writes=[B_stg])
                            kb.emit("dve", lambda e, part=part: e.tensor_tensor(out=woa[:, part * 4:(part + 1) * 4, :], in0=stg[0:64],
                                                                              in1=gatebc[0:64, s, 0, :].unsqueeze(1).to_broadcast([64, 4, D]),
                                                                              op=ALU.mult), [B_stg, B_gatebc], [B_wo])
                        else:
                            kb.dma("sp", lambda e: e.dma_start(out=stg[:], in_=wor_d[:]), "ld_wostg", writes=[B_stg])
                            kb.emit("dve", lambda e: e.tensor_tensor(out=wor[:], in0=stg[:], in1=g1b, op=ALU.mult),
                                    [B_stg, B_gatebc], [B_wo])
                    kb.barrier()
                    kb.flush()
                xt0 = sb("xto", [128, D], F32, po_)
                B_xt0 = kb.buf()
                x1t = [sb("x1t%d" % i, [128, D], F32, po_) for i in range(2)]
                B_x1t = [kb.buf(), kb.buf()]
                h2t = [sb("h2t%d" % i, [128, 8, 128], BF16, po_) for i in range(2)]
                B_h2t = [kb.buf(), kb.buf()]
                m2bc = sb("m2bc", [128, 2, D], F32, po_)
                B_m2bc = kb.buf()
                kb.dma("sp", lambda e: e.dma_start(out=m2bc[:].rearrange("p a d -> p (a d)"), in_=mod2_d[s:s + 1, :].partition_broadcast(128)),
                       "ld_m2bc", reads=[B_mod2d], writes=[B_m2bc])
                h2k = [sb("h2k%d" % i, [128, D], BF16, po_) for i in range(2)]
                h2kf = sb("h2kf", [128, D], F32, po_)
                B_h2k = [kb.buf(), kb.buf()]
                B_h2kf = kb.buf()
                nrm = NormT(po_, "o", ntp=2)
                pmx = [ps("pmx%d" % i, [128, D], F32, po_) for i in range(2)]
                B_pmx = [kb.pbuf(), kb.pbuf()]
                plg = ps("plg", [128, 64], F32, po_)
                B_plg = kb.pbuf()
                lga = sb("lga", [128, NT, 36], F32, po_)
                B_lga = kb.buf()
                xt1 = sb("xto1", [128, D], F32, po_)
                xts = [xt0, xt1]
                B_xts = [B_xt0, kb.buf()]

                def op_S1(i):
                    j = i % 2
                    tsl = slice(i * 128, (i + 1) * 128)
                    for hh in range(2):
                        for h in range(8):
                            kb.emit("pe", lambda e, j=j, h=h, hh=hh, tsl=tsl: e.matmul(
                                pmx[j][:, hh * 512:(hh + 1) * 512], lhsT=OT[0:64, h, tsl], rhs=woa[0:64, h, hh * 512:(hh + 1) * 512],
                                start=(h == 0), stop=False), [B_OT, B_wo], [B_pmx[j]], inc=False)
                        for c in range(4):
                            kb.emit("pe", lambda e, j=j, c=c, hh=hh, tsl=tsl: e.matmul(
                                pmx[j][:, hh * 512:(hh + 1) * 512], lhsT=recT[:, c, tsl], rhs=wor[:, c, hh * 512:(hh + 1) * 512],
                                start=False, stop=(c == 3)), [B_recT, B_wo], [B_pmx[j]], inc=(c == 3 and hh == 1))
                    kb.dma("sp", lambda e, i=i, j=j: e.dma_start(out=xts[j][:], in_=x_d[s, i * 128:(i + 1) * 128, :]), "ld_xto%d" % j, writes=[B_xts[j]])
                    kb.emit("dve", lambda e, j=j: e.tensor_tensor(out=x1t[j][:], in0=pmx[j][:], in1=xts[j][:], op=ALU.add),
                            [B_pmx[j], B_xts[j]], [B_x1t[j]])
                    kb.dma("sp", lambda e, i=i, j=j: e.dma_start(out=x1_d[s, i * 128:(i + 1) * 128, :], in_=x1t[j][:]), "st_x1_%d" % j,
                           reads=[B_x1t[j]], writes=[B_x1d[s][i]])

                def op_S2(i):
                    j = i % 2
                    gi = s * NT + i
                    xn_, B_xn_ = nrm.run(x1t[j][:], B_x1t[j], s, 2, h2t[j][:], B_h2t[j])
                    kb.emit("pool", lambda e, xn_=xn_: e.tensor_tensor(out=h2kf[:], in0=xn_[:], in1=m2bc[:, 0, :], op=ALU.mult),
                            [B_xn_, B_m2bc], [B_h2kf])
                    kb.emit("pool", lambda e, j=j: e.tensor_tensor(out=h2k[j][:], in0=h2kf[:], in1=m2bc[:, 1, :], op=ALU.add),
                            [B_h2kf, B_m2bc], [B_h2k[j]])
                    kb.dma("sp", lambda e, gi=gi, j=j: e.dma_start(out=h2tok_d[gi * 128:(gi + 1) * 128, :], in_=h2k[j][:]), "st_h2_%d" % j,
                           reads=[B_h2k[j]], writes=[B_h2d[s][i]])
                    for kc in range(8):
                        kb.emit("pe", lambda e, j=j, kc=kc: e.matmul(plg[:, 0:36], lhsT=h2t[j][:, kc, :], rhs=wr[:, kc, :],
                                                                     start=(kc == 0), stop=(kc == 7)), [B_h2t[j], B_wr], [B_plg], inc=(kc == 7))
                    kb.emit("dve", lambda e, i=i: e.tensor_tensor(out=lga[:, i, :], in0=plg[:, 0:36], in1=brb[:], op=ALU.add), [B_plg, B_brb], [B_lga])

                op_S1(0)
                for i in range(NT):
                    if i + 1 < NT:
                        op_S1(i + 1)
                    op_S2(i)
                g0 = s * NT
                gsel = sb("gsel", [128, 3, NT, 4], F32, po_)
                tkb = sb("tkb", [128, 8, NT], F32, po_)
                elm = sb("elm", [128, 4, NT, 32], F32, po_)
                B_gsel, B_tkb, B_elm = kb.buf(), kb.buf(), kb.buf()
                gl = lga[:, :, 0:4]
                el = lga[:, :, 4:36]
                bc4 = lambda ap: ap.unsqueeze(2).to_broadcast([128, NT, 4])
                bc32 = lambda ap: ap.unsqueeze(2).to_broadcast([128, NT, 32])
                kb.emit("dve", lambda e: e.tensor_reduce(out=tkb[:, 0, :], in_=gl, axis=AX.X, op=ALU.max), [B_lga], [B_tkb])
                kb.emit("dve", lambda e: e.tensor_tensor(out=gsel[:, 0], in0=gl, in1=bc4(tkb[:, 0, :]), op=ALU.is_ge), [B_lga, B_tkb], [B_gsel])
                kb.emit("dve", lambda e: e.tensor_tensor(out=gsel[:, 2], in0=gl, in1=bc4(tkb[:, 0, :]), op=ALU.subtract), [B_lga, B_tkb], [B_gsel])
                kb.emit("act", lambda e: e.activation(out=gsel[:, 2], in_=gsel[:, 2], func=AF.Exp), [B_gsel], [B_gsel])
                kb.emit("dve", lambda e: e.tensor_reduce(out=tkb[:, 1, :], in_=gsel[:, 2], axis=AX.X, op=ALU.add), [B_gsel, B_tkb], [B_tkb])
                kb.emit("dve", lambda e: e.reciprocal(out=tkb[:, 2, :], in_=tkb[:, 1, :]), [B_tkb], [B_tkb])
                kb.emit("dve", lambda e: e.tensor_scalar(out=gsel[:, 1], in0=gsel[:, 0], scalar1=-1.0, scalar2=BIG, op0=ALU.add, op1=ALU.mult),
                        [B_gsel], [B_gsel])
                kb.emit("dve", lambda e: e.tensor_tensor(out=elm[:, 0].rearrange("p t (g e) -> p t g e", g=4),
                                                         in0=lga[:, :, 4:36].rearrange("p t (g e) -> p t g e", g=4),
                                                         in1=gsel[:, 1].unsqueeze(3).to_broadcast([128, NT, 4, 8]), op=ALU.add),
                        [B_lga, B_gsel], [B_elm])
                kb.emit("dve", lambda e: e.tensor_reduce(out=tkb[:, 3, :], in_=elm[:, 0], axis=AX.X, op=ALU.max), [B_elm, B_tkb], [B_tkb])
                kb.emit("dve", lambda e: e.tensor_tensor(out=elm[:, 1], in0=elm[:, 0], in1=bc32(tkb[:, 3, :]), op=ALU.is_ge), [B_elm, B_tkb], [B_elm])
                kb.emit("dve", lambda e: e.scalar_tensor_tensor(out=elm[:, 2], in0=elm[:, 1], scalar=-BIG, in1=elm[:, 0], op0=ALU.mult, op1=ALU.add),
                        [B_elm], [B_elm])
                kb.emit("dve", lambda e: e.tensor_reduce(out=tkb[:, 4, :], in_=elm[:, 2], axis=AX.X, op=ALU.max), [B_elm, B_tkb], [B_tkb])
                kb.emit("dve", lambda e: e.tensor_tensor(out=elm[:, 3], in0=elm[:, 2], in1=bc32(tkb[:, 4, :]), op=ALU.is_ge), [B_elm, B_tkb], [B_elm])
                kb.emit("dve", lambda e: e.tensor_tensor(out=tkb[:, 5, :], in0=tkb[:, 4, :], in1=tkb[:, 3, :], op=ALU.subtract), [B_tkb], [B_tkb])
                kb.emit("act", lambda e: e.activation(out=tkb[:, 5, :], in_=tkb[:, 5, :], func=AF.Exp), [B_tkb], [B_tkb])
                kb.emit("dve", lambda e: e.tensor_scalar(out=tkb[:, 6, :], in0=tkb[:, 5, :], scalar1=1.0, scalar2=None, op0=ALU.add), [B_tkb], [B_tkb])
                kb.emit("dve", lambda e: e.reciprocal(out=tkb[:, 6, :], in_=tkb[:, 6, :]), [B_tkb], [B_tkb])
                kb.emit("dve", lambda e: e.tensor_tensor(out=WK[:, g0:g0 + NT, 0], in0=tkb[:, 6, :], in1=tkb[:, 2, :], op=ALU.mult), [B_tkb], B_EW[g0:g0 + NT])
                kb.emit("dve", lambda e: e.tensor_tensor(out=WK[:, g0:g0 + NT, 1], in0=WK[:, g0:g0 + NT, 0], in1=tkb[:, 5, :], op=ALU.mult),
                        [B_tkb] + B_EW[g0:g0 + NT], B_EW[g0:g0 + NT])
                for k2 in range(2):
                    kb.emit("dve", lambda e, k2=k2: e.tensor_tensor(out=elm[:, 0], in0=elm[:, 1 + 2 * k2], in1=iota_e[:].unsqueeze(1).to_broadcast([128, NT, 32]),
                                                                    op=ALU.mult), [B_elm, B_iota], [B_elm])
                    kb.emit("dve", lambda e, k2=k2: e.tensor_reduce(out=EIDX[:, g0:g0 + NT, k2], in_=elm[:, 0], axis=AX.X, op=ALU.add),
                            [B_elm] + B_EW[g0:g0 + NT], B_EW[g0:g0 + NT])
                kb.barrier()
                kb.flush()
            if stop == "mix":
                break

    if stop == "mix":
        if dbg and "EIDX" in dbg:
            dbg_store("EIDX", EIDX[:].rearrange("p i e -> p (i e)"), B_EW)
            dbg_store("WK", WK[:].rearrange("p i e -> p (i e)"), B_EW)
            kb.barrier()
            kb.flush()

    issue_cv(1000)
    if stop is None or stop == "moe":
        NG = nseq_run * NT
        with ExitStack() as pe_:
            cstm = sb("cstm", [128, 192], F32, pe_)
            crow = sb("crow", [1, 3200], F32, pe_)
            B_cstm, B_crow = kb.buf(), kb.buf()
            kb.dma("sp", lambda e: e.dma_start(out=cstm[:], in_=cstm_d[:]), "ld_cstm", writes=[B_cstm])
            kb.dma("sp", lambda e: e.dma_start(out=crow[:], in_=crow_d[:]), "ld_crow", writes=[B_crow])
            sltb = sb("sltb", [128, 128], BF16, pe_)
            B_sltb = kb.buf()
            kb.emit("dve", lambda e: e.tensor_copy(out=sltb[:], in_=cstm[:, 0:128]), [B_cstm], [B_sltb])
            NGT = NSEQ * NT
            Mb = sb("Mb", [128, NGT, 32], BF16, pe_)
            CS = sb("CS", [128, NGT + 1, 32], F32, pe_)
            RK = sb("RK", [128, NGT, 32], F32, pe_)
            oh = sb("oh", [128, NGT, 2, 32], F32, pe_)
            ohr = sb("ohr", [128, NGT, 32], F32, pe_)
            B_Mb, B_CS, B_RK, B_oh, B_ohr = (kb.buf() for _ in range(5))
            SLOTF = sb("SLOTF", [128, NGT, 2], F32, pe_)
            SLOT = sb("SLOT", [128, NGT, 2], I32, pe_)
            B_slotf, B_slot = kb.buf(), kb.buf()
            IDXW = sb("IDXW", [128, NBB, 2], I32, pe_)
            B_idxw = kb.buf()
            with ExitStack() as pr_:
                pcs = ps("pcs", [128, 1024], F32, pr_)
                B_pcs = kb.pbuf()
                prk = ps("prk", [128, 1024], F32, pr_)
                B_prk = kb.pbuf()
                pbcr = ps("pbcr", [128, 96], F32, pr_)
                B_pbcr = kb.pbuf()
                kb.emit("dve", lambda e: e.tensor_tensor(out=oh[:].rearrange("p g k e -> p (g k) e"),
                                                         in0=iota_e[:].unsqueeze(1).to_broadcast([128, NGT * 2, 32]),
                                                         in1=EIDX[:].rearrange("p g k -> p (g k)").unsqueeze(2).to_broadcast([128, NGT * 2, 32]),
                                                         op=ALU.is_equal), [B_iota] + B_EW, [B_oh])
                kb.emit("dve", lambda e: e.tensor_tensor(out=Mb[:], in0=oh[:, :, 0, :], in1=oh[:, :, 1, :], op=ALU.add), [B_oh], [B_Mb])
                Mbf = Mb[:].rearrange("p g e -> p (g e)")
                for hh in range(2):
                    kb.emit("pe", lambda e, hh=hh: e.matmul(pcs[:, hh * 512:(hh + 1) * 512], lhsT=onesb[:], rhs=Mbf[:, hh * 512:(hh + 1) * 512], start=True, stop=True),
                            [B_onesb, B_Mb], [B_pcs], inc=(hh == 1))
                for hh in range(2):
                    kb.emit("pe", lambda e, hh=hh: e.matmul(prk[:, hh * 512:(hh + 1) * 512], lhsT=sltb[:], rhs=Mbf[:, hh * 512:(hh + 1) * 512], start=True, stop=True),
                            [B_sltb, B_Mb], [B_prk], inc=(hh == 1))
                kb.emit("pool", lambda e: e.memset(CS[:, 0, :], 0.0), [], [B_CS])
                for g in range(NGT):
                    kb.emit("dve", lambda e, g=g: e.tensor_tensor(out=CS[:, g + 1, :], in0=CS[:, g, :], in1=pcs[:, g * 32:(g + 1) * 32], op=ALU.add),
                            [B_CS, B_pcs], [B_CS])
                kb.emit("dve", lambda e: e.tensor_tensor(out=RK[:].rearrange("p g e -> p (g e)"), in0=prk[:], in1=CS[:, 0:NGT, :].rearrange("p g e -> p (g e)"), op=ALU.add),
                        [B_prk, B_CS], [B_RK])
                rw = sb("rw", [1, 8, 64], F32, pr_)
                g1 = sb("g1", [1, 2048], F32, pr_)
                B_rw, B_g1 = kb.buf(), kb.buf()
                kb.emit("pool", lambda e: e.memset(rw[:], 0.0), [], [B_rw])
                kb.emit("dve", lambda e: e.tensor_copy(out=rw[:, 0, 0:32], in_=CS[0:1, NGT, :]), [B_CS, B_rw], [B_rw])
                kb.emit("dve", lambda e: e.tensor_tensor(out=g1[:, 0:512].rearrange("p (a b) -> p a b", a=32),
                                                         in0=rw[:, 0, 0:32].unsqueeze(2).to_broadcast([1, 32, 16]),
                                                         in1=crow[:, 0:16].unsqueeze(1).to_broadcast([1, 32, 16]), op=ALU.is_gt),
                        [B_rw, B_crow], [B_g1])
                kb.emit("dve", lambda e: e.tensor_reduce(out=rw[:, 1, 0:32], in_=g1[:, 0:512].rearrange("p (a b) -> p a b", a=32), axis=AX.X, op=ALU.add),
                        [B_g1, B_rw], [B_rw])
                kb.emit("dve", lambda e: e.tensor_tensor(out=g1[:, 0:1024].rearrange("p (a b) -> p a b", a=32),
                                                         in0=rw[:, 1, 0:32].unsqueeze(1).to_broadcast([1, 32, 32]),
                                                         in1=crow[:, 16:1040].rearrange("p (a b) -> p a b", a=32), op=ALU.mult),
                        [B_rw, B_crow], [B_g1])
                kb.emit("dve", lambda e: e.tensor_reduce(out=rw[:, 2, 0:32], in_=g1[:, 0:1024].rearrange("p (a b) -> p a b", a=32), axis=AX.X, op=ALU.add),
                        [B_g1, B_rw], [B_rw])
                kb.emit("dve", lambda e: e.tensor_tensor(out=rw[:, 3, 0:32], in0=rw[:, 2, 0:32], in1=rw[:, 1, 0:32], op=ALU.subtract), [B_rw], [B_rw])
                kb.emit("dve", lambda e: e.tensor_scalar(out=rw[:, 3, 0:32], in0=rw[:, 3, 0:32], scalar1=256.0, scalar2=None, op0=ALU.mult), [B_rw], [B_rw])
                kb.emit("dve", lambda e: e.tensor_tensor(out=g1[:].rearrange("p (a b) -> p a b", a=64),
                                                         in0=rw[:, 2, 0:32].unsqueeze(1).to_broadcast([1, 64, 32]),
                                                         in1=crow[:, 1040:3088].rearrange("p (a b) -> p a b", a=64), op=ALU.is_le),
                        [B_rw, B_crow], [B_g1])
                kb.emit("dve", lambda e: e.tensor_reduce(out=rw[:, 4, :], in_=g1[:].rearrange("p (a b) -> p a b", a=64), axis=AX.X, op=ALU.add),
                        [B_g1, B_rw], [B_rw])
                kb.emit("dve", lambda e: e.tensor_scalar(out=rw[:, 4, :], in0=rw[:, 4, :], scalar1=31.0, scalar2=None, op0=ALU.min), [B_rw], [B_rw])
                kb.emit("dve", lambda e: e.tensor_tensor(out=rw[:, 5, 2:64], in0=rw[:, 4, 2:64], in1=rw[:, 4, 0:62], op=ALU.is_equal), [B_rw], [B_rw])
                kb.emit("dve", lambda e: e.tensor_scalar(out=rw[:, 5, :], in0=rw[:, 5, :], scalar1=5.0e8, scalar2=None, op0=ALU.mult), [B_rw], [B_rw])
                kb.emit("dve", lambda e: e.scalar_tensor_tensor(out=rw[:, 6, :], in0=rw[:, 4, :], scalar=128.0, in1=rw[:, 5, :], op0=ALU.mult, op1=ALU.add),
                        [B_rw], [B_rw])
                kb.emit("pe", lambda e: e.matmul(pbcr[:, 0:64], lhsT=ones_f[0:1, 0:128], rhs=rw[:, 6, :], start=True, stop=True), [B_cst, B_rw], [B_pbcr], inc=False)
                kb.emit("pe", lambda e: e.matmul(pbcr[:, 64:96], lhsT=ones_f[0:1, 0:128], rhs=rw[:, 3, 0:32], start=True, stop=True), [B_cst, B_rw], [B_pbcr])
                bcs = sb("bcs", [128, 96], F32, pr_)
                idf = sb("idf", [128, NBB, 2], F32, pr_)
                B_bcs, B_idf = kb.buf(), kb.buf()
                kb.emit("dve", lambda e: e.tensor_copy(out=bcs[:], in_=pbcr[:]), [B_pbcr], [B_bcs])
                kb.emit("dve", lambda e: e.tensor_scalar(out=idf[:, :, 0], in0=bcs[:, 0:64], scalar1=cstm[:, 160:161], scalar2=None, op0=ALU.add),
                        [B_bcs, B_cstm], [B_idf])
                kb.emit("dve", lambda e: e.tensor_scalar(out=idf[:, :, 1], in0=idf[:, :, 0], scalar1=1.0, scalar2=None, op0=ALU.add), [B_idf], [B_idf])
                kb.emit("dve", lambda e: e.tensor_copy(out=IDXW[:], in_=idf[:]), [B_idf], [B_idxw])
                kb.emit("dve", lambda e: e.tensor_tensor(out=RK[:], in0=RK[:], in1=bcs[:, 64:96].unsqueeze(1).to_broadcast([128, NGT, 32]), op=ALU.add),
                        [B_RK, B_bcs], [B_RK])
                for k2 in range(2):
                    kb.emit("dve", lambda e, k2=k2: e.tensor_tensor(out=ohr[:], in0=oh[:, :, k2, :], in1=RK[:], op=ALU.mult), [B_oh, B_RK], [B_ohr])
                    kb.emit("dve", lambda e, k2=k2: e.tensor_reduce(out=SLOTF[:, :, k2], in_=ohr[:], axis=AX.X, op=ALU.add), [B_ohr, B_slotf], [B_slotf])
                kb.emit("dve", lambda e: e.tensor_copy(out=SLOT[:], in_=SLOTF[:]), [B_slotf], [B_slot])
                if dbg and "SLOT" in dbg:
                    dbg_store("SLOT", SLOTF[:].rearrange("p i e -> p (i e)"), [B_slotf])
                    dbg_store("BE", rw[:].rearrange("p a b -> p (a b)"), [B_rw])
                kb.barrier()
                kb.flush()
            with nullcontext(pe_) as pd_:
                hk = [sb("hk%d" % i, [128, D], BF16, pd_) for i in range(2)]
                B_hk = [kb.buf(), kb.buf()]
                for gi in range(NG):
                    j = gi % 2
                    s_, i_ = gi // NT, gi % NT
                    kb.dma("sp", lambda e, gi=gi, j=j: e.dma_start(out=hk[j][:], in_=h2tok_d[gi * 128:(gi + 1) * 128, :]), "ld_hk%d" % j,
                           reads=[B_h2d[s_][i_]], writes=[B_hk[j]])
                    for k2 in range(2):
                        kb.dma("pool", lambda e, gi=gi, j=j, k2=k2: e.indirect_dma_start(
                            out=xs_d[:, :], out_offset=bass.IndirectOffsetOnAxis(ap=SLOT[:, gi, k2:k2 + 1], axis=0), in_=hk[j][:], in_offset=None),
                            "sc_xs%d" % j, reads=[B_hk[j], B_slot, B_xsz], writes=[B_xs2[j]])
            B_ys2 = [kb.buf("ys0"), kb.buf("ys1")]
            with nullcontext(pe_) as px_:
                wall = [sb("wall%d" % i, [128, 3 * 4096], BF16, px_) for i in range(2)]
                wge = [wall[i][:, 0:4096].rearrange("p (k n) -> p k n", k=8) for i in range(2)]
                wue = [wall[i][:, 4096:8192].rearrange("p (k n) -> p k n", k=8) for i in range(2)]
                wde = [wall[i][:, 8192:12288].rearrange("p (k n) -> p k n", k=4) for i in range(2)]
                B_we = [kb.buf(), kb.buf()]
                xb = [sb("xb%d" % i, [128, 2, D], BF16, px_) for i in range(2)]
                B_xb = [kb.buf(), kb.buf()]
                xsT = [sb("xsT%d" % i, [128, 8, 256], BF16, px_) for i in range(2)]
                B_xsT = [kb.buf(), kb.buf()]
                hid = [sb("hid%d" % i, [128, 4, 256], BF16, px_) for i in range(2)]
                B_hid = [kb.buf(), kb.buf()]
                sgt = [sb("sgt%d" % i, [128, 256], F32, px_) for i in range(2)]
                B_sgt = [kb.buf(), kb.buf()]
                ysb = [sb("ysb%d" % i, [128, D], F32, px_) for i in range(2)]
                B_ysb = [kb.buf(), kb.buf()]
                ptx = [ps("ptx%d" % i, [128, 8, 128], BF16, px_) for i in range(2)]
                B_ptx = [kb.pbuf(), kb.pbuf()]
                pgu = [ps("pgu%d" % i, [128, 256], F32, px_) for i in range(4)]
                B_pgu = [kb.pbuf() for _ in range(4)]
                py = ps("py", [128, D], F32, px_)
                B_py = kb.pbuf()
                nbb_run = (NBB - 1) if stop is None else 8
                cnt_g = 0
                cnt_t = 0
                cnt_y = 0
                for b in range(nbb_run):
                    wj = b % 2
                    kb.dma("pool", lambda e, b=b, wj=wj: e.indirect_dma_start(
                        out=wall[wj][:, :], out_offset=None, in_=wbf_d[:, :],
                        in_offset=bass.IndirectOffsetOnAxis(ap=IDXW[:, b, 0:1], axis=0),
                        bounds_check=kb.const_reg(e, NEXP * 128 - 1), oob_is_err=False), "ld_we%d" % wj, reads=[B_idxw, B_wbf], writes=[B_we[wj]])
                    xj = b % 2
                    for bb_ in ([0, 1] if b == 0 else [b + 1]):
                        if bb_ < nbb_run:
                            kb.dma("sp", lambda e, bb_=bb_: e.dma_start(out=xb[bb_ % 2][:], in_=xs_d[bb_ * 256:(bb_ + 1) * 256, :].rearrange("(t p) d -> p t d", p=128)),
                                   "ld_xb%d" % (bb_ % 2), reads=B_xs2, writes=[B_xb[bb_ % 2]])
                    for t2 in range(2):
                        tj = cnt_t % 2
                        cnt_t += 1
                        for kc in range(8):
                            kb.emit("pe", lambda e, xj=xj, t2=t2, kc=kc, tj=tj: e.transpose(out=ptx[tj][:, kc, :], in_=xb[xj][:, t2, kc * 128:(kc + 1) * 128],
                                                                                       identity=identb[:]), [B_xb[xj], B_identb], [B_ptx[tj]], inc=(kc == 7))
                        kb.emit("act", lambda e, xj=xj, t2=t2, tj=tj: e.copy(out=xsT[xj][:, :, t2 * 128:(t2 + 1) * 128], in_=ptx[tj][:]), [B_ptx[tj]], [B_xsT[xj]])
                    hj = b % 2
                    for m in range(4):
                        pg_i = (cnt_g % 2) * 2
                        cnt_g += 1
                        for kc in range(8):
                            kb.emit("pe", lambda e, wj=wj, m=m, kc=kc, xj=xj, pg_i=pg_i: e.matmul(
                                pgu[pg_i][:], lhsT=wge[wj][:, kc, m * 128:(m + 1) * 128], rhs=xsT[xj][:, kc, :],
                                start=(kc == 0), stop=(kc == 7)), [B_we[wj], B_xsT[xj]], [B_pgu[pg_i]], inc=(kc == 7))
                        for kc in range(8):
                            kb.emit("pe", lambda e, wj=wj, m=m, kc=kc, xj=xj, pg_i=pg_i: e.matmul(
                                pgu[pg_i + 1][:], lhsT=wue[wj][:, kc, m * 128:(m + 1) * 128], rhs=xsT[xj][:, kc, :],
                                start=(kc == 0), stop=(kc == 7)), [B_we[wj], B_xsT[xj]], [B_pgu[pg_i + 1]], inc=(kc == 7))
                        sj = m % 2
                        kb.emit("act", lambda e, pg_i=pg_i, sj=sj: e.activation(out=sgt[sj][:], in_=pgu[pg_i][:], func=AF.Silu), [B_pgu[pg_i]], [B_sgt[sj]])
                        kb.emit("dve", lambda e, pg_i=pg_i, sj=sj, hj=hj, m=m: e.tensor_tensor(out=hid[hj][:, m, :], in0=sgt[sj][:], in1=pgu[pg_i + 1][:], op=ALU.mult),
                                [B_sgt[sj], B_pgu[pg_i + 1]], [B_hid[hj]])
                    for t2 in range(2):
                        yj = cnt_y % 2
                        cnt_y += 1
                        for hh in range(2):
                            for m in range(4):
                                kb.emit("pe", lambda e, wj=wj, m=m, hh=hh, hj=hj, t2=t2: e.matmul(
                                    py[:, hh * 512:(hh + 1) * 512], lhsT=hid[hj][:, m, t2 * 128:(t2 + 1) * 128],
                                    rhs=wde[wj][:, m, hh * 512:(hh + 1) * 512], start=(m == 0), stop=(m == 3)),
                                    [B_hid[hj], B_we[wj]], [B_py], inc=(m == 3 and hh == 1))
                        kb.emit("act", lambda e, yj=yj: e.copy(out=ysb[yj][:], in_=py[:]), [B_py], [B_ysb[yj]])
                        kb.dma("sp", lambda e, b=b, t2=t2, yj=yj: e.dma_start(out=ys_d[b * 256 + t2 * 128:b * 256 + (t2 + 1) * 128, :], in_=ysb[yj][:]),
                               "st_ys%d" % yj, reads=[B_ysb[yj]], writes=[B_ys2[yj]])
            with nullcontext(pe_) as pc_:
                xr = [sb("xr%d" % i, [128, D], F32, pc_) for i in range(2)]
                yg = [sb("yg%d" % i, [128, 2, D], F32, pc_) for i in range(2)]
                B_xr = [kb.buf(), kb.buf()]
                B_yg = [kb.buf(), kb.buf()]
                for gi in range(NG):
                    j = gi % 2
                    s_, i_ = gi // NT, gi % NT
                    kb.dma("sp", lambda e, s_=s_, i_=i_, j=j: e.dma_start(out=xr[j][:], in_=x1_d[s_, i_ * 128:(i_ + 1) * 128, :]), "ld_xr%d" % j,
                           reads=[B_x1d[s_][i_]], writes=[B_xr[j]])
                    for k2 in range(2):
                        kb.dma("pool", lambda e, gi=gi, j=j, k2=k2: e.indirect_dma_start(
                            out=yg[j][:, k2, :], out_offset=None, in_=ys_d[:, :],
                            in_offset=bass.IndirectOffsetOnAxis(ap=SLOT[:, gi, k2:k2 + 1], axis=0)), "ld_yg%d" % j,
                            reads=B_ys2 + [B_slot], writes=[B_yg[j]])
                    kb.emit("dve", lambda e, gi=gi, j=j: e.tensor_scalar(out=yg[j][:, 0, :], in0=yg[j][:, 0, :], scalar1=WK[:, gi, 0:1], scalar2=None, op0=ALU.mult),
                            [B_yg[j], B_EW[gi]], [B_yg[j]])
                    kb.emit("dve", lambda e, gi=gi, j=j: e.scalar_tensor_tensor(out=yg[j][:, 0, :], in0=yg[j][:, 1, :], scalar=WK[:, gi, 1:2], in1=yg[j][:, 0, :],
                                                                              op0=ALU.mult, op1=ALU.add), [B_yg[j], B_EW[gi]], [B_yg[j]])
                    kb.emit("dve", lambda e, s_=s_, j=j: e.tensor_tensor(out=yg[j][:, 0, :], in0=yg[j][:, 0, :], in1=gatebc[:, s_, 1, :], op=ALU.mult),
                            [B_yg[j], B_gatebc], [B_yg[j]])
                    kb.emit("dve", lambda e, j=j: e.tensor_tensor(out=xr[j][:], in0=xr[j][:], in1=yg[j][:, 0, :], op=ALU.add), [B_xr[j], B_yg[j]], [B_xr[j]])
                    kb.dma("sp", lambda e, s_=s_, i_=i_, j=j: e.dma_start(out=out_d[s_, i_ * 128:(i_ + 1) * 128, :], in_=xr[j][:]), "st_out%d" % j,
                           reads=[B_xr[j]])
                kb.barrier()
                kb.flush()

    kb.barrier()
    kb.flush()
    kb.es.close()
    return nc


def _consts():
    c = np.zeros((128, 1024), np.float32)
    idx = np.arange(128)
    same = (idx[:, None] // 64) == (idx[None, :] // 64)
    triF = ((idx[:, None] <= idx[None, :]) & same).astype(np.float32)
    triB = triF.T.copy()
    c[:, 0:128] = triF
    c[:, 128:256] = triB
    c[:, 256:384] = triB - np.eye(128, dtype=np.float32)
    c[:, 384:512] = triF - np.eye(128, dtype=np.float32)
    c[:, 512] = (idx < 64)
    c[:, 513] = (idx >= 64)
    half = 16
    inv_freq = (10000.0 ** (-np.arange(half, dtype=np.float32) / half)).astype(np.float32)
    c[:, 514:530] = inv_freq[None, :]
    c[0, 530] = 1.0
    c[1, 531] = 1.0
    c[0, 532:660] = 1.0
    c[1, 660:788] = 1.0
    c[:, 788:916] = 1.0
    c[:, 916] = 1.0 / 384
    c[:, 917] = 1.0 / 256
    c[:, 918] = -np.pi
    c[:, 919] = 1e-6
    return c


def _cstm():
    c = np.zeros((128, 192), np.float32)
    idx = np.arange(128)
    c[:, 0:128] = (idx[:, None] < idx[None, :]).astype(np.float32)
    c[:, 128:160] = np.arange(32, dtype=np.float32)[None, :]
    c[:, 160] = idx
    return c


def _crow():
    r = np.zeros((1, 3200), np.float32)
    r[0, 0:16] = 256.0 * np.arange(16)
    e = np.arange(32)
    r[0, 16:1040] = (e[None, :] <= e[:, None]).astype(np.float32).reshape(-1)
    r[0, 1040:3088] = np.repeat(np.arange(64, dtype=np.float32), 32)
    return r


def _kc(w):
    K, N = w.shape
    return np.ascontiguousarray(w.reshape(K // 128, 128, N).transpose(1, 0, 2))


def make_in_maps(inp):
    f = lambda a: np.ascontiguousarray(np.asarray(a, dtype=np.float32))
    x = f(inp["x"]); c = f(inp["c"]); pos = np.asarray(inp["positions"]).astype(np.int32)
    shared = {
        "ada_w": _kc(f(inp["ada_w"])[0]),
        "ada_b": f(inp["ada_b"])[0][None, :],
        "g1": np.ascontiguousarray(f(inp["norm1_g"])[0].reshape(8, 128).T),
        "g2": np.ascontiguousarray(f(inp["norm2_g"])[0].reshape(8, 128).T),
        "w_in": _kc(f(inp["w_in"])[0]),
        "qa_g": np.ascontiguousarray(f(inp["mla_qa_g"])[0].reshape(3, 128).T),
        "wq_up": _kc(f(inp["mla_wq_up"])[0]),
        "kva_g": np.ascontiguousarray(f(inp["mla_kva_g"])[0].reshape(2, 128).T),
        "wkv_up": _kc(f(inp["mla_wkv_up"])[0]),
        "qn_g": f(inp["mla_qn_g"])[0][None, :],
        "kn_g": f(inp["mla_kn_g"])[0][None, :],
        "lb_logits": f(inp["hg_lb_logits"]).reshape(1, -1),
        "hg_norm_g": f(inp["hg_norm_g"])[0][None, :],
        "w_out_a": np.ascontiguousarray(f(inp["w_out"])[0][:512].reshape(8, 64, D).transpose(1, 0, 2)),
        "w_out_r": _kc(f(inp["w_out"])[0][512:]),
        "w_router": _kc(np.concatenate([f(inp["router_group_w"])[0], f(inp["router_expert_w"])[0]], axis=1)),
        "b_router": np.concatenate([f(inp["router_group_b"])[0], f(inp["router_expert_b"])[0]])[None, :],
        "w_gate": np.ascontiguousarray(f(inp["w_gate"])[0].reshape(NEXP, 8, 128, 512).transpose(0, 2, 1, 3)),
        "w_up": np.ascontiguousarray(f(inp["w_up"])[0].reshape(NEXP, 8, 128, 512).transpose(0, 2, 1, 3)),
        "w_down": np.ascontiguousarray(f(inp["w_down"])[0].reshape(NEXP, 4, 128, D).transpose(0, 2, 1, 3)),
        "ident_bf": np.eye(128, dtype=np.float32).astype(ml_dtypes.bfloat16),
        "consts_f": _consts(),
        "cstm": _cstm(),
        "crow": _crow(),
        "g2row": f(inp["norm2_g"])[0][None, :],
    }
    maps = []
    for i in range(NCORES):
        b0 = NSEQ * i
        m = dict(shared)
        m["x"] = np.ascontiguousarray(x[b0:b0 + NSEQ])
        p = pos[b0:b0 + NSEQ].reshape(NSEQ, NT, 128)
        m["pos"] = np.ascontiguousarray(p.transpose(2, 0, 1).reshape(128, NSEQ * NT))
        m["cT"] = np.ascontiguousarray(c[b0:b0 + NSEQ].reshape(NSEQ, 8, 128).transpose(2, 1, 0))
        maps.append(m)
    return maps


def kernel(**inputs):
    nc = build()
    in_maps = make_in_maps(inputs)
    res = run_bass_kernel_spmd(nc, in_maps, core_ids=list(range(NCORES)))
    outs = [np.asarray(r["out"]).reshape(NSEQ, S, D) for r in res.results]
    return np.concatenate(outs, axis=0).astype(np.float32)
```

```python
import numpy as np
import ml_dtypes
from contextlib import ExitStack, nullcontext
import concourse.bass as bass
import concourse.mybir as mybir
from concourse.bass_utils import run_bass_kernel_spmd

F32 = mybir.dt.float32
BF16 = mybir.dt.bfloat16
I32 = mybir.dt.int32
AF = mybir.ActivationFunctionType
ALU = mybir.AluOpType
AX = mybir.AxisListType

NCORES = 8
D = 1024
S = 2048
NT = S // 128
NSEQ = 2
EPS = 1e-6
INCOLS = 3232
NEXP = 32
BIG = 1.0e30


class Buf:
    __slots__ = ("name", "w", "r", "excl")

    def __init__(self, name, excl=False):
        self.name = name
        self.excl = excl
        self.w = None
        self.r = {}


class KB:
    ENG = ("pe", "act", "dve", "pool", "sp")

    def __init__(self, nc):
        self.nc = nc
        self.es = ExitStack()
        self.sems = {}
        self.cnt = {}
        self.waited = {e: {} for e in self.ENG}
        self.prog = {e: [] for e in self.ENG}
        for e in self.ENG:
            self.sems[e] = self.es.enter_context(nc.semaphore("s_" + e))
            self.cnt[e] = 0
        self.nbuf = 0
        self.ninst = 0
        self.nflush = 0
        self.regs = {}

    def buf(self, name=None):
        self.nbuf += 1
        return Buf(name or ("b%d" % self.nbuf))

    def pbuf(self, name=None):
        self.nbuf += 1
        return Buf(name or ("p%d" % self.nbuf), excl=True)

    def dsem(self, key):
        if key not in self.sems:
            self.sems[key] = self.es.enter_context(self.nc.semaphore("d_" + key))
            self.cnt[key] = 0
        return key

    def _waits(self, eng, reads, writes):
        need = {}

        def add(dep):
            if dep is None:
                return
            k, v, e2 = dep
            if need.get(k, 0) < v:
                need[k] = v

        for b in reads:
            add(b.w)
        strict = (eng == "pool")
        for b in writes:
            if b.w is not None and (b.w[2] != eng or strict):
                add(b.w)
            for k, (v, e2) in b.r.items():
                if e2 != eng or strict:
                    add((k, v, e2))
        out = []
        wd = self.waited[eng]
        for k, v in need.items():
            if wd.get(k, 0) < v:
                wd[k] = v
                out.append((k, v))
        return out

    def emit(self, eng, fn, reads=(), writes=(), inc=True):
        if any(b.excl for b in reads):
            writes = list(writes) + [b for b in reads if b.excl and b not in writes]
            reads = [b for b in reads if not b.excl]
        waits = self._waits(eng, reads, writes)
        val = self.cnt[eng] + 1
        if inc:
            self.cnt[eng] = val
        rec_w = (eng, val, eng)
        for b in reads:
            old = b.r.get(eng)
            if old is None or old[0] < val:
                b.r[eng] = (val, eng)
        for b in writes:
            b.w = rec_w
            b.r = {}
        self.prog[eng].append((waits, fn, eng if inc else None, 1))
        self.ninst += 1

    def dma(self, q, fn, key, reads=(), writes=()):
        self.dsem(key)
        waits = self._waits(q, reads, writes)
        val = self.cnt[key] + 16
        self.cnt[key] = val
        for b in reads:
            old = b.r.get(key)
            if old is None or old[0] < val:
                b.r[key] = (val, "dma")
        for b in writes:
            b.w = (key, val, "dma")
            b.r = {}
        self.prog[q].append((waits, fn, key, 16))
        self.ninst += 1

    def barrier(self):
        tgt = {k: v for k, v in self.cnt.items() if v > 0}
        for e in self.ENG:
            waits = []
            for k, v in tgt.items():
                if k == e:
                    continue
                if self.waited[e].get(k, 0) < v:
                    self.waited[e][k] = v
                    waits.append((k, v))
            if waits:
                self.prog[e].append((waits, None, None, 0))

    def flush(self):
        nc = self.nc
        progs = self.prog
        sems = self.sems

        def run(engname, eh):
            for waits, fn, inckey, incv in progs[engname]:
                for k, v in waits:
                    eh.wait_ge(sems[k], v)
                if fn is not None:
                    ins = fn(eh)
                    if inckey is not None:
                        ins.then_inc(sems[inckey], incv)

        with nc.Block() as block:
            @block.tensor
            def _(e):
                run("pe", e)

            @block.scalar
            def _(e):
                run("act", e)

            @block.vector
            def _(e):
                run("dve", e)

            @block.gpsimd
            def _(e):
                run("pool", e)

            @block.sync
            def _(e):
                run("sp", e)
        self.prog = {e: [] for e in self.ENG}
        self.nflush += 1

    def const_reg(self, e, val):
        key = (self.nflush, val)
        if key not in self.regs:
            self.regs[key] = e.to_reg(val)
        return self.regs[key]


def build(dbg=None, stop=None):
    nc = bass.Bass("TRN2", target_bir_lowering=False)
    kb = KB(nc)
    es = kb.es

    def din(name, shape, dt=F32):
        return nc.dram_tensor(name, list(shape), dt, kind="ExternalInput").ap()

    x_d = din("x", [NSEQ, S, D])
    pos_d = din("pos", [128, NSEQ * NT], I32)
    cT_d = din("cT", [128, 8, NSEQ])
    adaw_d = din("ada_w", [128, 8, 6 * D])
    adab_d = din("ada_b", [1, 6 * D])
    g1_d = din("g1", [128, 8])
    g2_d = din("g2", [128, 8])
    win_d = din("w_in", [128, 8, INCOLS])
    qag_d = din("qa_g", [128, 3])
    wq_d = din("wq_up", [128, 3, 768])
    kvag_d = din("kva_g", [128, 2])
    wkv_d = din("wkv_up", [128, 2, 1024])
    qng_d = din("qn_g", [1, 96])
    kng_d = din("kn_g", [1, 96])
    lbl_d = din("lb_logits", [1, 2 * 2 * 512])
    hgn_d = din("hg_norm_g", [1, 128])
    woa_d = din("w_out_a", [64, 8, D])
    wor_d = din("w_out_r", [128, 4, D])
    wr_d = din("w_router", [128, 8, 36])
    br_d = din("b_router", [1, 36])
    wg_d = din("w_gate", [NEXP, 128, 8, 512])
    wu_d = din("w_up", [NEXP, 128, 8, 512])
    wd_d = din("w_down", [NEXP, 128, 4, D])
    identb_d = din("ident_bf", [128, 128], BF16)
    consts_d = din("consts_f", [128, 1024])
    out_d = nc.dram_tensor("out", [NSEQ, S, D], F32, kind="ExternalOutput").ap()
    x1_d = nc.dram_tensor("x1_scratch", [NSEQ, S, D], F32, kind="Internal").ap()
    h2tok_d = nc.dram_tensor("h2tok_scratch", [NSEQ * S, D], BF16, kind="Internal").ap()
    mod2_d = nc.dram_tensor("mod2_scratch", [NSEQ, 2 * D], F32, kind="Internal").ap()
    NBB = 64
    xs_d = nc.dram_tensor("xs_scratch", [NBB * 256, D], BF16, kind="Internal").ap()
    ys_d = nc.dram_tensor("ys_scratch", [NBB * 256, D], F32, kind="Internal").ap()
    g2row_d = din("g2row", [1, D])
    wbf_d = nc.dram_tensor("wbf", [NEXP * 128, 3 * 4096], BF16, kind="Internal").ap()
    cstm_d = din("cstm", [128, 192])
    crow_d = din("crow", [1, 3200])
    dbg_d = {}
    if dbg:
        for k, shp in dbg.items():
            dbg_d[k] = nc.dram_tensor("dbg_" + k, list(shp), F32, kind="ExternalOutput").ap()

    uid = [0]

    def sb(name, shape, dt=F32, stack=es):
        uid[0] += 1
        return stack.enter_context(nc.sbuf_tensor("sb%d_%s" % (uid[0], name), list(shape), dt))

    def ps(name, shape, dt=F32, stack=es):
        uid[0] += 1
        return stack.enter_context(nc.psum_tensor("ps%d_%s" % (uid[0], name), list(shape), dt))

    identb = sb("identb", [128, 128], BF16)
    cst = sb("cst", [128, 1024])
    B_identb, B_cst = kb.buf("identb"), kb.buf("cst")
    kb.dma("sp", lambda e: e.dma_start(out=identb[:], in_=identb_d[:]), "ld_identb", writes=[B_identb])
    kb.dma("sp", lambda e: e.dma_start(out=cst[:], in_=consts_d[:]), "ld_cst", writes=[B_cst])
    ones_f = cst[:, 788:916]
    B_wbf = kb.buf("wbf")
    wsrc = [wg_d.rearrange("e p (h k) n -> e p h (k n)", h=2), wu_d.rearrange("e p (h k) n -> e p h (k n)", h=2),
            wd_d.rearrange("e p (h k) n -> e p h (k n)", h=2)]
    B_xsz = kb.buf("xsz")
    B_xs2 = [kb.buf("xs0"), kb.buf("xs1")]
    pending_cv = []
    if stop is None or stop == "moe":
        for ex in range(NEXP):
            for m_ in range(3):
                pending_cv.append((ex, m_))

    def issue_cv(n=1):
        for _ in range(n):
            if not pending_cv:
                return
            ex, m_ = pending_cv.pop(0)
            kb.dma("pool", lambda e, ex=ex, m_=m_: e.dma_start(
                out=wbf_d[ex * 128:(ex + 1) * 128, m_ * 4096:(m_ + 1) * 4096].rearrange("p (h c) -> p h c", h=2), in_=wsrc[m_][ex]),
                "cv_w", writes=[B_wbf])

    B_modrow = kb.buf("modrow")
    B_mod2d = kb.buf("mod2d")
    modcol = sb("modcol", [128, NSEQ, 4, 8])
    B_modcol = kb.buf("modcol")
    gatebc = sb("gatebc", [128, NSEQ, 2, D], BF16)
    B_gatebc = kb.buf("gatebc")

    with ExitStack() as p0:
        modrow = sb("modrow", [2, 6 * D], F32, p0)
        cT = sb("cT", [128, 8, NSEQ], F32, p0)
        cact = sb("cact", [128, 8, NSEQ], F32, p0)
        adab = sb("adab", [2, 6 * D], F32, p0)
        g12 = sb("g12", [128, 2, 8], F32, p0)
        B_cT, B_cact, B_adab, B_g12 = kb.buf(), kb.buf(), kb.buf(), kb.buf()
        kb.dma("sp", lambda e: e.dma_start(out=cT[:], in_=cT_d[:]), "ld_cT", writes=[B_cT])
        kb.dma("sp", lambda e: e.dma_start(out=adab[0:1, :], in_=adab_d[:]), "ld_adab", writes=[B_adab])
        kb.dma("sp", lambda e: e.dma_start(out=adab[1:2, :], in_=adab_d[:]), "ld_adab", writes=[B_adab])
        kb.dma("sp", lambda e: e.dma_start(out=g12[:, 0, :], in_=g1_d[:]), "ld_g12", writes=[B_g12])
        kb.dma("sp", lambda e: e.dma_start(out=g12[:, 1, :], in_=g2_d[:]), "ld_g12", writes=[B_g12])
        kb.emit("act", lambda e: e.activation(out=cact[:], in_=cT[:], func=AF.Silu), [B_cT], [B_cact])
        wbuf = [sb("adaw%d" % i, [128, 8, 512], F32, p0) for i in range(2)]
        B_wbuf = [kb.buf(), kb.buf()]
        pmod = [ps("pmod%d" % i, [2, 512], F32, p0) for i in range(2)]
        B_pmod = [kb.pbuf(), kb.pbuf()]
        for n in range(12):
            j = n % 2
            kb.dma("sp", lambda e, n=n, j=j: e.dma_start(out=wbuf[j][:], in_=adaw_d[:, :, n * 512:(n + 1) * 512]),
                   "ld_adaw%d" % j, writes=[B_wbuf[j]])
            for kc in range(8):
                kb.emit("pe", lambda e, j=j, kc=kc: e.matmul(pmod[j][:], lhsT=cact[:, kc, :], rhs=wbuf[j][:, kc, :],
                                                              start=(kc == 0), stop=(kc == 7)),
                        [B_cact, B_wbuf[j]], [B_pmod[j]], inc=(kc == 7))
            kb.emit("dve", lambda e, n=n, j=j: e.tensor_tensor(out=modrow[:, n * 512:(n + 1) * 512], in0=pmod[j][:],
                                                               in1=adab[:, n * 512:(n + 1) * 512], op=ALU.add),
                    [B_pmod[j], B_adab], [B_modrow])
        pcol = ps("pcol", [128, 64], F32, p0)
        B_pcol = kb.pbuf()
        col_src = [1, 0, 4, 3]
        for s in range(NSEQ):
            for j in range(4):
                for kc in range(8):
                    c0 = col_src[j] * D + kc * 128
                    idx = (s * 4 + j) * 8 + kc
                    kb.emit("pe", lambda e, s=s, c0=c0, idx=idx: e.matmul(
                        pcol[:, idx:idx + 1], lhsT=modrow[0:2, c0:c0 + 128], rhs=cst[0:2, 530 + s:531 + s],
                        start=True, stop=True), [B_modrow, B_cst], [B_pcol], inc=(j == 3 and kc == 7 and s == NSEQ - 1))
        kb.emit("dve", lambda e: e.tensor_copy(out=modcol[:].rearrange("p s j k -> p (s j k)"), in_=pcol[:]),
                [B_pcol], [B_modcol])
        for s in range(NSEQ):
            for jj, gi in ((0, 0), (2, 1)):
                kb.emit("dve", lambda e, s=s, jj=jj, gi=gi: e.scalar_tensor_tensor(
                    out=modcol[:, s, jj, :], in0=modcol[:, s, jj, :], scalar=1.0, in1=g12[:, gi, :],
                    op0=ALU.add, op1=ALU.mult), [B_modcol, B_g12], [B_modcol])
        pbc = [ps("pbc%d" % i, [128, 512], F32, p0) for i in range(2)]
        B_pbc = [kb.pbuf(), kb.pbuf()]
        t = 0
        for s in range(NSEQ):
            for g, base in ((0, 2 * D), (1, 5 * D)):
                for hh in range(2):
                    j = t % 2
                    t += 1
                    kb.emit("pe", lambda e, s=s, base=base, hh=hh, j=j: e.matmul(
                        pbc[j][:], lhsT=cst[0:2, 532 + s * 128:532 + (s + 1) * 128],
                        rhs=modrow[0:2, base + hh * 512:base + (hh + 1) * 512], start=True, stop=True),
                        [B_modrow, B_cst], [B_pbc[j]])
                    kb.emit("act", lambda e, s=s, g=g, hh=hh, j=j: e.copy(
                        out=gatebc[:, s, g, hh * 512:(hh + 1) * 512], in_=pbc[j][:]), [B_pbc[j]], [B_gatebc])
        if dbg and "modcol" in dbg:
            kb.dma("sp", lambda e: e.dma_start(out=dbg_d["modcol"][:], in_=modcol[:].rearrange("p s j k -> p (s j k)")),
                   "st_dbg", reads=[B_modcol])
        g2r = sb("g2r", [2, D], F32, p0)
        B_g2r = kb.buf()
        for r_ in range(2):
            kb.dma("sp", lambda e, r_=r_: e.dma_start(out=g2r[r_:r_ + 1, :], in_=g2row_d[:]), "ld_g2r", writes=[B_g2r])
        kb.emit("dve", lambda e: e.scalar_tensor_tensor(out=g2r[:], in0=modrow[:, 4 * D:5 * D], scalar=1.0, in1=g2r[:], op0=ALU.add, op1=ALU.mult),
                [B_modrow, B_g2r], [B_g2r])
        kb.dma("sp", lambda e: e.dma_start(out=mod2_d[:, 0:D], in_=g2r[:]), "st_mod2", reads=[B_g2r], writes=[B_mod2d])
        kb.dma("sp", lambda e: e.dma_start(out=mod2_d[:, D:2 * D], in_=modrow[:, 3 * D:4 * D]), "st_mod2", reads=[B_modrow], writes=[B_mod2d])
        kb.barrier()
        kb.flush()

    onesb = sb("onesb", [128, 128], BF16)
    B_onesb = kb.buf()
    kb.emit("pool", lambda e: e.memset(onesb[:], 1.0), [], [B_onesb])
    wq = sb("wq", [128, 3, 768], BF16)
    wkv = sb("wkv", [128, 2, 1024], BF16)
    gq_bc = sb("gq_bc", [128, 8, 96])
    gk_bc = sb("gk_bc", [128, 8, 96])
    cosT = sb("cosT", [128, NSEQ * NT, 16])
    sinT = sb("sinT", [128, NSEQ * NT, 16])
    B_wq, B_wkv, B_gq, B_gk, B_cs = (kb.buf() for _ in range(5))
    with ExitStack() as pw:
        wq_f = sb("wq_f", [128, 3, 768], F32, pw)
        wkv_f = sb("wkv_f", [128, 2, 1024], F32, pw)
        qag = sb("qag", [128, 3], F32, pw)
        kvag = sb("kvag", [128, 2], F32, pw)
        g96 = sb("g96", [128, 2, 96], F32, pw)
        posi = sb("posi", [128, NSEQ * NT], I32, pw)
        posf = sb("posf", [128, NSEQ * NT], F32, pw)
        ang = sb("ang", [128, NSEQ * NT, 16], F32, pw)
        ang2 = sb("ang2", [128, NSEQ * NT, 16], F32, pw)
        B_wqf, B_wkvf, B_qag, B_kvag, B_g96, B_posi, B_posf, B_ang, B_ang2 = (kb.buf() for _ in range(9))
        kb.dma("sp", lambda e: e.dma_start(out=wq_f[:], in_=wq_d[:]), "ld_wqf", writes=[B_wqf])
        kb.dma("sp", lambda e: e.dma_start(out=wkv_f[:], in_=wkv_d[:]), "ld_wkvf", writes=[B_wkvf])
        kb.dma("sp", lambda e: e.dma_start(out=qag[:], in_=qag_d[:]), "ld_qag", writes=[B_qag])
        kb.dma("sp", lambda e: e.dma_start(out=kvag[:], in_=kvag_d[:]), "ld_kvag", writes=[B_kvag])
        kb.dma("sp", lambda e: e.dma_start(out=g96[:, 0, :], in_=qng_d.partition_broadcast(128)), "ld_g96", writes=[B_g96])
        kb.dma("sp", lambda e: e.dma_start(out=g96[:, 1, :], in_=kng_d.partition_broadcast(128)), "ld_g96", writes=[B_g96])
        kb.dma("sp", lambda e: e.dma_start(out=posi[:], in_=pos_d[:]), "ld_pos", writes=[B_posi])
        for c in range(3):
            kb.emit("dve", lambda e, c=c: e.tensor_scalar_mul(out=wq[:, c, :], in0=wq_f[:, c, :], scalar1=qag[:, c:c + 1]),
                    [B_wqf, B_qag], [B_wq])
        for c in range(2):
            kb.emit("dve", lambda e, c=c: e.tensor_scalar_mul(out=wkv[:, c, :], in0=wkv_f[:, c, :], scalar1=kvag[:, c:c + 1]),
                    [B_wkvf, B_kvag], [B_wkv])
        kb.emit("dve", lambda e: e.tensor_scalar_mul(out=gq_bc[:], in0=g96[:, 0:1, :].to_broadcast([128, 8, 96]),
                                                     scalar1=float(96 ** -0.5)), [B_g96], [B_gq])
        kb.emit("dve", lambda e: e.tensor_copy(out=gk_bc[:], in_=g96[:, 1:2, :].to_broadcast([128, 8, 96])), [B_g96], [B_gk])
        kb.emit("dve", lambda e: e.tensor_copy(out=posf[:], in_=posi[:]), [B_posi], [B_posf])
        kb.emit("dve", lambda e: e.tensor_tensor(out=ang[:], in0=posf[:].unsqueeze(2).to_broadcast([128, NSEQ * NT, 16]),
                                                 in1=cst[:, 514:530].unsqueeze(1).to_broadcast([128, NSEQ * NT, 16]),
                                                 op=ALU.mult), [B_posf, B_cst], [B_ang])
        PI = float(np.pi)
        angi = sb("angi", [128, NSEQ * NT, 16], I32, pw)
        B_angi = kb.buf()

        def sin_table(dst, shift):
            kb.emit("dve", lambda e: e.tensor_scalar(out=ang2[:], in0=ang[:], scalar1=float(1.0 / (2 * PI)), scalar2=None, op0=ALU.mult),
                    [B_ang], [B_ang2])
            kb.emit("dve", lambda e: e.tensor_copy(out=angi[:], in_=ang2[:]), [B_ang2], [B_angi])
            kb.emit("dve", lambda e: e.tensor_copy(out=ang2[:], in_=angi[:]), [B_angi], [B_ang2])
            kb.emit("dve", lambda e: e.scalar_tensor_tensor(out=ang2[:], in0=ang2[:], scalar=-2 * PI, in1=ang[:], op0=ALU.mult, op1=ALU.add),
                    [B_ang2, B_ang], [B_ang2])
            if shift != 0.0:
                kb.emit("dve", lambda e: e.tensor_scalar(out=ang2[:], in0=ang2[:], scalar1=float(shift), scalar2=None, op0=ALU.add),
                        [B_ang2], [B_ang2])
            kb.emit("dve", lambda e: e.tensor_scalar(out=angi[:].bitcast(F32), in0=ang2[:], scalar1=PI, scalar2=-2 * PI, op0=ALU.is_gt, op1=ALU.mult),
                    [B_ang2], [B_angi])
            kb.emit("dve", lambda e: e.tensor_tensor(out=ang2[:], in0=ang2[:], in1=angi[:].bitcast(F32), op=ALU.add), [B_ang2, B_angi], [B_ang2])
            kb.emit("dve", lambda e: e.tensor_scalar(out=angi[:].bitcast(F32), in0=ang2[:], scalar1=-PI, scalar2=2 * PI, op0=ALU.is_lt, op1=ALU.mult),
                    [B_ang2], [B_angi])
            kb.emit("dve", lambda e: e.tensor_tensor(out=ang2[:], in0=ang2[:], in1=angi[:].bitcast(F32), op=ALU.add), [B_ang2, B_angi], [B_ang2])
            kb.emit("act", lambda e: e.activation(out=dst[:], in_=ang2[:], func=AF.Sin), [B_ang2], [B_cs])

        sin_table(sinT, 0.0)
        sin_table(cosT, PI / 2)
        kb.barrier()
        kb.flush()

    wr = sb("wr", [128, 8, 36], BF16)
    brb = sb("brb", [128, 36])
    EIDX = sb("EIDX", [128, NSEQ * NT, 2])
    WK = sb("WK", [128, NSEQ * NT, 2])
    iota_e = sb("iota_e", [128, 32])
    B_wr, B_brb, B_iota = kb.buf(), kb.buf(), kb.buf()
    B_EW = [kb.buf() for _ in range(NSEQ * NT)]
    kb.dma("sp", lambda e: e.dma_start(out=iota_e[:], in_=cstm_d[:, 128:160]), "ld_iota", writes=[B_iota])
    kb.dma("pool", lambda e: e.dma_start(out=wr[:], in_=wr_d[:]), "ld_wr", writes=[B_wr])
    kb.dma("sp", lambda e: e.dma_start(out=brb[:], in_=br_d.partition_broadcast(128)), "ld_brb", writes=[B_brb])
    B_x1d = [[kb.buf() for _ in range(NT)] for _ in range(NSEQ)]
    B_h2d = [[kb.buf() for _ in range(NT)] for _ in range(NSEQ)]

    def dbg_store(name, ap, bufs):
        if dbg and name in dbg:
            kb.dma("sp", lambda e: e.dma_start(out=dbg_d[name][:], in_=ap), "st_dbg", reads=bufs)

    class NormT:
        def __init__(self, stack, tag, ntp=2, dve_evac=False):
            self.dve_evac = dve_evac
            self.xn = [sb("xn%s%d" % (tag, i), [128, D], BF16, stack) for i in range(2)]
            self.st = sb("st" + tag, [128, 2, 4], F32, stack)
            tps = [ps("tp%s%d" % (tag, i), [128, 8, 128], BF16, stack) for i in range(ntp)]
            btp = [kb.pbuf() for _ in range(ntp)]
            self.tp = [tps[i % ntp] for i in range(2)]
            self.B_tp = [btp[i % ntp] for i in range(2)]
            self.B_xn = [kb.buf(), kb.buf()]
            self.B_st = [kb.buf(), kb.buf()]
            self.n = 0

        def run(self, src, B_src, s, jg, dst, B_dst):
            j = self.n % 2
            self.n += 1
            issue_cv(1)
            st, xn, tp = self.st, self.xn[j], self.tp[j]
            junk = xn
            B_st, B_xn, B_tp = self.B_st[j], self.B_xn[j], self.B_tp[j]
            kb.emit("act", lambda e: e.activation(out=junk[:], in_=src, func=AF.Square, accum_out=st[:, j, 0:1]),
                    [B_src], [B_xn, B_st])
            kb.emit("act", lambda e: e.activation(out=st[:, j, 1:2], in_=st[:, j, 0:1], func=AF.Ln, scale=1.0 / D, bias=cst[:, 919:920]),
                    [B_st, B_cst], [B_st])
            kb.emit("act", lambda e: e.activation(out=st[:, j, 2:3], in_=st[:, j, 1:2], func=AF.Exp, scale=-0.5), [B_st], [B_st])
            kb.emit("dve", lambda e: e.tensor_scalar_mul(out=xn[:], in0=src, scalar1=st[:, j, 2:3]), [B_src, B_st], [B_xn])
            for kc in range(8):
                kb.emit("pe", lambda e, kc=kc: e.transpose(out=tp[:, kc, :], in_=xn[:, kc * 128:(kc + 1) * 128], identity=identb[:]),
                        [B_xn, B_identb], [B_tp], inc=(kc == 7))
            if self.dve_evac:
                kb.emit("dve", lambda e: e.tensor_tensor(out=dst, in0=tp[:], in1=modcol[:, s, jg, :].unsqueeze(2).to_broadcast([128, 8, 128]), op=ALU.mult),
                        [B_tp, B_modcol], [B_dst])
                kb.emit("pool", lambda e: e.tensor_tensor(out=dst, in0=dst, in1=modcol[:, s, jg + 1, :].unsqueeze(2).to_broadcast([128, 8, 128]), op=ALU.add),
                        [B_dst, B_modcol], [B_dst])
            else:
                for kc in range(8):
                    kb.emit("act", lambda e, kc=kc: e.activation(out=dst[:, kc, :], in_=tp[:, kc, :], func=AF.Identity,
                                                                 bias=modcol[:, s, jg + 1, kc:kc + 1], scale=modcol[:, s, jg, kc:kc + 1]),
                            [B_tp, B_modcol], [B_dst])
            return xn, B_xn

    nseq_run = NSEQ if stop is None else 1
    for s in range(nseq_run):
        with ExitStack() as sq_:
            OT = sb("OT", [64, 8, S], BF16, sq_)
            B_OT = kb.buf()

            with ExitStack() as pm:
                QT = sb("QT", [128, 8, S if stop != "hT" else 4], BF16, pm)
                KT = sb("KT", [128, 8, S if stop != "hT" else 4], BF16, pm)
                V2 = sb("V2", [128, NT, 8, 65], BF16, pm)
                rstdk = sb("rstdk", [128, NT, 8], F32, pm)
                B_QT, B_KT, B_V2, B_rk = kb.buf(), kb.buf(), kb.buf(), kb.buf()
                kb.emit("pool", lambda e: e.memset(V2[:, :, :, 64:65], 1.0), [], [B_V2])
                with ExitStack() as pb:
                    winm = sb("winm", [128, 8, 672], BF16, pb)
                    B_winm = kb.buf()
                    kb.dma("pool", lambda e: e.dma_start(out=winm[:], in_=win_d[:, :, 0:672]), "ld_winm", writes=[B_winm])
                    xt = [sb("xt0", [128, D], F32, pb), sb("xt1", [128, D], F32, pb)]
                    B_xt = [kb.buf(), kb.buf()]
                    nrm = NormT(pb, "a", ntp=1)
                    hTc = sb("hTc", [128, 8, 512], BF16, pb)
                    B_hTc = [kb.buf() for _ in range(4)]
                    latT = sb("latT", [128, 5, 512], BF16, pb)
                    sqT = sb("sqT", [128, 5, 512], BF16, pb)
                    B_latT, B_sqT = kb.buf(), kb.buf()
                    plat = [ps("plat%d" % i, [128, 512], F32, pb) for i in range(2)]
                    B_plat = [kb.pbuf(), kb.pbuf()]
                    pq = ps("pq", [128, 1024], F32, pb)
                    B_pq = kb.pbuf()
                    pkv = ps("pkv", [128, 1024], F32, pb)
                    B_pkv = kb.pbuf()
                    psm = ps("psm", [128, 64], F32, pb)
                    B_pss = kb.pbuf()
                    B_pkr = B_pss
                    ptq = nrm.tp[0]
                    B_ptq = nrm.B_tp[0]
                    rst4 = sb("rst", [128, 4, 4], F32, pb)
                    B_rst = kb.buf()
                    hstk = sb("hstk", [128, 3, 8], F32, pb)
                    rpk = sb("rpk", [128, 4, 1, 16], F32, pb)
                    B_hstk, B_rpk = kb.buf(), kb.buf()
                    qf = sb("qf", [128, 8, 96], F32, pb)
                    qsq = sb("qsq", [128, 8, 96], F32, pb)
                    qn = sb("qn", [128, 8, 96], F32, pb)
                    hst = sb("hst", [128, 3, 8], F32, pb)
                    rp_q = sb("rp", [128, 4, 8, 16], F32, pb)
                    qfin = sb("qfin", [128, 8, 96], BF16, pb)
                    kvf = sb("kvf", [128, 8, 128], F32, pb)
                    ksq = sb("ksq", [128, 8, 64], F32, pb)
                    krf = sb("krf", [128, 3, 32], F32, pb)
                    kst = sb("kst", [128, 4], F32, pb)
                    kfin = sb("kfin", [128, 8, 96], BF16, pb)
                    B_qf, B_qsq, B_qn, B_hst, B_rp_q, B_qfin, B_kvf, B_ksq, B_krf, B_kst, B_kfin = (kb.buf() for _ in range(11))

                    def rope(src3, dst3, cs_i, nh, Bsrc, Bdst, rp=None, B_rp=None):
                        if rp is None:
                            rp, B_rp = rp_q, B_rp_q
                        cb = cosT[:, cs_i:cs_i + 1, :].to_broadcast([128, nh, 16])
                        sbb = sinT[:, cs_i:cs_i + 1, :].to_broadcast([128, nh, 16])
                        x1 = src3[:, :, 0:16]
                        x2 = src3[:, :, 16:32]
                        r = rp[:, :, 0:nh, :]
                        kb.emit("dve", lambda e: e.tensor_tensor(out=r[:, 0], in0=x1, in1=cb, op=ALU.mult), [Bsrc, B_cs], [B_rp])
                        kb.emit("dve", lambda e: e.tensor_tensor(out=r[:, 1], in0=x2, in1=sbb, op=ALU.mult), [Bsrc, B_cs], [B_rp])
                        kb.emit("dve", lambda e: e.tensor_tensor(out=r[:, 2], in0=x2, in1=cb, op=ALU.mult), [Bsrc, B_cs], [B_rp])
                        kb.emit("dve", lambda e: e.tensor_tensor(out=r[:, 3], in0=x1, in1=sbb, op=ALU.mult), [Bsrc, B_cs], [B_rp])
                        kb.emit("dve", lambda e: e.tensor_tensor(out=dst3[:, :, 0:16], in0=r[:, 0], in1=r[:, 1], op=ALU.subtract),
                                [B_rp], [Bdst])
                        kb.emit("dve", lambda e: e.tensor_tensor(out=dst3[:, :, 16:32], in0=r[:, 2], in1=r[:, 3], op=ALU.add),
                                [B_rp], [Bdst])

                    ncc = 4 if stop != "hT" else 1
                    def ld_x(i):
                        j = i % 2
                        kb.dma("sp", lambda e: e.dma_start(out=xt[j][:], in_=x_d[s, i * 128:(i + 1) * 128, :]), "ld_xt%d" % j, writes=[B_xt[j]])

                    ld_x(0)
                    ld_x(1)
                    for cc in range(ncc):
                        for ti in range(4):
                            i = cc * 4 + ti
                            j = i % 2
                            nrm.run(xt[j][:], B_xt[j], s, 0, hTc[:, :, ti * 128:(ti + 1) * 128], B_hTc[ti])
                            if i + 2 < 4 * ncc:
                                ld_x(i + 2)
                        if stop == "hT":
                            break
                        for jc in range(5):
                            pj = jc % 2
                            for kc in range(8):
                                kb.emit("pe", lambda e, pj=pj, jc=jc, kc=kc: e.matmul(
                                    plat[pj][:], lhsT=winm[:, kc, jc * 128:(jc + 1) * 128], rhs=hTc[:, kc, :],
                                    start=(kc == 0), stop=(kc == 7)), [B_winm] + B_hTc, [B_plat[pj]], inc=(kc == 7))
                            kb.emit("act", lambda e, pj=pj, jc=jc: e.copy(out=latT[:, jc, :], in_=plat[pj][:]), [B_plat[pj]], [B_latT])
                            kb.emit("act", lambda e, pj=pj, jc=jc: e.activation(out=sqT[:, jc, :], in_=plat[pj][:], func=AF.Square),
                                    [B_plat[pj]], [B_sqT])
                        for ti in range(4):
                            t0 = ti * 128
                            rst = rst4[:, ti, :]
                            for jc in range(5):
                                col = 0 if jc < 3 else 1
                                kb.emit("pe", lambda e, jc=jc, col=col, t0=t0: e.matmul(
                                    psm[:, col:col + 1], lhsT=sqT[:, jc, t0:t0 + 128], rhs=onesb[:, 0:1],
                                    start=(jc in (0, 3)), stop=(jc in (2, 4))), [B_sqT, B_onesb], [B_pss], inc=(jc == 4))
                            kb.emit("dve", lambda e, rst=rst: e.tensor_tensor(out=rst[:, 0:2], in0=psm[:, 0:2], in1=cst[:, 916:918], op=ALU.mult),
                                    [B_pss, B_cst], [B_rst])
                            kb.emit("act", lambda e, rst=rst: e.activation(out=rst[:, 2:4], in_=rst[:, 0:2], func=AF.Ln, bias=cst[:, 919:920]),
                                    [B_rst, B_cst], [B_rst])
                            kb.emit("act", lambda e, rst=rst: e.activation(out=rst[:, 2:4], in_=rst[:, 2:4], func=AF.Exp, scale=-0.5), [B_rst], [B_rst])

                        def qchain(ti):
                            i = cc * 4 + ti
                            gi = s * NT + i
                            t0 = ti * 128
                            rst = rst4[:, ti, :]
                            for c in range(3):
                                kb.emit("pe", lambda e, c=c: e.matmul(pq[:, 0:512], lhsT=latT[:, c, t0:t0 + 128], rhs=wq[:, c, 0:512],
                                                                      start=(c == 0), stop=(c == 2)), [B_latT, B_wq], [B_pq], inc=False)
                            for c in range(3):
                                kb.emit("pe", lambda e, c=c: e.matmul(pq[:, 512:768], lhsT=latT[:, c, t0:t0 + 128], rhs=wq[:, c, 512:768],
                                                                      start=(c == 0), stop=(c == 2)), [B_latT, B_wq], [B_pq], inc=(c == 2))
                            yield
                            qf2 = qf[:].rearrange("p h d -> p (h d)")
                            kb.emit("act", lambda e: e.activation(out=qf2, in_=pq[:, 0:768], func=AF.Copy, scale=rst[:, 2:3]), [B_pq, B_rst], [B_qf])
                            yield
                            kb.emit("act", lambda e: e.activation(out=qsq[:], in_=qf[:], func=AF.Square), [B_qf], [B_qsq])
                            yield
                            kb.emit("dve", lambda e: e.tensor_reduce(out=hst[:, 0, :], in_=qsq[:], axis=AX.X, op=ALU.add), [B_qsq], [B_hst])
                            yield
                            kb.emit("act", lambda e: e.activation(out=hst[:, 1, :], in_=hst[:, 0, :], func=AF.Ln, scale=1.0 / 96, bias=cst[:, 919:920]),
                                    [B_hst, B_cst], [B_hst])
                            kb.emit("act", lambda e: e.activation(out=hst[:, 2, :], in_=hst[:, 1, :], func=AF.Exp, scale=-0.5), [B_hst], [B_hst])
                            yield
                            kb.emit("dve", lambda e: e.tensor_tensor(out=qn[:], in0=qf[:], in1=hst[:, 2, :].unsqueeze(2).to_broadcast([128, 8, 96]),
                                                                     op=ALU.mult), [B_qf, B_hst], [B_qn])
                            yield
                            kb.emit("dve", lambda e: e.tensor_tensor(out=qn[:], in0=qn[:], in1=gq_bc[:], op=ALU.mult), [B_qn, B_gq], [B_qn])
                            yield
                            kb.emit("act", lambda e: e.copy(out=qfin[:, :, 0:64], in_=qn[:, :, 0:64]), [B_qn], [B_qfin])
                            rope(qn[:, :, 64:96], qfin[:, :, 64:96], gi, 8, B_qn, B_qfin)
                            yield
                            for h in range(8):
                                kb.emit("pe", lambda e, h=h: e.transpose(out=ptq[0:96, h, :], in_=qfin[:, h, :], identity=identb[:]),
                                        [B_qfin, B_identb], [B_ptq], inc=(h == 7))
                            kb.emit("act", lambda e: e.copy(out=QT[0:96, :, i * 128:(i + 1) * 128], in_=ptq[0:96, :, :]), [B_ptq], [B_QT])
                            yield

                        def kchain(ti):
                            i = cc * 4 + ti
                            gi = s * NT + i
                            t0 = ti * 128
                            rst = rst4[:, ti, :]
                            hst_ = hstk
                            for hh in range(2):
                                for c in range(2):
                                    kb.emit("pe", lambda e, c=c, hh=hh: e.matmul(
                                        pkv[:, hh * 512:(hh + 1) * 512], lhsT=latT[:, 3 + c, t0:t0 + 128], rhs=wkv[:, c, hh * 512:(hh + 1) * 512],
                                        start=(c == 0), stop=(c == 1)), [B_latT, B_wkv], [B_pkv], inc=(c == 1 and hh == 1))
                            for kc in range(8):
                                kb.emit("pe", lambda e, kc=kc: e.matmul(psm[:, 32:64], lhsT=hTc[:, kc, t0:t0 + 128], rhs=winm[:, kc, 640:672],
                                                                        start=(kc == 0), stop=(kc == 7)), [B_hTc[ti], B_winm], [B_pkr], inc=(kc == 7))
                            yield
                            kvf2 = kvf[:].rearrange("p h d -> p (h d)")
                            kb.emit("act", lambda e: e.activation(out=kvf2, in_=pkv[:], func=AF.Copy, scale=rst[:, 3:4]), [B_pkv, B_rst], [B_kvf])
                            yield
                            kb.emit("pool", lambda e: e.tensor_copy(out=V2[:, i, :, 0:64], in_=kvf[:, :, 64:128]), [B_kvf], [B_V2])
                            kb.emit("act", lambda e: e.copy(out=krf[:, 0, :], in_=psm[:, 32:64]), [B_pkr], [B_krf])
                            kb.emit("act", lambda e: e.activation(out=krf[:, 1, :], in_=krf[:, 0, :], func=AF.Square, accum_out=kst[:, 0:1]),
                                    [B_krf], [B_krf, B_kst])
                            yield
                            kb.emit("act", lambda e: e.activation(out=ksq[:], in_=kvf[:, :, 0:64], func=AF.Square), [B_kvf], [B_ksq])
                            yield
                            kb.emit("dve", lambda e: e.tensor_reduce(out=hst_[:, 0, :], in_=ksq[:], axis=AX.X, op=ALU.add), [B_ksq], [B_hstk])
                            kb.emit("dve", lambda e: e.tensor_scalar(out=hst_[:, 1, :], in0=hst_[:, 0, :], scalar1=kst[:, 0:1], scalar2=1.0 / 96,
                                                                     op0=ALU.add, op1=ALU.mult), [B_hstk, B_kst], [B_hstk])
                            yield
                            kb.emit("act", lambda e: e.activation(out=hst_[:, 2, :], in_=hst_[:, 1, :], func=AF.Ln, bias=cst[:, 919:920]),
                                    [B_hstk, B_cst], [B_hstk])
                            kb.emit("act", lambda e: e.activation(out=rstdk[:, i, :], in_=hst_[:, 2, :], func=AF.Exp, scale=-0.5), [B_hstk], [B_rk])
                            yield
                            kb.emit("dve", lambda e: e.tensor_tensor(out=kfin[:, :, 0:64], in0=kvf[:, :, 0:64], in1=gk_bc[:, :, 0:64], op=ALU.mult),
                                    [B_kvf, B_gk], [B_kfin])
                            yield
                            kb.emit("dve", lambda e: e.tensor_tensor(out=krf[:, 1, :], in0=krf[:, 0, :], in1=gk_bc[:, 0, 64:96], op=ALU.mult),
                                    [B_krf, B_gk], [B_krf])
                            rope(krf[:, 1:2, :], krf[:, 2:3, :], gi, 1, B_krf, B_krf, rpk, B_rpk)
                            yield
                            kb.emit("dve", lambda e: e.tensor_copy(out=kfin[:, :, 64:96], in_=krf[:, 2:3, :].to_broadcast([128, 8, 32])),
                                    [B_krf], [B_kfin])
                            for h in range(8):
                                kb.emit("pe", lambda e, h=h: e.transpose(out=ptq[0:96, h, :], in_=kfin[:, h, :], identity=identb[:]),
                                        [B_kfin, B_identb], [B_ptq], inc=(h == 7))
                            kb.emit("act", lambda e: e.copy(out=KT[0:96, :, i * 128:(i + 1) * 128], in_=ptq[0:96, :, :]), [B_ptq], [B_KT])
                            yield

                        def run_il(gens):
                            gens = list(gens)
                            while gens:
                                for g in list(gens):
                                    try:
                                        next(g)
                                    except StopIteration:
                                        gens.remove(g)

                        run_il([qchain(0)])
                        for ti in range(4):
                            gl_ = [kchain(ti)]
                            if ti + 1 < 4:
                                gl_.append(qchain(ti + 1))
                            run_il(gl_)
                    if dbg and "hT" in dbg:
                        hTf = sb("hTf", [128, 8, 512], F32, pb)
                        B_hTf = kb.buf()
                        kb.emit("dve", lambda e: e.tensor_copy(out=hTf[:], in_=hTc[:]), B_hTc, [B_hTf])
                        dbg_store("hT", hTf[:].rearrange("p k t -> p (k t)"), [B_hTf])
                    kb.barrier()
                    kb.flush()
                if stop == "hT":
                    break
                if dbg and "QT" in dbg:
                    with ExitStack() as pd:
                        tf = sb("QTf", [128, 8, 512], F32, pd)
                        B_tf = kb.buf()
                        for nm, src, Bs in (("QT", QT, B_QT), ("KT", KT, B_KT)):
                            dv = dbg_d[nm].rearrange("p (k t) -> p k t", k=8)
                            for cc in range(4):
                                kb.emit("dve", lambda e, src=src, cc=cc: e.tensor_copy(out=tf[0:96], in_=src[0:96, :, cc * 512:(cc + 1) * 512]), [Bs], [B_tf])
                                kb.dma("sp", lambda e, dv=dv, cc=cc: e.dma_start(out=dv[:, :, cc * 512:(cc + 1) * 512], in_=tf[0:96]), "st_dbg", reads=[B_tf])
                        dbg_store("rstdk", rstdk[:].rearrange("p i h -> p (i h)"), [B_rk])
                        kb.barrier(); kb.flush()
                if stop == "QK":
                    break

                with ExitStack() as pat:
                    pst = [ps("pst%d" % i, [128, 512], F32, pat) for i in range(4)]
                    B_pst = [kb.pbuf() for _ in range(4)]
                    po = [ps("po%d" % i, [128, 512], F32, pat) for i in range(2)]
                    B_po = [kb.pbuf() for _ in range(2)]
                    pbc2 = ps("pbc2", [64, 512], F32, pat)
                    B_pbc2 = kb.pbuf()
                    pT = [sb("pT%d" % i, [128, 512], BF16, pat) for i in range(4)]
                    B_pT = [kb.buf() for _ in range(4)]
                    rec = sb("rec", [128, 512], F32, pat)
                    B_rec = kb.buf()
                    recb = sb("recb", [64, 512], F32, pat)
                    B_recb = kb.buf()
                    if s == 0 and (stop is None or stop == "moe"):
                        zt = sb("zt", [128, 2048], BF16, pat)
                        B_zt = kb.buf()
                        kb.emit("pool", lambda e: e.memset(zt[:], 0.0), [], [B_zt])
                        xs_v = xs_d.rearrange("(c p r) d -> c p (r d)", p=128, r=2)
                        for c_ in range(xs_v.shape[0]):
                            kb.dma("sp", lambda e, c_=c_: e.dma_start(out=xs_v[c_], in_=zt[:]), "zf_xs", reads=[B_zt], writes=[B_xsz])
                    units = [(h, qc) for h in range(8) for qc in range(4)]
                    steps = [(u, kt) for u in range(len(units)) for kt in range(NT)]

                    def emit_S(n):
                        u, kt = steps[n]
                        h, qc = units[u]
                        j = n % 4
                        kb.emit("pe", lambda e: e.matmul(pst[j][:], lhsT=KT[0:96, h, kt * 128:(kt + 1) * 128],
                                                         rhs=QT[0:96, h, qc * 512:(qc + 1) * 512], start=True, stop=True),
                                [B_KT, B_QT], [B_pst[j]])
                        kb.emit("act", lambda e: e.activation(out=pT[j][:], in_=pst[j][:], func=AF.Exp, scale=rstdk[:, kt, h:h + 1]),
                                [B_pst[j], B_rk], [B_pT[j]])

                    def emit_PV(n):
                        u, kt = steps[n]
                        h, qc = units[u]
                        j = n % 4
                        a = u % 2
                        kb.emit("pe", lambda e: e.matmul(po[a][0:65, :], lhsT=V2[:, kt, h, :], rhs=pT[j][:], start=(kt == 0), stop=(kt == NT - 1)),
                                [B_V2, B_pT[j]], [B_po[a]], inc=(kt == NT - 1))
                        if kt == NT - 1:
                            kb.emit("dve", lambda e: e.reciprocal(out=rec[64:65, :], in_=po[a][64:65, :]), [B_po[a]], [B_rec])
                            kb.emit("pe", lambda e: e.matmul(pbc2[:], lhsT=ones_f[64:65, 0:64], rhs=rec[64:65, :], start=True, stop=True),
                                    [B_rec, B_cst], [B_pbc2])
                            kb.emit("act", lambda e: e.copy(out=recb[:], in_=pbc2[:]), [B_pbc2], [B_recb])
                            kb.emit("dve", lambda e: e.tensor_tensor(out=OT[0:64, h, qc * 512:(qc + 1) * 512], in0=po[a][0:64, :],
                                                                     in1=recb[:], op=ALU.mult), [B_po[a], B_recb], [B_OT])

                    LA = 3
                    for n in range(len(steps) + LA):
                        if n < len(steps):
                            emit_S(n)
                        if n >= LA:
                            emit_PV(n - LA)
                    kb.barrier()
                    kb.flush()
            if dbg and "OT" in dbg:
                with ExitStack() as pd:
                    tf = sb("OTf", [64, 8, S], F32, pd)
                    B_tf = kb.buf()
                    kb.emit("dve", lambda e: e.tensor_copy(out=tf[:], in_=OT[:]), [B_OT], [B_tf])
                    dbg_store("OT", tf[:].rearrange("p k t -> p (k t)"), [B_tf])
                    kb.barrier(); kb.flush()
            if stop == "attn":
                break


            recT = sb("recT", [128, 4, S], BF16, sq_)
            B_recT = kb.buf()
            with ExitStack() as ph:
                winh = sb("winh", [128, 8, 2560], BF16, ph)
                B_winh5 = [kb.buf() for _ in range(5)]
                for cb in (0, 3, 1, 4, 2):
                    kb.dma("pool", lambda e, cb=cb: e.dma_start(out=winh[:, :, cb * 512:(cb + 1) * 512],
                                                                in_=win_d[:, :, 672 + cb * 512:672 + (cb + 1) * 512]),
                           "ld_winh%d" % cb, writes=[B_winh5[cb]])
                ofw = sb("ofw", [128, NT, 512], BF16, ph)
                B_ofw = [kb.buf() for _ in range(NT)]
                lbc = sb("lbc", [128, 2, 512], F32, ph)
                oml = sb("oml", [128, 2, 512], F32, ph)
                hgn = sb("hgn", [128, 128], F32, ph)
                B_lb, B_hgn = kb.buf(), kb.buf()
                with ExitStack() as pl:
                    lraw = sb("lraw", [128, 2, 2, 512], F32, pl)
                    B_lraw = kb.buf()
                    kb.dma("sp", lambda e: e.dma_start(out=lraw[:].rearrange("p a b n -> p (a b n)"), in_=lbl_d.partition_broadcast(128)),
                           "ld_lraw", writes=[B_lraw])
                    kb.dma("sp", lambda e: e.dma_start(out=hgn[:], in_=hgn_d.partition_broadcast(128)), "ld_hgn", writes=[B_hgn])
                    kb.emit("dve", lambda e: e.tensor_tensor(out=lbc[:], in0=lraw[:, :, 0, :], in1=lraw[:, :, 1, :], op=ALU.subtract),
                            [B_lraw], [B_lb])
                    kb.emit("act", lambda e: e.activation(out=lbc[:], in_=lbc[:], func=AF.Sigmoid), [B_lb], [B_lb])
                    kb.emit("dve", lambda e: e.tensor_scalar(out=oml[:], in0=lbc[:], scalar1=-1.0, scalar2=1.0, op0=ALU.mult, op1=ALU.add),
                            [B_lb], [B_lb])
                    kb.barrier()
                    kb.flush()
                xt0 = sb("xth", [128, D], F32, ph)
                B_xt0 = kb.buf()
                nrm = NormT(ph, "h", ntp=1, dve_evac=True)
                hTt2 = [sb("hTt%d" % i, [128, 8, 128], BF16, ph) for i in range(2)]
                B_hTt2 = [kb.buf(), kb.buf()]
                pg = [ps("pg%d" % i, [128, 512], F32, ph) for i in range(2)]
                B_pg = [kb.pbuf() for _ in range(2)]
                pgn = [0]

                def next_pg():
                    j = pgn[0] % 2
                    pgn[0] += 1
                    return pg[j], B_pg[j]

                PA = ps("hPA", [128, 4, 128], F32, ph)
                PK = ps("hPK", [128, 4, 128], F32, ph)
                PI = ps("hPI", [128, 4, 128], F32, ph)
                PAo = ps("hPAo", [128, 4, 128], F32, ph)
                PBo = ps("hPBo", [128, 4, 128], F32, ph)
                B_PA, B_PK, B_PI, B_PAo, B_PBo = (kb.pbuf() for _ in range(5))
                ptr = nrm.tp[0]
                B_ptr = nrm.B_tp[0]
                qq2 = [sb("hq_q%d" % i, [128, 512], F32, ph) for i in range(2)]
                vv = [sb("hq_v%d" % i, [128, 512], BF16, ph) for i in range(3)]
                gg = [sb("hq_g%d" % i, [128, 512], BF16, ph) for i in range(3)]
                sg = sb("hq_sg", [128, 512], F32, ph)
                ff = sb("hq_f", [128, 512], F32, ph)
                kk = sb("hq_k", [128, 512], F32, ph)
                lf = sb("hq_lf", [128, 512], F32, ph)
                eb = sb("hq_eb", [128, 512], F32, ph)
                enb = sb("hq_enb", [128, 512], F32, ph)
                er = sb("hq_er", [128, 512], F32, ph)
                qkd = sb("hq_qkd", [128, 8, 128], BF16, ph)
                kend = [sb("hq_kend%d" % i, [128, 2, 512], BF16, ph) for i in range(2)]
                qkT = [sb("hq_qkT%d" % i, [128, 8, 128], BF16, ph) for i in range(2)]
                qAB = [sb("hq_qAB%d" % i, [128, 2, 4, 128], BF16, ph) for i in range(2)]
                dec = [sb("hq_dec%d" % i, [128, 4, 2], F32, ph) for i in range(2)]
                attm = sb("hq_attm", [128, 4, 128], BF16, ph)
                Sst = sb("hq_S", [128, 4, 128], F32, ph)
                Sbf = sb("hq_Sb", [128, 4, 128], BF16, ph)
                osum = sb("hq_osum", [128, 4, 128], F32, ph)
                osq = sb("hq_osq", [128, 4, 128], F32, ph)
                ost = sb("hq_ost", [128, 3, 4], F32, ph)
                recb_ = sb("hq_rec", [128, 512], BF16, ph)
                (B_sg, B_ff, B_kk, B_lf, B_eb, B_enb, B_er, B_qkd, B_attm, B_osum, B_osq, B_ost, B_rec2) = (kb.buf() for _ in range(13))
                B_qq2 = [kb.buf(), kb.buf()]
                B_vv = [kb.buf(), kb.buf(), kb.buf()]
                B_gg = [kb.buf(), kb.buf(), kb.buf()]
                B_kend = [kb.buf(), kb.buf()]
                B_qkT = [kb.buf(), kb.buf()]
                B_qAB = [kb.buf(), kb.buf()]
                B_dec = [kb.buf(), kb.buf()]
                B_S = [kb.buf() for _ in range(4)]
                B_Sb = [kb.buf() for _ in range(4)]
                for st_ in range(2):
                    kb.emit("pool", lambda e, st_=st_: e.memset(qAB[st_][:], 0.0), [], [B_qAB[st_]])

                def proj(cb, hTt, B_hTt):
                    p, B_p = next_pg()
                    for kc in range(8):
                        kb.emit("pe", lambda e, kc=kc: e.matmul(p[:], lhsT=hTt[:, kc, :], rhs=winh[:, kc, cb * 512:(cb + 1) * 512],
                                                                start=(kc == 0), stop=(kc == 7)), [B_hTt, B_winh5[cb]], [B_p], inc=(kc == 7))
                    return p, B_p

                def stageA1(d, i, n):
                    hTt, B_hTt = hTt2[n % 2], B_hTt2[n % 2]
                    qq, B_qq = qq2[n % 2], B_qq2[n % 2]
                    s3 = n % 3
                    kb.dma("sp", lambda e: e.dma_start(out=xt0[:], in_=x_d[s, i * 128:(i + 1) * 128, :]), "ld_xth", writes=[B_xt0])
                    nrm.run(xt0[:], B_xt0, s, 0, hTt[:], B_hTt)
                    yield
                    p, B_p = proj(0, hTt, B_hTt)
                    kb.emit("act", lambda e: e.activation(out=qq[:], in_=p[:], func=AF.Silu), [B_p], [B_qq])
                    yield
                    if d == 1:
                        p3, B_p3 = proj(4, hTt, B_hTt)
                        kb.emit("act", lambda e: e.activation(out=gg[s3][:], in_=p3[:], func=AF.Silu), [B_p3], [B_gg[s3]])
                        yield
                    p2, B_p2 = proj(3, hTt, B_hTt)
                    kb.emit("act", lambda e: e.copy(out=vv[s3][:], in_=p2[:]), [B_p2], [B_vv[s3]])
                    yield

                def stageA(d, i, n):
                    st = n % 2
                    hTt, B_hTt = hTt2[n % 2], B_hTt2[n % 2]
                    qq, B_qq = qq2[n % 2], B_qq2[n % 2]
                    tri_c = cst[:, 0:128] if d == 0 else cst[:, 128:256]
                    rev_c = cst[:, 256:384] if d == 0 else cst[:, 384:512]
                    p4, B_p4 = proj(1 + d, hTt, B_hTt)
                    kb.emit("act", lambda e: e.activation(out=sg[:], in_=p4[:], func=AF.Sigmoid), [B_p4], [B_sg])
                    kb.emit("dve", lambda e: e.tensor_tensor(out=ff[:], in0=sg[:], in1=oml[:, d, :], op=ALU.mult), [B_sg, B_lb], [B_ff])
                    kb.emit("dve", lambda e: e.tensor_tensor(out=ff[:], in0=ff[:], in1=lbc[:, d, :], op=ALU.add), [B_ff, B_lb], [B_ff])
                    kb.emit("dve", lambda e: e.tensor_scalar(out=kk[:], in0=ff[:], scalar1=-1.0, scalar2=1.0, op0=ALU.mult, op1=ALU.add),
                            [B_ff], [B_kk])
                    kb.emit("act", lambda e: e.activation(out=lf[:], in_=ff[:], func=AF.Ln), [B_ff], [B_lf])
                    yield
                    pb_, B_pb = next_pg()
                    kb.emit("pe", lambda e: e.matmul(pb_[:], lhsT=tri_c, rhs=lf[:], start=True, stop=True), [B_cst, B_lf], [B_pb])
                    kb.emit("act", lambda e: e.activation(out=eb[:], in_=pb_[:], func=AF.Exp), [B_pb], [B_eb])
                    kb.emit("act", lambda e: e.activation(out=enb[:], in_=pb_[:], func=AF.Exp, scale=-1.0), [B_pb], [B_enb])
                    yield
                    pr_, B_pr = next_pg()
                    kb.emit("pe", lambda e: e.matmul(pr_[:], lhsT=rev_c, rhs=lf[:], start=True, stop=True), [B_cst, B_lf], [B_pr])
                    kb.emit("act", lambda e: e.activation(out=er[:], in_=pr_[:], func=AF.Exp), [B_pr], [B_er])
                    yield
                    pd_, B_pd = next_pg()
                    for h in range(4):
                        kb.emit("pe", lambda e, h=h: e.matmul(pd_[:, 2 * h:2 * h + 2], lhsT=lf[:, h * 128:(h + 1) * 128],
                                                              rhs=cst[:, 512:514], start=True, stop=True), [B_lf, B_cst], [B_pd], inc=(h == 3))
                    kb.emit("act", lambda e: e.activation(out=dec[st][:].rearrange("p h c -> p (h c)"), in_=pd_[:, 0:8], func=AF.Exp),
                            [B_pd], [B_dec[st]])
                    kb.emit("dve", lambda e: e.tensor_tensor(out=qkd[:, 0:4, :].rearrange("p h k -> p (h k)"), in0=qq[:], in1=eb[:], op=ALU.mult),
                            [B_qq, B_eb], [B_qkd])
                    kb.emit("dve", lambda e: e.tensor_tensor(out=qkd[:, 4:8, :].rearrange("p h k -> p (h k)"), in0=kk[:], in1=enb[:], op=ALU.mult),
                            [B_kk, B_enb], [B_qkd])
                    for c2 in range(2):
                        kb.emit("dve", lambda e, c2=c2: e.scalar_tensor_tensor(out=kend[st][:, c2, :], in0=er[:], scalar=cst[:, 512 + c2:513 + c2],
                                                                             in1=kk[:], op0=ALU.mult, op1=ALU.mult),
                                [B_kk, B_er, B_cst], [B_kend[st]])
                    yield
                    for j8 in range(8):
                        kb.emit("pe", lambda e, j8=j8: e.transpose(out=ptr[:, j8, :], in_=qkd[:, j8, :], identity=identb[:]),
                                [B_qkd, B_identb], [B_ptr], inc=(j8 == 7))
                    kb.emit("act", lambda e: e.copy(out=qkT[st][:], in_=ptr[:]), [B_ptr], [B_qkT[st]])
                    kb.emit("dve", lambda e: e.tensor_copy(out=qAB[st][:, 0, :, 0:64], in_=ptr[:, 0:4, 0:64]), [B_ptr], [B_qAB[st]])
                    kb.emit("dve", lambda e: e.tensor_copy(out=qAB[st][:, 1, :, 64:128], in_=ptr[:, 0:4, 64:128]), [B_ptr], [B_qAB[st]])
                    yield

                def stageB(d, i, n):
                    st = n % 2
                    s3 = n % 3
                    tri_c = cst[:, 0:128] if d == 0 else cst[:, 128:256]
                    corder = [0, 1] if d == 0 else [1, 0]
                    for h in range(4):
                        kb.emit("pe", lambda e, h=h: e.matmul(PA[:, h, :], lhsT=qkT[st][:, 4 + h, :], rhs=qkT[st][:, h, :], start=True, stop=True),
                                [B_qkT[st]], [B_PA], inc=(h == 3))
                    yield
                    kb.emit("dve", lambda e: e.tensor_tensor(out=attm[:], in0=PA[:], in1=tri_c.unsqueeze(1).to_broadcast([128, 4, 128]), op=ALU.mult),
                            [B_PA, B_cst], [B_attm])
                    yield
                    for ci, cidx in enumerate(corder):
                        Po_, B_Po_ = (PAo, B_PAo) if ci == 0 else (PBo, B_PBo)
                        for h in range(4):
                            hs = slice(h * 128, (h + 1) * 128)
                            if ci == 0:
                                kb.emit("pe", lambda e, h=h, hs=hs: e.matmul(PI[:, h, :], lhsT=attm[:, h, :], rhs=vv[s3][:, hs], start=True, stop=True),
                                        [B_attm, B_vv[s3]], [B_PI], inc=False)
                            kb.emit("pe", lambda e, h=h, cidx=cidx, Po_=Po_: e.matmul(Po_[:, h, :], lhsT=qAB[st][:, cidx, h, :], rhs=Sbf[:, h, :],
                                                                                    start=True, stop=True), [B_qAB[st], B_Sb[h]], [B_Po_], inc=False)
                            kb.emit("pe", lambda e, h=h, cidx=cidx, hs=hs: e.matmul(PK[:, h, :], lhsT=kend[st][:, cidx, hs], rhs=vv[s3][:, hs],
                                                                                  start=True, stop=True), [B_kend[st], B_vv[s3]], [B_PK], inc=(h == 3))
                        yield
                        for h in range(4):
                            kb.emit("dve", lambda e, h=h, cidx=cidx: e.scalar_tensor_tensor(
                                out=Sst[:, h, :], in0=Sst[:, h, :], scalar=dec[st][:, h, cidx:cidx + 1], in1=PK[:, h, :], op0=ALU.mult, op1=ALU.add),
                                [B_S[h], B_dec[st], B_PK], [B_S[h]])
                            kb.emit("pool", lambda e, h=h: e.tensor_copy(out=Sbf[:, h, :], in_=Sst[:, h, :]), [B_S[h]], [B_Sb[h]])
                        yield
                    kb.emit("act", lambda e: e.copy(out=osum[:], in_=PI[:]), [B_PI], [B_osum])
                    kb.emit("dve", lambda e: e.tensor_tensor(out=osum[:], in0=osum[:], in1=PAo[:], op=ALU.add), [B_osum, B_PAo], [B_osum])
                    if d == 0:
                        kb.emit("dve", lambda e: e.tensor_tensor(out=ofw[:, i, :], in0=osum[:].rearrange("p h v -> p (h v)"),
                                                                 in1=PBo[:].rearrange("p h v -> p (h v)"), op=ALU.add), [B_osum, B_PBo], [B_ofw[i]])
                        yield
                        return
                    kb.emit("dve", lambda e: e.tensor_tensor(out=osum[:], in0=osum[:], in1=PBo[:], op=ALU.add), [B_osum, B_PBo], [B_osum])
                    kb.emit("dve", lambda e: e.tensor_tensor(out=osum[:].rearrange("p h v -> p (h v)"), in0=osum[:].rearrange("p h v -> p (h v)"),
                                                             in1=ofw[:, i, :], op=ALU.add), [B_osum, B_ofw[i]], [B_osum])
                    yield
                    kb.emit("act", lambda e: e.activation(out=osq[:], in_=osum[:], func=AF.Square), [B_osum], [B_osq])
                    kb.emit("dve", lambda e: e.tensor_reduce(out=ost[:, 0, :], in_=osq[:], axis=AX.X, op=ALU.add), [B_osq], [B_ost])
                    kb.emit("act", lambda e: e.activation(out=ost[:, 1, :], in_=ost[:, 0, :], func=AF.Ln, scale=1.0 / 128, bias=cst[:, 919:920]),
                            [B_ost, B_cst], [B_ost])
                    kb.emit("act", lambda e: e.activation(out=ost[:, 2, :], in_=ost[:, 1, :], func=AF.Exp, scale=-0.5), [B_ost], [B_ost])
                    kb.emit("dve", lambda e: e.tensor_tensor(out=osum[:], in0=osum[:], in1=ost[:, 2, :].unsqueeze(2).to_broadcast([128, 4, 128]),
                                                             op=ALU.mult), [B_osum, B_ost], [B_osum])
                    kb.emit("dve", lambda e: e.tensor_tensor(out=osum[:], in0=osum[:], in1=hgn[:].unsqueeze(1).to_broadcast([128, 4, 128]),
                                                             op=ALU.mult), [B_osum, B_hgn], [B_osum])
                    kb.emit("dve", lambda e: e.tensor_tensor(out=recb_[:], in0=osum[:].rearrange("p h v -> p (h v)"), in1=gg[s3][:], op=ALU.mult),
                            [B_osum, B_gg[s3]], [B_rec2])
                    yield
                    for h in range(4):
                        kb.emit("pe", lambda e, h=h: e.transpose(out=ptr[:, h, :], in_=recb_[:, h * 128:(h + 1) * 128], identity=identb[:]),
                                [B_rec2, B_identb], [B_ptr], inc=(h == 3))
                    kb.emit("act", lambda e: e.copy(out=recT[:, :, i * 128:(i + 1) * 128], in_=ptr[:, 0:4, :]), [B_ptr], [B_recT])
                    yield

                def run_interleaved(gens):
                    gens = [g for g in gens if g is not None]
                    while gens:
                        for g in list(gens):
                            try:
                                next(g)
                            except StopIteration:
                                gens.remove(g)

                for d in range(2):
                    kb.emit("pool", lambda e: e.memset(Sst[:], 0.0), [], B_S)
                    kb.emit("pool", lambda e: e.memset(Sbf[:], 0.0), [], B_Sb)
                    order = list(range(NT)) if d == 0 else list(range(NT - 1, -1, -1))
                    run_interleaved([stageA1(d, order[0], 0)])
                    run_interleaved([stageA1(d, order[1], 1), stageA(d, order[0], 0)])
                    for n in range(NT):
                        g1 = stageA1(d, order[n + 2], n + 2) if n + 2 < NT else None
                        g2 = stageA(d, order[n + 1], n + 1) if n + 1 < NT else None
                        run_interleaved([g1, g2, stageB(d, order[n], n)])
                kb.barrier()
                kb.flush()
            if dbg and "recT" in dbg:
                with ExitStack() as pd:
                    tf = sb("recTf", [128, 4, S], F32, pd)
                    B_tf = kb.buf()
                    kb.emit("dve", lambda e: e.tensor_copy(out=tf[:], in_=recT[:]), [B_recT], [B_tf])
                    dbg_store("recT", tf[:].rearrange("p k t -> p (k t)"), [B_tf])
                    kb.barrier(); kb.flush()
            if stop in ("hgrn", "hgrn1"):
                break


            with ExitStack() as po_:
                woa = sb("woa", [64, 8, D], BF16, po_)
                wor = sb("wor", [128, 4, D], BF16, po_)
                B_wo = kb.buf()
                with ExitStack() as pst_:
                    stg = sb("wostg", [128, 4, D], F32, pst_)
                    B_stg = kb.buf()
                    g1b = gatebc[:, s, 0, :].unsqueeze(1).to_broadcast([128, 4, D])
                    for part in range(3):
                        if part < 2:
                            kb.dma("sp", lambda e, part=part: e.dma_start(out=stg[0:64], in_=woa_d[:, part * 4:(part + 1) * 4, :]),
                                   "ld_wostg", writes=[B_stg])
                            kb.emit("dve", lambda e, part=part: e.tensor_tensor(out=woa[:, part * 4:(part + 1) * 4, :], in0=stg[0:64],
                                                                              in1=gatebc[0:64, s, 0, :].unsqueeze(1).to_broadcast([64, 4, D]),
                                                                              op=ALU.mult), [B_stg, B_gatebc], [B_wo])
                        else:
                            kb.dma("sp", lambda e: e.dma_start(out=stg[:], in_=wor_d[:]), "ld_wostg", writes=[B_stg])
                            kb.emit("dve", lambda e: e.tensor_tensor(out=wor[:], in0=stg[:], in1=g1b, op=ALU.mult),
                                    [B_stg, B_gatebc], [B_wo])
                    kb.barrier()
                    kb.flush()
                xt0 = sb("xto", [128, D], F32, po_)
                B_xt0 = kb.buf()
                x1t = [sb("x1t%d" % i, [128, D], F32, po_) for i in range(2)]
                B_x1t = [kb.buf(), kb.buf()]
                h2t = [sb("h2t%d" % i, [128, 8, 128], BF16, po_) for i in range(2)]
                B_h2t = [kb.buf(), kb.buf()]
                m2bc = sb("m2bc", [128, 2, D], F32, po_)
                B_m2bc = kb.buf()
                kb.dma("sp", lambda e: e.dma_start(out=m2bc[:].rearrange("p a d -> p (a d)"), in_=mod2_d[s:s + 1, :].partition_broadcast(128)),
                       "ld_m2bc", reads=[B_mod2d], writes=[B_m2bc])
                h2k = [sb("h2k%d" % i, [128, D], BF16, po_) for i in range(2)]
                h2kf = sb("h2kf", [128, D], F32, po_)
                B_h2k = [kb.buf(), kb.buf()]
                B_h2kf = kb.buf()
                nrm = NormT(po_, "o", ntp=2)
                pmx = [ps("pmx%d" % i, [128, D], F32, po_) for i in range(2)]
                B_pmx = [kb.pbuf(), kb.pbuf()]
                plg = ps("plg", [128, 64], F32, po_)
                B_plg = kb.pbuf()
                lga = sb("lga", [128, NT, 36], F32, po_)
                B_lga = kb.buf()
                xt1 = sb("xto1", [128, D], F32, po_)
                xts = [xt0, xt1]
                B_xts = [B_xt0, kb.buf()]

                def op_S1(i):
                    j = i % 2
                    tsl = slice(i * 128, (i + 1) * 128)
                    for hh in range(2):
                        for h in range(8):
                            kb.emit("pe", lambda e, j=j, h=h, hh=hh, tsl=tsl: e.matmul(
                                pmx[j][:, hh * 512:(hh + 1) * 512], lhsT=OT[0:64, h, tsl], rhs=woa[0:64, h, hh * 512:(hh + 1) * 512],
                                start=(h == 0), stop=False), [B_OT, B_wo], [B_pmx[j]], inc=False)
                        for c in range(4):
                            kb.emit("pe", lambda e, j=j, c=c, hh=hh, tsl=tsl: e.matmul(
                                pmx[j][:, hh * 512:(hh + 1) * 512], lhsT=recT[:, c, tsl], rhs=wor[:, c, hh * 512:(hh + 1) * 512],
                                start=False, stop=(c == 3)), [B_recT, B_wo], [B_pmx[j]], inc=(c == 3 and hh == 1))
                    kb.dma("sp", lambda e, i=i, j=j: e.dma_start(out=xts[j][:], in_=x_d[s, i * 128:(i + 1) * 128, :]), "ld_xto%d" % j, writes=[B_xts[j]])
                    kb.emit("dve", lambda e, j=j: e.tensor_tensor(out=x1t[j][:], in0=pmx[j][:], in1=xts[j][:], op=ALU.add),
                            [B_pmx[j], B_xts[j]], [B_x1t[j]])
                    kb.dma("sp", lambda e, i=i, j=j: e.dma_start(out=x1_d[s, i * 128:(i + 1) * 128, :], in_=x1t[j][:]), "st_x1_%d" % j,
                           reads=[B_x1t[j]], writes=[B_x1d[s][i]])

                def op_S2(i):
                    j = i % 2
                    gi = s * NT + i
                    xn_, B_xn_ = nrm.run(x1t[j][:], B_x1t[j], s, 2, h2t[j][:], B_h2t[j])
                    kb.emit("pool", lambda e, xn_=xn_: e.tensor_tensor(out=h2kf[:], in0=xn_[:], in1=m2bc[:, 0, :], op=ALU.mult),
                            [B_xn_, B_m2bc], [B_h2kf])
                    kb.emit("pool", lambda e, j=j: e.tensor_tensor(out=h2k[j][:], in0=h2kf[:], in1=m2bc[:, 1, :], op=ALU.add),
                            [B_h2kf, B_m2bc], [B_h2k[j]])
                    kb.dma("sp", lambda e, gi=gi, j=j: e.dma_start(out=h2tok_d[gi * 128:(gi + 1) * 128, :], in_=h2k[j][:]), "st_h2_%d" % j,
                           reads=[B_h2k[j]], writes=[B_h2d[s][i]])
                    for kc in range(8):
                        kb.emit("pe", lambda e, j=j, kc=kc: e.matmul(plg[:, 0:36], lhsT=h2t[j][:, kc, :], rhs=wr[:, kc, :],
                                                                     start=(kc == 0), stop=(kc == 7)), [B_h2t[j], B_wr], [B_plg], inc=(kc == 7))
                    kb.emit("dve", lambda e, i=i: e.tensor_tensor(out=lga[:, i, :], in0=plg[:, 0:36], in1=brb[:], op=ALU.add), [B_plg, B_brb], [B_lga])

                op_S1(0)
                for i in range(NT):
                    if i + 1 < NT:
                        op_S1(i + 1)
                    op_S2(i)
                g0 = s * NT
                gsel = sb("gsel", [128, 3, NT, 4], F32, po_)
                tkb = sb("tkb", [128, 8, NT], F32, po_)
                elm = sb("elm", [128, 4, NT, 32], F32, po_)
                B_gsel, B_tkb, B_elm = kb.buf(), kb.buf(), kb.buf()
                gl = lga[:, :, 0:4]
                el = lga[:, :, 4:36]
                bc4 = lambda ap: ap.unsqueeze(2).to_broadcast([128, NT, 4])
                bc32 = lambda ap: ap.unsqueeze(2).to_broadcast([128, NT, 32])
                kb.emit("dve", lambda e: e.tensor_reduce(out=tkb[:, 0, :], in_=gl, axis=AX.X, op=ALU.max), [B_lga], [B_tkb])
                kb.emit("dve", lambda e: e.tensor_tensor(out=gsel[:, 0], in0=gl, in1=bc4(tkb[:, 0, :]), op=ALU.is_ge), [B_lga, B_tkb], [B_gsel])
                kb.emit("dve", lambda e: e.tensor_tensor(out=gsel[:, 2], in0=gl, in1=bc4(tkb[:, 0, :]), op=ALU.subtract), [B_lga, B_tkb], [B_gsel])
                kb.emit("act", lambda e: e.activation(out=gsel[:, 2], in_=gsel[:, 2], func=AF.Exp), [B_gsel], [B_gsel])
                kb.emit("dve", lambda e: e.tensor_reduce(out=tkb[:, 1, :], in_=gsel[:, 2], axis=AX.X, op=ALU.add), [B_gsel, B_tkb], [B_tkb])
                kb.emit("dve", lambda e: e.reciprocal(out=tkb[:, 2, :], in_=tkb[:, 1, :]), [B_tkb], [B_tkb])
                kb.emit("dve", lambda e: e.tensor_scalar(out=gsel[:, 1], in0=gsel[:, 0], scalar1=-1.0, scalar2=BIG, op0=ALU.add, op1=ALU.mult),
                        [B_gsel], [B_gsel])
                kb.emit("dve", lambda e: e.tensor_tensor(out=elm[:, 0].rearrange("p t (g e) -> p t g e", g=4),
                                                         in0=lga[:, :, 4:36].rearrange("p t (g e) -> p t g e", g=4),
                                                         in1=gsel[:, 1].unsqueeze(3).to_broadcast([128, NT, 4, 8]), op=ALU.add),
                        [B_lga, B_gsel], [B_elm])
                kb.emit("dve", lambda e: e.tensor_reduce(out=tkb[:, 3, :], in_=elm[:, 0], axis=AX.X, op=ALU.max), [B_elm, B_tkb], [B_tkb])
                kb.emit("dve", lambda e: e.tensor_tensor(out=elm[:, 1], in0=elm[:, 0], in1=bc32(tkb[:, 3, :]), op=ALU.is_ge), [B_elm, B_tkb], [B_elm])
                kb.emit("dve", lambda e: e.scalar_tensor_tensor(out=elm[:, 2], in0=elm[:, 1], scalar=-BIG, in1=elm[:, 0], op0=ALU.mult, op1=ALU.add),
                        [B_elm], [B_elm])
                kb.emit("dve", lambda e: e.tensor_reduce(out=tkb[:, 4, :], in_=elm[:, 2], axis=AX.X, op=ALU.max), [B_elm, B_tkb], [B_tkb])
                kb.emit("dve", lambda e: e.tensor_tensor(out=elm[:, 3], in0=elm[:, 2], in1=bc32(tkb[:, 4, :]), op=ALU.is_ge), [B_elm, B_tkb], [B_elm])
                kb.emit("dve", lambda e: e.tensor_tensor(out=tkb[:, 5, :], in0=tkb[:, 4, :], in1=tkb[:, 3, :], op=ALU.subtract), [B_tkb], [B_tkb])
                kb.emit("act", lambda e: e.activation(out=tkb[:, 5, :], in_=tkb[:, 5, :], func=AF.Exp), [B_tkb], [B_tkb])
                kb.emit("dve", lambda e: e.tensor_scalar(out=tkb[:, 6, :], in0=tkb[:, 5, :], scalar1=1.0, scalar2=None, op0=ALU.add), [B_tkb], [B_tkb])
                kb.emit("dve", lambda e: e.reciprocal(out=tkb[:, 6, :], in_=tkb[:, 6, :]), [B_tkb], [B_tkb])
                kb.emit("dve", lambda e: e.tensor_tensor(out=WK[:, g0:g0 + NT, 0], in0=tkb[:, 6, :], in1=tkb[:, 2, :], op=ALU.mult), [B_tkb], B_EW[g0:g0 + NT])
                kb.emit("dve", lambda e: e.tensor_tensor(out=WK[:, g0:g0 + NT, 1], in0=WK[:, g0:g0 + NT, 0], in1=tkb[:, 5, :], op=ALU.mult),
                        [B_tkb] + B_EW[g0:g0 + NT], B_EW[g0:g0 + NT])
                for k2 in range(2):
                    kb.emit("dve", lambda e, k2=k2: e.tensor_tensor(out=elm[:, 0], in0=elm[:, 1 + 2 * k2], in1=iota_e[:].unsqueeze(1).to_broadcast([128, NT, 32]),
                                                                    op=ALU.mult), [B_elm, B_iota], [B_elm])
                    kb.emit("dve", lambda e, k2=k2: e.tensor_reduce(out=EIDX[:, g0:g0 + NT, k2], in_=elm[:, 0], axis=AX.X, op=ALU.add),
                            [B_elm] + B_EW[g0:g0 + NT], B_EW[g0:g0 + NT])
                kb.barrier()
                kb.flush()
            if stop == "mix":
                break

    if stop == "mix":
        if dbg and "EIDX" in dbg:
            dbg_store("EIDX", EIDX[:].rearrange("p i e -> p (i e)"), B_EW)
            dbg_store("WK", WK[:].rearrange("p i e -> p (i e)"), B_EW)
            kb.barrier()
            kb.flush()

    issue_cv(1000)
    if stop is None or stop == "moe":
        NG = nseq_run * NT
        with ExitStack() as pe_:
            cstm = sb("cstm", [128, 192], F32, pe_)
            crow = sb("crow", [1, 3200], F32, pe_)
            B_cstm, B_crow = kb.buf(), kb.buf()
            kb.dma("sp", lambda e: e.dma_start(out=cstm[:], in_=cstm_d[:]), "ld_cstm", writes=[B_cstm])
            kb.dma("sp", lambda e: e.dma_start(out=crow[:], in_=crow_d[:]), "ld_crow", writes=[B_crow])
            sltb = sb("sltb", [128, 128], BF16, pe_)
            B_sltb = kb.buf()
            kb.emit("dve", lambda e: e.tensor_copy(out=sltb[:], in_=cstm[:, 0:128]), [B_cstm], [B_sltb])
            NGT = NSEQ * NT
            Mb = sb("Mb", [128, NGT, 32], BF16, pe_)
            CS = sb("CS", [128, NGT + 1, 32], F32, pe_)
            RK = sb("RK", [128, NGT, 32], F32, pe_)
            oh = sb("oh", [128, NGT, 2, 32], F32, pe_)
            ohr = sb("ohr", [128, NGT, 32], F32, pe_)
            B_Mb, B_CS, B_RK, B_oh, B_ohr = (kb.buf() for _ in range(5))
            SLOTF = sb("SLOTF", [128, NGT, 2], F32, pe_)
            SLOT = sb("SLOT", [128, NGT, 2], I32, pe_)
            B_slotf, B_slot = kb.buf(), kb.buf()
            IDXW = sb("IDXW", [128, NBB, 2], I32, pe_)
            B_idxw = kb.buf()
            with ExitStack() as pr_:
                pcs = ps("pcs", [128, 1024], F32, pr_)
                B_pcs = kb.pbuf()
                prk = ps("prk", [128, 1024], F32, pr_)
                B_prk = kb.pbuf()
                pbcr = ps("pbcr", [128, 96], F32, pr_)
                B_pbcr = kb.pbuf()
                kb.emit("dve", lambda e: e.tensor_tensor(out=oh[:].rearrange("p g k e -> p (g k) e"),
                                                         in0=iota_e[:].unsqueeze(1).to_broadcast([128, NGT * 2, 32]),
                                                         in1=EIDX[:].rearrange("p g k -> p (g k)").unsqueeze(2).to_broadcast([128, NGT * 2, 32]),
                                                         op=ALU.is_equal), [B_iota] + B_EW, [B_oh])
                kb.emit("dve", lambda e: e.tensor_tensor(out=Mb[:], in0=oh[:, :, 0, :], in1=oh[:, :, 1, :], op=ALU.add), [B_oh], [B_Mb])
                Mbf = Mb[:].rearrange("p g e -> p (g e)")
                for hh in range(2):
                    kb.emit("pe", lambda e, hh=hh: e.matmul(pcs[:, hh * 512:(hh + 1) * 512], lhsT=onesb[:], rhs=Mbf[:, hh * 512:(hh + 1) * 512], start=True, stop=True),
                            [B_onesb, B_Mb], [B_pcs], inc=(hh == 1))
                for hh in range(2):
                    kb.emit("pe", lambda e, hh=hh: e.matmul(prk[:, hh * 512:(hh + 1) * 512], lhsT=sltb[:], rhs=Mbf[:, hh * 512:(hh + 1) * 512], start=True, stop=True),
                            [B_sltb, B_Mb], [B_prk], inc=(hh == 1))
                kb.emit("pool", lambda e: e.memset(CS[:, 0, :], 0.0), [], [B_CS])
                for g in range(NGT):
                    kb.emit("dve", lambda e, g=g: e.tensor_tensor(out=CS[:, g + 1, :], in0=CS[:, g, :], in1=pcs[:, g * 32:(g + 1) * 32], op=ALU.add),
                            [B_CS, B_pcs], [B_CS])
                kb.emit("dve", lambda e: e.tensor_tensor(out=RK[:].rearrange("p g e -> p (g e)"), in0=prk[:], in1=CS[:, 0:NGT, :].rearrange("p g e -> p (g e)"), op=ALU.add),
                        [B_prk, B_CS], [B_RK])
                rw = sb("rw", [1, 8, 64], F32, pr_)
                g1 = sb("g1", [1, 2048], F32, pr_)
                B_rw, B_g1 = kb.buf(), kb.buf()
                kb.emit("pool", lambda e: e.memset(rw[:], 0.0), [], [B_rw])
                kb.emit("dve", lambda e: e.tensor_copy(out=rw[:, 0, 0:32], in_=CS[0:1, NGT, :]), [B_CS, B_rw], [B_rw])
                kb.emit("dve", lambda e: e.tensor_tensor(out=g1[:, 0:512].rearrange("p (a b) -> p a b", a=32),
                                                         in0=rw[:, 0, 0:32].unsqueeze(2).to_broadcast([1, 32, 16]),
                                                         in1=crow[:, 0:16].unsqueeze(1).to_broadcast([1, 32, 16]), op=ALU.is_gt),
                        [B_rw, B_crow], [B_g1])
                kb.emit("dve", lambda e: e.tensor_reduce(out=rw[:, 1, 0:32], in_=g1[:, 0:512].rearrange("p (a b) -> p a b", a=32), axis=AX.X, op=ALU.add),
                        [B_g1, B_rw], [B_rw])
                kb.emit("dve", lambda e: e.tensor_tensor(out=g1[:, 0:1024].rearrange("p (a b) -> p a b", a=32),
                                                         in0=rw[:, 1, 0:32].unsqueeze(1).to_broadcast([1, 32, 32]),
                                                         in1=crow[:, 16:1040].rearrange("p (a b) -> p a b", a=32), op=ALU.mult),
                        [B_rw, B_crow], [B_g1])
                kb.emit("dve", lambda e: e.tensor_reduce(out=rw[:, 2, 0:32], in_=g1[:, 0:1024].rearrange("p (a b) -> p a b", a=32), axis=AX.X, op=ALU.add),
                        [B_g1, B_rw], [B_rw])
                kb.emit("dve", lambda e: e.tensor_tensor(out=rw[:, 3, 0:32], in0=rw[:, 2, 0:32], in1=rw[:, 1, 0:32], op=ALU.subtract), [B_rw], [B_rw])
                kb.emit("dve", lambda e: e.tensor_scalar(out=rw[:, 3, 0:32], in0=rw[:, 3, 0:32], scalar1=256.0, scalar2=None, op0=ALU.mult), [B_rw], [B_rw])
                kb.emit("dve", lambda e: e.tensor_tensor(out=g1[:].rearrange("p (a b) -> p a b", a=64),
                                                         in0=rw[:, 2, 0:32].unsqueeze(1).to_broadcast([1, 64, 32]),
                                                         in1=crow[:, 1040:3088].rearrange("p (a b) -> p a b", a=64), op=ALU.is_le),
                        [B_rw, B_crow], [B_g1])
                kb.emit("dve", lambda e: e.tensor_reduce(out=rw[:, 4, :], in_=g1[:].rearrange("p (a b) -> p a b", a=64), axis=AX.X, op=ALU.add),
                        [B_g1, B_rw], [B_rw])
                kb.emit("dve", lambda e: e.tensor_scalar(out=rw[:, 4, :], in0=rw[:, 4, :], scalar1=31.0, scalar2=None, op0=ALU.min), [B_rw], [B_rw])
                kb.emit("dve", lambda e: e.tensor_tensor(out=rw[:, 5, 2:64], in0=rw[:, 4, 2:64], in1=rw[:, 4, 0:62], op=ALU.is_equal), [B_rw], [B_rw])
                kb.emit("dve", lambda e: e.tensor_scalar(out=rw[:, 5, :], in0=rw[:, 5, :], scalar1=5.0e8, scalar2=None, op0=ALU.mult), [B_rw], [B_rw])
                kb.emit("dve", lambda e: e.scalar_tensor_tensor(out=rw[:, 6, :], in0=rw[:, 4, :], scalar=128.0, in1=rw[:, 5, :], op0=ALU.mult, op1=ALU.add),
                        [B_rw], [B_rw])
                kb.emit("pe", lambda e: e.matmul(pbcr[:, 0:64], lhsT=ones_f[0:1, 0:128], rhs=rw[:, 6, :], start=True, stop=True), [B_cst, B_rw], [B_pbcr], inc=False)
                kb.emit("pe", lambda e: e.matmul(pbcr[:, 64:96], lhsT=ones_f[0:1, 0:128], rhs=rw[:, 3, 0:32], start=True, stop=True), [B_cst, B_rw], [B_pbcr])
                bcs = sb("bcs", [128, 96], F32, pr_)
                idf = sb("idf", [128, NBB, 2], F32, pr_)
                B_bcs, B_idf = kb.buf(), kb.buf()
                kb.emit("dve", lambda e: e.tensor_copy(out=bcs[:], in_=pbcr[:]), [B_pbcr], [B_bcs])
                kb.emit("dve", lambda e: e.tensor_scalar(out=idf[:, :, 0], in0=bcs[:, 0:64], scalar1=cstm[:, 160:161], scalar2=None, op0=ALU.add),
                        [B_bcs, B_cstm], [B_idf])
                kb.emit("dve", lambda e: e.tensor_scalar(out=idf[:, :, 1], in0=idf[:, :, 0], scalar1=1.0, scalar2=None, op0=ALU.add), [B_idf], [B_idf])
                kb.emit("dve", lambda e: e.tensor_copy(out=IDXW[:], in_=idf[:]), [B_idf], [B_idxw])
                kb.emit("dve", lambda e: e.tensor_tensor(out=RK[:], in0=RK[:], in1=bcs[:, 64:96].unsqueeze(1).to_broadcast([128, NGT, 32]), op=ALU.add),
                        [B_RK, B_bcs], [B_RK])
                for k2 in range(2):
                    kb.emit("dve", lambda e, k2=k2: e.tensor_tensor(out=ohr[:], in0=oh[:, :, k2, :], in1=RK[:], op=ALU.mult), [B_oh, B_RK], [B_ohr])
                    kb.emit("dve", lambda e, k2=k2: e.tensor_reduce(out=SLOTF[:, :, k2], in_=ohr[:], axis=AX.X, op=ALU.add), [B_ohr, B_slotf], [B_slotf])
                kb.emit("dve", lambda e: e.tensor_copy(out=SLOT[:], in_=SLOTF[:]), [B_slotf], [B_slot])
                if dbg and "SLOT" in dbg:
                    dbg_store("SLOT", SLOTF[:].rearrange("p i e -> p (i e)"), [B_slotf])
                    dbg_store("BE", rw[:].rearrange("p a b -> p (a b)"), [B_rw])
                kb.barrier()
                kb.flush()
            with nullcontext(pe_) as pd_:
                hk = [sb("hk%d" % i, [128, D], BF16, pd_) for i in range(2)]
                B_hk = [kb.buf(), kb.buf()]
                for gi in range(NG):
                    j = gi % 2
                    s_, i_ = gi // NT, gi % NT
                    kb.dma("sp", lambda e, gi=gi, j=j: e.dma_start(out=hk[j][:], in_=h2tok_d[gi * 128:(gi + 1) * 128, :]), "ld_hk%d" % j,
                           reads=[B_h2d[s_][i_]], writes=[B_hk[j]])
                    for k2 in range(2):
                        kb.dma("pool", lambda e, gi=gi, j=j, k2=k2: e.indirect_dma_start(
                            out=xs_d[:, :], out_offset=bass.IndirectOffsetOnAxis(ap=SLOT[:, gi, k2:k2 + 1], axis=0), in_=hk[j][:], in_offset=None),
                            "sc_xs%d" % j, reads=[B_hk[j], B_slot, B_xsz], writes=[B_xs2[j]])
            B_ys2 = [kb.buf("ys0"), kb.buf("ys1")]
            with nullcontext(pe_) as px_:
                wall = [sb("wall%d" % i, [128, 3 * 4096], BF16, px_) for i in range(2)]
                wge = [wall[i][:, 0:4096].rearrange("p (k n) -> p k n", k=8) for i in range(2)]
                wue = [wall[i][:, 4096:8192].rearrange("p (k n) -> p k n", k=8) for i in range(2)]
                wde = [wall[i][:, 8192:12288].rearrange("p (k n) -> p k n", k=4) for i in range(2)]
                B_we = [kb.buf(), kb.buf()]
                xb = [sb("xb%d" % i, [128, 2, D], BF16, px_) for i in range(2)]
                B_xb = [kb.buf(), kb.buf()]
                xsT = [sb("xsT%d" % i, [128, 8, 256], BF16, px_) for i in range(2)]
                B_xsT = [kb.buf(), kb.buf()]
                hid = [sb("hid%d" % i, [128, 4, 256], BF16, px_) for i in range(2)]
                B_hid = [kb.buf(), kb.buf()]
                sgt = [sb("sgt%d" % i, [128, 256], F32, px_) for i in range(2)]
                B_sgt = [kb.buf(), kb.buf()]
                ysb = [sb("ysb%d" % i, [128, D], F32, px_) for i in range(2)]
                B_ysb = [kb.buf(), kb.buf()]
                ptx0 = ps("ptx0", [128, 8, 128], BF16, px_)
                B_ptx0 = kb.pbuf()
                ptx = [ptx0, ptx0]
                B_ptx = [B_ptx0, B_ptx0]
                pgu = [ps("pgu%d" % i, [128, 256], F32, px_) for i in range(4)]
                B_pgu = [kb.pbuf() for _ in range(4)]
                pyh = [ps("pyh%d" % i, [128, 512], F32, px_) for i in range(3)]
                B_pyh = [kb.pbuf() for _ in range(3)]
                cnt_hb = 0
                nbb_run = (NBB - 1) if stop is None else 8
                cnt_g = 0
                cnt_t = 0
                cnt_y = 0
                for b in range(nbb_run):
                    wj = b % 2
                    kb.dma("pool", lambda e, b=b, wj=wj: e.indirect_dma_start(
                        out=wall[wj][:, :], out_offset=None, in_=wbf_d[:, :],
                        in_offset=bass.IndirectOffsetOnAxis(ap=IDXW[:, b, 0:1], axis=0),
                        bounds_check=kb.const_reg(e, NEXP * 128 - 1), oob_is_err=False), "ld_we%d" % wj, reads=[B_idxw, B_wbf], writes=[B_we[wj]])
                    xj = b % 2
                    for bb_ in ([0, 1] if b == 0 else [b + 1]):
                        if bb_ < nbb_run:
                            kb.dma("sp", lambda e, bb_=bb_: e.dma_start(out=xb[bb_ % 2][:], in_=xs_d[bb_ * 256:(bb_ + 1) * 256, :].rearrange("(t p) d -> p t d", p=128)),
                                   "ld_xb%d" % (bb_ % 2), reads=B_xs2, writes=[B_xb[bb_ % 2]])
                    for t2 in range(2):
                        tj = cnt_t % 2
                        cnt_t += 1
                        for kc in range(8):
                            kb.emit("pe", lambda e, xj=xj, t2=t2, kc=kc, tj=tj: e.transpose(out=ptx[tj][:, kc, :], in_=xb[xj][:, t2, kc * 128:(kc + 1) * 128],
                                                                                       identity=identb[:]), [B_xb[xj], B_identb], [B_ptx[tj]], inc=(kc == 7))
                        kb.emit("act", lambda e, xj=xj, t2=t2, tj=tj: e.copy(out=xsT[xj][:, :, t2 * 128:(t2 + 1) * 128], in_=ptx[tj][:]), [B_ptx[tj]], [B_xsT[xj]])
                    hj = b % 2
                    for m in range(4):
                        pg_i = (cnt_g % 2) * 2
                        cnt_g += 1
                        for kc in range(8):
                            kb.emit("pe", lambda e, wj=wj, m=m, kc=kc, xj=xj, pg_i=pg_i: e.matmul(
                                pgu[pg_i][:], lhsT=wge[wj][:, kc, m * 128:(m + 1) * 128], rhs=xsT[xj][:, kc, :],
                                start=(kc == 0), stop=(kc == 7)), [B_we[wj], B_xsT[xj]], [B_pgu[pg_i]], inc=(kc == 7))
                        for kc in range(8):
                            kb.emit("pe", lambda e, wj=wj, m=m, kc=kc, xj=xj, pg_i=pg_i: e.matmul(
                                pgu[pg_i + 1][:], lhsT=wue[wj][:, kc, m * 128:(m + 1) * 128], rhs=xsT[xj][:, kc, :],
                                start=(kc == 0), stop=(kc == 7)), [B_we[wj], B_xsT[xj]], [B_pgu[pg_i + 1]], inc=(kc == 7))
                        sj = m % 2
                        kb.emit("act", lambda e, pg_i=pg_i, sj=sj: e.activation(out=sgt[sj][:], in_=pgu[pg_i][:], func=AF.Silu), [B_pgu[pg_i]], [B_sgt[sj]])
                        kb.emit("dve", lambda e, pg_i=pg_i, sj=sj, hj=hj, m=m: e.tensor_tensor(out=hid[hj][:, m, :], in0=sgt[sj][:], in1=pgu[pg_i + 1][:], op=ALU.mult),
                                [B_sgt[sj], B_pgu[pg_i + 1]], [B_hid[hj]])
                    for t2 in range(2):
                        yj = cnt_y % 2
                        cnt_y += 1
                        for hh in range(2):
                            hb = cnt_hb % 3
                            cnt_hb += 1
                            for m in range(4):
                                kb.emit("pe", lambda e, wj=wj, m=m, hh=hh, hj=hj, t2=t2, hb=hb: e.matmul(
                                    pyh[hb][:], lhsT=hid[hj][:, m, t2 * 128:(t2 + 1) * 128],
                                    rhs=wde[wj][:, m, hh * 512:(hh + 1) * 512], start=(m == 0), stop=(m == 3)),
                                    [B_hid[hj], B_we[wj]], [B_pyh[hb]], inc=(m == 3))
                            kb.emit("act", lambda e, yj=yj, hh=hh, hb=hb: e.copy(out=ysb[yj][:, hh * 512:(hh + 1) * 512], in_=pyh[hb][:]),
                                    [B_pyh[hb]], [B_ysb[yj]])
                        kb.dma("sp", lambda e, b=b, t2=t2, yj=yj: e.dma_start(out=ys_d[b * 256 + t2 * 128:b * 256 + (t2 + 1) * 128, :], in_=ysb[yj][:]),
                               "st_ys%d" % yj, reads=[B_ysb[yj]], writes=[B_ys2[yj]])
            with nullcontext(pe_) as pc_:
                xr = [sb("xr%d" % i, [128, D], F32, pc_) for i in range(2)]
                yg = [sb("yg%d" % i, [128, 2, D], F32, pc_) for i in range(2)]
                B_xr = [kb.buf(), kb.buf()]
                B_yg = [kb.buf(), kb.buf()]
                for gi in range(NG):
                    j = gi % 2
                    s_, i_ = gi // NT, gi % NT
                    kb.dma("sp", lambda e, s_=s_, i_=i_, j=j: e.dma_start(out=xr[j][:], in_=x1_d[s_, i_ * 128:(i_ + 1) * 128, :]), "ld_xr%d" % j,
                           reads=[B_x1d[s_][i_]], writes=[B_xr[j]])
                    for k2 in range(2):
                        kb.dma("pool", lambda e, gi=gi, j=j, k2=k2: e.indirect_dma_start(
                            out=yg[j][:, k2, :], out_offset=None, in_=ys_d[:, :],
                            in_offset=bass.IndirectOffsetOnAxis(ap=SLOT[:, gi, k2:k2 + 1], axis=0)), "ld_yg%d" % j,
                            reads=B_ys2 + [B_slot], writes=[B_yg[j]])
                    kb.emit("dve", lambda e, gi=gi, j=j: e.tensor_scalar(out=yg[j][:, 0, :], in0=yg[j][:, 0, :], scalar1=WK[:, gi, 0:1], scalar2=None, op0=ALU.mult),
                            [B_yg[j], B_EW[gi]], [B_yg[j]])
                    kb.emit("dve", lambda e, gi=gi, j=j: e.scalar_tensor_tensor(out=yg[j][:, 0, :], in0=yg[j][:, 1, :], scalar=WK[:, gi, 1:2], in1=yg[j][:, 0, :],
                                                                              op0=ALU.mult, op1=ALU.add), [B_yg[j], B_EW[gi]], [B_yg[j]])
                    kb.emit("dve", lambda e, s_=s_, j=j: e.tensor_tensor(out=yg[j][:, 0, :], in0=yg[j][:, 0, :], in1=gatebc[:, s_, 1, :], op=ALU.mult),
                            [B_yg[j], B_gatebc], [B_yg[j]])
                    kb.emit("dve", lambda e, j=j: e.tensor_tensor(out=xr[j][:], in0=xr[j][:], in1=yg[j][:, 0, :], op=ALU.add), [B_xr[j], B_yg[j]], [B_xr[j]])
                    kb.dma("sp", lambda e, s_=s_, i_=i_, j=j: e.dma_start(out=out_d[s_, i_ * 128:(i_ + 1) * 128, :], in_=xr[j][:]), "st_out%d" % j,
                           reads=[B_xr[j]])
                kb.barrier()
                kb.flush()

    kb.barrier()
    kb.flush()
    kb.es.close()
    return nc


def _consts():
    c = np.zeros((128, 1024), np.float32)
    idx = np.arange(128)
    same = (idx[:, None] // 64) == (idx[None, :] // 64)
    triF = ((idx[:, None] <= idx[None, :]) & same).astype(np.float32)
    triB = triF.T.copy()
    c[:, 0:128] = triF
    c[:, 128:256] = triB
    c[:, 256:384] = triB - np.eye(128, dtype=np.float32)
    c[:, 384:512] = triF - np.eye(128, dtype=np.float32)
    c[:, 512] = (idx < 64)
    c[:, 513] = (idx >= 64)
    half = 16
    inv_freq = (10000.0 ** (-np.arange(half, dtype=np.float32) / half)).astype(np.float32)
    c[:, 514:530] = inv_freq[None, :]
    c[0, 530] = 1.0
    c[1, 531] = 1.0
    c[0, 532:660] = 1.0
    c[1, 660:788] = 1.0
    c[:, 788:916] = 1.0
    c[:, 916] = 1.0 / 384
    c[:, 917] = 1.0 / 256
    c[:, 918] = -np.pi
    c[:, 919] = 1e-6
    return c


def _cstm():
    c = np.zeros((128, 192), np.float32)
    idx = np.arange(128)
    c[:, 0:128] = (idx[:, None] < idx[None, :]).astype(np.float32)
    c[:, 128:160] = np.arange(32, dtype=np.float32)[None, :]
    c[:, 160] = idx
    return c


def _crow():
    r = np.zeros((1, 3200), np.float32)
    r[0, 0:16] = 256.0 * np.arange(16)
    e = np.arange(32)
    r[0, 16:1040] = (e[None, :] <= e[:, None]).astype(np.float32).reshape(-1)
    r[0, 1040:3088] = np.repeat(np.arange(64, dtype=np.float32), 32)
    return r


def _kc(w):
    K, N = w.shape
    return np.ascontiguousarray(w.reshape(K // 128, 128, N).transpose(1, 0, 2))


def make_in_maps(inp):
    f = lambda a: np.ascontiguousarray(np.asarray(a, dtype=np.float32))
    x = f(inp["x"]); c = f(inp["c"]); pos = np.asarray(inp["positions"]).astype(np.int32)
    shared = {
        "ada_w": _kc(f(inp["ada_w"])[0]),
        "ada_b": f(inp["ada_b"])[0][None, :],
        "g1": np.ascontiguousarray(f(inp["norm1_g"])[0].reshape(8, 128).T),
        "g2": np.ascontiguousarray(f(inp["norm2_g"])[0].reshape(8, 128).T),
        "w_in": _kc(f(inp["w_in"])[0]),
        "qa_g": np.ascontiguousarray(f(inp["mla_qa_g"])[0].reshape(3, 128).T),
        "wq_up": _kc(f(inp["mla_wq_up"])[0]),
        "kva_g": np.ascontiguousarray(f(inp["mla_kva_g"])[0].reshape(2, 128).T),
        "wkv_up": _kc(f(inp["mla_wkv_up"])[0]),
        "qn_g": f(inp["mla_qn_g"])[0][None, :],
        "kn_g": f(inp["mla_kn_g"])[0][None, :],
        "lb_logits": f(inp["hg_lb_logits"]).reshape(1, -1),
        "hg_norm_g": f(inp["hg_norm_g"])[0][None, :],
        "w_out_a": np.ascontiguousarray(f(inp["w_out"])[0][:512].reshape(8, 64, D).transpose(1, 0, 2)),
        "w_out_r": _kc(f(inp["w_out"])[0][512:]),
        "w_router": _kc(np.concatenate([f(inp["router_group_w"])[0], f(inp["router_expert_w"])[0]], axis=1)),
        "b_router": np.concatenate([f(inp["router_group_b"])[0], f(inp["router_expert_b"])[0]])[None, :],
        "w_gate": np.ascontiguousarray(f(inp["w_gate"])[0].reshape(NEXP, 8, 128, 512).transpose(0, 2, 1, 3)),
        "w_up": np.ascontiguousarray(f(inp["w_up"])[0].reshape(NEXP, 8, 128, 512).transpose(0, 2, 1, 3)),
        "w_down": np.ascontiguousarray(f(inp["w_down"])[0].reshape(NEXP, 4, 128, D).transpose(0, 2, 1, 3)),
        "ident_bf": np.eye(128, dtype=np.float32).astype(ml_dtypes.bfloat16),
        "consts_f": _consts(),
        "cstm": _cstm(),
        "crow": _crow(),
        "g2row": f(inp["norm2_g"])[0][None, :],
    }
    maps = []
    for i in range(NCORES):
        b0 = NSEQ * i
        m = dict(shared)
        m["x"] = np.ascontiguousarray(x[b0:b0 + NSEQ])
        p = pos[b0:b0 + NSEQ].reshape(NSEQ, NT, 128)
        m["pos"] = np.ascontiguousarray(p.transpose(2, 0, 1).reshape(128, NSEQ * NT))
        m["cT"] = np.ascontiguousarray(c[b0:b0 + NSEQ].reshape(NSEQ, 8, 128).transpose(2, 1, 0))
        maps.append(m)
    return maps


def kernel(**inputs):
    nc = build()
    in_maps = make_in_maps(inputs)
    res = run_bass_kernel_spmd(nc, in_maps, core_ids=list(range(NCORES)))
    outs = [np.asarray(r["out"]).reshape(NSEQ, S, D) for r in res.results]
    return np.concatenate(outs, axis=0).astype(np.float32)
```

```python
import numpy as np
import ml_dtypes
from contextlib import ExitStack, nullcontext
import concourse.bass as bass
import concourse.mybir as mybir
from concourse.bass_utils import run_bass_kernel_spmd

F32 = mybir.dt.float32
BF16 = mybir.dt.bfloat16
I32 = mybir.dt.int32
AF = mybir.ActivationFunctionType
ALU = mybir.AluOpType
AX = mybir.AxisListType

NCORES = 8
D = 1024
S = 2048
NT = S // 128
NSEQ = 2
EPS = 1e-6
INCOLS = 3232
NEXP = 32
BIG = 1.0e30


class Buf:
    __slots__ = ("name", "w", "r", "excl")

    def __init__(self, name, excl=False):
        self.name = name
        self.excl = excl
        self.w = None
        self.r = {}


class KB:
    ENG = ("pe", "act", "dve", "pool", "sp")

    def __init__(self, nc):
        self.nc = nc
        self.es = ExitStack()
        self.sems = {}
        self.cnt = {}
        self.waited = {e: {} for e in self.ENG}
        self.prog = {e: [] for e in self.ENG}
        for e in self.ENG:
            self.sems[e] = self.es.enter_context(nc.semaphore("s_" + e))
            self.cnt[e] = 0
        self.nbuf = 0
        self.ninst = 0
        self.nflush = 0
        self.regs = {}

    def buf(self, name=None):
        self.nbuf += 1
        return Buf(name or ("b%d" % self.nbuf))

    def pbuf(self, name=None):
        self.nbuf += 1
        return Buf(name or ("p%d" % self.nbuf), excl=True)

    def dsem(self, key):
        if key not in self.sems:
            self.sems[key] = self.es.enter_context(self.nc.semaphore("d_" + key))
            self.cnt[key] = 0
        return key

    def _waits(self, eng, reads, writes):
        need = {}

        def add(dep):
            if dep is None:
                return
            k, v, e2 = dep
            if need.get(k, 0) < v:
                need[k] = v

        for b in reads:
            add(b.w)
        strict = (eng == "pool")
        for b in writes:
            if b.w is not None and (b.w[2] != eng or strict):
                add(b.w)
            for k, (v, e2) in b.r.items():
                if e2 != eng or strict:
                    add((k, v, e2))
        out = []
        wd = self.waited[eng]
        for k, v in need.items():
            if wd.get(k, 0) < v:
                wd[k] = v
                out.append((k, v))
        return out

    def emit(self, eng, fn, reads=(), writes=(), inc=True):
        if any(b.excl for b in reads):
            writes = list(writes) + [b for b in reads if b.excl and b not in writes]
            reads = [b for b in reads if not b.excl]
        waits = self._waits(eng, reads, writes)
        val = self.cnt[eng] + 1
        if inc:
            self.cnt[eng] = val
        rec_w = (eng, val, eng)
        for b in reads:
            old = b.r.get(eng)
            if old is None or old[0] < val:
                b.r[eng] = (val, eng)
        for b in writes:
            b.w = rec_w
            b.r = {}
        self.prog[eng].append((waits, fn, eng if inc else None, 1))
        self.ninst += 1

    def dma(self, q, fn, key, reads=(), writes=()):
        self.dsem(key)
        waits = self._waits(q, reads, writes)
        val = self.cnt[key] + 16
        self.cnt[key] = val
        for b in reads:
            old = b.r.get(key)
            if old is None or old[0] < val:
                b.r[key] = (val, "dma")
        for b in writes:
            b.w = (key, val, "dma")
            b.r = {}
        self.prog[q].append((waits, fn, key, 16))
        self.ninst += 1

    def barrier(self):
        tgt = {k: v for k, v in self.cnt.items() if v > 0}
        for e in self.ENG:
            waits = []
            for k, v in tgt.items():
                if k == e:
                    continue
                if self.waited[e].get(k, 0) < v:
                    self.waited[e][k] = v
                    waits.append((k, v))
            if waits:
                self.prog[e].append((waits, None, None, 0))

    def flush(self):
        nc = self.nc
        progs = self.prog
        sems = self.sems

        def run(engname, eh):
            for waits, fn, inckey, incv in progs[engname]:
                for k, v in waits:
                    eh.wait_ge(sems[k], v)
                if fn is not None:
                    ins = fn(eh)
                    if inckey is not None:
                        ins.then_inc(sems[inckey], incv)

        with nc.Block() as block:
            @block.tensor
            def _(e):
                run("pe", e)

            @block.scalar
            def _(e):
                run("act", e)

            @block.vector
            def _(e):
                run("dve", e)

            @block.gpsimd
            def _(e):
                run("pool", e)

            @block.sync
            def _(e):
                run("sp", e)
        self.prog = {e: [] for e in self.ENG}
        self.nflush += 1

    def const_reg(self, e, val):
        key = (self.nflush, val)
        if key not in self.regs:
            self.regs[key] = e.to_reg(val)
        return self.regs[key]


def build(dbg=None, stop=None):
    nc = bass.Bass("TRN2", target_bir_lowering=False)
    kb = KB(nc)
    es = kb.es

    def din(name, shape, dt=F32):
        return nc.dram_tensor(name, list(shape), dt, kind="ExternalInput").ap()

    x_d = din("x", [NSEQ, S, D])
    pos_d = din("pos", [128, NSEQ * NT], I32)
    cT_d = din("cT", [128, 8, NSEQ])
    adaw_d = din("ada_w", [128, 8, 6 * D])
    adab_d = din("ada_b", [1, 6 * D])
    g1_d = din("g1", [128, 8])
    g2_d = din("g2", [128, 8])
    win_d = din("w_in", [128, 8, INCOLS])
    qag_d = din("qa_g", [128, 3])
    wq_d = din("wq_up", [128, 3, 768])
    kvag_d = din("kva_g", [128, 2])
    wkv_d = din("wkv_up", [128, 2, 1024])
    qng_d = din("qn_g", [1, 96])
    kng_d = din("kn_g", [1, 96])
    lbl_d = din("lb_logits", [1, 2 * 2 * 512])
    hgn_d = din("hg_norm_g", [1, 128])
    woa_d = din("w_out_a", [64, 8, D])
    wor_d = din("w_out_r", [128, 4, D])
    wr_d = din("w_router", [128, 8, 36])
    br_d = din("b_router", [1, 36])
    wg_d = din("w_gate", [NEXP, 128, 8, 512])
    wu_d = din("w_up", [NEXP, 128, 8, 512])
    wd_d = din("w_down", [NEXP, 128, 4, D])
    identb_d = din("ident_bf", [128, 128], BF16)
    consts_d = din("consts_f", [128, 1024])
    out_d = nc.dram_tensor("out", [NSEQ, S, D], F32, kind="ExternalOutput").ap()
    x1_d = nc.dram_tensor("x1_scratch", [NSEQ, S, D], F32, kind="Internal").ap()
    h2tok_d = nc.dram_tensor("h2tok_scratch", [NSEQ * S, D], BF16, kind="Internal").ap()
    mod2_d = nc.dram_tensor("mod2_scratch", [NSEQ, 2 * D], F32, kind="Internal").ap()
    NBB = 64
    xs_d = nc.dram_tensor("xs_scratch", [NBB * 256, D], BF16, kind="Internal").ap()
    ys_d = nc.dram_tensor("ys_scratch", [NBB * 256, D], F32, kind="Internal").ap()
    g2row_d = din("g2row", [1, D])
    wbf_d = nc.dram_tensor("wbf", [NEXP * 128, 3 * 4096], BF16, kind="Internal").ap()
    cstm_d = din("cstm", [128, 192])
    crow_d = din("crow", [1, 3200])
    dbg_d = {}
    if dbg:
        for k, shp in dbg.items():
            dbg_d[k] = nc.dram_tensor("dbg_" + k, list(shp), F32, kind="ExternalOutput").ap()

    uid = [0]

    def sb(name, shape, dt=F32, stack=es):
        uid[0] += 1
        return stack.enter_context(nc.sbuf_tensor("sb%d_%s" % (uid[0], name), list(shape), dt))

    def ps(name, shape, dt=F32, stack=es):
        uid[0] += 1
        return stack.enter_context(nc.psum_tensor("ps%d_%s" % (uid[0], name), list(shape), dt))

    identb = sb("identb", [128, 128], BF16)
    cst = sb("cst", [128, 1024])
    B_identb, B_cst = kb.buf("identb"), kb.buf("cst")
    kb.dma("sp", lambda e: e.dma_start(out=identb[:], in_=identb_d[:]), "ld_identb", writes=[B_identb])
    kb.dma("sp", lambda e: e.dma_start(out=cst[:], in_=consts_d[:]), "ld_cst", writes=[B_cst])
    ones_f = cst[:, 788:916]
    B_wbf = kb.buf("wbf")
    wsrc = [wg_d.rearrange("e p (h k) n -> e p h (k n)", h=2), wu_d.rearrange("e p (h k) n -> e p h (k n)", h=2),
            wd_d.rearrange("e p (h k) n -> e p h (k n)", h=2)]
    B_xsz = kb.buf("xsz")
    B_xs2 = [kb.buf("xs0"), kb.buf("xs1")]
    pending_cv = []
    if stop is None or stop == "moe":
        for ex in range(NEXP):
            for m_ in range(3):
                pending_cv.append((ex, m_))

    def issue_cv(n=1):
        for _ in range(n):
            if not pending_cv:
                return
            ex, m_ = pending_cv.pop(0)
            kb.dma("pool", lambda e, ex=ex, m_=m_: e.dma_start(
                out=wbf_d[ex * 128:(ex + 1) * 128, m_ * 4096:(m_ + 1) * 4096].rearrange("p (h c) -> p h c", h=2), in_=wsrc[m_][ex]),
                "cv_w", writes=[B_wbf])

    B_modrow = kb.buf("modrow")
    B_mod2d = kb.buf("mod2d")
    modcol = sb("modcol", [128, NSEQ, 4, 8])
    B_modcol = kb.buf("modcol")
    gatebc = sb("gatebc", [128, NSEQ, 2, D], BF16)
    B_gatebc = kb.buf("gatebc")

    with ExitStack() as p0:
        modrow = sb("modrow", [2, 6 * D], F32, p0)
        cT = sb("cT", [128, 8, NSEQ], F32, p0)
        cact = sb("cact", [128, 8, NSEQ], F32, p0)
        adab = sb("adab", [2, 6 * D], F32, p0)
        g12 = sb("g12", [128, 2, 8], F32, p0)
        B_cT, B_cact, B_adab, B_g12 = kb.buf(), kb.buf(), kb.buf(), kb.buf()
        kb.dma("sp", lambda e: e.dma_start(out=cT[:], in_=cT_d[:]), "ld_cT", writes=[B_cT])
        kb.dma("sp", lambda e: e.dma_start(out=adab[0:1, :], in_=adab_d[:]), "ld_adab", writes=[B_adab])
        kb.dma("sp", lambda e: e.dma_start(out=adab[1:2, :], in_=adab_d[:]), "ld_adab", writes=[B_adab])
        kb.dma("sp", lambda e: e.dma_start(out=g12[:, 0, :], in_=g1_d[:]), "ld_g12", writes=[B_g12])
        kb.dma("sp", lambda e: e.dma_start(out=g12[:, 1, :], in_=g2_d[:]), "ld_g12", writes=[B_g12])
        kb.emit("act", lambda e: e.activation(out=cact[:], in_=cT[:], func=AF.Silu), [B_cT], [B_cact])
        wbuf = [sb("adaw%d" % i, [128, 8, 512], F32, p0) for i in range(2)]
        B_wbuf = [kb.buf(), kb.buf()]
        pmod = [ps("pmod%d" % i, [2, 512], F32, p0) for i in range(2)]
        B_pmod = [kb.pbuf(), kb.pbuf()]
        for n in range(12):
            j = n % 2
            kb.dma("sp", lambda e, n=n, j=j: e.dma_start(out=wbuf[j][:], in_=adaw_d[:, :, n * 512:(n + 1) * 512]),
                   "ld_adaw%d" % j, writes=[B_wbuf[j]])
            for kc in range(8):
                kb.emit("pe", lambda e, j=j, kc=kc: e.matmul(pmod[j][:], lhsT=cact[:, kc, :], rhs=wbuf[j][:, kc, :],
                                                              start=(kc == 0), stop=(kc == 7)),
                        [B_cact, B_wbuf[j]], [B_pmod[j]], inc=(kc == 7))
            kb.emit("dve", lambda e, n=n, j=j: e.tensor_tensor(out=modrow[:, n * 512:(n + 1) * 512], in0=pmod[j][:],
                                                               in1=adab[:, n * 512:(n + 1) * 512], op=ALU.add),
                    [B_pmod[j], B_adab], [B_modrow])
        pcol = ps("pcol", [128, 64], F32, p0)
        B_pcol = kb.pbuf()
        col_src = [1, 0, 4, 3]
        for s in range(NSEQ):
            for j in range(4):
                for kc in range(8):
                    c0 = col_src[j] * D + kc * 128
                    idx = (s * 4 + j) * 8 + kc
                    kb.emit("pe", lambda e, s=s, c0=c0, idx=idx: e.matmul(
                        pcol[:, idx:idx + 1], lhsT=modrow[0:2, c0:c0 + 128], rhs=cst[0:2, 530 + s:531 + s],
                        start=True, stop=True), [B_modrow, B_cst], [B_pcol], inc=(j == 3 and kc == 7 and s == NSEQ - 1))
        kb.emit("dve", lambda e: e.tensor_copy(out=modcol[:].rearrange("p s j k -> p (s j k)"), in_=pcol[:]),
                [B_pcol], [B_modcol])
        for s in range(NSEQ):
            for jj, gi in ((0, 0), (2, 1)):
                kb.emit("dve", lambda e, s=s, jj=jj, gi=gi: e.scalar_tensor_tensor(
                    out=modcol[:, s, jj, :], in0=modcol[:, s, jj, :], scalar=1.0, in1=g12[:, gi, :],
                    op0=ALU.add, op1=ALU.mult), [B_modcol, B_g12], [B_modcol])
        pbc = [ps("pbc%d" % i, [128, 512], F32, p0) for i in range(2)]
        B_pbc = [kb.pbuf(), kb.pbuf()]
        t = 0
        for s in range(NSEQ):
            for g, base in ((0, 2 * D), (1, 5 * D)):
                for hh in range(2):
                    j = t % 2
                    t += 1
                    kb.emit("pe", lambda e, s=s, base=base, hh=hh, j=j: e.matmul(
                        pbc[j][:], lhsT=cst[0:2, 532 + s * 128:532 + (s + 1) * 128],
                        rhs=modrow[0:2, base + hh * 512:base + (hh + 1) * 512], start=True, stop=True),
                        [B_modrow, B_cst], [B_pbc[j]])
                    kb.emit("act", lambda e, s=s, g=g, hh=hh, j=j: e.copy(
                        out=gatebc[:, s, g, hh * 512:(hh + 1) * 512], in_=pbc[j][:]), [B_pbc[j]], [B_gatebc])
        if dbg and "modcol" in dbg:
            kb.dma("sp", lambda e: e.dma_start(out=dbg_d["modcol"][:], in_=modcol[:].rearrange("p s j k -> p (s j k)")),
                   "st_dbg", reads=[B_modcol])
        g2r = sb("g2r", [2, D], F32, p0)
        B_g2r = kb.buf()
        for r_ in range(2):
            kb.dma("sp", lambda e, r_=r_: e.dma_start(out=g2r[r_:r_ + 1, :], in_=g2row_d[:]), "ld_g2r", writes=[B_g2r])
        kb.emit("dve", lambda e: e.scalar_tensor_tensor(out=g2r[:], in0=modrow[:, 4 * D:5 * D], scalar=1.0, in1=g2r[:], op0=ALU.add, op1=ALU.mult),
                [B_modrow, B_g2r], [B_g2r])
        kb.dma("sp", lambda e: e.dma_start(out=mod2_d[:, 0:D], in_=g2r[:]), "st_mod2", reads=[B_g2r], writes=[B_mod2d])
        kb.dma("sp", lambda e: e.dma_start(out=mod2_d[:, D:2 * D], in_=modrow[:, 3 * D:4 * D]), "st_mod2", reads=[B_modrow], writes=[B_mod2d])
        kb.barrier()
        kb.flush()

    onesb = sb("onesb", [128, 128], BF16)
    B_onesb = kb.buf()
    kb.emit("pool", lambda e: e.memset(onesb[:], 1.0), [], [B_onesb])
    wq = sb("wq", [128, 3, 768], BF16)
    wkv = sb("wkv", [128, 2, 1024], BF16)
    gq_bc = sb("gq_bc", [128, 8, 96])
    gk_bc = sb("gk_bc", [128, 8, 96])
    cosT = sb("cosT", [128, NSEQ * NT, 16])
    sinT = sb("sinT", [128, NSEQ * NT, 16])
    B_wq, B_wkv, B_gq, B_gk, B_cs = (kb.buf() for _ in range(5))
    with ExitStack() as pw:
        wq_f = sb("wq_f", [128, 3, 768], F32, pw)
        wkv_f = sb("wkv_f", [128, 2, 1024], F32, pw)
        qag = sb("qag", [128, 3], F32, pw)
        kvag = sb("kvag", [128, 2], F32, pw)
        g96 = sb("g96", [128, 2, 96], F32, pw)
        posi = sb("posi", [128, NSEQ * NT], I32, pw)
        posf = sb("posf", [128, NSEQ * NT], F32, pw)
        ang = sb("ang", [128, NSEQ * NT, 16], F32, pw)
        ang2 = sb("ang2", [128, NSEQ * NT, 16], F32, pw)
        B_wqf, B_wkvf, B_qag, B_kvag, B_g96, B_posi, B_posf, B_ang, B_ang2 = (kb.buf() for _ in range(9))
        kb.dma("sp", lambda e: e.dma_start(out=wq_f[:], in_=wq_d[:]), "ld_wqf", writes=[B_wqf])
        kb.dma("sp", lambda e: e.dma_start(out=wkv_f[:], in_=wkv_d[:]), "ld_wkvf", writes=[B_wkvf])
        kb.dma("sp", lambda e: e.dma_start(out=qag[:], in_=qag_d[:]), "ld_qag", writes=[B_qag])
        kb.dma("sp", lambda e: e.dma_start(out=kvag[:], in_=kvag_d[:]), "ld_kvag", writes=[B_kvag])
        kb.dma("sp", lambda e: e.dma_start(out=g96[:, 0, :], in_=qng_d.partition_broadcast(128)), "ld_g96", writes=[B_g96])
        kb.dma("sp", lambda e: e.dma_start(out=g96[:, 1, :], in_=kng_d.partition_broadcast(128)), "ld_g96", writes=[B_g96])
        kb.dma("sp", lambda e: e.dma_start(out=posi[:], in_=pos_d[:]), "ld_pos", writes=[B_posi])
        for c in range(3):
            kb.emit("dve", lambda e, c=c: e.tensor_scalar_mul(out=wq[:, c, :], in0=wq_f[:, c, :], scalar1=qag[:, c:c + 1]),
                    [B_wqf, B_qag], [B_wq])
        for c in range(2):
            kb.emit("dve", lambda e, c=c: e.tensor_scalar_mul(out=wkv[:, c, :], in0=wkv_f[:, c, :], scalar1=kvag[:, c:c + 1]),
                    [B_wkvf, B_kvag], [B_wkv])
        kb.emit("dve", lambda e: e.tensor_scalar_mul(out=gq_bc[:], in0=g96[:, 0:1, :].to_broadcast([128, 8, 96]),
                                                     scalar1=float(96 ** -0.5)), [B_g96], [B_gq])
        kb.emit("dve", lambda e: e.tensor_copy(out=gk_bc[:], in_=g96[:, 1:2, :].to_broadcast([128, 8, 96])), [B_g96], [B_gk])
        kb.emit("dve", lambda e: e.tensor_copy(out=posf[:], in_=posi[:]), [B_posi], [B_posf])
        kb.emit("dve", lambda e: e.tensor_tensor(out=ang[:], in0=posf[:].unsqueeze(2).to_broadcast([128, NSEQ * NT, 16]),
                                                 in1=cst[:, 514:530].unsqueeze(1).to_broadcast([128, NSEQ * NT, 16]),
                                                 op=ALU.mult), [B_posf, B_cst], [B_ang])
        PI = float(np.pi)
        angi = sb("angi", [128, NSEQ * NT, 16], I32, pw)
        B_angi = kb.buf()

        def sin_table(dst, shift):
            kb.emit("dve", lambda e: e.tensor_scalar(out=ang2[:], in0=ang[:], scalar1=float(1.0 / (2 * PI)), scalar2=None, op0=ALU.mult),
                    [B_ang], [B_ang2])
            kb.emit("dve", lambda e: e.tensor_copy(out=angi[:], in_=ang2[:]), [B_ang2], [B_angi])
            kb.emit("dve", lambda e: e.tensor_copy(out=ang2[:], in_=angi[:]), [B_angi], [B_ang2])
            kb.emit("dve", lambda e: e.scalar_tensor_tensor(out=ang2[:], in0=ang2[:], scalar=-2 * PI, in1=ang[:], op0=ALU.mult, op1=ALU.add),
                    [B_ang2, B_ang], [B_ang2])
            if shift != 0.0:
                kb.emit("dve", lambda e: e.tensor_scalar(out=ang2[:], in0=ang2[:], scalar1=float(shift), scalar2=None, op0=ALU.add),
                        [B_ang2], [B_ang2])
            kb.emit("dve", lambda e: e.tensor_scalar(out=angi[:].bitcast(F32), in0=ang2[:], scalar1=PI, scalar2=-2 * PI, op0=ALU.is_gt, op1=ALU.mult),
                    [B_ang2], [B_angi])
            kb.emit("dve", lambda e: e.tensor_tensor(out=ang2[:], in0=ang2[:], in1=angi[:].bitcast(F32), op=ALU.add), [B_ang2, B_angi], [B_ang2])
            kb.emit("dve", lambda e: e.tensor_scalar(out=angi[:].bitcast(F32), in0=ang2[:], scalar1=-PI, scalar2=2 * PI, op0=ALU.is_lt, op1=ALU.mult),
                    [B_ang2], [B_angi])
            kb.emit("dve", lambda e: e.tensor_tensor(out=ang2[:], in0=ang2[:], in1=angi[:].bitcast(F32), op=ALU.add), [B_ang2, B_angi], [B_ang2])
            kb.emit("act", lambda e: e.activation(out=dst[:], in_=ang2[:], func=AF.Sin), [B_ang2], [B_cs])

        sin_table(sinT, 0.0)
        sin_table(cosT, PI / 2)
        kb.barrier()
        kb.flush()

    wr = sb("wr", [128, 8, 36], BF16)
    brb = sb("brb", [128, 36])
    EIDX = sb("EIDX", [128, NSEQ * NT, 2])
    WK = sb("WK", [128, NSEQ * NT, 2])
    iota_e = sb("iota_e", [128, 32])
    B_wr, B_brb, B_iota = kb.buf(), kb.buf(), kb.buf()
    B_EW = [kb.buf() for _ in range(NSEQ * NT)]
    kb.dma("sp", lambda e: e.dma_start(out=iota_e[:], in_=cstm_d[:, 128:160]), "ld_iota", writes=[B_iota])
    kb.dma("pool", lambda e: e.dma_start(out=wr[:], in_=wr_d[:]), "ld_wr", writes=[B_wr])
    kb.dma("sp", lambda e: e.dma_start(out=brb[:], in_=br_d.partition_broadcast(128)), "ld_brb", writes=[B_brb])
    B_x1d = [[kb.buf() for _ in range(NT)] for _ in range(NSEQ)]
    B_h2d = [[kb.buf() for _ in range(NT)] for _ in range(NSEQ)]

    def dbg_store(name, ap, bufs):
        if dbg and name in dbg:
            kb.dma("sp", lambda e: e.dma_start(out=dbg_d[name][:], in_=ap), "st_dbg", reads=bufs)

    class NormT:
        def __init__(self, stack, tag, ntp=2, dve_evac=False):
            self.dve_evac = dve_evac
            self.xn = [sb("xn%s%d" % (tag, i), [128, D], BF16, stack) for i in range(2)]
            self.st = sb("st" + tag, [128, 2, 4], F32, stack)
            tps = [ps("tp%s%d" % (tag, i), [128, 8, 128], BF16, stack) for i in range(ntp)]
            btp = [kb.pbuf() for _ in range(ntp)]
            self.tp = [tps[i % ntp] for i in range(2)]
            self.B_tp = [btp[i % ntp] for i in range(2)]
            self.B_xn = [kb.buf(), kb.buf()]
            self.B_st = [kb.buf(), kb.buf()]
            self.n = 0

        def run(self, src, B_src, s, jg, dst, B_dst):
            j = self.n % 2
            self.n += 1
            issue_cv(1)
            st, xn, tp = self.st, self.xn[j], self.tp[j]
            junk = xn
            B_st, B_xn, B_tp = self.B_st[j], self.B_xn[j], self.B_tp[j]
            kb.emit("act", lambda e: e.activation(out=junk[:], in_=src, func=AF.Square, accum_out=st[:, j, 0:1]),
                    [B_src], [B_xn, B_st])
            kb.emit("act", lambda e: e.activation(out=st[:, j, 1:2], in_=st[:, j, 0:1], func=AF.Ln, scale=1.0 / D, bias=cst[:, 919:920]),
                    [B_st, B_cst], [B_st])
            kb.emit("act", lambda e: e.activation(out=st[:, j, 2:3], in_=st[:, j, 1:2], func=AF.Exp, scale=-0.5), [B_st], [B_st])
            kb.emit("dve", lambda e: e.tensor_scalar_mul(out=xn[:], in0=src, scalar1=st[:, j, 2:3]), [B_src, B_st], [B_xn])
            for kc in range(8):
                kb.emit("pe", lambda e, kc=kc: e.transpose(out=tp[:, kc, :], in_=xn[:, kc * 128:(kc + 1) * 128], identity=identb[:]),
                        [B_xn, B_identb], [B_tp], inc=(kc == 7))
            if self.dve_evac:
                kb.emit("dve", lambda e: e.tensor_tensor(out=dst, in0=tp[:], in1=modcol[:, s, jg, :].unsqueeze(2).to_broadcast([128, 8, 128]), op=ALU.mult),
                        [B_tp, B_modcol], [B_dst])
                kb.emit("pool", lambda e: e.tensor_tensor(out=dst, in0=dst, in1=modcol[:, s, jg + 1, :].unsqueeze(2).to_broadcast([128, 8, 128]), op=ALU.add),
                        [B_dst, B_modcol], [B_dst])
            else:
                for kc in range(8):
                    kb.emit("act", lambda e, kc=kc: e.activation(out=dst[:, kc, :], in_=tp[:, kc, :], func=AF.Identity,
                                                                 bias=modcol[:, s, jg + 1, kc:kc + 1], scale=modcol[:, s, jg, kc:kc + 1]),
                            [B_tp, B_modcol], [B_dst])
            return xn, B_xn

    nseq_run = NSEQ if stop is None else 1
    for s in range(nseq_run):
        with ExitStack() as sq_:
            OT = sb("OT", [64, 8, S], BF16, sq_)
            B_OT = kb.buf()

            with ExitStack() as pm:
                QT = sb("QT", [128, 8, S if stop != "hT" else 4], BF16, pm)
                KT = sb("KT", [128, 8, S if stop != "hT" else 4], BF16, pm)
                V2 = sb("V2", [128, NT, 8, 65], BF16, pm)
                rstdk = sb("rstdk", [128, NT, 8], F32, pm)
                B_QT, B_KT, B_V2, B_rk = kb.buf(), kb.buf(), kb.buf(), kb.buf()
                kb.emit("pool", lambda e: e.memset(V2[:, :, :, 64:65], 1.0), [], [B_V2])
                with ExitStack() as pb:
                    winm = sb("winm", [128, 8, 672], BF16, pb)
                    B_winm = kb.buf()
                    kb.dma("pool", lambda e: e.dma_start(out=winm[:], in_=win_d[:, :, 0:672]), "ld_winm", writes=[B_winm])
                    xt = [sb("xt0", [128, D], F32, pb), sb("xt1", [128, D], F32, pb)]
                    B_xt = [kb.buf(), kb.buf()]
                    nrm = NormT(pb, "a", ntp=1)
                    hTc = sb("hTc", [128, 8, 512], BF16, pb)
                    B_hTc = [kb.buf() for _ in range(4)]
                    latT = sb("latT", [128, 5, 512], BF16, pb)
                    sqT = sb("sqT", [128, 5, 512], BF16, pb)
                    B_latT, B_sqT = kb.buf(), kb.buf()
                    plat = [ps("plat%d" % i, [128, 512], F32, pb) for i in range(2)]
                    B_plat = [kb.pbuf(), kb.pbuf()]
                    pq = ps("pq", [128, 1024], F32, pb)
                    B_pq = kb.pbuf()
                    pkv = ps("pkv", [128, 1024], F32, pb)
                    B_pkv = kb.pbuf()
                    psm = ps("psm", [128, 64], F32, pb)
                    B_pss = kb.pbuf()
                    B_pkr = B_pss
                    ptq = nrm.tp[0]
                    B_ptq = nrm.B_tp[0]
                    rst4 = sb("rst", [128, 4, 4], F32, pb)
                    B_rst = kb.buf()
                    hstk = sb("hstk", [128, 3, 8], F32, pb)
                    rpk = sb("rpk", [128, 4, 1, 16], F32, pb)
                    B_hstk, B_rpk = kb.buf(), kb.buf()
                    qf = sb("qf", [128, 8, 96], F32, pb)
                    qsq = sb("qsq", [128, 8, 96], F32, pb)
                    qn = sb("qn", [128, 8, 96], F32, pb)
                    hst = sb("hst", [128, 3, 8], F32, pb)
                    rp_q = sb("rp", [128, 4, 8, 16], F32, pb)
                    qfin = sb("qfin", [128, 8, 96], BF16, pb)
                    kvf = sb("kvf", [128, 8, 128], F32, pb)
                    ksq = sb("ksq", [128, 8, 64], F32, pb)
                    krf = sb("krf", [128, 3, 32], F32, pb)
                    kst = sb("kst", [128, 4], F32, pb)
                    kfin = sb("kfin", [128, 8, 96], BF16, pb)
                    B_qf, B_qsq, B_qn, B_hst, B_rp_q, B_qfin, B_kvf, B_ksq, B_krf, B_kst, B_kfin = (kb.buf() for _ in range(11))

                    def rope(src3, dst3, cs_i, nh, Bsrc, Bdst, rp=None, B_rp=None):
                        if rp is None:
                            rp, B_rp = rp_q, B_rp_q
                        cb = cosT[:, cs_i:cs_i + 1, :].to_broadcast([128, nh, 16])
                        sbb = sinT[:, cs_i:cs_i + 1, :].to_broadcast([128, nh, 16])
                        x1 = src3[:, :, 0:16]
                        x2 = src3[:, :, 16:32]
                        r = rp[:, :, 0:nh, :]
                        kb.emit("dve", lambda e: e.tensor_tensor(out=r[:, 0], in0=x1, in1=cb, op=ALU.mult), [Bsrc, B_cs], [B_rp])
                        kb.emit("dve", lambda e: e.tensor_tensor(out=r[:, 1], in0=x2, in1=sbb, op=ALU.mult), [Bsrc, B_cs], [B_rp])
                        kb.emit("dve", lambda e: e.tensor_tensor(out=r[:, 2], in0=x2, in1=cb, op=ALU.mult), [Bsrc, B_cs], [B_rp])
                        kb.emit("dve", lambda e: e.tensor_tensor(out=r[:, 3], in0=x1, in1=sbb, op=ALU.mult), [Bsrc, B_cs], [B_rp])
                        kb.emit("dve", lambda e: e.tensor_tensor(out=dst3[:, :, 0:16], in0=r[:, 0], in1=r[:, 1], op=ALU.subtract),
                                [B_rp], [Bdst])
                        kb.emit("dve", lambda e: e.tensor_tensor(out=dst3[:, :, 16:32], in0=r[:, 2], in1=r[:, 3], op=ALU.add),
                                [B_rp], [Bdst])

                    ncc = 4 if stop != "hT" else 1
                    def ld_x(i):
                        j = i % 2
                        kb.dma("sp", lambda e: e.dma_start(out=xt[j][:], in_=x_d[s, i * 128:(i + 1) * 128, :]), "ld_xt%d" % j, writes=[B_xt[j]])

                    ld_x(0)
                    ld_x(1)
                    for cc in range(ncc):
                        for ti in range(4):
                            i = cc * 4 + ti
                            j = i % 2
                            nrm.run(xt[j][:], B_xt[j], s, 0, hTc[:, :, ti * 128:(ti + 1) * 128], B_hTc[ti])
                            if i + 2 < 4 * ncc:
                                ld_x(i + 2)
                        if stop == "hT":
                            break
                        for jc in range(5):
                            pj = jc % 2
                            for kc in range(8):
                                kb.emit("pe", lambda e, pj=pj, jc=jc, kc=kc: e.matmul(
                                    plat[pj][:], lhsT=winm[:, kc, jc * 128:(jc + 1) * 128], rhs=hTc[:, kc, :],
                                    start=(kc == 0), stop=(kc == 7)), [B_winm] + B_hTc, [B_plat[pj]], inc=(kc == 7))
                            kb.emit("act", lambda e, pj=pj, jc=jc: e.copy(out=latT[:, jc, :], in_=plat[pj][:]), [B_plat[pj]], [B_latT])
                            kb.emit("act", lambda e, pj=pj, jc=jc: e.activation(out=sqT[:, jc, :], in_=plat[pj][:], func=AF.Square),
                                    [B_plat[pj]], [B_sqT])
                        for ti in range(4):
                            t0 = ti * 128
                            rst = rst4[:, ti, :]
                            for jc in range(5):
                                col = 0 if jc < 3 else 1
                                kb.emit("pe", lambda e, jc=jc, col=col, t0=t0: e.matmul(
                                    psm[:, col:col + 1], lhsT=sqT[:, jc, t0:t0 + 128], rhs=onesb[:, 0:1],
                                    start=(jc in (0, 3)), stop=(jc in (2, 4))), [B_sqT, B_onesb], [B_pss], inc=(jc == 4))
                            kb.emit("dve", lambda e, rst=rst: e.tensor_tensor(out=rst[:, 0:2], in0=psm[:, 0:2], in1=cst[:, 916:918], op=ALU.mult),
                                    [B_pss, B_cst], [B_rst])
                            kb.emit("act", lambda e, rst=rst: e.activation(out=rst[:, 2:4], in_=rst[:, 0:2], func=AF.Ln, bias=cst[:, 919:920]),
                                    [B_rst, B_cst], [B_rst])
                            kb.emit("act", lambda e, rst=rst: e.activation(out=rst[:, 2:4], in_=rst[:, 2:4], func=AF.Exp, scale=-0.5), [B_rst], [B_rst])

                        def qchain(ti):
                            i = cc * 4 + ti
                            gi = s * NT + i
                            t0 = ti * 128
                            rst = rst4[:, ti, :]
                            for c in range(3):
                                kb.emit("pe", lambda e, c=c: e.matmul(pq[:, 0:512], lhsT=latT[:, c, t0:t0 + 128], rhs=wq[:, c, 0:512],
                                                                      start=(c == 0), stop=(c == 2)), [B_latT, B_wq], [B_pq], inc=False)
                            for c in range(3):
                                kb.emit("pe", lambda e, c=c: e.matmul(pq[:, 512:768], lhsT=latT[:, c, t0:t0 + 128], rhs=wq[:, c, 512:768],
                                                                      start=(c == 0), stop=(c == 2)), [B_latT, B_wq], [B_pq], inc=(c == 2))
                            yield
                            qf2 = qf[:].rearrange("p h d -> p (h d)")
                            kb.emit("act", lambda e: e.activation(out=qf2, in_=pq[:, 0:768], func=AF.Copy, scale=rst[:, 2:3]), [B_pq, B_rst], [B_qf])
                            yield
                            kb.emit("act", lambda e: e.activation(out=qsq[:], in_=qf[:], func=AF.Square), [B_qf], [B_qsq])
                            yield
                            kb.emit("dve", lambda e: e.tensor_reduce(out=hst[:, 0, :], in_=qsq[:], axis=AX.X, op=ALU.add), [B_qsq], [B_hst])
                            yield
                            kb.emit("act", lambda e: e.activation(out=hst[:, 1, :], in_=hst[:, 0, :], func=AF.Ln, scale=1.0 / 96, bias=cst[:, 919:920]),
                                    [B_hst, B_cst], [B_hst])
                            kb.emit("act", lambda e: e.activation(out=hst[:, 2, :], in_=hst[:, 1, :], func=AF.Exp, scale=-0.5), [B_hst], [B_hst])
                            yield
                            kb.emit("dve", lambda e: e.tensor_tensor(out=qn[:], in0=qf[:], in1=hst[:, 2, :].unsqueeze(2).to_broadcast([128, 8, 96]),
                                                                     op=ALU.mult), [B_qf, B_hst], [B_qn])
                            yield
                            kb.emit("dve", lambda e: e.tensor_tensor(out=qn[:], in0=qn[:], in1=gq_bc[:], op=ALU.mult), [B_qn, B_gq], [B_qn])
                            yield
                            kb.emit("act", lambda e: e.copy(out=qfin[:, :, 0:64], in_=qn[:, :, 0:64]), [B_qn], [B_qfin])
                            rope(qn[:, :, 64:96], qfin[:, :, 64:96], gi, 8, B_qn, B_qfin)
                            yield
                            for h in range(8):
                                kb.emit("pe", lambda e, h=h: e.transpose(out=ptq[0:96, h, :], in_=qfin[:, h, :], identity=identb[:]),
                                        [B_qfin, B_identb], [B_ptq], inc=(h == 7))
                            kb.emit("act", lambda e: e.copy(out=QT[0:96, :, i * 128:(i + 1) * 128], in_=ptq[0:96, :, :]), [B_ptq], [B_QT])
                            yield

                        def kchain(ti):
                            i = cc * 4 + ti
                            gi = s * NT + i
                            t0 = ti * 128
                            rst = rst4[:, ti, :]
                            hst_ = hstk
                            for hh in range(2):
                                for c in range(2):
                                    kb.emit("pe", lambda e, c=c, hh=hh: e.matmul(
                                        pkv[:, hh * 512:(hh + 1) * 512], lhsT=latT[:, 3 + c, t0:t0 + 128], rhs=wkv[:, c, hh * 512:(hh + 1) * 512],
                                        start=(c == 0), stop=(c == 1)), [B_latT, B_wkv], [B_pkv], inc=(c == 1 and hh == 1))
                            for kc in range(8):
                                kb.emit("pe", lambda e, kc=kc: e.matmul(psm[:, 32:64], lhsT=hTc[:, kc, t0:t0 + 128], rhs=winm[:, kc, 640:672],
                                                                        start=(kc == 0), stop=(kc == 7)), [B_hTc[ti], B_winm], [B_pkr], inc=(kc == 7))
                            yield
                            kvf2 = kvf[:].rearrange("p h d -> p (h d)")
                            kb.emit("act", lambda e: e.activation(out=kvf2, in_=pkv[:], func=AF.Copy, scale=rst[:, 3:4]), [B_pkv, B_rst], [B_kvf])
                            yield
                            kb.emit("pool", lambda e: e.tensor_copy(out=V2[:, i, :, 0:64], in_=kvf[:, :, 64:128]), [B_kvf], [B_V2])
                            kb.emit("act", lambda e: e.copy(out=krf[:, 0, :], in_=psm[:, 32:64]), [B_pkr], [B_krf])
                            kb.emit("act", lambda e: e.activation(out=krf[:, 1, :], in_=krf[:, 0, :], func=AF.Square, accum_out=kst[:, 0:1]),
                                    [B_krf], [B_krf, B_kst])
                            yield
                            kb.emit("act", lambda e: e.activation(out=ksq[:], in_=kvf[:, :, 0:64], func=AF.Square), [B_kvf], [B_ksq])
                            yield
                            kb.emit("dve", lambda e: e.tensor_reduce(out=hst_[:, 0, :], in_=ksq[:], axis=AX.X, op=ALU.add), [B_ksq], [B_hstk])
                            kb.emit("dve", lambda e: e.tensor_scalar(out=hst_[:, 1, :], in0=hst_[:, 0, :], scalar1=kst[:, 0:1], scalar2=1.0 / 96,
                                                                     op0=ALU.add, op1=ALU.mult), [B_hstk, B_kst], [B_hstk])
                            yield
                            kb.emit("act", lambda e: e.activation(out=hst_[:, 2, :], in_=hst_[:, 1, :], func=AF.Ln, bias=cst[:, 919:920]),
                                    [B_hstk, B_cst], [B_hstk])
                            kb.emit("act", lambda e: e.activation(out=rstdk[:, i, :], in_=hst_[:, 2, :], func=AF.Exp, scale=-0.5), [B_hstk], [B_rk])
                            yield
                            kb.emit("dve", lambda e: e.tensor_tensor(out=kfin[:, :, 0:64], in0=kvf[:, :, 0:64], in1=gk_bc[:, :, 0:64], op=ALU.mult),
                                    [B_kvf, B_gk], [B_kfin])
                            yield
                            kb.emit("dve", lambda e: e.tensor_tensor(out=krf[:, 1, :], in0=krf[:, 0, :], in1=gk_bc[:, 0, 64:96], op=ALU.mult),
                                    [B_krf, B_gk], [B_krf])
                            rope(krf[:, 1:2, :], krf[:, 2:3, :], gi, 1, B_krf, B_krf, rpk, B_rpk)
                            yield
                            kb.emit("dve", lambda e: e.tensor_copy(out=kfin[:, :, 64:96], in_=krf[:, 2:3, :].to_broadcast([128, 8, 32])),
                                    [B_krf], [B_kfin])
                            for h in range(8):
                                kb.emit("pe", lambda e, h=h: e.transpose(out=ptq[0:96, h, :], in_=kfin[:, h, :], identity=identb[:]),
                                        [B_kfin, B_identb], [B_ptq], inc=(h == 7))
                            kb.emit("act", lambda e: e.copy(out=KT[0:96, :, i * 128:(i + 1) * 128], in_=ptq[0:96, :, :]), [B_ptq], [B_KT])
                            yield

                        def run_il(gens):
                            gens = list(gens)
                            while gens:
                                for g in list(gens):
                                    try:
                                        next(g)
                                    except StopIteration:
                                        gens.remove(g)

                        run_il([qchain(0)])
                        for ti in range(4):
                            gl_ = [kchain(ti)]
                            if ti + 1 < 4:
                                gl_.append(qchain(ti + 1))
                            run_il(gl_)
                    if dbg and "hT" in dbg:
                        hTf = sb("hTf", [128, 8, 512], F32, pb)
                        B_hTf = kb.buf()
                        kb.emit("dve", lambda e: e.tensor_copy(out=hTf[:], in_=hTc[:]), B_hTc, [B_hTf])
                        dbg_store("hT", hTf[:].rearrange("p k t -> p (k t)"), [B_hTf])
                    kb.barrier()
                    kb.flush()
                if stop == "hT":
                    break
                if dbg and "QT" in dbg:
                    with ExitStack() as pd:
                        tf = sb("QTf", [128, 8, 512], F32, pd)
                        B_tf = kb.buf()
                        for nm, src, Bs in (("QT", QT, B_QT), ("KT", KT, B_KT)):
                            dv = dbg_d[nm].rearrange("p (k t) -> p k t", k=8)
                            for cc in range(4):
                                kb.emit("dve", lambda e, src=src, cc=cc: e.tensor_copy(out=tf[0:96], in_=src[0:96, :, cc * 512:(cc + 1) * 512]), [Bs], [B_tf])
                                kb.dma("sp", lambda e, dv=dv, cc=cc: e.dma_start(out=dv[:, :, cc * 512:(cc + 1) * 512], in_=tf[0:96]), "st_dbg", reads=[B_tf])
                        dbg_store("rstdk", rstdk[:].rearrange("p i h -> p (i h)"), [B_rk])
                        kb.barrier(); kb.flush()
                if stop == "QK":
                    break

                with ExitStack() as pat:
                    pst = [ps("pst%d" % i, [128, 512], F32, pat) for i in range(4)]
                    B_pst = [kb.pbuf() for _ in range(4)]
                    po = [ps("po%d" % i, [128, 512], F32, pat) for i in range(2)]
                    B_po = [kb.pbuf() for _ in range(2)]
                    pbc2 = ps("pbc2", [64, 512], F32, pat)
                    B_pbc2 = kb.pbuf()
                    pT = [sb("pT%d" % i, [128, 512], BF16, pat) for i in range(4)]
                    B_pT = [kb.buf() for _ in range(4)]
                    rec = sb("rec", [128, 512], F32, pat)
                    B_rec = kb.buf()
                    recb = sb("recb", [64, 512], F32, pat)
                    B_recb = kb.buf()
                    if s == 0 and (stop is None or stop == "moe"):
                        zt = sb("zt", [128, 2048], BF16, pat)
                        B_zt = kb.buf()
                        kb.emit("pool", lambda e: e.memset(zt[:], 0.0), [], [B_zt])
                        xs_v = xs_d.rearrange("(c p r) d -> c p (r d)", p=128, r=2)
                        for c_ in range(xs_v.shape[0]):
                            kb.dma("sp", lambda e, c_=c_: e.dma_start(out=xs_v[c_], in_=zt[:]), "zf_xs", reads=[B_zt], writes=[B_xsz])
                    units = [(h, qc) for h in range(8) for qc in range(4)]
                    steps = [(u, kt) for u in range(len(units)) for kt in range(NT)]

                    def emit_S(n):
                        u, kt = steps[n]
                        h, qc = units[u]
                        j = n % 4
                        kb.emit("pe", lambda e: e.matmul(pst[j][:], lhsT=KT[0:96, h, kt * 128:(kt + 1) * 128],
                                                         rhs=QT[0:96, h, qc * 512:(qc + 1) * 512], start=True, stop=True),
                                [B_KT, B_QT], [B_pst[j]])
                        kb.emit("act", lambda e: e.activation(out=pT[j][:], in_=pst[j][:], func=AF.Exp, scale=rstdk[:, kt, h:h + 1]),
                                [B_pst[j], B_rk], [B_pT[j]])

                    def emit_PV(n):
                        u, kt = steps[n]
                        h, qc = units[u]
                        j = n % 4
                        a = u % 2
                        kb.emit("pe", lambda e: e.matmul(po[a][0:65, :], lhsT=V2[:, kt, h, :], rhs=pT[j][:], start=(kt == 0), stop=(kt == NT - 1)),
                                [B_V2, B_pT[j]], [B_po[a]], inc=(kt == NT - 1))
                        if kt == NT - 1:
                            kb.emit("dve", lambda e: e.reciprocal(out=rec[64:65, :], in_=po[a][64:65, :]), [B_po[a]], [B_rec])
                            kb.emit("pe", lambda e: e.matmul(pbc2[:], lhsT=ones_f[64:65, 0:64], rhs=rec[64:65, :], start=True, stop=True),
                                    [B_rec, B_cst], [B_pbc2])
                            kb.emit("act", lambda e: e.copy(out=recb[:], in_=pbc2[:]), [B_pbc2], [B_recb])
                            kb.emit("dve", lambda e: e.tensor_tensor(out=OT[0:64, h, qc * 512:(qc + 1) * 512], in0=po[a][0:64, :],
                                                                     in1=recb[:], op=ALU.mult), [B_po[a], B_recb], [B_OT])

                    LA = 3
                    for n in range(len(steps) + LA):
                        if n < len(steps):
                            emit_S(n)
                        if n >= LA:
                            emit_PV(n - LA)
                    kb.barrier()
                    kb.flush()
            if dbg and "OT" in dbg:
                with ExitStack() as pd:
                    tf = sb("OTf", [64, 8, S], F32, pd)
                    B_tf = kb.buf()
                    kb.emit("dve", lambda e: e.tensor_copy(out=tf[:], in_=OT[:]), [B_OT], [B_tf])
                    dbg_store("OT", tf[:].rearrange("p k t -> p (k t)"), [B_tf])
                    kb.barrier(); kb.flush()
            if stop == "attn":
                break


            recT = sb("recT", [128, 4, S], BF16, sq_)
            B_recT = kb.buf()
            with ExitStack() as ph:
                winh = sb("winh", [128, 8, 2560], BF16, ph)
                B_winh5 = [kb.buf() for _ in range(5)]
                for cb in (0, 3, 1, 4, 2):
                    kb.dma("pool", lambda e, cb=cb: e.dma_start(out=winh[:, :, cb * 512:(cb + 1) * 512],
                                                                in_=win_d[:, :, 672 + cb * 512:672 + (cb + 1) * 512]),
                           "ld_winh%d" % cb, writes=[B_winh5[cb]])
                ofw = sb("ofw", [128, NT, 512], BF16, ph)
                B_ofw = [kb.buf() for _ in range(NT)]
                lbc = sb("lbc", [128, 2, 512], F32, ph)
                oml = sb("oml", [128, 2, 512], F32, ph)
                hgn = sb("hgn", [128, 128], F32, ph)
                B_lb, B_hgn = kb.buf(), kb.buf()
                with ExitStack() as pl:
                    lraw = sb("lraw", [128, 2, 2, 512], F32, pl)
                    B_lraw = kb.buf()
                    kb.dma("sp", lambda e: e.dma_start(out=lraw[:].rearrange("p a b n -> p (a b n)"), in_=lbl_d.partition_broadcast(128)),
                           "ld_lraw", writes=[B_lraw])
                    kb.dma("sp", lambda e: e.dma_start(out=hgn[:], in_=hgn_d.partition_broadcast(128)), "ld_hgn", writes=[B_hgn])
                    kb.emit("dve", lambda e: e.tensor_tensor(out=lbc[:], in0=lraw[:, :, 0, :], in1=lraw[:, :, 1, :], op=ALU.subtract),
                            [B_lraw], [B_lb])
                    kb.emit("act", lambda e: e.activation(out=lbc[:], in_=lbc[:], func=AF.Sigmoid), [B_lb], [B_lb])
                    kb.emit("dve", lambda e: e.tensor_scalar(out=oml[:], in0=lbc[:], scalar1=-1.0, scalar2=1.0, op0=ALU.mult, op1=ALU.add),
                            [B_lb], [B_lb])
                    kb.barrier()
                    kb.flush()
                xt0 = sb("xth", [128, D], F32, ph)
                B_xt0 = kb.buf()
                nrm = NormT(ph, "h", ntp=1, dve_evac=True)
                hTt2 = [sb("hTt%d" % i, [128, 8, 128], BF16, ph) for i in range(2)]
                B_hTt2 = [kb.buf(), kb.buf()]
                pg = [ps("pg%d" % i, [128, 512], F32, ph) for i in range(2)]
                B_pg = [kb.pbuf() for _ in range(2)]
                pgn = [0]

                def next_pg():
                    j = pgn[0] % 2
                    pgn[0] += 1
                    return pg[j], B_pg[j]

                PA = ps("hPA", [128, 4, 128], F32, ph)
                PK = ps("hPK", [128, 4, 128], F32, ph)
                PI = ps("hPI", [128, 4, 128], F32, ph)
                PAo = ps("hPAo", [128, 4, 128], F32, ph)
                PBo = ps("hPBo", [128, 4, 128], F32, ph)
                B_PA, B_PK, B_PI, B_PAo, B_PBo = (kb.pbuf() for _ in range(5))
                ptr = nrm.tp[0]
                B_ptr = nrm.B_tp[0]
                qq2 = [sb("hq_q%d" % i, [128, 512], F32, ph) for i in range(2)]
                vv = [sb("hq_v%d" % i, [128, 512], BF16, ph) for i in range(3)]
                gg = [sb("hq_g%d" % i, [128, 512], BF16, ph) for i in range(3)]
                sg = sb("hq_sg", [128, 512], F32, ph)
                ff = sb("hq_f", [128, 512], F32, ph)
                kk = sb("hq_k", [128, 512], F32, ph)
                lf = sb("hq_lf", [128, 512], F32, ph)
                eb = sb("hq_eb", [128, 512], F32, ph)
                enb = sb("hq_enb", [128, 512], F32, ph)
                er = sb("hq_er", [128, 512], F32, ph)
                qkd = sb("hq_qkd", [128, 8, 128], BF16, ph)
                kend = [sb("hq_kend%d" % i, [128, 2, 512], BF16, ph) for i in range(2)]
                qkT = [sb("hq_qkT%d" % i, [128, 8, 128], BF16, ph) for i in range(2)]
                qAB = [sb("hq_qAB%d" % i, [128, 2, 4, 128], BF16, ph) for i in range(2)]
                dec = [sb("hq_dec%d" % i, [128, 4, 2], F32, ph) for i in range(2)]
                attm = sb("hq_attm", [128, 4, 128], BF16, ph)
                Sst = sb("hq_S", [128, 4, 128], F32, ph)
                Sbf = sb("hq_Sb", [128, 4, 128], BF16, ph)
                osum = sb("hq_osum", [128, 4, 128], F32, ph)
                osq = sb("hq_osq", [128, 4, 128], F32, ph)
                ost = sb("hq_ost", [128, 3, 4], F32, ph)
                recb_ = sb("hq_rec", [128, 512], BF16, ph)
                (B_sg, B_ff, B_kk, B_lf, B_eb, B_enb, B_er, B_qkd, B_attm, B_osum, B_osq, B_ost, B_rec2) = (kb.buf() for _ in range(13))
                B_qq2 = [kb.buf(), kb.buf()]
                B_vv = [kb.buf(), kb.buf(), kb.buf()]
                B_gg = [kb.buf(), kb.buf(), kb.buf()]
                B_kend = [kb.buf(), kb.buf()]
                B_qkT = [kb.buf(), kb.buf()]
                B_qAB = [kb.buf(), kb.buf()]
                B_dec = [kb.buf(), kb.buf()]
                B_S = [kb.buf() for _ in range(4)]
                B_Sb = [kb.buf() for _ in range(4)]
                for st_ in range(2):
                    kb.emit("pool", lambda e, st_=st_: e.memset(qAB[st_][:], 0.0), [], [B_qAB[st_]])

                def proj(cb, hTt, B_hTt):
                    p, B_p = next_pg()
                    for kc in range(8):
                        kb.emit("pe", lambda e, kc=kc: e.matmul(p[:], lhsT=hTt[:, kc, :], rhs=winh[:, kc, cb * 512:(cb + 1) * 512],
                                                                start=(kc == 0), stop=(kc == 7)), [B_hTt, B_winh5[cb]], [B_p], inc=(kc == 7))
                    return p, B_p

                def stageA1(d, i, n):
                    hTt, B_hTt = hTt2[n % 2], B_hTt2[n % 2]
                    qq, B_qq = qq2[n % 2], B_qq2[n % 2]
                    s3 = n % 3
                    kb.dma("sp", lambda e: e.dma_start(out=xt0[:], in_=x_d[s, i * 128:(i + 1) * 128, :]), "ld_xth", writes=[B_xt0])
                    nrm.run(xt0[:], B_xt0, s, 0, hTt[:], B_hTt)
                    yield
                    p, B_p = proj(0, hTt, B_hTt)
                    kb.emit("act", lambda e: e.activation(out=qq[:], in_=p[:], func=AF.Silu), [B_p], [B_qq])
                    yield
                    if d == 1:
                        p3, B_p3 = proj(4, hTt, B_hTt)
                        kb.emit("act", lambda e: e.activation(out=gg[s3][:], in_=p3[:], func=AF.Silu), [B_p3], [B_gg[s3]])
                        yield
                    p2, B_p2 = proj(3, hTt, B_hTt)
                    kb.emit("act", lambda e: e.copy(out=vv[s3][:], in_=p2[:]), [B_p2], [B_vv[s3]])
                    yield

                def stageA(d, i, n):
                    st = n % 2
                    hTt, B_hTt = hTt2[n % 2], B_hTt2[n % 2]
                    qq, B_qq = qq2[n % 2], B_qq2[n % 2]
                    tri_c = cst[:, 0:128] if d == 0 else cst[:, 128:256]
                    rev_c = cst[:, 256:384] if d == 0 else cst[:, 384:512]
                    p4, B_p4 = proj(1 + d, hTt, B_hTt)
                    kb.emit("act", lambda e: e.activation(out=sg[:], in_=p4[:], func=AF.Sigmoid), [B_p4], [B_sg])
                    kb.emit("dve", lambda e: e.tensor_tensor(out=ff[:], in0=sg[:], in1=oml[:, d, :], op=ALU.mult), [B_sg, B_lb], [B_ff])
                    kb.emit("dve", lambda e: e.tensor_tensor(out=ff[:], in0=ff[:], in1=lbc[:, d, :], op=ALU.add), [B_ff, B_lb], [B_ff])
                    kb.emit("dve", lambda e: e.tensor_scalar(out=kk[:], in0=ff[:], scalar1=-1.0, scalar2=1.0, op0=ALU.mult, op1=ALU.add),
                            [B_ff], [B_kk])
                    kb.emit("act", lambda e: e.activation(out=lf[:], in_=ff[:], func=AF.Ln), [B_ff], [B_lf])
                    yield
                    pb_, B_pb = next_pg()
                    kb.emit("pe", lambda e: e.matmul(pb_[:], lhsT=tri_c, rhs=lf[:], start=True, stop=True), [B_cst, B_lf], [B_pb])
                    kb.emit("act", lambda e: e.activation(out=eb[:], in_=pb_[:], func=AF.Exp), [B_pb], [B_eb])
                    kb.emit("act", lambda e: e.activation(out=enb[:], in_=pb_[:], func=AF.Exp, scale=-1.0), [B_pb], [B_enb])
                    yield
                    pr_, B_pr = next_pg()
                    kb.emit("pe", lambda e: e.matmul(pr_[:], lhsT=rev_c, rhs=lf[:], start=True, stop=True), [B_cst, B_lf], [B_pr])
                    kb.emit("act", lambda e: e.activation(out=er[:], in_=pr_[:], func=AF.Exp), [B_pr], [B_er])
                    yield
                    pd_, B_pd = next_pg()
                    for h in range(4):
                        kb.emit("pe", lambda e, h=h: e.matmul(pd_[:, 2 * h:2 * h + 2], lhsT=lf[:, h * 128:(h + 1) * 128],
                                                              rhs=cst[:, 512:514], start=True, stop=True), [B_lf, B_cst], [B_pd], inc=(h == 3))
                    kb.emit("act", lambda e: e.activation(out=dec[st][:].rearrange("p h c -> p (h c)"), in_=pd_[:, 0:8], func=AF.Exp),
                            [B_pd], [B_dec[st]])
                    kb.emit("dve", lambda e: e.tensor_tensor(out=qkd[:, 0:4, :].rearrange("p h k -> p (h k)"), in0=qq[:], in1=eb[:], op=ALU.mult),
                            [B_qq, B_eb], [B_qkd])
                    kb.emit("dve", lambda e: e.tensor_tensor(out=qkd[:, 4:8, :].rearrange("p h k -> p (h k)"), in0=kk[:], in1=enb[:], op=ALU.mult),
                            [B_kk, B_enb], [B_qkd])
                    for c2 in range(2):
                        kb.emit("dve", lambda e, c2=c2: e.scalar_tensor_tensor(out=kend[st][:, c2, :], in0=er[:], scalar=cst[:, 512 + c2:513 + c2],
                                                                             in1=kk[:], op0=ALU.mult, op1=ALU.mult),
                                [B_kk, B_er, B_cst], [B_kend[st]])
                    yield
                    for j8 in range(8):
                        kb.emit("pe", lambda e, j8=j8: e.transpose(out=ptr[:, j8, :], in_=qkd[:, j8, :], identity=identb[:]),
                                [B_qkd, B_identb], [B_ptr], inc=(j8 == 7))
                    kb.emit("act", lambda e: e.copy(out=qkT[st][:], in_=ptr[:]), [B_ptr], [B_qkT[st]])
                    kb.emit("dve", lambda e: e.tensor_copy(out=qAB[st][:, 0, :, 0:64], in_=ptr[:, 0:4, 0:64]), [B_ptr], [B_qAB[st]])
                    kb.emit("dve", lambda e: e.tensor_copy(out=qAB[st][:, 1, :, 64:128], in_=ptr[:, 0:4, 64:128]), [B_ptr], [B_qAB[st]])
                    yield

                def stageB(d, i, n):
                    st = n % 2
                    s3 = n % 3
                    tri_c = cst[:, 0:128] if d == 0 else cst[:, 128:256]
                    corder = [0, 1] if d == 0 else [1, 0]
                    for h in range(4):
                        kb.emit("pe", lambda e, h=h: e.matmul(PA[:, h, :], lhsT=qkT[st][:, 4 + h, :], rhs=qkT[st][:, h, :], start=True, stop=True),
                                [B_qkT[st]], [B_PA], inc=(h == 3))
                    yield
                    kb.emit("dve", lambda e: e.tensor_tensor(out=attm[:], in0=PA[:], in1=tri_c.unsqueeze(1).to_broadcast([128, 4, 128]), op=ALU.mult),
                            [B_PA, B_cst], [B_attm])
                    yield
                    for ci, cidx in enumerate(corder):
                        Po_, B_Po_ = (PAo, B_PAo) if ci == 0 else (PBo, B_PBo)
                        for h in range(4):
                            hs = slice(h * 128, (h + 1) * 128)
                            if ci == 0:
                                kb.emit("pe", lambda e, h=h, hs=hs: e.matmul(PI[:, h, :], lhsT=attm[:, h, :], rhs=vv[s3][:, hs], start=True, stop=True),
                                        [B_attm, B_vv[s3]], [B_PI], inc=False)
                            kb.emit("pe", lambda e, h=h, cidx=cidx, Po_=Po_: e.matmul(Po_[:, h, :], lhsT=qAB[st][:, cidx, h, :], rhs=Sbf[:, h, :],
                                                                                    start=True, stop=True), [B_qAB[st], B_Sb[h]], [B_Po_], inc=False)
                            kb.emit("pe", lambda e, h=h, cidx=cidx, hs=hs: e.matmul(PK[:, h, :], lhsT=kend[st][:, cidx, hs], rhs=vv[s3][:, hs],
                                                                                  start=True, stop=True), [B_kend[st], B_vv[s3]], [B_PK], inc=(h == 3))
                        yield
                        for h in range(4):
                            kb.emit("dve", lambda e, h=h, cidx=cidx: e.scalar_tensor_tensor(
                                out=Sst[:, h, :], in0=Sst[:, h, :], scalar=dec[st][:, h, cidx:cidx + 1], in1=PK[:, h, :], op0=ALU.mult, op1=ALU.add),
                                [B_S[h], B_dec[st], B_PK], [B_S[h]])
                            kb.emit("pool", lambda e, h=h: e.tensor_copy(out=Sbf[:, h, :], in_=Sst[:, h, :]), [B_S[h]], [B_Sb[h]])
                        yield
                    kb.emit("act", lambda e: e.copy(out=osum[:], in_=PI[:]), [B_PI], [B_osum])
                    kb.emit("dve", lambda e: e.tensor_tensor(out=osum[:], in0=osum[:], in1=PAo[:], op=ALU.add), [B_osum, B_PAo], [B_osum])
                    if d == 0:
                        kb.emit("dve", lambda e: e.tensor_tensor(out=ofw[:, i, :], in0=osum[:].rearrange("p h v -> p (h v)"),
                                                                 in1=PBo[:].rearrange("p h v -> p (h v)"), op=ALU.add), [B_osum, B_PBo], [B_ofw[i]])
                        yield
                        return
                    kb.emit("dve", lambda e: e.tensor_tensor(out=osum[:], in0=osum[:], in1=PBo[:], op=ALU.add), [B_osum, B_PBo], [B_osum])
                    kb.emit("dve", lambda e: e.tensor_tensor(out=osum[:].rearrange("p h v -> p (h v)"), in0=osum[:].rearrange("p h v -> p (h v)"),
                                                             in1=ofw[:, i, :], op=ALU.add), [B_osum, B_ofw[i]], [B_osum])
                    yield
                    kb.emit("act", lambda e: e.activation(out=osq[:], in_=osum[:], func=AF.Square), [B_osum], [B_osq])
                    kb.emit("dve", lambda e: e.tensor_reduce(out=ost[:, 0, :], in_=osq[:], axis=AX.X, op=ALU.add), [B_osq], [B_ost])
                    kb.emit("act", lambda e: e.activation(out=ost[:, 1, :], in_=ost[:, 0, :], func=AF.Ln, scale=1.0 / 128, bias=cst[:, 919:920]),
                            [B_ost, B_cst], [B_ost])
                    kb.emit("act", lambda e: e.activation(out=ost[:, 2, :], in_=ost[:, 1, :], func=AF.Exp, scale=-0.5), [B_ost], [B_ost])
                    kb.emit("dve", lambda e: e.tensor_tensor(out=osum[:], in0=osum[:], in1=ost[:, 2, :].unsqueeze(2).to_broadcast([128, 4, 128]),
                                                             op=ALU.mult), [B_osum, B_ost], [B_osum])
                    kb.emit("dve", lambda e: e.tensor_tensor(out=osum[:], in0=osum[:], in1=hgn[:].unsqueeze(1).to_broadcast([128, 4, 128]),
                                                             op=ALU.mult), [B_osum, B_hgn], [B_osum])
                    kb.emit("dve", lambda e: e.tensor_tensor(out=recb_[:], in0=osum[:].rearrange("p h v -> p (h v)"), in1=gg[s3][:], op=ALU.mult),
                            [B_osum, B_gg[s3]], [B_rec2])
                    yield
                    for h in range(4):
                        kb.emit("pe", lambda e, h=h: e.transpose(out=ptr[:, h, :], in_=recb_[:, h * 128:(h + 1) * 128], identity=identb[:]),
                                [B_rec2, B_identb], [B_ptr], inc=(h == 3))
                    kb.emit("act", lambda e: e.copy(out=recT[:, :, i * 128:(i + 1) * 128], in_=ptr[:, 0:4, :]), [B_ptr], [B_recT])
                    yield

                def run_interleaved(gens):
                    gens = [g for g in gens if g is not None]
                    while gens:
                        for g in list(gens):
                            try:
                                next(g)
                            except StopIteration:
                                gens.remove(g)

                for d in range(2):
                    kb.emit("pool", lambda e: e.memset(Sst[:], 0.0), [], B_S)
                    kb.emit("pool", lambda e: e.memset(Sbf[:], 0.0), [], B_Sb)
                    order = list(range(NT)) if d == 0 else list(range(NT - 1, -1, -1))
                    run_interleaved([stageA1(d, order[0], 0)])
                    run_interleaved([stageA1(d, order[1], 1), stageA(d, order[0], 0)])
                    for n in range(NT):
                        g1 = stageA1(d, order[n + 2], n + 2) if n + 2 < NT else None
                        g2 = stageA(d, order[n + 1], n + 1) if n + 1 < NT else None
                        run_interleaved([g1, g2, stageB(d, order[n], n)])
                kb.barrier()
                kb.flush()
            if dbg and "recT" in dbg:
                with ExitStack() as pd:
                    tf = sb("recTf", [128, 4, S], F32, pd)
                    B_tf = kb.buf()
                    kb.emit("dve", lambda e: e.tensor_copy(out=tf[:], in_=recT[:]), [B_recT], [B_tf])
                    dbg_store("recT", tf[:].rearrange("p k t -> p (k t)"), [B_tf])
                    kb.barrier(); kb.flush()
            if stop in ("hgrn", "hgrn1"):
                break


            with ExitStack() as po_:
                woa = sb("woa", [64, 8, D], BF16, po_)
                wor = sb("wor", [128, 4, D], BF16, po_)
                B_wo = kb.buf()
                with nullcontext(po_) as pst_:
                    stg = sb("wostg", [128, 4, D], F32, pst_)
                    B_stg = kb.buf()
                    g1b = gatebc[:, s, 0, :].unsqueeze(1).to_broadcast([128, 4, D])
                    for part in range(3):
                        if part < 2:
                            kb.dma("sp", lambda e, part=part: e.dma_start(out=stg[0:64], in_=woa_d[:, part * 4:(part + 1) * 4, :]),
                                   "ld_wostg", writes=[B_stg])
                            kb.emit("dve", lambda e, part=part: e.tensor_tensor(out=woa[:, part * 4:(part + 1) * 4, :], in0=stg[0:64],
                                                                              in1=gatebc[0:64, s, 0, :].unsqueeze(1).to_broadcast([64, 4, D]),
                                                                              op=ALU.mult), [B_stg, B_gatebc], [B_wo])
                        else:
                            kb.dma("sp", lambda e: e.dma_start(out=stg[:], in_=wor_d[:]), "ld_wostg", writes=[B_stg])
                            kb.emit("dve", lambda e: e.tensor_tensor(out=wor[:], in0=stg[:], in1=g1b, op=ALU.mult),
                                    [B_stg, B_gatebc], [B_wo])
                xt0 = sb("xto", [128, D], F32, po_)
                B_xt0 = kb.buf()
                x1t = [sb("x1t%d" % i, [128, D], F32, po_) for i in range(2)]
                B_x1t = [kb.buf(), kb.buf()]
                h2t = [sb("h2t%d" % i, [128, 8, 128], BF16, po_) for i in range(2)]
                B_h2t = [kb.buf(), kb.buf()]
                m2bc = sb("m2bc", [128, 2, D], F32, po_)
                B_m2bc = kb.buf()
                kb.dma("sp", lambda e: e.dma_start(out=m2bc[:].rearrange("p a d -> p (a d)"), in_=mod2_d[s:s + 1, :].partition_broadcast(128)),
                       "ld_m2bc", reads=[B_mod2d], writes=[B_m2bc])
                h2k = [sb("h2k%d" % i, [128, D], BF16, po_) for i in range(2)]
                h2kf = sb("h2kf", [128, D], F32, po_)
                B_h2k = [kb.buf(), kb.buf()]
                B_h2kf = kb.buf()
                nrm = NormT(po_, "o", ntp=2)
                pmx = [ps("pmx%d" % i, [128, D], F32, po_) for i in range(2)]
                B_pmx = [kb.pbuf(), kb.pbuf()]
                plg = ps("plg", [128, 64], F32, po_)
                B_plg = kb.pbuf()
                lga = sb("lga", [128, NT, 36], F32, po_)
                B_lga = kb.buf()
                xt1 = sb("xto1", [128, D], F32, po_)
                xts = [xt0, xt1]
                B_xts = [B_xt0, kb.buf()]

                def op_S1(i):
                    j = i % 2
                    tsl = slice(i * 128, (i + 1) * 128)
                    for hh in range(2):
                        for h in range(8):
                            kb.emit("pe", lambda e, j=j, h=h, hh=hh, tsl=tsl: e.matmul(
                                pmx[j][:, hh * 512:(hh + 1) * 512], lhsT=OT[0:64, h, tsl], rhs=woa[0:64, h, hh * 512:(hh + 1) * 512],
                                start=(h == 0), stop=False), [B_OT, B_wo], [B_pmx[j]], inc=False)
                        for c in range(4):
                            kb.emit("pe", lambda e, j=j, c=c, hh=hh, tsl=tsl: e.matmul(
                                pmx[j][:, hh * 512:(hh + 1) * 512], lhsT=recT[:, c, tsl], rhs=wor[:, c, hh * 512:(hh + 1) * 512],
                                start=False, stop=(c == 3)), [B_recT, B_wo], [B_pmx[j]], inc=(c == 3 and hh == 1))
                    kb.dma("sp", lambda e, i=i, j=j: e.dma_start(out=xts[j][:], in_=x_d[s, i * 128:(i + 1) * 128, :]), "ld_xto%d" % j, writes=[B_xts[j]])
                    kb.emit("dve", lambda e, j=j: e.tensor_tensor(out=x1t[j][:], in0=pmx[j][:], in1=xts[j][:], op=ALU.add),
                            [B_pmx[j], B_xts[j]], [B_x1t[j]])
                    kb.dma("sp", lambda e, i=i, j=j: e.dma_start(out=x1_d[s, i * 128:(i + 1) * 128, :], in_=x1t[j][:]), "st_x1_%d" % j,
                           reads=[B_x1t[j]], writes=[B_x1d[s][i]])

                def op_S2(i):
                    j = i % 2
                    gi = s * NT + i
                    xn_, B_xn_ = nrm.run(x1t[j][:], B_x1t[j], s, 2, h2t[j][:], B_h2t[j])
                    kb.emit("pool", lambda e, xn_=xn_: e.tensor_tensor(out=h2kf[:], in0=xn_[:], in1=m2bc[:, 0, :], op=ALU.mult),
                            [B_xn_, B_m2bc], [B_h2kf])
                    kb.emit("pool", lambda e, j=j: e.tensor_tensor(out=h2k[j][:], in0=h2kf[:], in1=m2bc[:, 1, :], op=ALU.add),
                            [B_h2kf, B_m2bc], [B_h2k[j]])
                    kb.dma("sp", lambda e, gi=gi, j=j: e.dma_start(out=h2tok_d[gi * 128:(gi + 1) * 128, :], in_=h2k[j][:]), "st_h2_%d" % j,
                           reads=[B_h2k[j]], writes=[B_h2d[s][i]])
                    for kc in range(8):
                        kb.emit("pe", lambda e, j=j, kc=kc: e.matmul(plg[:, 0:36], lhsT=h2t[j][:, kc, :], rhs=wr[:, kc, :],
                                                                     start=(kc == 0), stop=(kc == 7)), [B_h2t[j], B_wr], [B_plg], inc=(kc == 7))
                    kb.emit("dve", lambda e, i=i: e.tensor_tensor(out=lga[:, i, :], in0=plg[:, 0:36], in1=brb[:], op=ALU.add), [B_plg, B_brb], [B_lga])

                op_S1(0)
                for i in range(NT):
                    if i + 1 < NT:
                        op_S1(i + 1)
                    op_S2(i)
                g0 = s * NT
                gsel = sb("gsel", [128, 3, NT, 4], F32, po_)
                tkb = sb("tkb", [128, 8, NT], F32, po_)
                elm = sb("elm", [128, 4, NT, 32], F32, po_)
                B_gsel, B_tkb, B_elm = kb.buf(), kb.buf(), kb.buf()
                gl = lga[:, :, 0:4]
                el = lga[:, :, 4:36]
                bc4 = lambda ap: ap.unsqueeze(2).to_broadcast([128, NT, 4])
                bc32 = lambda ap: ap.unsqueeze(2).to_broadcast([128, NT, 32])
                kb.emit("dve", lambda e: e.tensor_reduce(out=tkb[:, 0, :], in_=gl, axis=AX.X, op=ALU.max), [B_lga], [B_tkb])
                kb.emit("dve", lambda e: e.tensor_tensor(out=gsel[:, 0], in0=gl, in1=bc4(tkb[:, 0, :]), op=ALU.is_ge), [B_lga, B_tkb], [B_gsel])
                kb.emit("dve", lambda e: e.tensor_tensor(out=gsel[:, 2], in0=gl, in1=bc4(tkb[:, 0, :]), op=ALU.subtract), [B_lga, B_tkb], [B_gsel])
                kb.emit("act", lambda e: e.activation(out=gsel[:, 2], in_=gsel[:, 2], func=AF.Exp), [B_gsel], [B_gsel])
                kb.emit("dve", lambda e: e.tensor_reduce(out=tkb[:, 1, :], in_=gsel[:, 2], axis=AX.X, op=ALU.add), [B_gsel, B_tkb], [B_tkb])
                kb.emit("dve", lambda e: e.reciprocal(out=tkb[:, 2, :], in_=tkb[:, 1, :]), [B_tkb], [B_tkb])
                kb.emit("dve", lambda e: e.tensor_scalar(out=gsel[:, 1], in0=gsel[:, 0], scalar1=-1.0, scalar2=BIG, op0=ALU.add, op1=ALU.mult),
                        [B_gsel], [B_gsel])
                kb.emit("dve", lambda e: e.tensor_tensor(out=elm[:, 0].rearrange("p t (g e) -> p t g e", g=4),
                                                         in0=lga[:, :, 4:36].rearrange("p t (g e) -> p t g e", g=4),
                                                         in1=gsel[:, 1].unsqueeze(3).to_broadcast([128, NT, 4, 8]), op=ALU.add),
                        [B_lga, B_gsel], [B_elm])
                kb.emit("dve", lambda e: e.tensor_reduce(out=tkb[:, 3, :], in_=elm[:, 0], axis=AX.X, op=ALU.max), [B_elm, B_tkb], [B_tkb])
                kb.emit("dve", lambda e: e.tensor_tensor(out=elm[:, 1], in0=elm[:, 0], in1=bc32(tkb[:, 3, :]), op=ALU.is_ge), [B_elm, B_tkb], [B_elm])
                kb.emit("dve", lambda e: e.scalar_tensor_tensor(out=elm[:, 2], in0=elm[:, 1], scalar=-BIG, in1=elm[:, 0], op0=ALU.mult, op1=ALU.add),
                        [B_elm], [B_elm])
                kb.emit("dve", lambda e: e.tensor_reduce(out=tkb[:, 4, :], in_=elm[:, 2], axis=AX.X, op=ALU.max), [B_elm, B_tkb], [B_tkb])
                kb.emit("dve", lambda e: e.tensor_tensor(out=elm[:, 3], in0=elm[:, 2], in1=bc32(tkb[:, 4, :]), op=ALU.is_ge), [B_elm, B_tkb], [B_elm])
                kb.emit("dve", lambda e: e.tensor_tensor(out=tkb[:, 5, :], in0=tkb[:, 4, :], in1=tkb[:, 3, :], op=ALU.subtract), [B_tkb], [B_tkb])
                kb.emit("act", lambda e: e.activation(out=tkb[:, 5, :], in_=tkb[:, 5, :], func=AF.Exp), [B_tkb], [B_tkb])
                kb.emit("dve", lambda e: e.tensor_scalar(out=tkb[:, 6, :], in0=tkb[:, 5, :], scalar1=1.0, scalar2=None, op0=ALU.add), [B_tkb], [B_tkb])
                kb.emit("dve", lambda e: e.reciprocal(out=tkb[:, 6, :], in_=tkb[:, 6, :]), [B_tkb], [B_tkb])
                kb.emit("dve", lambda e: e.tensor_tensor(out=WK[:, g0:g0 + NT, 0], in0=tkb[:, 6, :], in1=tkb[:, 2, :], op=ALU.mult), [B_tkb], B_EW[g0:g0 + NT])
                kb.emit("dve", lambda e: e.tensor_tensor(out=WK[:, g0:g0 + NT, 1], in0=WK[:, g0:g0 + NT, 0], in1=tkb[:, 5, :], op=ALU.mult),
                        [B_tkb] + B_EW[g0:g0 + NT], B_EW[g0:g0 + NT])
                for k2 in range(2):
                    kb.emit("dve", lambda e, k2=k2: e.tensor_tensor(out=elm[:, 0], in0=elm[:, 1 + 2 * k2], in1=iota_e[:].unsqueeze(1).to_broadcast([128, NT, 32]),
                                                                    op=ALU.mult), [B_elm, B_iota], [B_elm])
                    kb.emit("dve", lambda e, k2=k2: e.tensor_reduce(out=EIDX[:, g0:g0 + NT, k2], in_=elm[:, 0], axis=AX.X, op=ALU.add),
                            [B_elm] + B_EW[g0:g0 + NT], B_EW[g0:g0 + NT])
                kb.barrier()
                kb.flush()
            if stop == "mix":
                break

    if stop == "mix":
        if dbg and "EIDX" in dbg:
            dbg_store("EIDX", EIDX[:].rearrange("p i e -> p (i e)"), B_EW)
            dbg_store("WK", WK[:].rearrange("p i e -> p (i e)"), B_EW)
            kb.barrier()
            kb.flush()

    issue_cv(1000)
    if stop is None or stop == "moe":
        NG = nseq_run * NT
        with ExitStack() as pe_:
            cstm = sb("cstm", [128, 192], F32, pe_)
            crow = sb("crow", [1, 3200], F32, pe_)
            B_cstm, B_crow = kb.buf(), kb.buf()
            kb.dma("sp", lambda e: e.dma_start(out=cstm[:], in_=cstm_d[:]), "ld_cstm", writes=[B_cstm])
            kb.dma("sp", lambda e: e.dma_start(out=crow[:], in_=crow_d[:]), "ld_crow", writes=[B_crow])
            sltb = sb("sltb", [128, 128], BF16, pe_)
            B_sltb = kb.buf()
            kb.emit("dve", lambda e: e.tensor_copy(out=sltb[:], in_=cstm[:, 0:128]), [B_cstm], [B_sltb])
            NGT = NSEQ * NT
            Mb = sb("Mb", [128, NGT, 32], BF16, pe_)
            CS = sb("CS", [128, NGT + 1, 32], F32, pe_)
            RK = sb("RK", [128, NGT, 32], F32, pe_)
            oh = sb("oh", [128, NGT, 2, 32], F32, pe_)
            ohr = sb("ohr", [128, NGT, 32], F32, pe_)
            B_Mb, B_CS, B_RK, B_oh, B_ohr = (kb.buf() for _ in range(5))
            SLOTF = sb("SLOTF", [128, NGT, 2], F32, pe_)
            SLOT = sb("SLOT", [128, NGT, 2], I32, pe_)
            B_slotf, B_slot = kb.buf(), kb.buf()
            IDXW = sb("IDXW", [128, NBB, 2], I32, pe_)
            B_idxw = kb.buf()
            with ExitStack() as pr_:
                pcs = ps("pcs", [128, 1024], F32, pr_)
                B_pcs = kb.pbuf()
                prk = ps("prk", [128, 1024], F32, pr_)
                B_prk = kb.pbuf()
                pbcr = ps("pbcr", [128, 96], F32, pr_)
                B_pbcr = kb.pbuf()
                kb.emit("dve", lambda e: e.tensor_tensor(out=oh[:].rearrange("p g k e -> p (g k) e"),
                                                         in0=iota_e[:].unsqueeze(1).to_broadcast([128, NGT * 2, 32]),
                                                         in1=EIDX[:].rearrange("p g k -> p (g k)").unsqueeze(2).to_broadcast([128, NGT * 2, 32]),
                                                         op=ALU.is_equal), [B_iota] + B_EW, [B_oh])
                kb.emit("dve", lambda e: e.tensor_tensor(out=Mb[:], in0=oh[:, :, 0, :], in1=oh[:, :, 1, :], op=ALU.add), [B_oh], [B_Mb])
                Mbf = Mb[:].rearrange("p g e -> p (g e)")
                for hh in range(2):
                    kb.emit("pe", lambda e, hh=hh: e.matmul(pcs[:, hh * 512:(hh + 1) * 512], lhsT=onesb[:], rhs=Mbf[:, hh * 512:(hh + 1) * 512], start=True, stop=True),
                            [B_onesb, B_Mb], [B_pcs], inc=(hh == 1))
                for hh in range(2):
                    kb.emit("pe", lambda e, hh=hh: e.matmul(prk[:, hh * 512:(hh + 1) * 512], lhsT=sltb[:], rhs=Mbf[:, hh * 512:(hh + 1) * 512], start=True, stop=True),
                            [B_sltb, B_Mb], [B_prk], inc=(hh == 1))
                kb.emit("pool", lambda e: e.memset(CS[:, 0, :], 0.0), [], [B_CS])
                for g in range(NGT):
                    kb.emit("dve", lambda e, g=g: e.tensor_tensor(out=CS[:, g + 1, :], in0=CS[:, g, :], in1=pcs[:, g * 32:(g + 1) * 32], op=ALU.add),
                            [B_CS, B_pcs], [B_CS])
                kb.emit("dve", lambda e: e.tensor_tensor(out=RK[:].rearrange("p g e -> p (g e)"), in0=prk[:], in1=CS[:, 0:NGT, :].rearrange("p g e -> p (g e)"), op=ALU.add),
                        [B_prk, B_CS], [B_RK])
                rw = sb("rw", [1, 8, 64], F32, pr_)
                g1 = sb("g1", [1, 2048], F32, pr_)
                B_rw, B_g1 = kb.buf(), kb.buf()
                kb.emit("pool", lambda e: e.memset(rw[:], 0.0), [], [B_rw])
                kb.emit("dve", lambda e: e.tensor_copy(out=rw[:, 0, 0:32], in_=CS[0:1, NGT, :]), [B_CS, B_rw], [B_rw])
                kb.emit("dve", lambda e: e.tensor_tensor(out=g1[:, 0:512].rearrange("p (a b) -> p a b", a=32),
                                                         in0=rw[:, 0, 0:32].unsqueeze(2).to_broadcast([1, 32, 16]),
                                                         in1=crow[:, 0:16].unsqueeze(1).to_broadcast([1, 32, 16]), op=ALU.is_gt),
                        [B_rw, B_crow], [B_g1])
                kb.emit("dve", lambda e: e.tensor_reduce(out=rw[:, 1, 0:32], in_=g1[:, 0:512].rearrange("p (a b) -> p a b", a=32), axis=AX.X, op=ALU.add),
                        [B_g1, B_rw], [B_rw])
                kb.emit("dve", lambda e: e.tensor_tensor(out=g1[:, 0:1024].rearrange("p (a b) -> p a b", a=32),
                                                         in0=rw[:, 1, 0:32].unsqueeze(1).to_broadcast([1, 32, 32]),
                                                         in1=crow[:, 16:1040].rearrange("p (a b) -> p a b", a=32), op=ALU.mult),
                        [B_rw, B_crow], [B_g1])
                kb.emit("dve", lambda e: e.tensor_reduce(out=rw[:, 2, 0:32], in_=g1[:, 0:1024].rearrange("p (a b) -> p a b", a=32), axis=AX.X, op=ALU.add),
                        [B_g1, B_rw], [B_rw])
                kb.emit("dve", lambda e: e.tensor_tensor(out=rw[:, 3, 0:32], in0=rw[:, 2, 0:32], in1=rw[:, 1, 0:32], op=ALU.subtract), [B_rw], [B_rw])
                kb.emit("dve", lambda e: e.tensor_scalar(out=rw[:, 3, 0:32], in0=rw[:, 3, 0:32], scalar1=256.0, scalar2=None, op0=ALU.mult), [B_rw], [B_rw])
                kb.emit("dve", lambda e: e.tensor_tensor(out=g1[:].rearrange("p (a b) -> p a b", a=64),
                                                         in0=rw[:, 2, 0:32].unsqueeze(1).to_broadcast([1, 64, 32]),
                                                         in1=crow[:, 1040:3088].rearrange("p (a b) -> p a b", a=64), op=ALU.is_le),
                        [B_rw, B_crow], [B_g1])
                kb.emit("dve", lambda e: e.tensor_reduce(out=rw[:, 4, :], in_=g1[:].rearrange("p (a b) -> p a b", a=64), axis=AX.X, op=ALU.add),
                        [B_g1, B_rw], [B_rw])
                kb.emit("dve", lambda e: e.tensor_scalar(out=rw[:, 4, :], in0=rw[:, 4, :], scalar1=31.0, scalar2=None, op0=ALU.min), [B_rw], [B_rw])
                kb.emit("dve", lambda e: e.tensor_tensor(out=rw[:, 5, 2:64], in0=rw[:, 4, 2:64], in1=rw[:, 4, 0:62], op=ALU.is_equal), [B_rw], [B_rw])
                kb.emit("dve", lambda e: e.tensor_scalar(out=rw[:, 5, :], in0=rw[:, 5, :], scalar1=5.0e8, scalar2=None, op0=ALU.mult), [B_rw], [B_rw])
                kb.emit("dve", lambda e: e.scalar_tensor_tensor(out=rw[:, 6, :], in0=rw[:, 4, :], scalar=128.0, in1=rw[:, 5, :], op0=ALU.mult, op1=ALU.add),
                        [B_rw], [B_rw])
                kb.emit("pe", lambda e: e.matmul(pbcr[:, 0:64], lhsT=ones_f[0:1, 0:128], rhs=rw[:, 6, :], start=True, stop=True), [B_cst, B_rw], [B_pbcr], inc=False)
                kb.emit("pe", lambda e: e.matmul(pbcr[:, 64:96], lhsT=ones_f[0:1, 0:128], rhs=rw[:, 3, 0:32], start=True, stop=True), [B_cst, B_rw], [B_pbcr])
                bcs = sb("bcs", [128, 96], F32, pr_)
                idf = sb("idf", [128, NBB, 2], F32, pr_)
                B_bcs, B_idf = kb.buf(), kb.buf()
                kb.emit("dve", lambda e: e.tensor_copy(out=bcs[:], in_=pbcr[:]), [B_pbcr], [B_bcs])
                kb.emit("dve", lambda e: e.tensor_scalar(out=idf[:, :, 0], in0=bcs[:, 0:64], scalar1=cstm[:, 160:161], scalar2=None, op0=ALU.add),
                        [B_bcs, B_cstm], [B_idf])
                kb.emit("dve", lambda e: e.tensor_scalar(out=idf[:, :, 1], in0=idf[:, :, 0], scalar1=1.0, scalar2=None, op0=ALU.add), [B_idf], [B_idf])
                kb.emit("dve", lambda e: e.tensor_copy(out=IDXW[:], in_=idf[:]), [B_idf], [B_idxw])
                kb.emit("dve", lambda e: e.tensor_tensor(out=RK[:], in0=RK[:], in1=bcs[:, 64:96].unsqueeze(1).to_broadcast([128, NGT, 32]), op=ALU.add),
                        [B_RK, B_bcs], [B_RK])
                for k2 in range(2):
                    kb.emit("dve", lambda e, k2=k2: e.tensor_tensor(out=ohr[:], in0=oh[:, :, k2, :], in1=RK[:], op=ALU.mult), [B_oh, B_RK], [B_ohr])
                    kb.emit("dve", lambda e, k2=k2: e.tensor_reduce(out=SLOTF[:, :, k2], in_=ohr[:], axis=AX.X, op=ALU.add), [B_ohr, B_slotf], [B_slotf])
                kb.emit("dve", lambda e: e.tensor_copy(out=SLOT[:], in_=SLOTF[:]), [B_slotf], [B_slot])
                if dbg and "SLOT" in dbg:
                    dbg_store("SLOT", SLOTF[:].rearrange("p i e -> p (i e)"), [B_slotf])
                    dbg_store("BE", rw[:].rearrange("p a b -> p (a b)"), [B_rw])
                kb.barrier()
                kb.flush()
            with nullcontext(pe_) as pd_:
                hk = [sb("hk%d" % i, [128, D], BF16, pd_) for i in range(2)]
                B_hk = [kb.buf(), kb.buf()]
                for gi in range(NG):
                    j = gi % 2
                    s_, i_ = gi // NT, gi % NT
                    kb.dma("sp", lambda e, gi=gi, j=j: e.dma_start(out=hk[j][:], in_=h2tok_d[gi * 128:(gi + 1) * 128, :]), "ld_hk%d" % j,
                           reads=[B_h2d[s_][i_]], writes=[B_hk[j]])
                    for k2 in range(2):
                        kb.dma("pool", lambda e, gi=gi, j=j, k2=k2: e.indirect_dma_start(
                            out=xs_d[:, :], out_offset=bass.IndirectOffsetOnAxis(ap=SLOT[:, gi, k2:k2 + 1], axis=0), in_=hk[j][:], in_offset=None),
                            "sc_xs%d" % j, reads=[B_hk[j], B_slot, B_xsz], writes=[B_xs2[j]])
            B_ys2 = [kb.buf("ys0"), kb.buf("ys1")]
            with nullcontext(pe_) as px_:
                wall = [sb("wall%d" % i, [128, 3 * 4096], BF16, px_) for i in range(2)]
                wge = [wall[i][:, 0:4096].rearrange("p (k n) -> p k n", k=8) for i in range(2)]
                wue = [wall[i][:, 4096:8192].rearrange("p (k n) -> p k n", k=8) for i in range(2)]
                wde = [wall[i][:, 8192:12288].rearrange("p (k n) -> p k n", k=4) for i in range(2)]
                B_we = [kb.buf(), kb.buf()]
                xb = [sb("xb%d" % i, [128, 2, D], BF16, px_) for i in range(2)]
                B_xb = [kb.buf(), kb.buf()]
                xsT = [sb("xsT%d" % i, [128, 8, 256], BF16, px_) for i in range(2)]
                B_xsT = [kb.buf(), kb.buf()]
                hid = [sb("hid%d" % i, [128, 4, 256], BF16, px_) for i in range(2)]
                B_hid = [kb.buf(), kb.buf()]
                sgt = [sb("sgt%d" % i, [128, 256], F32, px_) for i in range(2)]
                B_sgt = [kb.buf(), kb.buf()]
                ysb = [sb("ysb%d" % i, [128, D], F32, px_) for i in range(2)]
                B_ysb = [kb.buf(), kb.buf()]
                ptx0 = ps("ptx0", [128, 8, 128], BF16, px_)
                B_ptx0 = kb.pbuf()
                ptx = [ptx0, ptx0]
                B_ptx = [B_ptx0, B_ptx0]
                pgu = [ps("pgu%d" % i, [128, 256], F32, px_) for i in range(4)]
                B_pgu = [kb.pbuf() for _ in range(4)]
                pyh = [ps("pyh%d" % i, [128, 512], F32, px_) for i in range(3)]
                B_pyh = [kb.pbuf() for _ in range(3)]
                cnt_hb = 0
                nbb_run = (NBB - 1) if stop is None else 8
                cnt_g = 0
                cnt_t = 0
                cnt_y = 0
                for b in range(nbb_run):
                    wj = b % 2
                    kb.dma("pool", lambda e, b=b, wj=wj: e.indirect_dma_start(
                        out=wall[wj][:, :], out_offset=None, in_=wbf_d[:, :],
                        in_offset=bass.IndirectOffsetOnAxis(ap=IDXW[:, b, 0:1], axis=0),
                        bounds_check=kb.const_reg(e, NEXP * 128 - 1), oob_is_err=False), "ld_we%d" % wj, reads=[B_idxw, B_wbf], writes=[B_we[wj]])
                    xj = b % 2
                    for bb_ in ([0, 1] if b == 0 else [b + 1]):
                        if bb_ < nbb_run:
                            kb.dma("sp", lambda e, bb_=bb_: e.dma_start(out=xb[bb_ % 2][:], in_=xs_d[bb_ * 256:(bb_ + 1) * 256, :].rearrange("(t p) d -> p t d", p=128)),
                                   "ld_xb%d" % (bb_ % 2), reads=B_xs2, writes=[B_xb[bb_ % 2]])
                    for t2 in range(2):
                        tj = cnt_t % 2
                        cnt_t += 1
                        for kc in range(8):
                            kb.emit("pe", lambda e, xj=xj, t2=t2, kc=kc, tj=tj: e.transpose(out=ptx[tj][:, kc, :], in_=xb[xj][:, t2, kc * 128:(kc + 1) * 128],
                                                                                       identity=identb[:]), [B_xb[xj], B_identb], [B_ptx[tj]], inc=(kc == 7))
                        kb.emit("act", lambda e, xj=xj, t2=t2, tj=tj: e.copy(out=xsT[xj][:, :, t2 * 128:(t2 + 1) * 128], in_=ptx[tj][:]), [B_ptx[tj]], [B_xsT[xj]])
                    hj = b % 2
                    for m in range(4):
                        pg_i = (cnt_g % 2) * 2
                        cnt_g += 1
                        for kc in range(8):
                            kb.emit("pe", lambda e, wj=wj, m=m, kc=kc, xj=xj, pg_i=pg_i: e.matmul(
                                pgu[pg_i][:], lhsT=wge[wj][:, kc, m * 128:(m + 1) * 128], rhs=xsT[xj][:, kc, :],
                                start=(kc == 0), stop=(kc == 7)), [B_we[wj], B_xsT[xj]], [B_pgu[pg_i]], inc=(kc == 7))
                        for kc in range(8):
                            kb.emit("pe", lambda e, wj=wj, m=m, kc=kc, xj=xj, pg_i=pg_i: e.matmul(
                                pgu[pg_i + 1][:], lhsT=wue[wj][:, kc, m * 128:(m + 1) * 128], rhs=xsT[xj][:, kc, :],
                                start=(kc == 0), stop=(kc == 7)), [B_we[wj], B_xsT[xj]], [B_pgu[pg_i + 1]], inc=(kc == 7))
                        sj = m % 2
                        kb.emit("act", lambda e, pg_i=pg_i, sj=sj: e.activation(out=sgt[sj][:], in_=pgu[pg_i][:], func=AF.Silu), [B_pgu[pg_i]], [B_sgt[sj]])
                        kb.emit("dve", lambda e, pg_i=pg_i, sj=sj, hj=hj, m=m: e.tensor_tensor(out=hid[hj][:, m, :], in0=sgt[sj][:], in1=pgu[pg_i + 1][:], op=ALU.mult),
                                [B_sgt[sj], B_pgu[pg_i + 1]], [B_hid[hj]])
                    for t2 in range(2):
                        yj = cnt_y % 2
                        cnt_y += 1
                        for hh in range(2):
                            hb = cnt_hb % 3
                            cnt_hb += 1
                            for m in range(4):
                                kb.emit("pe", lambda e, wj=wj, m=m, hh=hh, hj=hj, t2=t2, hb=hb: e.matmul(
                                    pyh[hb][:], lhsT=hid[hj][:, m, t2 * 128:(t2 + 1) * 128],
                                    rhs=wde[wj][:, m, hh * 512:(hh + 1) * 512], start=(m == 0), stop=(m == 3)),
                                    [B_hid[hj], B_we[wj]], [B_pyh[hb]], inc=(m == 3))
                            kb.emit("act", lambda e, yj=yj, hh=hh, hb=hb: e.copy(out=ysb[yj][:, hh * 512:(hh + 1) * 512], in_=pyh[hb][:]),
                                    [B_pyh[hb]], [B_ysb[yj]])
                        kb.dma("sp", lambda e, b=b, t2=t2, yj=yj: e.dma_start(out=ys_d[b * 256 + t2 * 128:b * 256 + (t2 + 1) * 128, :], in_=ysb[yj][:]),
                               "st_ys%d" % yj, reads=[B_ysb[yj]], writes=[B_ys2[yj]])
            with nullcontext(pe_) as pc_:
                xr = [sb("xr%d" % i, [128, D], F32, pc_) for i in range(2)]
                yg = [sb("yg%d" % i, [128, 2, D], F32, pc_) for i in range(2)]
                B_xr = [kb.buf(), kb.buf()]
                B_yg = [kb.buf(), kb.buf()]
                for gi in range(NG):
                    j = gi % 2
                    s_, i_ = gi // NT, gi % NT
                    kb.dma("sp", lambda e, s_=s_, i_=i_, j=j: e.dma_start(out=xr[j][:], in_=x1_d[s_, i_ * 128:(i_ + 1) * 128, :]), "ld_xr%d" % j,
                           reads=[B_x1d[s_][i_]], writes=[B_xr[j]])
                    for k2 in range(2):
                        kb.dma("pool", lambda e, gi=gi, j=j, k2=k2: e.indirect_dma_start(
                            out=yg[j][:, k2, :], out_offset=None, in_=ys_d[:, :],
                            in_offset=bass.IndirectOffsetOnAxis(ap=SLOT[:, gi, k2:k2 + 1], axis=0)), "ld_yg%d" % j,
                            reads=B_ys2 + [B_slot], writes=[B_yg[j]])
                    kb.emit("dve", lambda e, gi=gi, j=j: e.tensor_scalar(out=yg[j][:, 0, :], in0=yg[j][:, 0, :], scalar1=WK[:, gi, 0:1], scalar2=None, op0=ALU.mult),
                            [B_yg[j], B_EW[gi]], [B_yg[j]])
                    kb.emit("dve", lambda e, gi=gi, j=j: e.scalar_tensor_tensor(out=yg[j][:, 0, :], in0=yg[j][:, 1, :], scalar=WK[:, gi, 1:2], in1=yg[j][:, 0, :],
                                                                              op0=ALU.mult, op1=ALU.add), [B_yg[j], B_EW[gi]], [B_yg[j]])
                    kb.emit("dve", lambda e, s_=s_, j=j: e.tensor_tensor(out=yg[j][:, 0, :], in0=yg[j][:, 0, :], in1=gatebc[:, s_, 1, :], op=ALU.mult),
                            [B_yg[j], B_gatebc], [B_yg[j]])
                    kb.emit("dve", lambda e, j=j: e.tensor_tensor(out=xr[j][:], in0=xr[j][:], in1=yg[j][:, 0, :], op=ALU.add), [B_xr[j], B_yg[j]], [B_xr[j]])
                    kb.dma("sp", lambda e, s_=s_, i_=i_, j=j: e.dma_start(out=out_d[s_, i_ * 128:(i_ + 1) * 128, :], in_=xr[j][:]), "st_out%d" % j,
                           reads=[B_xr[j]])
                kb.barrier()
                kb.flush()

    kb.barrier()
    kb.flush()
    kb.es.close()
    return nc


def _consts():
    c = np.zeros((128, 1024), np.float32)
    idx = np.arange(128)
    same = (idx[:, None] // 64) == (idx[None, :] // 64)
    triF = ((idx[:, None] <= idx[None, :]) & same).astype(np.float32)
    triB = triF.T.copy()
    c[:, 0:128] = triF
    c[:, 128:256] = triB
    c[:, 256:384] = triB - np.eye(128, dtype=np.float32)
    c[:, 384:512] = triF - np.eye(128, dtype=np.float32)
    c[:, 512] = (idx < 64)
    c[:, 513] = (idx >= 64)
    half = 16
    inv_freq = (10000.0 ** (-np.arange(half, dtype=np.float32) / half)).astype(np.float32)
    c[:, 514:530] = inv_freq[None, :]
    c[0, 530] = 1.0
    c[1, 531] = 1.0
    c[0, 532:660] = 1.0
    c[1, 660:788] = 1.0
    c[:, 788:916] = 1.0
    c[:, 916] = 1.0 / 384
    c[:, 917] = 1.0 / 256
    c[:, 918] = -np.pi
    c[:, 919] = 1e-6
    return c


def _cstm():
    c = np.zeros((128, 192), np.float32)
    idx = np.arange(128)
    c[:, 0:128] = (idx[:, None] < idx[None, :]).astype(np.float32)
    c[:, 128:160] = np.arange(32, dtype=np.float32)[None, :]
    c[:, 160] = idx
    return c


def _crow():
    r = np.zeros((1, 3200), np.float32)
    r[0, 0:16] = 256.0 * np.arange(16)
    e = np.arange(32)
    r[0, 16:1040] = (e[None, :] <= e[:, None]).astype(np.float32).reshape(-1)
    r[0, 1040:3088] = np.repeat(np.arange(64, dtype=np.float32), 32)
    return r


def _kc(w):
    K, N = w.shape
    return np.ascontiguousarray(w.reshape(K // 128, 128, N).transpose(1, 0, 2))


def make_in_maps(inp):
    f = lambda a: np.ascontiguousarray(np.asarray(a, dtype=np.float32))
    x = f(inp["x"]); c = f(inp["c"]); pos = np.asarray(inp["positions"]).astype(np.int32)
    shared = {
        "ada_w": _kc(f(inp["ada_w"])[0]),
        "ada_b": f(inp["ada_b"])[0][None, :],
        "g1": np.ascontiguousarray(f(inp["norm1_g"])[0].reshape(8, 128).T),
        "g2": np.ascontiguousarray(f(inp["norm2_g"])[0].reshape(8, 128).T),
        "w_in": _kc(f(inp["w_in"])[0]),
        "qa_g": np.ascontiguousarray(f(inp["mla_qa_g"])[0].reshape(3, 128).T),
        "wq_up": _kc(f(inp["mla_wq_up"])[0]),
        "kva_g": np.ascontiguousarray(f(inp["mla_kva_g"])[0].reshape(2, 128).T),
        "wkv_up": _kc(f(inp["mla_wkv_up"])[0]),
        "qn_g": f(inp["mla_qn_g"])[0][None, :],
        "kn_g": f(inp["mla_kn_g"])[0][None, :],
        "lb_logits": f(inp["hg_lb_logits"]).reshape(1, -1),
        "hg_norm_g": f(inp["hg_norm_g"])[0][None, :],
        "w_out_a": np.ascontiguousarray(f(inp["w_out"])[0][:512].reshape(8, 64, D).transpose(1, 0, 2)),
        "w_out_r": _kc(f(inp["w_out"])[0][512:]),
        "w_router": _kc(np.concatenate([f(inp["router_group_w"])[0], f(inp["router_expert_w"])[0]], axis=1)),
        "b_router": np.concatenate([f(inp["router_group_b"])[0], f(inp["router_expert_b"])[0]])[None, :],
        "w_gate": np.ascontiguousarray(f(inp["w_gate"])[0].reshape(NEXP, 8, 128, 512).transpose(0, 2, 1, 3)),
        "w_up": np.ascontiguousarray(f(inp["w_up"])[0].reshape(NEXP, 8, 128, 512).transpose(0, 2, 1, 3)),
        "w_down": np.ascontiguousarray(f(inp["w_down"])[0].reshape(NEXP, 4, 128, D).transpose(0, 2, 1, 3)),
        "ident_bf": np.eye(128, dtype=np.float32).astype(ml_dtypes.bfloat16),
        "consts_f": _consts(),
        "cstm": _cstm(),
        "crow": _crow(),
        "g2row": f(inp["norm2_g"])[0][None, :],
    }
    maps = []
    for i in range(NCORES):
        b0 = NSEQ * i
        m = dict(shared)
        m["x"] = np.ascontiguousarray(x[b0:b0 + NSEQ])
        p = pos[b0:b0 + NSEQ].reshape(NSEQ, NT, 128)
        m["pos"] = np.ascontiguousarray(p.transpose(2, 0, 1).reshape(128, NSEQ * NT))
        m["cT"] = np.ascontiguousarray(c[b0:b0 + NSEQ].reshape(NSEQ, 8, 128).transpose(2, 1, 0))
        maps.append(m)
    return maps


def kernel(**inputs):
    nc = build()
    in_maps = make_in_maps(inputs)
    res = run_bass_kernel_spmd(nc, in_maps, core_ids=list(range(NCORES)))
    outs = [np.asarray(r["out"]).reshape(NSEQ, S, D) for r in res.results]
    return np.concatenate(outs, axis=0).astype(np.float32)
```
